# Optimizing a Trainium2 kernel written in Bass

```python
import jax, jax.numpy as jnp
from jax import lax
import numpy as np

D_MODEL = 1024
BATCH = 2
SEQ = 8192
DEPTH = 2

A_HEADS = 8
HEAD_DIM = 64
A_WIDTH = A_HEADS * HEAD_DIM
IDX_HEADS = 4
IDX_DIM = 64
TOPK_MAX = 256
Q_BLOCK = 128
ATTN_SCALE = HEAD_DIM ** -0.5
IDX_SCALE = (IDX_DIM ** -0.5) * (IDX_HEADS ** -0.5)

ROPE_THETA = 500000.0
ROT_DIM = HEAD_DIM // 4

B_HEADS = 4
B_DK = 32
B_DV = 64
B_KWIDTH = B_HEADS * B_DK
B_VWIDTH = B_HEADS * B_DV
GATE_RANK = 16
GATE_TAU = 16.0
CHUNK = 64

POOL_WINDOWS = (2, 4, 8, 16)
C_GROUPS = 4
C_GROUP_DIM = 64
C_WIDTH = C_GROUPS * C_GROUP_DIM

MIX_WIDTH = A_WIDTH + B_VWIDTH + C_WIDTH

SPLIT_SIZES = (
    A_WIDTH, A_WIDTH, A_WIDTH,
    IDX_HEADS * IDX_DIM, IDX_DIM, IDX_HEADS,
    B_KWIDTH, B_KWIDTH, B_VWIDTH, B_VWIDTH,
    GATE_RANK,
    C_WIDTH,
)
IN_WIDTH = sum(SPLIT_SIZES)

D_FF = 2816
CONV_WIDTH = 3
EPS = 1e-6

kernel_name = "hymba_dsa_gla_pool_convglu"


def rms_norm(x, g):
    xf = x.astype(jnp.float32)
    y = xf * lax.rsqrt(jnp.mean(xf * xf, axis=-1, keepdims=True) + EPS)
    return (y * g.astype(jnp.float32)).astype(x.dtype)


def rope_tables(positions):
    inv = ROPE_THETA ** (-jnp.arange(0, ROT_DIM, 2, dtype=jnp.float32) / ROT_DIM)
    ang = positions.astype(jnp.float32)[..., None] * inv
    return jnp.cos(ang), jnp.sin(ang)


def partial_rope(x, cos, sin):
    half = ROT_DIM // 2
    x1 = x[..., :half].astype(jnp.float32)
    x2 = x[..., half:ROT_DIM].astype(jnp.float32)
    c = cos[:, :, None, :]
    s = sin[:, :, None, :]
    rot = jnp.concatenate([x1 * c - x2 * s, x2 * c + x1 * s], axis=-1).astype(x.dtype)
    return jnp.concatenate([rot, x[..., ROT_DIM:]], axis=-1)


def split_cols(p):
    outs = []
    off = 0
    for n in SPLIT_SIZES:
        outs.append(p[..., off:off + n])
        off += n
    return outs


def dsa_attention(q, k, v, qi, ki, wi):
    nbat, S, H, Dh = q.shape
    n_keep = min(TOPK_MAX, S // 4)
    nb = S // Q_BLOCK
    key_pos = jnp.arange(S)
    ki_f = ki.astype(jnp.float32)

    def to_blocks(a):
        return a.reshape(nbat, nb, Q_BLOCK, *a.shape[2:]).swapaxes(0, 1)

    def block(args):
        qb, qib, wib, qpos = args
        logits = jnp.einsum('bqhd,bsd->bqhs', qib.astype(jnp.float32), ki_f)
        score = jnp.einsum('bqh,bqhs->bqs', wib.astype(jnp.float32), jax.nn.relu(logits)) * IDX_SCALE
        causal = key_pos[None, :] <= qpos[:, None]
        score = jnp.where(causal[None], score, -jnp.inf)
        _, idx = lax.top_k(score, n_keep)
        valid = idx <= qpos[None, :, None]
        kg = jax.vmap(lambda kk, ii: kk[ii])(k, idx)
        vg = jax.vmap(lambda vv, ii: vv[ii])(v, idx)
        s = jnp.einsum('bqhd,bqkhd->bhqk', qb.astype(jnp.float32), kg.astype(jnp.float32)) * ATTN_SCALE
        s = jnp.where(valid[:, None], s, -jnp.inf)
        p = jax.nn.softmax(s, axis=-1)
        o = jnp.einsum('bhqk,bqkhd->bqhd', p, vg.astype(jnp.float32))
        return o.astype(q.dtype)

    qpos_blocks = jnp.arange(S).reshape(nb, Q_BLOCK)
    out = lax.map(block, (to_blocks(q), to_blocks(qi), to_blocks(wi), qpos_blocks))
    return out.swapaxes(0, 1).reshape(nbat, S, H, Dh)


def gla_chunked(q, k, v, log_a):
    nbat, S, H, DK = q.shape
    DV = v.shape[-1]
    nc = S // CHUNK

    def chunks(a):
        return a.astype(jnp.float32).reshape(nbat, nc, CHUNK, H, a.shape[-1]).transpose(1, 0, 3, 2, 4)

    qc = chunks(q * (DK ** -0.5))
    kc, vc, gc = chunks(k), chunks(v), chunks(log_a)
    tri = jnp.tril(jnp.ones((CHUNK, CHUNK), dtype=bool))

    def step(state, inp):
        qq, kk, vv, gg = inp
        b = jnp.cumsum(gg, axis=2)
        o_inter = jnp.einsum('bhcd,bhde->bhce', qq * jnp.exp(b), state)
        diff = b[:, :, :, None, :] - b[:, :, None, :, :]
        decay = jnp.exp(jnp.where(tri[:, :, None], diff, -jnp.inf))
        att = jnp.einsum('bhid,bhjd,bhijd->bhij', qq, kk, decay)
        o_intra = jnp.einsum('bhij,bhje->bhie', att, vv)
        b_last = b[:, :, -1:, :]
        new_state = state * jnp.exp(b_last[:, :, 0, :, None]) + jnp.einsum(
            'bhcd,bhce->bhde', kk * jnp.exp(b_last - b), vv)
        return new_state, o_inter + o_intra

    state0 = jnp.zeros((nbat, H, DK, DV), jnp.float32)
    _, o = lax.scan(step, state0, (qc, kc, vc, gc))
    return o.transpose(1, 0, 3, 2, 4).reshape(nbat, S, H, DV)


def multiscale_pool(u, w_pool, pool_scale):
    nbat, S, _ = u.shape
    uf = u.astype(jnp.float32).reshape(nbat, S, C_GROUPS, C_GROUP_DIM)
    cs = jnp.concatenate([jnp.zeros((nbat, 1, C_GROUPS, C_GROUP_DIM), jnp.float32),
                          jnp.cumsum(uf, axis=1)], axis=1)
    pos = jnp.arange(S)
    win = jnp.array(POOL_WINDOWS, dtype=jnp.int32)
    lo_idx = jnp.maximum(pos[:, None] + 1 - win[None, :], 0)
    lo = cs[:, lo_idx, jnp.arange(C_GROUPS)[None, :]]
    cnt = jnp.minimum(pos[:, None] + 1, win[None, :]).astype(jnp.float32)
    pooled = (cs[:, 1:] - lo) / cnt[None, :, :, None] - uf
    y = jnp.einsum('bsgd,gde->bsge', pooled, w_pool.astype(jnp.float32))
    y = y * pool_scale.astype(jnp.float32).reshape(C_GROUPS, C_GROUP_DIM)
    return y.reshape(nbat, S, C_WIDTH).astype(u.dtype)


def conv_glu_ffn(x, w_up, conv_w, conv_b, w_down):
    S = x.shape[1]
    h = x @ w_up
    a, b = h[..., :D_FF], h[..., D_FF:]
    ap = jnp.pad(a, ((0, 0), (CONV_WIDTH - 1, 0), (0, 0)))
    conv = conv_b + ap[:, 0:S] * conv_w[0]
    for j in range(1, CONV_WIDTH):
        conv = conv + ap[:, j:j + S] * conv_w[j]
    return (jax.nn.silu(conv) * b) @ w_down


def hybrid_layer(x, cos, sin, norm1_g, w_in, w_gate_up, b_gate, gla_norm_g, w_pool, pool_scale,
                 w_out, norm2_g, w_up, conv_w, conv_b, w_down):
    nbat, S, _ = x.shape
    h = rms_norm(x, norm1_g)
    proj = h @ w_in
    (qa, ka, va, qi, ki, wi, qb, kb, vb, rb, gl, uc) = split_cols(proj)

    qa = partial_rope(qa.reshape(nbat, S, A_HEADS, HEAD_DIM), cos, sin)
    ka = partial_rope(ka.reshape(nbat, S, A_HEADS, HEAD_DIM), cos, sin)
    va = va.reshape(nbat, S, A_HEADS, HEAD_DIM)
    qi = partial_rope(qi.reshape(nbat, S, IDX_HEADS, IDX_DIM), cos, sin)
    ki = partial_rope(ki[:, :, None, :], cos, sin)[:, :, 0, :]
    o_a = dsa_attention(qa, ka, va, qi, ki, wi).reshape(nbat, S, A_WIDTH)

    z = (gl @ w_gate_up + b_gate).astype(jnp.float32)
    log_a = (jax.nn.log_sigmoid(z) / GATE_TAU).reshape(nbat, S, B_HEADS, B_DK)
    o_b = gla_chunked(qb.reshape(nbat, S, B_HEADS, B_DK), kb.reshape(nbat, S, B_HEADS, B_DK),
                      vb.reshape(nbat, S, B_HEADS, B_DV), log_a)
    o_b = rms_norm(o_b, gla_norm_g).reshape(nbat, S, B_VWIDTH)
    o_b = (o_b * jax.nn.silu(rb.astype(jnp.float32))).astype(x.dtype)

    o_c = multiscale_pool(uc, w_pool, pool_scale)

    mix = jnp.concatenate([o_a.astype(x.dtype), o_b, o_c], axis=-1) @ w_out
    x = x + mix
    x = x + conv_glu_ffn(rms_norm(x, norm2_g), w_up, conv_w, conv_b, w_down)
    return x


def setup_inputs(seed: int = 0) -> dict:
    key = jax.random.key(seed)
    ks = jax.random.split(key, 16)
    f32 = jnp.float32
    nrm = lambda k, shape, s: jax.random.normal(k, shape, f32) * s
    return {
        "x": jax.random.normal(ks[0], (BATCH, SEQ, D_MODEL), f32),
        "positions": jnp.broadcast_to(jnp.arange(SEQ, dtype=jnp.int32)[None, :], (BATCH, SEQ)),
        "norm1_g": 1.0 + nrm(ks[1], (DEPTH, D_MODEL), 0.02),
        "w_in": nrm(ks[2], (DEPTH, D_MODEL, IN_WIDTH), D_MODEL ** -0.5),
        "w_gate_up": nrm(ks[3], (DEPTH, GATE_RANK, B_KWIDTH), GATE_RANK ** -0.5),
        "b_gate": nrm(ks[4], (DEPTH, B_KWIDTH), 0.01),
        "gla_norm_g": 1.0 + nrm(ks[5], (DEPTH, B_HEADS, B_DV), 0.02),
        "w_pool": nrm(ks[6], (DEPTH, C_GROUPS, C_GROUP_DIM, C_GROUP_DIM), C_GROUP_DIM ** -0.5),
        "pool_scale": 1.0 + nrm(ks[7], (DEPTH, C_WIDTH), 0.1),
        "w_out": nrm(ks[8], (DEPTH, MIX_WIDTH, D_MODEL), MIX_WIDTH ** -0.5),
        "norm2_g": 1.0 + nrm(ks[9], (DEPTH, D_MODEL), 0.02),
        "w_up": nrm(ks[10], (DEPTH, D_MODEL, 2 * D_FF), D_MODEL ** -0.5),
        "conv_w": nrm(ks[11], (DEPTH, CONV_WIDTH, D_FF), CONV_WIDTH ** -0.5),
        "conv_b": nrm(ks[12], (DEPTH, D_FF), 0.01),
        "w_down": nrm(ks[13], (DEPTH, D_FF, D_MODEL), D_FF ** -0.5),
        "final_norm_g": 1.0 + nrm(ks[14], (D_MODEL,), 0.02),
    }


def reference(x, positions, norm1_g, w_in, w_gate_up, b_gate, gla_norm_g, w_pool, pool_scale,
              w_out, norm2_g, w_up, conv_w, conv_b, w_down, final_norm_g):
    cos, sin = rope_tables(positions)
    for l in range(DEPTH):
        x = hybrid_layer(x, cos, sin, norm1_g[l], w_in[l], w_gate_up[l], b_gate[l], gla_norm_g[l],
                         w_pool[l], pool_scale[l], w_out[l], norm2_g[l], w_up[l], conv_w[l],
                         conv_b[l], w_down[l])
    return rms_norm(x, final_norm_g)
```

```python
import numpy as np
import concourse.bass as bass
import concourse.mybir as mybir
from concourse.bass_utils import run_bass_kernel_spmd
from contextlib import ExitStack
import math
import ml_dtypes

F32 = mybir.dt.float32
BF16 = mybir.dt.bfloat16
I32 = mybir.dt.int32
ALU = mybir.AluOpType
AF = mybir.ActivationFunctionType
AX = mybir.AxisListType

ENGS = ("pe", "act", "dve", "pool", "sp")


class _Op:
    __slots__ = ("eng", "fn", "reads", "writes", "dma", "deps", "sig", "sigval", "dsem", "idx")


class Sched:
    def __init__(self, nc, n_dma_sems=6):
        self.nc = nc
        self.ops = []
        self.last_w = {}
        self.readers = {}
        self.n_dma_sems = n_dma_sems

    def add(self, eng, fn, reads=(), writes=(), dma=False):
        op = _Op()
        op.eng = eng
        op.fn = fn
        op.reads = tuple(reads)
        op.writes = tuple(writes)
        op.dma = dma
        op.idx = len(self.ops)
        deps = set()
        for r in op.reads:
            w = self.last_w.get(r)
            if w is not None:
                deps.add(w)
        for r in op.writes:
            w = self.last_w.get(r)
            if w is not None:
                deps.add(w)
            for rd in self.readers.get(r, ()):
                deps.add(rd)
        deps.discard(op.idx)
        op.deps = deps
        for r in op.reads:
            self.readers.setdefault(r, []).append(op.idx)
        for r in op.writes:
            self.last_w[r] = op.idx
            self.readers[r] = []
        op.sig = False
        op.sigval = None
        op.dsem = None
        self.ops.append(op)
        return op

    def pe(self, fn, reads=(), writes=()):
        return self.add("pe", fn, reads, writes)

    def act(self, fn, reads=(), writes=()):
        return self.add("act", fn, reads, writes)

    def dve(self, fn, reads=(), writes=()):
        return self.add("dve", fn, reads, writes)

    def pool(self, fn, reads=(), writes=()):
        return self.add("pool", fn, reads, writes)

    def dma(self, fn, reads=(), writes=(), q="sp"):
        return self.add(q, fn, reads, writes, dma=True)

    def emit(self, stack):
        nc = self.nc
        ops = self.ops
        for op in ops:
            for d in op.deps:
                dop = ops[d]
                if dop.eng == op.eng and not dop.dma:
                    if not (set(dop.writes) & set(op.reads)):
                        continue
                dop.sig = True
        for op in ops:
            if op.dma:
                op.sig = True
        esem = {e: stack.enter_context(nc.semaphore("s_" + e)) for e in ENGS}
        dsems = {}
        for q in ENGS:
            if any(o.dma and o.eng == q for o in ops):
                dsems[q] = [stack.enter_context(nc.semaphore("d_%s%d" % (q, i))) for i in range(self.n_dma_sems)]
        ecount = {e: 0 for e in ENGS}
        dcount = {q: [0] * self.n_dma_sems for q in dsems}
        drr = {q: 0 for q in dsems}
        prev_dma_wait = {}
        for op in ops:
            if op.dma:
                k = drr[op.eng]
                drr[op.eng] = (k + 1) % self.n_dma_sems
                prev_dma_wait[op.idx] = dcount[op.eng][k]
                dcount[op.eng][k] += 16
                op.dsem = (op.eng, k)
                op.sigval = dcount[op.eng][k]
            elif op.sig:
                ecount[op.eng] += 1
                op.sigval = ecount[op.eng]
        by_eng = {e: [o for o in ops if o.eng == e] for e in ENGS}
        block = stack.enter_context(nc.Block())
        self.n_waits = 0

        def run(ename, eobj):
            waited = {}
            for op in by_eng[ename]:
                need = {}
                for d in op.deps:
                    dop = ops[d]
                    if dop.dma:
                        key = ("d",) + dop.dsem
                        sem = dsems[dop.dsem[0]][dop.dsem[1]]
                    else:
                        if dop.eng == ename and not (set(dop.writes) & set(op.reads)):
                            continue
                        key = ("e", dop.eng)
                        sem = esem[dop.eng]
                    v = dop.sigval
                    if waited.get(key, 0) >= v:
                        continue
                    if key not in need or need[key][1] < v:
                        need[key] = (sem, v)
                if op.dma:
                    pv = prev_dma_wait[op.idx]
                    key = ("d",) + op.dsem
                    if pv > 0 and waited.get(key, 0) < pv:
                        if key not in need or need[key][1] < pv:
                            need[key] = (dsems[op.dsem[0]][op.dsem[1]], pv)
                for key, (sem, v) in need.items():
                    eobj.wait_ge(sem, v)
                    waited[key] = v
                    self.n_waits += 1
                ins = op.fn(eobj)
                if op.dma:
                    ins.then_inc(dsems[op.dsem[0]][op.dsem[1]], 16)
                elif op.sig:
                    ins.then_inc(esem[ename], 1)
            for q, lst in dsems.items():
                if q == ename:
                    for k, s in enumerate(lst):
                        if dcount[q][k] > 0:
                            eobj.wait_ge(s, dcount[q][k])

        if by_eng["pe"]:
            @block.tensor
            def _(e):
                run("pe", e)
        if by_eng["act"]:
            @block.scalar
            def _(e):
                run("act", e)
        if by_eng["dve"]:
            @block.vector
            def _(e):
                run("dve", e)
        if by_eng["pool"]:
            @block.gpsimd
            def _(e):
                run("pool", e)
        if by_eng["sp"]:
            @block.sync
            def _(e):
                run("sp", e)


NTILE = 20
INW = 2900
CH = [(0, 512), (512, 1024), (1024, 1536), (1536, 2048), (2048, 2560), (2560, 2900)]
UC0 = 2644
BFW = 1856
F32W = INW - BFW
TWO_PI = 2.0 * math.pi


def pool_mats(first):
    Mc = np.zeros((128, 4, 128), np.float32)
    Mh = np.zeros((128, 4, 128), np.float32)
    for gi, w in enumerate((2, 4, 8, 16)):
        for t in range(128):
            cnt = min(t + 1, w) if first else w
            for s in range(t - w + 1, t + 1):
                if s >= 0:
                    Mc[s, gi, t] += 1.0 / cnt
                elif not first:
                    Mh[128 + s, gi, t] += 1.0 / cnt
            Mc[t, gi, t] -= 1.0
    return Mc, Mh


def build_A():
    nc = bass.Bass("TRN2", target_bir_lowering=False)
    D = lambda name, shape, dt, kind="ExternalInput": nc.dram_tensor(name, shape, dt, kind=kind).ap()
    xtok = D("xtok", [NTILE * 128, 1024], F32)
    xT = D("xT", [1024, NTILE * 128], F32)
    w_in = D("w_in", [1024, INW], F32)
    g1 = D("g1", [128, 8], F32)
    pos = D("pos", [128, NTILE], I32)
    inv = D("inv", [128, 8], F32)
    wpool = D("wpool", [64, 4, 64], F32)
    pscale = D("pscale", [64, 4], F32)
    mcur = D("mcur", [128, 5, 4, 128], F32)
    mhal = D("mhal", [128, 5, 4, 128], F32)
    pbf = D("pbf", [2048, BFW], BF16, "ExternalOutput")
    pf32 = D("pf32", [2048, F32W], F32, "ExternalOutput")
    ocT = D("ocT", [64, 4, 2048], BF16, "ExternalOutput")

    with ExitStack() as stack:
        T = lambda name, shape, dt: stack.enter_context(nc.sbuf_tensor(name, shape, dt))
        P = lambda name, shape, dt: stack.enter_context(nc.psum_tensor(name, shape, dt))
        wbf = T("wbf", [128, 8, INW], BF16)
        wst = T("wst", [128, 2, INW], F32)
        g1t = T("g1t", [128, 8], F32)
        posi = T("posi", [128, NTILE], I32)
        posf = T("posf", [128, NTILE], F32)
        invt = T("invt", [128, 8], F32)
        ang = T("ang", [128, NTILE, 8], F32)
        kq = T("kq", [128, NTILE, 8], F32)
        angc = T("angc", [128, NTILE, 8], F32)
        kqi = T("kqi", [128, NTILE, 8], I32)
        cost = T("cost", [128, NTILE, 8], F32)
        sint = T("sint", [128, NTILE, 8], F32)
        wpt = T("wpt", [64, 4, 64], F32)
        pst = T("pst", [64, 4], F32)
        mct = T("mct", [128, 5, 4, 128], F32)
        mht = T("mht", [128, 5, 4, 128], F32)
        xtk = T("xtk", [128, 2, 1024], F32)
        sqj = T("sqj", [128, 1024], BF16)
        ss = T("ss", [128, 2], F32)
        rstd = T("rstd", [128, 2], F32)
        epst = T("epst", [128, 1], F32)
        xTt = T("xTt", [128, 2, 8, 128], F32)
        hT = T("hT", [128, 2, 8, 128], BF16)
        proj = T("proj", [128, 2, INW], F32)
        pb16 = T("pb16", [128, 2, BFW], BF16)
        rt = T("rt", [128, 4, 16, 8], F32)
        pooled = T("pooled", [64, 2, 4, 128], F32)
        oct_ = T("oct", [64, 2, 4, 128], BF16)
        ps = [P("ps%d" % i, [128, 512], F32) for i in range(4)]
        pp = [P("pp%d" % i, [64, 4, 128], F32) for i in range(2)]
        py = [P("py%d" % i, [64, 4, 128], F32) for i in range(2)]

        s = Sched(nc)
        s.dma(lambda e: e.dma_start(out=g1t[:], in_=g1), writes=["g1t"])
        s.dma(lambda e: e.dma_start(out=posi[:], in_=pos), writes=["posi"])
        s.dma(lambda e: e.dma_start(out=invt[:], in_=inv), writes=["invt"])
        s.dma(lambda e: e.dma_start(out=wpt[:], in_=wpool), writes=["wpt"])
        s.dma(lambda e: e.dma_start(out=pst[:], in_=pscale), writes=["pst"])
        s.dma(lambda e: e.dma_start(out=mct[:], in_=mcur), writes=["mct"])
        s.dma(lambda e: e.dma_start(out=mht[:], in_=mhal), writes=["mht"])
        s.dve(lambda e: e.memset(epst[:], 1e-6), writes=["epst"])
        s.dve(lambda e: e.tensor_copy(out=posf[:], in_=posi[:]), reads=["posi"], writes=["posf"])
        for t in range(NTILE):
            s.dve(lambda e, t=t: e.tensor_scalar(out=ang[:, t, :], in0=invt[:], scalar1=posf[:, t:t + 1], scalar2=None, op0=ALU.mult),
                  reads=["invt", "posf"], writes=["ang"])
        s.dve(lambda e: e.tensor_scalar(out=kqi[:], in0=ang[:], scalar1=1.0 / TWO_PI, scalar2=None, op0=ALU.mult), reads=["ang"], writes=["kqi"])
        s.dve(lambda e: e.tensor_copy(out=kq[:], in_=kqi[:]), reads=["kqi"], writes=["kq"])
        s.dve(lambda e: e.scalar_tensor_tensor(out=ang[:], in0=kq[:], scalar=-TWO_PI, in1=ang[:], op0=ALU.mult, op1=ALU.add),
              reads=["kq", "ang"], writes=["ang"])
        def wrap(y, name):
            s.dve(lambda e: e.tensor_scalar(out=kq[:], in0=y[:], scalar1=math.pi, scalar2=-TWO_PI, op0=ALU.is_gt, op1=ALU.mult),
                  reads=[name], writes=["kq"])
            s.dve(lambda e: e.tensor_tensor(out=y[:], in0=y[:], in1=kq[:], op=ALU.add), reads=[name, "kq"], writes=[name])
            s.dve(lambda e: e.tensor_scalar(out=kq[:], in0=y[:], scalar1=-math.pi, scalar2=TWO_PI, op0=ALU.is_lt, op1=ALU.mult),
                  reads=[name], writes=["kq"])
            s.dve(lambda e: e.tensor_tensor(out=y[:], in0=y[:], in1=kq[:], op=ALU.add), reads=[name, "kq"], writes=[name])
        s.dve(lambda e: e.tensor_scalar(out=angc[:], in0=ang[:], scalar1=math.pi / 2, scalar2=None, op0=ALU.add), reads=["ang"], writes=["angc"])
        wrap(ang, "ang")
        wrap(angc, "angc")
        s.act(lambda e: e.activation(out=sint[:], in_=ang[:], func=AF.Sin), reads=["ang"], writes=["sint"])
        s.act(lambda e: e.activation(out=cost[:], in_=angc[:], func=AF.Sin), reads=["angc"], writes=["cost"])

        for c in range(8):
            b = c % 2
            s.dma(lambda e, c=c, b=b: e.dma_start(out=wst[:, b, :], in_=w_in[c * 128:(c + 1) * 128, :]), writes=[("wst", b)])
            eng = s.dve if c % 2 == 0 else s.pool
            eng(lambda e, c=c, b=b: e.tensor_scalar(out=wbf[:, c, :], in0=wst[:, b, :], scalar1=g1t[:, c:c + 1], scalar2=None, op0=ALU.mult),
                reads=[("wst", b), "g1t"], writes=[("wbf", c)])

        own = 0
        for t in range(NTILE):
            k, i = divmod(t, 5)
            halo = (i == 0)
            b = t % 2
            pb = (t - 1) % 2
            s.dma(lambda e, t=t, b=b: e.dma_start(out=xtk[:, b, :], in_=xtok[t * 128:(t + 1) * 128, :]), writes=[("xtk", b)])
            s.dma(lambda e, t=t, b=b: e.dma_start(out=xTt[:, b, :, :], in_=xT[:, t * 128:(t + 1) * 128].rearrange("(c p) t -> p c t", p=128)),
                  writes=[("xTt", b)])
            s.act(lambda e, b=b: e.activation(out=sqj[:], in_=xtk[:, b, :], func=AF.Square, accum_out=ss[:, b:b + 1]),
                  reads=[("xtk", b)], writes=["sqj", ("ss", b)])
            s.act(lambda e, b=b: e.activation(out=ss[:, b:b + 1], in_=ss[:, b:b + 1], func=AF.Sqrt, scale=1.0 / 1024, bias=epst[:, 0:1]),
                  reads=[("ss", b), "epst"], writes=[("ss", b)])
            s.dve(lambda e, b=b: e.reciprocal(out=rstd[:, b:b + 1], in_=ss[:, b:b + 1]), reads=[("ss", b)], writes=[("rstd", b)])
            s.pool(lambda e, b=b: e.tensor_copy(out=hT[:, b, :, :], in_=xTt[:, b, :, :]), reads=[("xTt", b)], writes=[("hT", b)])
            chunks = [5] if halo else list(range(6))
            for n in chunks:
                n0, n1 = CH[n]
                pt = ps[n % 4]
                for c in range(8):
                    s.pe(lambda e, pt=pt, b=b, c=c, n0=n0, n1=n1: e.matmul(pt[:, 0:n1 - n0], lhsT=hT[:, b, c, :], rhs=wbf[:, c, n0:n1],
                                                                         start=(c == 0), stop=(c == 7)),
                         reads=[("hT", b), ("wbf", c)], writes=[("ps", n % 4)])
                if n % 2 == 0:
                    s.act(lambda e, pt=pt, b=b, n0=n0, n1=n1: e.activation(out=proj[:, b, n0:n1], in_=pt[:, 0:n1 - n0], func=AF.Copy,
                                                                         scale=rstd[:, b:b + 1]),
                          reads=[("ps", n % 4), ("rstd", b)], writes=[("proj", b, n)])
                else:
                    s.dve(lambda e, pt=pt, b=b, n0=n0, n1=n1: e.tensor_scalar(out=proj[:, b, n0:n1], in0=pt[:, 0:n1 - n0],
                                                                            scalar1=rstd[:, b:b + 1], scalar2=None, op0=ALU.mult),
                          reads=[("ps", n % 4), ("rstd", b)], writes=[("proj", b, n)])
            if not halo:
                for (c0, nh, regs) in ((0, 16, [("proj", b, 0), ("proj", b, 1)]), (1536, 5, [("proj", b, 3)])):
                    v = proj[:, b, c0:c0 + nh * 64].rearrange("p (h d) -> p h d", d=64)
                    x1 = v[:, :, 0:8]
                    x2 = v[:, :, 8:16]
                    cb = cost[:, t, :].unsqueeze(1).to_broadcast([128, nh, 8])
                    sb = sint[:, t, :].unsqueeze(1).to_broadcast([128, nh, 8])
                    t0 = rt[:, 0, 0:nh, :]
                    t1 = rt[:, 1, 0:nh, :]
                    t2 = rt[:, 2, 0:nh, :]
                    t3 = rt[:, 3, 0:nh, :]
                    R = regs + ["cost", "sint"]
                    s.dve(lambda e, t0=t0, x1=x1, cb=cb: e.tensor_tensor(out=t0, in0=x1, in1=cb, op=ALU.mult), reads=R, writes=["rt0"])
                    s.dve(lambda e, t1=t1, x2=x2, sb=sb: e.tensor_tensor(out=t1, in0=x2, in1=sb, op=ALU.mult), reads=R, writes=["rt1"])
                    s.dve(lambda e, t2=t2, x2=x2, cb=cb: e.tensor_tensor(out=t2, in0=x2, in1=cb, op=ALU.mult), reads=R, writes=["rt2"])
                    s.dve(lambda e, t3=t3, x1=x1, sb=sb: e.tensor_tensor(out=t3, in0=x1, in1=sb, op=ALU.mult), reads=R, writes=["rt3"])
                    s.dve(lambda e, t0=t0, t1=t1, x1=x1: e.tensor_tensor(out=x1, in0=t0, in1=t1, op=ALU.subtract),
                          reads=["rt0", "rt1", "rt2", "rt3"], writes=regs)
                    s.dve(lambda e, t2=t2, t3=t3, x2=x2: e.tensor_tensor(out=x2, in0=t2, in1=t3, op=ALU.add),
                          reads=["rt0", "rt1", "rt2", "rt3"], writes=regs)
                r0 = own * 128
                s.act(lambda e, b=b: e.activation(out=pb16[:, b, :], in_=proj[:, b, 0:BFW], func=AF.Copy),
                      reads=[("proj", b, n) for n in range(4)], writes=[("pb16", b)])
                s.dma(lambda e, b=b, r0=r0: e.dma_start(out=pbf[r0:r0 + 128, :], in_=pb16[:, b, :]), reads=[("pb16", b)])
                s.dma(lambda e, b=b, r0=r0: e.dma_start(out=pf32[r0:r0 + 128, :], in_=proj[:, b, BFW:INW]),
                      reads=[("proj", b, n) for n in (3, 4, 5)])
                mi = 0 if i > 1 else 1 + k
                ob = own % 2
                for gi in range(4):
                    s.pe(lambda e, ob=ob, b=b, gi=gi, mi=mi: e.matmul(pp[ob][:, gi, :], lhsT=proj[:, b, UC0 + gi * 64:UC0 + (gi + 1) * 64],
                                                                  rhs=mct[:, mi, gi, :], start=True, stop=False),
                         reads=[("proj", b, 5), "mct"], writes=[("pp", ob)])
                    s.pe(lambda e, ob=ob, pb=pb, gi=gi, mi=mi: e.matmul(pp[ob][:, gi, :], lhsT=proj[:, pb, UC0 + gi * 64:UC0 + (gi + 1) * 64],
                                                                    rhs=mht[:, mi, gi, :], start=False, stop=True),
                         reads=[("proj", pb, 5), "mht"], writes=[("pp", ob)])
                s.act(lambda e, ob=ob: e.activation(out=pooled[:, ob, :, :], in_=pp[ob][:], func=AF.Copy),
                      reads=[("pp", ob)], writes=[("pooled", ob)])
                for gi in range(4):
                    s.pe(lambda e, ob=ob, gi=gi: e.matmul(py[ob][:, gi, :], lhsT=wpt[:, gi, :], rhs=pooled[:, ob, gi, :], start=True, stop=True),
                         reads=[("pooled", ob), "wpt"], writes=[("py", ob)])
                for gi in range(4):
                    s.dve(lambda e, ob=ob, gi=gi: e.tensor_scalar(out=oct_[:, ob, gi, :], in0=py[ob][:, gi, :], scalar1=pst[:, gi:gi + 1],
                                                                scalar2=None, op0=ALU.mult),
                          reads=[("py", ob), "pst"], writes=[("oct", ob)])
                s.dma(lambda e, ob=ob, r0=r0: e.dma_start(out=ocT[:, :, r0:r0 + 128], in_=oct_[:, ob, :, :]), reads=[("oct", ob)])
                own += 1
        s.emit(stack)
        print("A: ops", len(s.ops), "waits", s.n_waits)
    return nc


def host_inputs_A(x, positions, norm1_g, w_in, w_pool, pool_scale):
    inv = (500000.0 ** (-np.arange(0, 16, 2, dtype=np.float32) / 16)).astype(np.float32)
    McG, MhG = pool_mats(False)
    McF, MhF = pool_mats(True)
    maps = []
    for c in range(8):
        b, j = divmod(c, 4)
        rows = []
        posl = []
        mcur = np.zeros((128, 5, 4, 128), np.float32)
        mhal = np.zeros((128, 5, 4, 128), np.float32)
        mcur[:, 0], mhal[:, 0] = McG, MhG
        for k in range(4):
            g = 4 * k + j
            t0 = 512 * g
            if g == 0:
                rows.append(np.zeros((128, 1024), np.float32))
                posl.append(np.zeros((128,), np.int32))
                mcur[:, 1 + k], mhal[:, 1 + k] = McF, MhF
            else:
                rows.append(x[b, t0 - 128:t0])
                posl.append(positions[b, t0 - 128:t0])
                mcur[:, 1 + k], mhal[:, 1 + k] = McG, MhG
            rows.append(x[b, t0:t0 + 512])
            posl.append(positions[b, t0:t0 + 512])
        xt = np.ascontiguousarray(np.concatenate(rows, 0))
        pl = np.concatenate(posl, 0).astype(np.int32)
        maps.append({
            "xtok": xt, "xT": np.ascontiguousarray(xt.T), "w_in": np.ascontiguousarray(w_in),
            "g1": np.ascontiguousarray(norm1_g.reshape(8, 128).T),
            "pos": np.ascontiguousarray(pl.reshape(NTILE, 128).T),
            "inv": np.ascontiguousarray(np.broadcast_to(inv[None, :], (128, 8))),
            "wpool": np.ascontiguousarray(w_pool.transpose(1, 0, 2)),
            "pscale": np.ascontiguousarray(pool_scale.reshape(4, 64).T),
            "mcur": mcur, "mhal": mhal,
        })
    return maps


NIT = 16
NEG = -1.0e30


def build_B(nslot=4, nit=NIT):
    nc = bass.Bass("TRN2", target_bir_lowering=False)
    D = lambda name, shape, dt, kind="ExternalInput": nc.dram_tensor(name, shape, dt, kind=kind).ap()
    kT = D("kT", [128, 4, 8192], BF16)
    vv = D("v", [8192, 8, 64], BF16)
    kiT = D("kiT", [64, 8192], BF16)
    qT = D("qT", [128, 4, 4, 512], BF16)
    qiT = D("qiT", [64, 4, 4, 512], BF16)
    wi = D("wi", [128, 16, 4], F32)
    qrel = D("qrel", [128, 16], F32)
    kpos = D("kpos", [128, 2048], F32)
    ident = D("ident", [128, 128], BF16)
    oaT = D("oaT", [64, 8, 2048], BF16, "ExternalOutput")

    with ExitStack() as stack:
        T = lambda name, shape, dt: stack.enter_context(nc.sbuf_tensor(name, shape, dt))
        P = lambda name, shape, dt: stack.enter_context(nc.psum_tensor(name, shape, dt))
        kit = T("kit", [64, 8192], BF16)
        sc = T("sc", [128, 8192], F32)
        mk = T("mk", [128, 4, 8192], BF16)
        kpt = T("kpt", [128, 2048], F32)
        rr = T("rr", [128, 4, 512], F32)
        qTt = T("qTt", [128, 1, 4, 512], BF16)
        qit = T("qit", [64, 1, 4, 512], BF16)
        wit = T("wit", [128, 16, 4], F32)
        qrt = T("qrt", [128, 16], F32)
        idt = T("idt", [128, 128], BF16)
        kTs = T("kTs", [128, 2, 4, 512], BF16)
        vraw = T("vraw", [128, 4, 512], BF16)
        vt = T("vt", [128, 2, 4, 8, 65], BF16)
        E = T("E", [128, 3, 512], BF16)
        Pm = T("Pm", [128, 3, 512], BF16)
        mT = T("mT", [128, 2, 512], BF16)
        sm = T("sm", [128, 8], F32)
        ones1 = T("ones1", [128, 64], F32)
        rec = T("rec", [128, 512], F32)
        bcs = T("bcs", [64, 512], F32)
        oT = T("oT", [64, 2, 512], BF16)
        pb = [P("pb%d" % i, [128, 512], F32) for i in range(8)]
        pTb = pb[2][:].bitcast(BF16)

        s = Sched(nc)
        s.dma(lambda e: e.dma_start(out=kit[:], in_=kiT), writes=["kit"])
        s.dma(lambda e: e.dma_start(out=wit[:], in_=wi), writes=["wit"])
        s.dma(lambda e: e.dma_start(out=qrt[:], in_=qrel), writes=["qrt"])
        s.dma(lambda e: e.dma_start(out=kpt[:], in_=kpos), writes=["kpt"])
        s.dma(lambda e: e.dma_start(out=idt[:], in_=ident), writes=["idt"])
        s.pool(lambda e: e.memset(ones1[:], 1.0), writes=["ones1"])
        for i in range(2):
            s.pool(lambda e, i=i: e.memset(vt[:, i, :, :, 64:65], 1.0), writes=[("vt", i)])
        cbias = rr[:].rearrange("p h n -> p (h n)")
        RRALL = [("rr", h) for h in range(4)]

        kbc = 0
        ec = 0
        mtc = 0
        otc = 0
        for k in range(nslot):
            L = 2048 * (k + 1)
            nkc = L // 512
            nkb = L // 128
            qb = 0
            s.dma(lambda e, k=k, qb=qb: e.dma_start(out=qTt[:, qb, :, :], in_=qT[:, k, :, :]), writes=[("qTt", qb)])
            s.dma(lambda e, k=k, qb=qb: e.dma_start(out=qit[:, qb, :, :], in_=qiT[:, k, :, :]), writes=[("qit", qb)])
            for qt in range(4):
                g = 4 * k + qt
                for n in range(nkc):
                    for h in range(4):
                        s.pe(lambda e, h=h, qb=qb, qt=qt, n=n: e.matmul(pb[h][:], lhsT=qit[:, qb, h, qt * 128:(qt + 1) * 128],
                                                                     rhs=kit[:, n * 512:(n + 1) * 512], start=True, stop=True),
                             reads=[("qit", qb), "kit"], writes=[("pb", h)])
                        s.act(lambda e, h=h: e.activation(out=rr[:, h, :], in_=pb[h][:], func=AF.Relu), reads=[("pb", h)], writes=[("rr", h)])
                        if h == 0:
                            s.dve(lambda e, n=n, g=g: e.tensor_scalar(out=sc[:, n * 512:(n + 1) * 512], in0=rr[:, 0, :], scalar1=wit[:, g, 0:1],
                                                                    scalar2=None, op0=ALU.mult),
                                  reads=[("rr", 0), "wit"], writes=[("sc", n)])
                        else:
                            s.dve(lambda e, n=n, g=g, h=h: e.scalar_tensor_tensor(out=sc[:, n * 512:(n + 1) * 512], in0=rr[:, h, :],
                                                                                 scalar=wit[:, g, h:h + 1], in1=sc[:, n * 512:(n + 1) * 512],
                                                                                 op0=ALU.mult, op1=ALU.add),
                                  reads=[("rr", h), "wit", ("sc", n)], writes=[("sc", n)])
                allsc = [("sc", n) for n in range(nkc)]
                s.dve(lambda e, L=L: e.tensor_reduce(out=sm[:, 0:1], in_=sc[:, 0:L], axis=AX.X, op=ALU.min), reads=allsc, writes=["lo"])
                s.dve(lambda e, g=g: e.tensor_scalar(out=cbias[:], in0=kpt[:], scalar1=qrt[:, g:g + 1], scalar2=NEG, op0=ALU.is_gt, op1=ALU.mult),
                      reads=["kpt", "qrt"], writes=RRALL)
                s.dve(lambda e, L=L: e.tensor_tensor(out=sc[:, L - 2048:L], in0=sc[:, L - 2048:L], in1=cbias[:], op=ALU.add),
                      reads=allsc + RRALL, writes=allsc)
                s.dve(lambda e, L=L: e.tensor_reduce(out=sm[:, 5:6], in_=sc[:, 0:L], axis=AX.X, op=ALU.max), reads=allsc, writes=["rmax"])
                s.dve(lambda e: e.tensor_tensor(out=sm[:, 1:2], in0=sm[:, 5:6], in1=sm[:, 0:1], op=ALU.subtract), reads=["rmax", "lo"], writes=["range"])
                for it in range(1, nit + 1):
                    f = 2.0 ** (-it)
                    s.dve(lambda e, f=f: e.tensor_scalar(out=sm[:, 2:3], in0=sm[:, 1:2], scalar1=f, scalar2=sm[:, 0:1], op0=ALU.mult, op1=ALU.add),
                          reads=["range", "lo"], writes=["mid"])
                    s.dve(lambda e, L=L, qt=qt: e.tensor_scalar(out=mk[:, qt, 0:L], in0=sc[:, 0:L], scalar1=sm[:, 2:3], scalar2=None,
                                                              op0=ALU.is_ge, op1=ALU.add, accum_out=sm[:, 3:4]),
                          reads=allsc + ["mid"], writes=[("mk", qt), "cnt"])
                    s.dve(lambda e, f=f: e.tensor_scalar(out=sm[:, 4:5], in0=sm[:, 3:4], scalar1=255.5, scalar2=f, op0=ALU.is_ge, op1=ALU.mult),
                          reads=["cnt"], writes=["pred"])
                    s.dve(lambda e: e.scalar_tensor_tensor(out=sm[:, 0:1], in0=sm[:, 4:5], scalar=sm[:, 1:2], in1=sm[:, 0:1], op0=ALU.mult, op1=ALU.add),
                          reads=["pred", "range", "lo"], writes=["lo"])
                s.dve(lambda e, L=L, qt=qt: e.tensor_scalar(out=mk[:, qt, 0:L], in0=sc[:, 0:L], scalar1=sm[:, 0:1], scalar2=None, op0=ALU.is_ge),
                      reads=allsc + ["lo"], writes=[("mk", qt)])
            for hp in range(2):
                for kb in range(nkb):
                    sbk, kl = divmod(kb, 4)
                    if kl == 0:
                        kbuf = kbc % 2
                        kbc += 1
                        s.dma(lambda e, sbk=sbk, kbuf=kbuf: e.dma_start(out=kTs[:, kbuf, :, :], in_=kT[:, :, sbk * 512:(sbk + 1) * 512]),
                              writes=[("kTs", kbuf)])
                        s.dma(lambda e, sbk=sbk: e.dma_start(out=vraw[:], in_=vv[sbk * 512:(sbk + 1) * 512].rearrange("(kb p) h d -> p kb (h d)", p=128)),
                              writes=["vraw"])
                        s.pool(lambda e, kbuf=kbuf: e.tensor_copy(out=vt[:, kbuf, :, :, 0:64], in_=vraw[:].rearrange("p kb (h d) -> p kb h d", d=64)),
                               reads=["vraw"], writes=[("vt", kbuf)])
                    mb = mtc % 2
                    mtc += 1
                    for qt in range(4):
                        s.pe(lambda e, qt=qt, kb=kb: e.transpose(pTb[:, qt * 128:(qt + 1) * 128], mk[:, qt, kb * 128:(kb + 1) * 128], idt[:]),
                             reads=[("mk", qt), "idt"], writes=[("pb", 2)])
                    s.act(lambda e, mb=mb: e.activation(out=mT[:, mb, :], in_=pTb[:, 0:512], func=AF.Copy), reads=[("pb", 2)], writes=[("mT", mb)])
                    for hl in range(4):
                        h = hp * 4 + hl
                        pr, hh = divmod(h, 2)
                        sb = hl % 2
                        eb = ec % 3
                        ec += 1
                        s.pe(lambda e, sb=sb, kbuf=kbuf, pr=pr, hh=hh, qb=qb, kl=kl: e.matmul(pb[sb][:], lhsT=kTs[hh * 64:(hh + 1) * 64, kbuf, pr, kl * 128:(kl + 1) * 128],
                                                                                    rhs=qTt[hh * 64:(hh + 1) * 64, qb, pr, :], start=True, stop=True),
                             reads=[("kTs", kbuf), ("qTt", qb)], writes=[("pb", sb)])
                        s.act(lambda e, sb=sb, eb=eb: e.activation(out=E[:, eb, :], in_=pb[sb][:], func=AF.Exp, scale=0.125),
                              reads=[("pb", sb)], writes=[("E", eb)])
                        eng = s.dve if (hl % 2 == 0) else s.pool
                        eng(lambda e, eb=eb, mb=mb: e.tensor_tensor(out=Pm[:, eb, :], in0=E[:, eb, :], in1=mT[:, mb, :], op=ALU.mult),
                            reads=[("E", eb), ("mT", mb)], writes=[("Pm", eb)])
                        s.pe(lambda e, hl=hl, kbuf=kbuf, h=h, eb=eb, kb=kb, nkb=nkb, kl=kl: e.matmul(pb[4 + hl][0:65, :], lhsT=vt[:, kbuf, kl, h, :], rhs=Pm[:, eb, :],
                                                                                          start=(kb == 0), stop=(kb == nkb - 1)),
                             reads=[("vt", kbuf), ("Pm", eb)], writes=[("pb", 4 + hl)])
                for hl in range(4):
                    h = hp * 4 + hl
                    ob = otc % 2
                    otc += 1
                    s.dve(lambda e, hl=hl: e.reciprocal(out=rec[64:65, :], in_=pb[4 + hl][64:65, :]), reads=[("pb", 4 + hl)], writes=["rec"])
                    s.pe(lambda e: e.matmul(pb[3][0:64, :], lhsT=ones1[64:65, :], rhs=rec[64:65, :], start=True, stop=True),
                         reads=["ones1", "rec"], writes=[("pb", 3)])
                    s.act(lambda e: e.activation(out=bcs[:], in_=pb[3][0:64, :], func=AF.Copy), reads=[("pb", 3)], writes=["bcs"])
                    s.dve(lambda e, hl=hl, ob=ob: e.tensor_tensor(out=oT[:, ob, :], in0=pb[4 + hl][0:64, :], in1=bcs[:], op=ALU.mult),
                          reads=[("pb", 4 + hl), "bcs"], writes=[("oT", ob)])
                    s.dma(lambda e, h=h, k=k, ob=ob: e.dma_start(out=oaT[:, h, k * 512:(k + 1) * 512], in_=oT[:, ob, :]), reads=[("oT", ob)])
        s.emit(stack)
        print("B: ops", len(s.ops), "waits", s.n_waits)
    return nc


def host_inputs_B(pbf_list, pf32_list):
    bf = ml_dtypes.bfloat16
    maps = []
    full = []
    for b in range(2):
        ka = np.zeros((8192, 512), bf)
        va = np.zeros((8192, 512), bf)
        ki = np.zeros((8192, 64), bf)
        for j in range(4):
            c = b * 4 + j
            for k in range(4):
                g = 4 * k + j
                blk = pbf_list[c][512 * k:512 * (k + 1)]
                ka[512 * g:512 * (g + 1)] = blk[:, 512:1024]
                va[512 * g:512 * (g + 1)] = blk[:, 1024:1536]
                ki[512 * g:512 * (g + 1)] = blk[:, 1792:1856]
        kTl = np.ascontiguousarray(ka.reshape(8192, 4, 2, 64).transpose(2, 3, 1, 0).reshape(128, 4, 8192))
        full.append((kTl, np.ascontiguousarray(va.reshape(8192, 8, 64)), np.ascontiguousarray(ki.T)))
    kpos = np.ascontiguousarray(np.broadcast_to(np.arange(2048, dtype=np.float32)[None, :], (128, 2048)))
    ident = np.eye(128).astype(bf)
    for c in range(8):
        b, j = divmod(c, 4)
        p = pbf_list[c]
        qa = p[:, 0:512].reshape(4, 512, 4, 2, 64)
        qTl = np.ascontiguousarray(qa.transpose(3, 4, 0, 2, 1).reshape(128, 4, 4, 512))
        qi = p[:, 1536:1792].reshape(4, 512, 4, 64)
        qiTl = np.ascontiguousarray(qi.transpose(3, 0, 2, 1))
        wi = np.ascontiguousarray(pf32_list[c][:, 0:4].reshape(16, 128, 4).transpose(1, 0, 2))
        qrel = np.zeros((128, 16), np.float32)
        for k in range(4):
            g = 4 * k + j
            for qt in range(4):
                qrel[:, 4 * k + qt] = 512 * g + 128 * qt + np.arange(128) - 2048 * k
        maps.append({"kT": full[b][0], "v": full[b][1], "kiT": full[b][2], "qT": qTl, "qiT": qiTl, "wi": wi, "qrel": qrel,
                     "kpos": kpos, "ident": ident})
    return maps


SEG = 2048
NSEG = 4
CPS = SEG // 64


def build_G():
    nc = bass.Bass("TRN2", target_bir_lowering=False)
    D = lambda name, shape, dt, kind="ExternalInput": nc.dram_tensor(name, shape, dt, kind=kind).ap()
    qT = D("qT", [32, 8192], F32)
    kT = D("kT", [32, 8192], F32)
    vtok = D("vtok", [64, 128, 64], F32)
    glT = D("glT", [16, 8192], F32)
    wg = D("wg", [16, 32], F32)
    bg = D("bg", [32, 1], F32)
    rT = D("rT", [64, 8192], F32)
    gng = D("gng", [64, 1], F32)
    resetm = D("resetm", [32, SEG], F32)
    tri = D("tri", [64, 64], F32)
    ident = D("ident", [32, 32], F32)
    obT = D("obT", [64, 8192], BF16, "ExternalOutput")

    with ExitStack() as stack:
        T = lambda name, shape, dt: stack.enter_context(nc.sbuf_tensor(name, shape, dt))
        P = lambda name, shape, dt: stack.enter_context(nc.psum_tensor(name, shape, dt))
        qs = T("qs", [32, SEG], F32)
        ks = T("ks", [32, SEG], F32)
        vs = T("vs", [64, CPS, 64], F32)
        gls = T("gls", [16, SEG], F32)
        rs_ = T("rs", [64, SEG], F32)
        wgt = T("wgt", [16, 32], F32)
        bgt = T("bgt", [32, 1], F32)
        nbg = T("nbg", [32, 1], F32)
        gnt = T("gnt", [64, 1], F32)
        rmt = T("rmt", [32, SEG], F32)
        trit = T("trit", [64, 64], F32)
        idt = T("idt", [32, 32], F32)
        ones = T("ones", [64, 64], F32)
        epst = T("epst", [64, 1], F32)
        t1 = T("t1", [32, SEG], F32)
        cum = T("cum", [32, SEG], F32)
        eb = T("eb", [32, SEG], F32)
        enb = T("enb", [32, SEG], F32)
        qt_ = T("qt", [32, SEG], F32)
        kt_ = T("kt", [32, SEG], F32)
        ktok = T("ktok", [64, 2, 32], F32)
        am = T("am", [64, 2, 64], F32)
        U = T("U", [32, 2, 64], F32)
        S = T("S", [32, 2, 64], F32)
        oTs = T("oTs", [64, SEG], F32)
        sqs = T("sqs", [64, SEG], F32)
        rsd = T("rsd", [64, 512], F32)
        sil = T("sil", [64, SEG], F32)
        outb = T("outb", [64, SEG], BF16)
        pz = [P("pz%d" % i, [64, 512], F32) for i in range(2)]
        pk = [P("pk%d" % i, [64, 512], F32) for i in range(2)]
        pt_ = [P("pt%d" % i, [64, 512], F32) for i in range(2)]
        po = P("po", [64, 512], F32)
        pu = P("pu", [64, 512], F32)

        s = Sched(nc)
        for (dst, src, nm) in ((wgt, wg, "wgt"), (bgt, bg, "bgt"), (gnt, gng, "gnt"), (rmt, resetm, "rmt"), (trit, tri, "trit"), (idt, ident, "idt")):
            s.dma(lambda e, dst=dst, src=src: e.dma_start(out=dst[:], in_=src), writes=[nm])
        s.dve(lambda e: e.memset(ones[:], 1.0), writes=["ones"])
        s.dve(lambda e: e.memset(epst[:], 1e-6), writes=["epst"])
        s.dve(lambda e: e.memset(S[:], 0.0), writes=[("S", 0), ("S", 1)])
        s.dve(lambda e: e.tensor_scalar(out=nbg[:], in0=bgt[:], scalar1=-1.0, scalar2=None, op0=ALU.mult), reads=["bgt"], writes=["nbg"])
        cc = 0
        for sgi in range(NSEG):
            c0 = sgi * SEG
            s.dma(lambda e, c0=c0: e.dma_start(out=qs[:], in_=qT[:, c0:c0 + SEG]), writes=["qs"])
            s.dma(lambda e, c0=c0: e.dma_start(out=ks[:], in_=kT[:, c0:c0 + SEG]), writes=["ks"])
            s.dma(lambda e, sgi=sgi: e.dma_start(out=vs[:], in_=vtok[:, sgi * CPS:(sgi + 1) * CPS, :]), writes=["vs"])
            s.dma(lambda e, c0=c0: e.dma_start(out=gls[:], in_=glT[:, c0:c0 + SEG]), writes=["gls"])
            s.dma(lambda e, c0=c0: e.dma_start(out=rs_[:], in_=rT[:, c0:c0 + SEG]), writes=["rs"])
            for pc in range(SEG // 512):
                pzb = pz[pc % 2]
                s.pe(lambda e, pzb=pzb, pc=pc: e.matmul(pzb[0:32, :], lhsT=wgt[:], rhs=gls[:, pc * 512:(pc + 1) * 512], start=True, stop=True),
                     reads=["wgt", "gls"], writes=[("pz", pc % 2)])
                s.act(lambda e, pzb=pzb, pc=pc: e.activation(out=t1[:, pc * 512:(pc + 1) * 512], in_=pzb[0:32, :], func=AF.Exp, scale=-1.0, bias=nbg[:, 0:1]),
                      reads=[("pz", pc % 2), "nbg"], writes=["t1"])
            s.act(lambda e: e.activation(out=t1[:], in_=t1[:], func=AF.Ln, bias=1.0), reads=["t1"], writes=["t1"])
            s.dve(lambda e: e.tensor_tensor_scan(out=cum[:], data0=rmt[:], data1=t1[:], initial=0.0, op0=ALU.mult, op1=ALU.add),
                  reads=["rmt", "t1"], writes=["cum"])
            s.act(lambda e: e.activation(out=eb[:], in_=cum[:], func=AF.Exp, scale=-1.0 / 16), reads=["cum"], writes=["eb"])
            s.act(lambda e: e.activation(out=enb[:], in_=cum[:], func=AF.Exp, scale=1.0 / 16), reads=["cum"], writes=["enb"])
            s.dve(lambda e: e.scalar_tensor_tensor(out=qt_[:], in0=qs[:], scalar=32.0 ** -0.5, in1=eb[:], op0=ALU.mult, op1=ALU.mult),
                  reads=["qs", "eb"], writes=["qt"])
            s.dve(lambda e: e.tensor_tensor(out=kt_[:], in0=ks[:], in1=enb[:], op=ALU.mult), reads=["ks", "enb"], writes=["kt"])
            s.act(lambda e: e.activation(out=sil[:], in_=rs_[:], func=AF.Silu), reads=["rs"], writes=["sil"])
            for c in range(CPS):
                p = cc % 2
                cc += 1
                cs = slice(c * 64, (c + 1) * 64)
                ac = eb[:, c * 64 + 63:c * 64 + 64]
                sp, sn = p, 1 - p
                s.pe(lambda e, p=p, cs=cs: e.transpose(pk[p][:, 0:32], kt_[:, cs], idt[:]), reads=["kt", "idt"], writes=[("pk", p)])
                s.act(lambda e, p=p: e.activation(out=ktok[:, p, :], in_=pk[p][:, 0:32], func=AF.Copy), reads=[("pk", p)], writes=[("ktok", p)])
                s.pe(lambda e, p=p, cs=cs: e.matmul(pt_[p][:, 0:64], lhsT=kt_[:, cs], rhs=qt_[:, cs], start=True, stop=True),
                     reads=["kt", "qt"], writes=[("pt", p)])
                s.dve(lambda e, p=p: e.tensor_tensor(out=am[:, p, :], in0=pt_[p][:, 0:64], in1=trit[:], op=ALU.mult),
                      reads=[("pt", p), "trit"], writes=[("am", p)])
                s.pe(lambda e, p=p, c=c: e.matmul(po[:, 0:64], lhsT=vs[:, c, :], rhs=am[:, p, :], start=True, stop=False),
                     reads=["vs", ("am", p)], writes=["po"])
                s.pe(lambda e, sp=sp, cs=cs: e.matmul(po[:, 0:64], lhsT=S[:, sp, :], rhs=qt_[:, cs], start=False, stop=True),
                     reads=[("S", sp), "qt"], writes=["po"])
                s.act(lambda e, cs=cs: e.activation(out=oTs[:, cs], in_=po[:, 0:64], func=AF.Copy), reads=["po"], writes=["oTs"])
                s.pe(lambda e, p=p, c=c: e.matmul(pu[0:32, 0:64], lhsT=ktok[:, p, :], rhs=vs[:, c, :], start=True, stop=True),
                     reads=[("ktok", p), "vs"], writes=["pu"])
                s.act(lambda e, p=p, ac=ac: e.activation(out=U[:, p, :], in_=pu[0:32, 0:64], func=AF.Copy, scale=ac), reads=["pu", "eb"], writes=[("U", p)])
                s.dve(lambda e, p=p, sp=sp, sn=sn, ac=ac: e.scalar_tensor_tensor(out=S[:, sn, :], in0=S[:, sp, :], scalar=ac, in1=U[:, p, :],
                                                                               op0=ALU.mult, op1=ALU.add),
                      reads=[("S", sp), ("U", p), "eb"], writes=[("S", sn)])
            s.act(lambda e: e.activation(out=sqs[:], in_=oTs[:], func=AF.Square), reads=["oTs"], writes=["sqs"])
            for pc in range(SEG // 512):
                pzb = pz[pc % 2]
                ps_ = slice(pc * 512, (pc + 1) * 512)
                s.pe(lambda e, pzb=pzb, ps_=ps_: e.matmul(pzb[:, :], lhsT=ones[:], rhs=sqs[:, ps_], start=True, stop=True),
                     reads=["ones", "sqs"], writes=[("pz", pc % 2)])
                s.act(lambda e, pzb=pzb: e.activation(out=rsd[:], in_=pzb[:, :], func=AF.Sqrt, scale=1.0 / 64, bias=epst[:, 0:1]),
                      reads=[("pz", pc % 2), "epst"], writes=["rsd"])
                s.dve(lambda e: e.reciprocal(out=rsd[:], in_=rsd[:]), reads=["rsd"], writes=["rsd"])
                s.dve(lambda e, ps_=ps_: e.tensor_tensor(out=oTs[:, ps_], in0=oTs[:, ps_], in1=rsd[:], op=ALU.mult), reads=["oTs", "rsd"], writes=["oTs"])
            s.dve(lambda e: e.scalar_tensor_tensor(out=outb[:], in0=oTs[:], scalar=gnt[:, 0:1], in1=sil[:], op0=ALU.mult, op1=ALU.mult),
                  reads=["oTs", "gnt", "sil"], writes=["outb"])
            s.dma(lambda e, c0=c0: e.dma_start(out=obT[:, c0:c0 + SEG], in_=outb[:]), reads=["outb"])
        s.emit(stack)
        print("G: ops", len(s.ops), "waits", s.n_waits)
    return nc


def host_inputs_G(pf32_full, w_gate_up, b_gate, gla_norm_g):
    maps = []
    rm = np.ones((32, SEG), np.float32)
    rm[:, ::64] = 0.0
    tri = np.triu(np.ones((64, 64), np.float32))
    for c in range(8):
        b, h = divmod(c, 4)
        p = pf32_full[b]
        maps.append({
            "qT": np.ascontiguousarray(p[:, 4 + 32 * h:4 + 32 * (h + 1)].T),
            "kT": np.ascontiguousarray(p[:, 132 + 32 * h:132 + 32 * (h + 1)].T),
            "vtok": np.ascontiguousarray(p[:, 260 + 64 * h:260 + 64 * (h + 1)].reshape(128, 64, 64).transpose(1, 0, 2)),
            "glT": np.ascontiguousarray(p[:, 772:788].T),
            "wg": np.ascontiguousarray(w_gate_up[:, 32 * h:32 * (h + 1)]),
            "bg": np.ascontiguousarray(b_gate[32 * h:32 * (h + 1)].reshape(32, 1)),
            "rT": np.ascontiguousarray(p[:, 516 + 64 * h:516 + 64 * (h + 1)].T),
            "gng": np.ascontiguousarray(gla_norm_g[h].reshape(64, 1)),
            "resetm": rm, "tri": tri, "ident": np.eye(32, dtype=np.float32),
        })
    return maps


DFF = 2816
NFC = 22
UW = 256
SLOTW = 2 + 512
NCOL = 4 * SLOTW


def build_F(final=False):
    nc = bass.Bass("TRN2", target_bir_lowering=False)
    D = lambda name, shape, dt, kind="ExternalInput": nc.dram_tensor(name, shape, dt, kind=kind).ap()
    mixT = D("mixT", [1024, NCOL], BF16)
    xT = D("xT", [1024, NCOL], F32)
    w_out = D("w_out", [1024, 1024], F32)
    w_up = D("w_up", [1024, 2 * DFF], F32)
    w_down = D("w_down", [DFF, 1024], F32)
    g2 = D("g2", [128, 8], F32)
    cw = D("cw", [128, NFC, 4], F32)
    hflag = D("hflag", [128, 4], F32)
    gf = D("gf", [128, 8], F32)
    xoT = D("xoT", [1024, 2048], F32, "ExternalOutput")

    with ExitStack() as stack:
        T = lambda name, shape, dt: stack.enter_context(nc.sbuf_tensor(name, shape, dt))
        P = lambda name, shape, dt: stack.enter_context(nc.psum_tensor(name, shape, dt))
        wo = T("wo", [128, 8, 1024], BF16)
        wu = T("wu", [128, 8, 2 * DFF], BF16)
        wd = T("wd", [128, NFC, 1024], BF16)
        wst = T("wst", [128, 2, 1024], F32)
        g2t = T("g2t", [128, 8], F32)
        gft = T("gft", [128, 8], F32)
        cwt = T("cwt", [128, NFC, 4], F32)
        hft = T("hft", [128, 4], F32)
        ones = T("ones", [128, 128], F32)
        epst = T("epst", [128, 1], F32)
        mx = T("mx", [128, 8, UW], BF16)
        xm = T("xm", [128, 8, UW], F32)
        sq = T("sq", [128, 2, UW], F32)
        rs = T("rs", [128, UW], F32)
        h2 = T("h2", [128, 8, UW], BF16)
        actT = T("actT", [128, NFC, UW], BF16)
        aext = T("aext", [128, 2, 2 + UW], F32)
        cv = T("cv", [128, 2, UW], F32)
        sg = T("sg", [128, 2, UW], F32)
        atail = T("atail", [128, NFC, 2], F32)
        pa = [P("pa%d" % i, [128, 512], F32) for i in range(8)]

        s = Sched(nc)
        for (dst, src, nm) in ((g2t, g2, "g2t"), (gft, gf, "gft"), (cwt, cw, "cwt"), (hft, hflag, "hft")):
            s.dma(lambda e, dst=dst, src=src: e.dma_start(out=dst[:], in_=src), writes=[nm])
        s.dve(lambda e: e.memset(ones[:], 1.0), writes=["ones"])
        s.dve(lambda e: e.memset(epst[:], 1e-6), writes=["epst"])
        wc = 0
        def load_w(dst_fn, src_fn, nrow_chunks, ncols, regname, scale_g):
            nonlocal wc
            for c in range(nrow_chunks):
                for c0 in range(0, ncols, 1024):
                    c1 = min(ncols, c0 + 1024)
                    b = wc % 2
                    wc += 1
                    s.dma(lambda e, b=b, c=c, c0=c0, c1=c1: e.dma_start(out=wst[:, b, 0:c1 - c0], in_=src_fn(c, c0, c1)), writes=[("wst", b)])
                    eng = s.dve if (wc % 2 == 0) else s.pool
                    if scale_g:
                        eng(lambda e, b=b, c=c, c0=c0, c1=c1: e.tensor_scalar(out=dst_fn(c, c0, c1), in0=wst[:, b, 0:c1 - c0], scalar1=g2t[:, c:c + 1],
                                                                            scalar2=None, op0=ALU.mult),
                            reads=[("wst", b), "g2t"], writes=[(regname, c)])
                    else:
                        eng(lambda e, b=b, c=c, c0=c0, c1=c1: e.tensor_copy(out=dst_fn(c, c0, c1), in_=wst[:, b, 0:c1 - c0]),
                            reads=[("wst", b)], writes=[(regname, c)])
        load_w(lambda c, c0, c1: wo[:, c, c0:c1], lambda c, c0, c1: w_out[c * 128:(c + 1) * 128, c0:c1], 8, 1024, "wo", False)
        load_w(lambda c, c0, c1: wu[:, c, c0:c1], lambda c, c0, c1: w_up[c * 128:(c + 1) * 128, c0:c1], 8, 2 * DFF, "wu", True)
        load_w(lambda c, c0, c1: wd[:, c, c0:c1], lambda c, c0, c1: w_down[c * 128:(c + 1) * 128, c0:c1], NFC, 1024, "wd", False)
        WO = [("wo", c) for c in range(8)]
        WU = [("wu", c) for c in range(8)]
        WD = [("wd", c) for c in range(NFC)]

        def unit(k, col0, n, halo, out0):
            s.dma(lambda e: e.dma_start(out=mx[:, :, 0:n], in_=mixT[:, col0:col0 + n].rearrange("(c p) t -> p c t", p=128)), writes=["mx"])
            s.dma(lambda e: e.dma_start(out=xm[:, :, 0:n], in_=xT[:, col0:col0 + n].rearrange("(c p) t -> p c t", p=128)), writes=["xm"])
            for dc in range(8):
                pt = pa[dc % 2]
                for c in range(8):
                    s.pe(lambda e, pt=pt, c=c, dc=dc: e.matmul(pt[:, 0:n], lhsT=wo[:, c, dc * 128:(dc + 1) * 128], rhs=mx[:, c, 0:n],
                                                              start=(c == 0), stop=(c == 7)),
                         reads=WO + ["mx"], writes=[("pa", dc % 2)])
                s.dve(lambda e, pt=pt, dc=dc: e.tensor_tensor(out=xm[:, dc, 0:n], in0=pt[:, 0:n], in1=xm[:, dc, 0:n], op=ALU.add),
                      reads=[("pa", dc % 2), "xm"], writes=["xm"])
                s.act(lambda e, dc=dc: e.activation(out=sq[:, dc % 2, 0:n], in_=xm[:, dc, 0:n], func=AF.Square), reads=["xm"], writes=[("sq", dc % 2)])
                s.pe(lambda e, dc=dc: e.matmul(pa[2][:, 0:n], lhsT=ones[:], rhs=sq[:, dc % 2, 0:n], start=(dc == 0), stop=(dc == 7)),
                     reads=["ones", ("sq", dc % 2)], writes=[("pa", 2)])
            s.act(lambda e: e.activation(out=rs[:, 0:n], in_=pa[2][:, 0:n], func=AF.Sqrt, scale=1.0 / 1024, bias=epst[:, 0:1]),
                  reads=[("pa", 2), "epst"], writes=["rs"])
            s.dve(lambda e: e.reciprocal(out=rs[:, 0:n], in_=rs[:, 0:n]), reads=["rs"], writes=["rs"])
            for dc in range(8):
                eng = s.dve if dc % 2 == 0 else s.pool
                eng(lambda e, dc=dc: e.tensor_tensor(out=h2[:, dc, 0:n], in0=xm[:, dc, 0:n], in1=rs[:, 0:n], op=ALU.mult),
                    reads=["xm", "rs"], writes=["h2"])
            for fc in range(NFC):
                ab = fc % 2
                pA = pa[3 + ab]
                pB = pa[5 + ab]
                for c in range(8):
                    s.pe(lambda e, pA=pA, c=c, fc=fc: e.matmul(pA[:, 0:n], lhsT=wu[:, c, fc * 128:(fc + 1) * 128], rhs=h2[:, c, 0:n],
                                                              start=(c == 0), stop=(c == 7)),
                         reads=WU + ["h2"], writes=[("pa", 3 + ab)])
                if halo:
                    s.dve(lambda e, pA=pA, fc=fc, k=k: e.tensor_scalar(out=atail[:, fc, :], in0=pA[:, 0:2], scalar1=hft[:, k:k + 1], scalar2=None,
                                                                     op0=ALU.mult),
                          reads=[("pa", 3 + ab), "hft"], writes=[("atail", fc)])
                    continue
                for c in range(8):
                    s.pe(lambda e, pB=pB, c=c, fc=fc: e.matmul(pB[:, 0:n], lhsT=wu[:, c, DFF + fc * 128:DFF + (fc + 1) * 128], rhs=h2[:, c, 0:n],
                                                              start=(c == 0), stop=(c == 7)),
                         reads=WU + ["h2"], writes=[("pa", 5 + ab)])
                s.act(lambda e, ab=ab, fc=fc: e.activation(out=aext[:, ab, 0:2], in_=atail[:, fc, :], func=AF.Copy),
                      reads=[("atail", fc)], writes=[("aext", ab)])
                s.act(lambda e, ab=ab, pA=pA: e.activation(out=aext[:, ab, 2:2 + n], in_=pA[:, 0:n], func=AF.Copy),
                      reads=[("pa", 3 + ab)], writes=[("aext", ab)])
                s.act(lambda e, ab=ab, fc=fc: e.activation(out=atail[:, fc, :], in_=aext[:, ab, n:n + 2], func=AF.Copy),
                      reads=[("aext", ab)], writes=[("atail", fc)])
                s.dve(lambda e, ab=ab, fc=fc: e.tensor_scalar(out=cv[:, ab, 0:n], in0=aext[:, ab, 2:2 + n], scalar1=cwt[:, fc, 2:3], scalar2=cwt[:, fc, 3:4],
                                                            op0=ALU.mult, op1=ALU.add),
                      reads=[("aext", ab), "cwt"], writes=[("cv", ab)])
                s.dve(lambda e, ab=ab, fc=fc: e.scalar_tensor_tensor(out=cv[:, ab, 0:n], in0=aext[:, ab, 1:1 + n], scalar=cwt[:, fc, 1:2], in1=cv[:, ab, 0:n],
                                                                   op0=ALU.mult, op1=ALU.add),
                      reads=[("aext", ab), "cwt", ("cv", ab)], writes=[("cv", ab)])
                s.dve(lambda e, ab=ab, fc=fc: e.scalar_tensor_tensor(out=cv[:, ab, 0:n], in0=aext[:, ab, 0:n], scalar=cwt[:, fc, 0:1], in1=cv[:, ab, 0:n],
                                                                   op0=ALU.mult, op1=ALU.add),
                      reads=[("aext", ab), "cwt", ("cv", ab)], writes=[("cv", ab)])
                s.act(lambda e, ab=ab: e.activation(out=sg[:, ab, 0:n], in_=cv[:, ab, 0:n], func=AF.Silu), reads=[("cv", ab)], writes=[("sg", ab)])
                s.dve(lambda e, ab=ab, fc=fc, pB=pB: e.tensor_tensor(out=actT[:, fc, 0:n], in0=pB[:, 0:n], in1=sg[:, ab, 0:n], op=ALU.mult),
                      reads=[("pa", 5 + ab), ("sg", ab)], writes=[("actT", fc)])
            if halo:
                return
            AT = [("actT", fc) for fc in range(NFC)]
            for dc in range(8):
                pt = pa[dc % 2]
                for fc in range(NFC):
                    s.pe(lambda e, pt=pt, fc=fc, dc=dc: e.matmul(pt[:, 0:n], lhsT=wd[:, fc, dc * 128:(dc + 1) * 128], rhs=actT[:, fc, 0:n],
                                                                start=(fc == 0), stop=(fc == NFC - 1)),
                         reads=WD + AT, writes=[("pa", dc % 2)])
                s.dve(lambda e, pt=pt, dc=dc: e.tensor_tensor(out=xm[:, dc, 0:n], in0=pt[:, 0:n], in1=xm[:, dc, 0:n], op=ALU.add),
                      reads=[("pa", dc % 2), "xm"], writes=["xm"])
            if final:
                for dc in range(8):
                    s.act(lambda e, dc=dc: e.activation(out=sq[:, dc % 2, 0:n], in_=xm[:, dc, 0:n], func=AF.Square), reads=["xm"], writes=[("sq", dc % 2)])
                    s.pe(lambda e, dc=dc: e.matmul(pa[2][:, 0:n], lhsT=ones[:], rhs=sq[:, dc % 2, 0:n], start=(dc == 0), stop=(dc == 7)),
                         reads=["ones", ("sq", dc % 2)], writes=[("pa", 2)])
                s.act(lambda e: e.activation(out=rs[:, 0:n], in_=pa[2][:, 0:n], func=AF.Sqrt, scale=1.0 / 1024, bias=epst[:, 0:1]),
                      reads=[("pa", 2), "epst"], writes=["rs"])
                s.dve(lambda e: e.reciprocal(out=rs[:, 0:n], in_=rs[:, 0:n]), reads=["rs"], writes=["rs"])
                for dc in range(8):
                    s.dve(lambda e, dc=dc: e.scalar_tensor_tensor(out=xm[:, dc, 0:n], in0=xm[:, dc, 0:n], scalar=gft[:, dc:dc + 1], in1=rs[:, 0:n],
                                                                op0=ALU.mult, op1=ALU.mult),
                          reads=["xm", "rs", "gft"], writes=["xm"])
            s.dma(lambda e: e.dma_start(out=xoT[:, out0:out0 + n].rearrange("(c p) t -> p c t", p=128), in_=xm[:, :, 0:n]), reads=["xm"])

        for k in range(4):
            unit(k, k * SLOTW, 2, True, None)
            for u in range(512 // UW):
                unit(k, k * SLOTW + 2 + u * UW, UW, False, k * 512 + u * UW)
        s.emit(stack)
        print("F: ops", len(s.ops), "waits", s.n_waits)
    return nc


def host_inputs_F(mix_list, x_full, w_out, norm2_g, w_up, conv_w, conv_b, w_down, final_g):
    bf = ml_dtypes.bfloat16
    maps = []
    cw = np.zeros((128, NFC, 4), np.float32)
    cw[:, :, 0:3] = conv_w.T.reshape(NFC, 128, 3).transpose(1, 0, 2)
    cw[:, :, 3] = conv_b.reshape(NFC, 128).T
    for c in range(8):
        b, j = divmod(c, 4)
        mcols, xcols = [], []
        hf = np.ones((128, 4), np.float32)
        for k in range(4):
            g = 4 * k + j
            t0 = 512 * g
            if g == 0:
                mcols.append(np.zeros((2, 1024), bf))
                xcols.append(np.zeros((2, 1024), np.float32))
                hf[:, k] = 0.0
            else:
                mcols.append(mix_list[b][t0 - 2:t0])
                xcols.append(x_full[b, t0 - 2:t0])
            mcols.append(mix_list[b][t0:t0 + 512])
            xcols.append(x_full[b, t0:t0 + 512])
        maps.append({
            "mixT": np.ascontiguousarray(np.concatenate(mcols, 0).T), "xT": np.ascontiguousarray(np.concatenate(xcols, 0).T),
            "w_out": np.ascontiguousarray(w_out), "w_up": np.ascontiguousarray(w_up), "w_down": np.ascontiguousarray(w_down),
            "g2": np.ascontiguousarray(norm2_g.reshape(8, 128).T), "cw": cw, "hflag": hf,
            "gf": np.ascontiguousarray(final_g.reshape(8, 128).T),
        })
    return maps


_CACHE = {}
CHECK = None


def _get(name, fn):
    if name not in _CACHE:
        _CACHE[name] = fn()
    return _CACHE[name]


def _run(nc, maps):
    res = run_bass_kernel_spmd(nc, maps, core_ids=list(range(8)))
    return res.results


def forward(x, positions, norm1_g, w_in, w_gate_up, b_gate, gla_norm_g, w_pool, pool_scale, w_out, norm2_g, w_up, conv_w, conv_b,
            w_down, final_norm_g):
    bf = ml_dtypes.bfloat16
    x = np.asarray(x, np.float32)
    positions = np.asarray(positions, np.int32)
    depth = norm1_g.shape[0]
    ncA = _get("A", build_A)
    ncB = _get("B", build_B)
    ncG = _get("G", build_G)
    for l in range(depth):
        last = (l == depth - 1)
        rA = _run(ncA, host_inputs_A(x, positions, np.asarray(norm1_g[l]), np.asarray(w_in[l]), np.asarray(w_pool[l]), np.asarray(pool_scale[l])))
        pbf = [np.asarray(r["pbf"]) for r in rA]
        pf32 = [np.asarray(r["pf32"]) for r in rA]
        ocT = [np.asarray(r["ocT"]) for r in rA]
        if CHECK:
            CHECK("A", l, dict(pbf=pbf, pf32=pf32, ocT=ocT))
        rB = _run(ncB, host_inputs_B(pbf, pf32))
        oaT = [np.asarray(r["oaT"]) for r in rB]
        if CHECK:
            CHECK("B", l, dict(oaT=oaT))
        pf_full = []
        for b in range(2):
            full = np.zeros((8192, pf32[0].shape[1]), np.float32)
            for j in range(4):
                for k in range(4):
                    g = 4 * k + j
                    full[512 * g:512 * (g + 1)] = pf32[b * 4 + j][512 * k:512 * (k + 1)]
            pf_full.append(full)
        rG = _run(ncG, host_inputs_G(pf_full, np.asarray(w_gate_up[l]), np.asarray(b_gate[l]), np.asarray(gla_norm_g[l])))
        obT = [np.asarray(r["obT"]) for r in rG]
        if CHECK:
            CHECK("G", l, dict(obT=obT))
        mix = []
        for b in range(2):
            m = np.zeros((8192, 1024), bf)
            for j in range(4):
                c = b * 4 + j
                oa = oaT[c].transpose(2, 1, 0).reshape(2048, 512)
                oc = ocT[c].transpose(2, 1, 0).reshape(2048, 256)
                for k in range(4):
                    g = 4 * k + j
                    m[512 * g:512 * (g + 1), 0:512] = oa[512 * k:512 * (k + 1)]
                    m[512 * g:512 * (g + 1), 768:1024] = oc[512 * k:512 * (k + 1)]
            for h in range(4):
                m[:, 512 + 64 * h:512 + 64 * (h + 1)] = obT[b * 4 + h].T
            mix.append(m)
        if CHECK:
            CHECK("mix", l, dict(mix=mix))
        ncF = _get("F%d" % int(last), lambda: build_F(final=last))
        rF = _run(ncF, host_inputs_F(mix, x, np.asarray(w_out[l]), np.asarray(norm2_g[l]), np.asarray(w_up[l]), np.asarray(conv_w[l]),
                                     np.asarray(conv_b[l]), np.asarray(w_down[l]), np.asarray(final_norm_g)))
        xn = np.zeros_like(x)
        for c in range(8):
            b, j = divmod(c, 4)
            xo = np.asarray(rF[c]["xoT"]).T
            for k in range(4):
                g = 4 * k + j
                xn[b, 512 * g:512 * (g + 1)] = xo[512 * k:512 * (k + 1)]
        x = xn
        if CHECK:
            CHECK("F", l, dict(x=x))
    return x


def kernel(**inputs):
    out = forward(**{k: np.asarray(v) for k, v in inputs.items()})
    return np.ascontiguousarray(out.astype(np.float32))
```

```python
import numpy as np
import concourse.bass as bass
import concourse.mybir as mybir
from concourse.bass_utils import run_bass_kernel_spmd
from contextlib import ExitStack
import math
import ml_dtypes

F32 = mybir.dt.float32
BF16 = mybir.dt.bfloat16
I32 = mybir.dt.int32
ALU = mybir.AluOpType
AF = mybir.ActivationFunctionType
AX = mybir.AxisListType

ENGS = ("pe", "act", "dve", "pool", "sp")


class _Op:
    __slots__ = ("eng", "fn", "reads", "writes", "dma", "deps", "sig", "sigval", "dsem", "idx")


class Sched:
    def __init__(self, nc, n_dma_sems=6):
        self.nc = nc
        self.ops = []
        self.last_w = {}
        self.readers = {}
        self.n_dma_sems = n_dma_sems

    def add(self, eng, fn, reads=(), writes=(), dma=False):
        op = _Op()
        op.eng = eng
        op.fn = fn
        op.reads = tuple(reads)
        op.writes = tuple(writes)
        op.dma = dma
        op.idx = len(self.ops)
        deps = set()
        for r in op.reads:
            w = self.last_w.get(r)
            if w is not None:
                deps.add(w)
        for r in op.writes:
            w = self.last_w.get(r)
            if w is not None:
                deps.add(w)
            for rd in self.readers.get(r, ()):
                deps.add(rd)
        deps.discard(op.idx)
        op.deps = deps
        for r in op.reads:
            self.readers.setdefault(r, []).append(op.idx)
        for r in op.writes:
            self.last_w[r] = op.idx
            self.readers[r] = []
        op.sig = False
        op.sigval = None
        op.dsem = None
        self.ops.append(op)
        return op

    def pe(self, fn, reads=(), writes=()):
        return self.add("pe", fn, reads, writes)

    def act(self, fn, reads=(), writes=()):
        return self.add("act", fn, reads, writes)

    def dve(self, fn, reads=(), writes=()):
        return self.add("dve", fn, reads, writes)

    def pool(self, fn, reads=(), writes=()):
        return self.add("pool", fn, reads, writes)

    def dma(self, fn, reads=(), writes=(), q="sp"):
        return self.add(q, fn, reads, writes, dma=True)

    def emit(self, stack):
        nc = self.nc
        ops = self.ops
        for op in ops:
            for d in op.deps:
                dop = ops[d]
                if dop.eng == op.eng and not dop.dma:
                    if not (set(dop.writes) & set(op.reads)):
                        continue
                dop.sig = True
        for op in ops:
            if op.dma:
                op.sig = True
        esem = {e: stack.enter_context(nc.semaphore("s_" + e)) for e in ENGS}
        dsems = {}
        for q in ENGS:
            if any(o.dma and o.eng == q for o in ops):
                dsems[q] = [stack.enter_context(nc.semaphore("d_%s%d" % (q, i))) for i in range(self.n_dma_sems)]
        ecount = {e: 0 for e in ENGS}
        dcount = {q: [0] * self.n_dma_sems for q in dsems}
        drr = {q: 0 for q in dsems}
        prev_dma_wait = {}
        for op in ops:
            if op.dma:
                k = drr[op.eng]
                drr[op.eng] = (k + 1) % self.n_dma_sems
                prev_dma_wait[op.idx] = dcount[op.eng][k]
                dcount[op.eng][k] += 16
                op.dsem = (op.eng, k)
                op.sigval = dcount[op.eng][k]
            elif op.sig:
                ecount[op.eng] += 1
                op.sigval = ecount[op.eng]
        by_eng = {e: [o for o in ops if o.eng == e] for e in ENGS}
        block = stack.enter_context(nc.Block())
        self.n_waits = 0

        def run(ename, eobj):
            waited = {}
            for op in by_eng[ename]:
                need = {}
                for d in op.deps:
                    dop = ops[d]
                    if dop.dma:
                        key = ("d",) + dop.dsem
                        sem = dsems[dop.dsem[0]][dop.dsem[1]]
                    else:
                        if dop.eng == ename and not (set(dop.writes) & set(op.reads)):
                            continue
                        key = ("e", dop.eng)
                        sem = esem[dop.eng]
                    v = dop.sigval
                    if waited.get(key, 0) >= v:
                        continue
                    if key not in need or need[key][1] < v:
                        need[key] = (sem, v)
                if op.dma:
                    pv = prev_dma_wait[op.idx]
                    key = ("d",) + op.dsem
                    if pv > 0 and waited.get(key, 0) < pv:
                        if key not in need or need[key][1] < pv:
                            need[key] = (dsems[op.dsem[0]][op.dsem[1]], pv)
                for key, (sem, v) in need.items():
                    eobj.wait_ge(sem, v)
                    waited[key] = v
                    self.n_waits += 1
                ins = op.fn(eobj)
                if op.dma:
                    ins.then_inc(dsems[op.dsem[0]][op.dsem[1]], 16)
                elif op.sig:
                    ins.then_inc(esem[ename], 1)
            for q, lst in dsems.items():
                if q == ename:
                    for k, s in enumerate(lst):
                        if dcount[q][k] > 0:
                            eobj.wait_ge(s, dcount[q][k])

        if by_eng["pe"]:
            @block.tensor
            def _(e):
                run("pe", e)
        if by_eng["act"]:
            @block.scalar
            def _(e):
                run("act", e)
        if by_eng["dve"]:
            @block.vector
            def _(e):
                run("dve", e)
        if by_eng["pool"]:
            @block.gpsimd
            def _(e):
                run("pool", e)
        if by_eng["sp"]:
            @block.sync
            def _(e):
                run("sp", e)


NTILE = 20
INW = 2900
CH = [(0, 512), (512, 1024), (1024, 1536), (1536, 2048), (2048, 2560), (2560, 2900)]
UC0 = 2644
BFW = 1856
F32W = INW - BFW
TWO_PI = 2.0 * math.pi


def pool_mats(first):
    Mc = np.zeros((128, 4, 128), np.float32)
    Mh = np.zeros((128, 4, 128), np.float32)
    for gi, w in enumerate((2, 4, 8, 16)):
        for t in range(128):
            cnt = min(t + 1, w) if first else w
            for s in range(t - w + 1, t + 1):
                if s >= 0:
                    Mc[s, gi, t] += 1.0 / cnt
                elif not first:
                    Mh[128 + s, gi, t] += 1.0 / cnt
            Mc[t, gi, t] -= 1.0
    return Mc, Mh


def build_A():
    nc = bass.Bass("TRN2", target_bir_lowering=False)
    D = lambda name, shape, dt, kind="ExternalInput": nc.dram_tensor(name, shape, dt, kind=kind).ap()
    xtok = D("xtok", [NTILE * 128, 1024], F32)
    xT = D("xT", [1024, NTILE * 128], F32)
    w_in = D("w_in", [1024, INW], F32)
    g1 = D("g1", [128, 8], F32)
    pos = D("pos", [128, NTILE], I32)
    inv = D("inv", [128, 8], F32)
    wpool = D("wpool", [64, 4, 64], F32)
    pscale = D("pscale", [64, 4], F32)
    mcur = D("mcur", [128, 5, 4, 128], F32)
    mhal = D("mhal", [128, 5, 4, 128], F32)
    pbf = D("pbf", [2048, BFW], BF16, "ExternalOutput")
    pf32 = D("pf32", [2048, F32W], F32, "ExternalOutput")
    ocT = D("ocT", [64, 4, 2048], BF16, "ExternalOutput")

    with ExitStack() as stack:
        T = lambda name, shape, dt: stack.enter_context(nc.sbuf_tensor(name, shape, dt))
        P = lambda name, shape, dt: stack.enter_context(nc.psum_tensor(name, shape, dt))
        wbf = T("wbf", [128, 8, INW], BF16)
        wst = T("wst", [128, 2, INW], F32)
        g1t = T("g1t", [128, 8], F32)
        posi = T("posi", [128, NTILE], I32)
        posf = T("posf", [128, NTILE], F32)
        invt = T("invt", [128, 8], F32)
        ang = T("ang", [128, NTILE, 8], F32)
        kq = T("kq", [128, NTILE, 8], F32)
        angc = T("angc", [128, NTILE, 8], F32)
        kqi = T("kqi", [128, NTILE, 8], I32)
        cost = T("cost", [128, NTILE, 8], F32)
        sint = T("sint", [128, NTILE, 8], F32)
        wpt = T("wpt", [64, 4, 64], F32)
        pst = T("pst", [64, 4], F32)
        mct = T("mct", [128, 5, 4, 128], F32)
        mht = T("mht", [128, 5, 4, 128], F32)
        xtk = T("xtk", [128, 2, 1024], F32)
        sqj = T("sqj", [128, 1024], BF16)
        ss = T("ss", [128, 2], F32)
        rstd = T("rstd", [128, 2], F32)
        epst = T("epst", [128, 1], F32)
        xTt = T("xTt", [128, 2, 8, 128], F32)
        hT = T("hT", [128, 2, 8, 128], BF16)
        proj = T("proj", [128, 2, INW], F32)
        pb16 = T("pb16", [128, 2, BFW], BF16)
        rt = T("rt", [128, 4, 16, 8], F32)
        pooled = T("pooled", [64, 2, 4, 128], F32)
        oct_ = T("oct", [64, 2, 4, 128], BF16)
        ps = [P("ps%d" % i, [128, 512], F32) for i in range(4)]
        pp = [P("pp%d" % i, [64, 4, 128], F32) for i in range(2)]
        py = [P("py%d" % i, [64, 4, 128], F32) for i in range(2)]

        s = Sched(nc)
        s.dma(lambda e: e.dma_start(out=g1t[:], in_=g1), writes=["g1t"])
        s.dma(lambda e: e.dma_start(out=posi[:], in_=pos), writes=["posi"])
        s.dma(lambda e: e.dma_start(out=invt[:], in_=inv), writes=["invt"])
        s.dma(lambda e: e.dma_start(out=wpt[:], in_=wpool), writes=["wpt"])
        s.dma(lambda e: e.dma_start(out=pst[:], in_=pscale), writes=["pst"])
        s.dma(lambda e: e.dma_start(out=mct[:], in_=mcur), writes=["mct"])
        s.dma(lambda e: e.dma_start(out=mht[:], in_=mhal), writes=["mht"])
        s.dve(lambda e: e.memset(epst[:], 1e-6), writes=["epst"])
        s.dve(lambda e: e.tensor_copy(out=posf[:], in_=posi[:]), reads=["posi"], writes=["posf"])
        for t in range(NTILE):
            s.dve(lambda e, t=t: e.tensor_scalar(out=ang[:, t, :], in0=invt[:], scalar1=posf[:, t:t + 1], scalar2=None, op0=ALU.mult),
                  reads=["invt", "posf"], writes=["ang"])
        s.dve(lambda e: e.tensor_scalar(out=kqi[:], in0=ang[:], scalar1=1.0 / TWO_PI, scalar2=None, op0=ALU.mult), reads=["ang"], writes=["kqi"])
        s.dve(lambda e: e.tensor_copy(out=kq[:], in_=kqi[:]), reads=["kqi"], writes=["kq"])
        s.dve(lambda e: e.scalar_tensor_tensor(out=ang[:], in0=kq[:], scalar=-TWO_PI, in1=ang[:], op0=ALU.mult, op1=ALU.add),
              reads=["kq", "ang"], writes=["ang"])
        def wrap(y, name):
            s.dve(lambda e: e.tensor_scalar(out=kq[:], in0=y[:], scalar1=math.pi, scalar2=-TWO_PI, op0=ALU.is_gt, op1=ALU.mult),
                  reads=[name], writes=["kq"])
            s.dve(lambda e: e.tensor_tensor(out=y[:], in0=y[:], in1=kq[:], op=ALU.add), reads=[name, "kq"], writes=[name])
            s.dve(lambda e: e.tensor_scalar(out=kq[:], in0=y[:], scalar1=-math.pi, scalar2=TWO_PI, op0=ALU.is_lt, op1=ALU.mult),
                  reads=[name], writes=["kq"])
            s.dve(lambda e: e.tensor_tensor(out=y[:], in0=y[:], in1=kq[:], op=ALU.add), reads=[name, "kq"], writes=[name])
        s.dve(lambda e: e.tensor_scalar(out=angc[:], in0=ang[:], scalar1=math.pi / 2, scalar2=None, op0=ALU.add), reads=["ang"], writes=["angc"])
        wrap(ang, "ang")
        wrap(angc, "angc")
        s.act(lambda e: e.activation(out=sint[:], in_=ang[:], func=AF.Sin), reads=["ang"], writes=["sint"])
        s.act(lambda e: e.activation(out=cost[:], in_=angc[:], func=AF.Sin), reads=["angc"], writes=["cost"])

        for c in range(8):
            b = c % 2
            s.dma(lambda e, c=c, b=b: e.dma_start(out=wst[:, b, :], in_=w_in[c * 128:(c + 1) * 128, :]), writes=[("wst", b)])
            eng = s.dve if c % 2 == 0 else s.pool
            eng(lambda e, c=c, b=b: e.tensor_scalar(out=wbf[:, c, :], in0=wst[:, b, :], scalar1=g1t[:, c:c + 1], scalar2=None, op0=ALU.mult),
                reads=[("wst", b), "g1t"], writes=[("wbf", c)])

        own = 0
        for t in range(NTILE):
            k, i = divmod(t, 5)
            halo = (i == 0)
            b = t % 2
            pb = (t - 1) % 2
            s.dma(lambda e, t=t, b=b: e.dma_start(out=xtk[:, b, :], in_=xtok[t * 128:(t + 1) * 128, :]), writes=[("xtk", b)])
            s.dma(lambda e, t=t, b=b: e.dma_start(out=xTt[:, b, :, :], in_=xT[:, t * 128:(t + 1) * 128].rearrange("(c p) t -> p c t", p=128)),
                  writes=[("xTt", b)])
            s.act(lambda e, b=b: e.activation(out=sqj[:], in_=xtk[:, b, :], func=AF.Square, accum_out=ss[:, b:b + 1]),
                  reads=[("xtk", b)], writes=["sqj", ("ss", b)])
            s.act(lambda e, b=b: e.activation(out=ss[:, b:b + 1], in_=ss[:, b:b + 1], func=AF.Sqrt, scale=1.0 / 1024, bias=epst[:, 0:1]),
                  reads=[("ss", b), "epst"], writes=[("ss", b)])
            s.dve(lambda e, b=b: e.reciprocal(out=rstd[:, b:b + 1], in_=ss[:, b:b + 1]), reads=[("ss", b)], writes=[("rstd", b)])
            s.pool(lambda e, b=b: e.tensor_copy(out=hT[:, b, :, :], in_=xTt[:, b, :, :]), reads=[("xTt", b)], writes=[("hT", b)])
            chunks = [5] if halo else list(range(6))
            for n in chunks:
                n0, n1 = CH[n]
                pt = ps[n % 4]
                for c in range(8):
                    s.pe(lambda e, pt=pt, b=b, c=c, n0=n0, n1=n1: e.matmul(pt[:, 0:n1 - n0], lhsT=hT[:, b, c, :], rhs=wbf[:, c, n0:n1],
                                                                         start=(c == 0), stop=(c == 7)),
                         reads=[("hT", b), ("wbf", c)], writes=[("ps", n % 4)])
                if n % 2 == 0:
                    s.act(lambda e, pt=pt, b=b, n0=n0, n1=n1: e.activation(out=proj[:, b, n0:n1], in_=pt[:, 0:n1 - n0], func=AF.Copy,
                                                                         scale=rstd[:, b:b + 1]),
                          reads=[("ps", n % 4), ("rstd", b)], writes=[("proj", b, n)])
                else:
                    s.dve(lambda e, pt=pt, b=b, n0=n0, n1=n1: e.tensor_scalar(out=proj[:, b, n0:n1], in0=pt[:, 0:n1 - n0],
                                                                            scalar1=rstd[:, b:b + 1], scalar2=None, op0=ALU.mult),
                          reads=[("ps", n % 4), ("rstd", b)], writes=[("proj", b, n)])
            if not halo:
                for (c0, nh, regs) in ((0, 16, [("proj", b, 0), ("proj", b, 1)]), (1536, 5, [("proj", b, 3)])):
                    v = proj[:, b, c0:c0 + nh * 64].rearrange("p (h d) -> p h d", d=64)
                    x1 = v[:, :, 0:8]
                    x2 = v[:, :, 8:16]
                    cb = cost[:, t, :].unsqueeze(1).to_broadcast([128, nh, 8])
                    sb = sint[:, t, :].unsqueeze(1).to_broadcast([128, nh, 8])
                    t0 = rt[:, 0, 0:nh, :]
                    t1 = rt[:, 1, 0:nh, :]
                    t2 = rt[:, 2, 0:nh, :]
                    t3 = rt[:, 3, 0:nh, :]
                    R = regs + ["cost", "sint"]
                    s.dve(lambda e, t0=t0, x1=x1, cb=cb: e.tensor_tensor(out=t0, in0=x1, in1=cb, op=ALU.mult), reads=R, writes=["rt0"])
                    s.dve(lambda e, t1=t1, x2=x2, sb=sb: e.tensor_tensor(out=t1, in0=x2, in1=sb, op=ALU.mult), reads=R, writes=["rt1"])
                    s.dve(lambda e, t2=t2, x2=x2, cb=cb: e.tensor_tensor(out=t2, in0=x2, in1=cb, op=ALU.mult), reads=R, writes=["rt2"])
                    s.dve(lambda e, t3=t3, x1=x1, sb=sb: e.tensor_tensor(out=t3, in0=x1, in1=sb, op=ALU.mult), reads=R, writes=["rt3"])
                    s.dve(lambda e, t0=t0, t1=t1, x1=x1: e.tensor_tensor(out=x1, in0=t0, in1=t1, op=ALU.subtract),
                          reads=["rt0", "rt1", "rt2", "rt3"], writes=regs)
                    s.dve(lambda e, t2=t2, t3=t3, x2=x2: e.tensor_tensor(out=x2, in0=t2, in1=t3, op=ALU.add),
                          reads=["rt0", "rt1", "rt2", "rt3"], writes=regs)
                r0 = own * 128
                s.act(lambda e, b=b: e.activation(out=pb16[:, b, :], in_=proj[:, b, 0:BFW], func=AF.Copy),
                      reads=[("proj", b, n) for n in range(4)], writes=[("pb16", b)])
                s.dma(lambda e, b=b, r0=r0: e.dma_start(out=pbf[r0:r0 + 128, :], in_=pb16[:, b, :]), reads=[("pb16", b)])
                s.dma(lambda e, b=b, r0=r0: e.dma_start(out=pf32[r0:r0 + 128, :], in_=proj[:, b, BFW:INW]),
                      reads=[("proj", b, n) for n in (3, 4, 5)])
                mi = 0 if i > 1 else 1 + k
                ob = own % 2
                for gi in range(4):
                    s.pe(lambda e, ob=ob, b=b, gi=gi, mi=mi: e.matmul(pp[ob][:, gi, :], lhsT=proj[:, b, UC0 + gi * 64:UC0 + (gi + 1) * 64],
                                                                  rhs=mct[:, mi, gi, :], start=True, stop=False),
                         reads=[("proj", b, 5), "mct"], writes=[("pp", ob)])
                    s.pe(lambda e, ob=ob, pb=pb, gi=gi, mi=mi: e.matmul(pp[ob][:, gi, :], lhsT=proj[:, pb, UC0 + gi * 64:UC0 + (gi + 1) * 64],
                                                                    rhs=mht[:, mi, gi, :], start=False, stop=True),
                         reads=[("proj", pb, 5), "mht"], writes=[("pp", ob)])
                s.act(lambda e, ob=ob: e.activation(out=pooled[:, ob, :, :], in_=pp[ob][:], func=AF.Copy),
                      reads=[("pp", ob)], writes=[("pooled", ob)])
                for gi in range(4):
                    s.pe(lambda e, ob=ob, gi=gi: e.matmul(py[ob][:, gi, :], lhsT=wpt[:, gi, :], rhs=pooled[:, ob, gi, :], start=True, stop=True),
                         reads=[("pooled", ob), "wpt"], writes=[("py", ob)])
                for gi in range(4):
                    s.dve(lambda e, ob=ob, gi=gi: e.tensor_scalar(out=oct_[:, ob, gi, :], in0=py[ob][:, gi, :], scalar1=pst[:, gi:gi + 1],
                                                                scalar2=None, op0=ALU.mult),
                          reads=[("py", ob), "pst"], writes=[("oct", ob)])
                s.dma(lambda e, ob=ob, r0=r0: e.dma_start(out=ocT[:, :, r0:r0 + 128], in_=oct_[:, ob, :, :]), reads=[("oct", ob)])
                own += 1
        s.emit(stack)
        print("A: ops", len(s.ops), "waits", s.n_waits)
    return nc


def host_inputs_A(x, positions, norm1_g, w_in, w_pool, pool_scale):
    inv = (500000.0 ** (-np.arange(0, 16, 2, dtype=np.float32) / 16)).astype(np.float32)
    McG, MhG = pool_mats(False)
    McF, MhF = pool_mats(True)
    maps = []
    for c in range(8):
        b, j = divmod(c, 4)
        rows = []
        posl = []
        mcur = np.zeros((128, 5, 4, 128), np.float32)
        mhal = np.zeros((128, 5, 4, 128), np.float32)
        mcur[:, 0], mhal[:, 0] = McG, MhG
        for k in range(4):
            g = 4 * k + j
            t0 = 512 * g
            if g == 0:
                rows.append(np.zeros((128, 1024), np.float32))
                posl.append(np.zeros((128,), np.int32))
                mcur[:, 1 + k], mhal[:, 1 + k] = McF, MhF
            else:
                rows.append(x[b, t0 - 128:t0])
                posl.append(positions[b, t0 - 128:t0])
                mcur[:, 1 + k], mhal[:, 1 + k] = McG, MhG
            rows.append(x[b, t0:t0 + 512])
            posl.append(positions[b, t0:t0 + 512])
        xt = np.ascontiguousarray(np.concatenate(rows, 0))
        pl = np.concatenate(posl, 0).astype(np.int32)
        maps.append({
            "xtok": xt, "xT": np.ascontiguousarray(xt.T), "w_in": np.ascontiguousarray(w_in),
            "g1": np.ascontiguousarray(norm1_g.reshape(8, 128).T),
            "pos": np.ascontiguousarray(pl.reshape(NTILE, 128).T),
            "inv": np.ascontiguousarray(np.broadcast_to(inv[None, :], (128, 8))),
            "wpool": np.ascontiguousarray(w_pool.transpose(1, 0, 2)),
            "pscale": np.ascontiguousarray(pool_scale.reshape(4, 64).T),
            "mcur": mcur, "mhal": mhal,
        })
    return maps


NIT = 16
NEG = -1.0e30


def build_B(nslot=4, nit=NIT):
    nc = bass.Bass("TRN2", target_bir_lowering=False)
    D = lambda name, shape, dt, kind="ExternalInput": nc.dram_tensor(name, shape, dt, kind=kind).ap()
    kT = D("kT", [128, 4, 8192], BF16)
    vv = D("v", [8192, 512], BF16)
    kiT = D("kiT", [64, 8192], BF16)
    qT = D("qT", [128, 4, 4, 512], BF16)
    qiT = D("qiT", [64, 4, 4, 512], BF16)
    wi = D("wi", [128, 16, 4], F32)
    qrel = D("qrel", [128, 16], F32)
    kpos = D("kpos", [128, 2048], F32)
    ident = D("ident", [128, 128], BF16)
    oaT = D("oaT", [64, 8, 2048], BF16, "ExternalOutput")

    with ExitStack() as stack:
        T = lambda name, shape, dt: stack.enter_context(nc.sbuf_tensor(name, shape, dt))
        P = lambda name, shape, dt: stack.enter_context(nc.psum_tensor(name, shape, dt))
        kit = T("kit", [64, 8192], BF16)
        sc = T("sc", [128, 8192], F32)
        mk = T("mk", [128, 4, 8192], BF16)
        kpt = T("kpt", [128, 2048], F32)
        rr = T("rr", [128, 4, 512], F32)
        qTt = T("qTt", [128, 1, 4, 512], BF16)
        qit = T("qit", [64, 1, 4, 512], BF16)
        wit = T("wit", [128, 16, 4], F32)
        qrt = T("qrt", [128, 16], F32)
        idt = T("idt", [128, 128], BF16)
        kTs = T("kTs", [128, 2, 4, 512], BF16)
        vraw = T("vraw", [128, 2, 4, 512], BF16)
        vt = T("vt", [128, 2, 4, 520], BF16)
        E = T("E", [128, 3, 512], BF16)
        Pm = T("Pm", [128, 3, 512], BF16)
        mT = T("mT", [128, 2, 512], BF16)
        sm = T("sm", [128, 8], F32)
        ones1 = T("ones1", [128, 64], F32)
        rec = T("rec", [128, 512], F32)
        bcs = T("bcs", [64, 512], F32)
        oT = T("oT", [64, 2, 512], BF16)
        pb = [P("pb%d" % i, [128, 512], F32) for i in range(8)]
        pTb = pb[2][:].bitcast(BF16)

        s = Sched(nc)
        s.dma(lambda e: e.dma_start(out=kit[:], in_=kiT), writes=["kit"])
        s.dma(lambda e: e.dma_start(out=wit[:], in_=wi), writes=["wit"])
        s.dma(lambda e: e.dma_start(out=qrt[:], in_=qrel), writes=["qrt"])
        s.dma(lambda e: e.dma_start(out=kpt[:], in_=kpos), writes=["kpt"])
        s.dma(lambda e: e.dma_start(out=idt[:], in_=ident), writes=["idt"])
        s.pool(lambda e: e.memset(ones1[:], 1.0), writes=["ones1"])
        s.dve(lambda e: e.memset(vt[:], 1.0), writes=[("vt", 0), ("vt", 1)])
        cbias = rr[:].rearrange("p h n -> p (h n)")
        RRALL = [("rr", h) for h in range(4)]

        kbc = 0
        ec = 0
        mtc = 0
        otc = 0
        for k in range(nslot):
            L = 2048 * (k + 1)
            nkc = L // 512
            nkb = L // 128
            qb = 0
            s.dma(lambda e, k=k, qb=qb: e.dma_start(out=qTt[:, qb, :, :], in_=qT[:, k, :, :]), writes=[("qTt", qb)])
            s.dma(lambda e, k=k, qb=qb: e.dma_start(out=qit[:, qb, :, :], in_=qiT[:, k, :, :]), writes=[("qit", qb)])
            for qt in range(4):
                g = 4 * k + qt
                for n in range(nkc):
                    for h in range(4):
                        s.pe(lambda e, h=h, qb=qb, qt=qt, n=n: e.matmul(pb[h][:], lhsT=qit[:, qb, h, qt * 128:(qt + 1) * 128],
                                                                     rhs=kit[:, n * 512:(n + 1) * 512], start=True, stop=True),
                             reads=[("qit", qb), "kit"], writes=[("pb", h)])
                        s.act(lambda e, h=h: e.activation(out=rr[:, h, :], in_=pb[h][:], func=AF.Relu), reads=[("pb", h)], writes=[("rr", h)])
                        if h == 0:
                            s.dve(lambda e, n=n, g=g: e.tensor_scalar(out=sc[:, n * 512:(n + 1) * 512], in0=rr[:, 0, :], scalar1=wit[:, g, 0:1],
                                                                    scalar2=None, op0=ALU.mult),
                                  reads=[("rr", 0), "wit"], writes=[("sc", n)])
                        else:
                            s.dve(lambda e, n=n, g=g, h=h: e.scalar_tensor_tensor(out=sc[:, n * 512:(n + 1) * 512], in0=rr[:, h, :],
                                                                                 scalar=wit[:, g, h:h + 1], in1=sc[:, n * 512:(n + 1) * 512],
                                                                                 op0=ALU.mult, op1=ALU.add),
                                  reads=[("rr", h), "wit", ("sc", n)], writes=[("sc", n)])
                allsc = [("sc", n) for n in range(nkc)]
                s.dve(lambda e, L=L: e.tensor_reduce(out=sm[:, 0:1], in_=sc[:, 0:L], axis=AX.X, op=ALU.min), reads=allsc, writes=["lo"])
                s.dve(lambda e, g=g: e.tensor_scalar(out=cbias[:], in0=kpt[:], scalar1=qrt[:, g:g + 1], scalar2=NEG, op0=ALU.is_gt, op1=ALU.mult),
                      reads=["kpt", "qrt"], writes=RRALL)
                s.dve(lambda e, L=L: e.tensor_tensor(out=sc[:, L - 2048:L], in0=sc[:, L - 2048:L], in1=cbias[:], op=ALU.add),
                      reads=allsc + RRALL, writes=allsc)
                s.dve(lambda e, L=L: e.tensor_reduce(out=sm[:, 5:6], in_=sc[:, 0:L], axis=AX.X, op=ALU.max), reads=allsc, writes=["rmax"])
                s.dve(lambda e: e.tensor_tensor(out=sm[:, 1:2], in0=sm[:, 5:6], in1=sm[:, 0:1], op=ALU.subtract), reads=["rmax", "lo"], writes=["range"])
                for it in range(1, nit + 1):
                    f = 2.0 ** (-it)
                    s.dve(lambda e, f=f: e.tensor_scalar(out=sm[:, 2:3], in0=sm[:, 1:2], scalar1=f, scalar2=sm[:, 0:1], op0=ALU.mult, op1=ALU.add),
                          reads=["range", "lo"], writes=["mid"])
                    s.dve(lambda e, L=L, qt=qt: e.tensor_scalar(out=mk[:, qt, 0:L], in0=sc[:, 0:L], scalar1=sm[:, 2:3], scalar2=None,
                                                              op0=ALU.is_ge, op1=ALU.add, accum_out=sm[:, 3:4]),
                          reads=allsc + ["mid"], writes=[("mk", qt), "cnt"])
                    s.dve(lambda e, f=f: e.tensor_scalar(out=sm[:, 4:5], in0=sm[:, 3:4], scalar1=255.5, scalar2=f, op0=ALU.is_ge, op1=ALU.mult),
                          reads=["cnt"], writes=["pred"])
                    s.dve(lambda e: e.scalar_tensor_tensor(out=sm[:, 0:1], in0=sm[:, 4:5], scalar=sm[:, 1:2], in1=sm[:, 0:1], op0=ALU.mult, op1=ALU.add),
                          reads=["pred", "range", "lo"], writes=["lo"])
                s.dve(lambda e, L=L, qt=qt: e.tensor_scalar(out=mk[:, qt, 0:L], in0=sc[:, 0:L], scalar1=sm[:, 0:1], scalar2=None, op0=ALU.is_ge),
                      reads=allsc + ["lo"], writes=[("mk", qt)])
            steps = [(hp, kb, hl) for hp in range(2) for kb in range(nkb) for hl in range(4)]
            nst = len(steps)
            info = {}

            def load_sb(hp, sbk):
                nonlocal kbc
                kbuf = kbc % 2
                kbc += 1
                info[("kbuf", hp, sbk)] = kbuf
                s.dma(lambda e, sbk=sbk, kbuf=kbuf: e.dma_start(out=kTs[:, kbuf, :, :], in_=kT[:, :, sbk * 512:(sbk + 1) * 512]),
                      writes=[("kTs", kbuf)])
                s.dma(lambda e, sbk=sbk, kbuf=kbuf: e.dma_start(out=vraw[:, kbuf, :, :], in_=vv[sbk * 512:(sbk + 1) * 512, :].rearrange("(kb p) c -> p kb c", p=128)),
                      writes=[("vraw", kbuf)])
                for kl_ in range(4):
                    s.dve(lambda e, kbuf=kbuf, kl_=kl_: e.tensor_copy(out=vt[:, kbuf, kl_, :].rearrange("p (h c) -> p h c", c=65)[:, :, 0:64],
                                                                   in_=vraw[:, kbuf, kl_, :].rearrange("p (h d) -> p h d", d=64)),
                          reads=[("vraw", kbuf)], writes=[("vt", kbuf)])

            def pre(hp, kb):
                nonlocal mtc
                sbk, kl = divmod(kb, 4)
                mb = mtc % 2
                mtc += 1
                info[("mb", hp, kb)] = mb
                for qt in range(4):
                    s.pe(lambda e, qt=qt, kb=kb: e.transpose(pTb[:, qt * 128:(qt + 1) * 128], mk[:, qt, kb * 128:(kb + 1) * 128], idt[:]),
                         reads=[("mk", qt), "idt"], writes=[("pb", 2)])
                s.act(lambda e, mb=mb: e.activation(out=mT[:, mb, :], in_=pTb[:, 0:512], func=AF.Copy), reads=[("pb", 2)], writes=[("mT", mb)])

            def ST(i):
                hp, kb, hl = steps[i]
                sbk, kl = divmod(kb, 4)
                kbuf = info[("kbuf", hp, sbk)]
                h = hp * 4 + hl
                pr, hh = divmod(h, 2)
                sb = i % 2
                s.pe(lambda e, sb=sb, kbuf=kbuf, pr=pr, hh=hh, kl=kl: e.matmul(pb[sb][:], lhsT=kTs[hh * 64:(hh + 1) * 64, kbuf, pr, kl * 128:(kl + 1) * 128],
                                                                            rhs=qTt[hh * 64:(hh + 1) * 64, 0, pr, :], start=True, stop=True),
                     reads=[("kTs", kbuf), ("qTt", 0)], writes=[("pb", sb)])

            def rest_a(i):
                sb = i % 2
                eb = i % 3
                s.act(lambda e, sb=sb, eb=eb: e.activation(out=E[:, eb, :], in_=pb[sb][:], func=AF.Exp, scale=0.125),
                      reads=[("pb", sb)], writes=[("E", eb)])

            def rest(i):
                nonlocal otc
                hp, kb, hl = steps[i]
                sbk, kl = divmod(kb, 4)
                kbuf = info[("kbuf", hp, sbk)]
                mb = info[("mb", hp, kb)]
                h = hp * 4 + hl
                sb = i % 2
                eb = i % 3
                eng = s.dve if (i % 2 == 0) else s.pool
                eng(lambda e, eb=eb, mb=mb: e.tensor_tensor(out=Pm[:, eb, :], in0=E[:, eb, :], in1=mT[:, mb, :], op=ALU.mult),
                    reads=[("E", eb), ("mT", mb)], writes=[("Pm", eb)])
                s.pe(lambda e, hl=hl, kbuf=kbuf, h=h, eb=eb, kb=kb, kl=kl: e.matmul(pb[4 + hl][0:65, :], lhsT=vt[:, kbuf, kl, h * 65:(h + 1) * 65], rhs=Pm[:, eb, :],
                                                                                 start=(kb == 0), stop=(kb == nkb - 1)),
                     reads=[("vt", kbuf), ("Pm", eb)], writes=[("pb", 4 + hl)])
                if kb == nkb - 1:
                    ob = otc % 2
                    otc += 1
                    s.dve(lambda e, hl=hl: e.reciprocal(out=rec[64:65, :], in_=pb[4 + hl][64:65, :]), reads=[("pb", 4 + hl)], writes=["rec"])
                    s.pe(lambda e: e.matmul(pb[3][0:64, :], lhsT=ones1[64:65, :], rhs=rec[64:65, :], start=True, stop=True),
                         reads=["ones1", "rec"], writes=[("pb", 3)])
                    s.act(lambda e: e.activation(out=bcs[:], in_=pb[3][0:64, :], func=AF.Copy), reads=[("pb", 3)], writes=["bcs"])
                    s.dve(lambda e, hl=hl, ob=ob: e.tensor_tensor(out=oT[:, ob, :], in0=pb[4 + hl][0:64, :], in1=bcs[:], op=ALU.mult),
                          reads=[("pb", 4 + hl), "bcs"], writes=[("oT", ob)])
                    s.dma(lambda e, h=h, k=k, ob=ob: e.dma_start(out=oaT[:, h, k * 512:(k + 1) * 512], in_=oT[:, ob, :]), reads=[("oT", ob)])

            DPIPE = 2
            order = [(hp_, sb_) for hp_ in range(2) for sb_ in range(nkb // 4)]
            load_sb(*order[0])
            for j in range(min(DPIPE, nst)):
                if steps[j][2] == 0:
                    pre(steps[j][0], steps[j][1])
                ST(j)
            for i in range(nst):
                if steps[i][2] == 0 and steps[i][1] % 4 == 0:
                    oi = order.index((steps[i][0], steps[i][1] // 4))
                    if oi + 1 < len(order):
                        load_sb(*order[oi + 1])
                rest_a(i)
                j = i + DPIPE
                if j < nst:
                    if steps[j][2] == 0:
                        pre(steps[j][0], steps[j][1])
                    ST(j)
                rest(i)
        s.emit(stack)
        print("B: ops", len(s.ops), "waits", s.n_waits)
    return nc


def host_inputs_B(pbf_list, pf32_list):
    bf = ml_dtypes.bfloat16
    maps = []
    full = []
    for b in range(2):
        ka = np.zeros((8192, 512), bf)
        va = np.zeros((8192, 512), bf)
        ki = np.zeros((8192, 64), bf)
        for j in range(4):
            c = b * 4 + j
            for k in range(4):
                g = 4 * k + j
                blk = pbf_list[c][512 * k:512 * (k + 1)]
                ka[512 * g:512 * (g + 1)] = blk[:, 512:1024]
                va[512 * g:512 * (g + 1)] = blk[:, 1024:1536]
                ki[512 * g:512 * (g + 1)] = blk[:, 1792:1856]
        kTl = np.ascontiguousarray(ka.reshape(8192, 4, 2, 64).transpose(2, 3, 1, 0).reshape(128, 4, 8192))
        full.append((kTl, np.ascontiguousarray(va), np.ascontiguousarray(ki.T)))
    kpos = np.ascontiguousarray(np.broadcast_to(np.arange(2048, dtype=np.float32)[None, :], (128, 2048)))
    ident = np.eye(128).astype(bf)
    for c in range(8):
        b, j = divmod(c, 4)
        p = pbf_list[c]
        qa = p[:, 0:512].reshape(4, 512, 4, 2, 64)
        qTl = np.ascontiguousarray(qa.transpose(3, 4, 0, 2, 1).reshape(128, 4, 4, 512))
        qi = p[:, 1536:1792].reshape(4, 512, 4, 64)
        qiTl = np.ascontiguousarray(qi.transpose(3, 0, 2, 1))
        wi = np.ascontiguousarray(pf32_list[c][:, 0:4].reshape(16, 128, 4).transpose(1, 0, 2))
        qrel = np.zeros((128, 16), np.float32)
        for k in range(4):
            g = 4 * k + j
            for qt in range(4):
                qrel[:, 4 * k + qt] = 512 * g + 128 * qt + np.arange(128) - 2048 * k
        maps.append({"kT": full[b][0], "v": full[b][1], "kiT": full[b][2], "qT": qTl, "qiT": qiTl, "wi": wi, "qrel": qrel,
                     "kpos": kpos, "ident": ident})
    return maps


SEG = 2048
NSEG = 4
CPS = SEG // 64


def build_G():
    nc = bass.Bass("TRN2", target_bir_lowering=False)
    D = lambda name, shape, dt, kind="ExternalInput": nc.dram_tensor(name, shape, dt, kind=kind).ap()
    qT = D("qT", [32, 8192], F32)
    kT = D("kT", [32, 8192], F32)
    vtok = D("vtok", [64, 128, 64], F32)
    glT = D("glT", [16, 8192], F32)
    wg = D("wg", [16, 32], F32)
    bg = D("bg", [32, 1], F32)
    rT = D("rT", [64, 8192], F32)
    gng = D("gng", [64, 1], F32)
    resetm = D("resetm", [32, SEG], F32)
    tri = D("tri", [64, 64], F32)
    ident = D("ident", [32, 32], F32)
    obT = D("obT", [64, 8192], BF16, "ExternalOutput")

    with ExitStack() as stack:
        T = lambda name, shape, dt: stack.enter_context(nc.sbuf_tensor(name, shape, dt))
        P = lambda name, shape, dt: stack.enter_context(nc.psum_tensor(name, shape, dt))
        qs = T("qs", [32, SEG], F32)
        ks = T("ks", [32, SEG], F32)
        vs = T("vs", [64, CPS, 64], F32)
        gls = T("gls", [16, SEG], F32)
        rs_ = T("rs", [64, SEG], F32)
        wgt = T("wgt", [16, 32], F32)
        bgt = T("bgt", [32, 1], F32)
        nbg = T("nbg", [32, 1], F32)
        gnt = T("gnt", [64, 1], F32)
        rmt = T("rmt", [32, SEG], F32)
        trit = T("trit", [64, 64], F32)
        idt = T("idt", [32, 32], F32)
        ones = T("ones", [64, 64], F32)
        epst = T("epst", [64, 1], F32)
        t1 = T("t1", [32, SEG], F32)
        cum = T("cum", [32, SEG], F32)
        eb = T("eb", [32, SEG], F32)
        enb = T("enb", [32, SEG], F32)
        qt_ = T("qt", [32, SEG], F32)
        kt_ = T("kt", [32, SEG], F32)
        ktok = T("ktok", [64, 2, 32], F32)
        am = T("am", [64, 2, 64], F32)
        U = T("U", [32, 2, 64], F32)
        S = T("S", [32, 2, 64], F32)
        oTs = T("oTs", [64, SEG], F32)
        sqs = T("sqs", [64, SEG], F32)
        rsd = T("rsd", [64, 512], F32)
        sil = T("sil", [64, SEG], F32)
        outb = T("outb", [64, SEG], BF16)
        pz = [P("pz%d" % i, [64, 512], F32) for i in range(2)]
        pk = [P("pk%d" % i, [64, 512], F32) for i in range(2)]
        pt_ = [P("pt%d" % i, [64, 512], F32) for i in range(2)]
        po = P("po", [64, 512], F32)
        pu = P("pu", [64, 512], F32)

        s = Sched(nc)
        for (dst, src, nm) in ((wgt, wg, "wgt"), (bgt, bg, "bgt"), (gnt, gng, "gnt"), (rmt, resetm, "rmt"), (trit, tri, "trit"), (idt, ident, "idt")):
            s.dma(lambda e, dst=dst, src=src: e.dma_start(out=dst[:], in_=src), writes=[nm])
        s.dve(lambda e: e.memset(ones[:], 1.0), writes=["ones"])
        s.dve(lambda e: e.memset(epst[:], 1e-6), writes=["epst"])
        s.dve(lambda e: e.memset(S[:], 0.0), writes=[("S", 0), ("S", 1)])
        s.dve(lambda e: e.tensor_scalar(out=nbg[:], in0=bgt[:], scalar1=-1.0, scalar2=None, op0=ALU.mult), reads=["bgt"], writes=["nbg"])
        cc = 0
        for sgi in range(NSEG):
            c0 = sgi * SEG
            s.dma(lambda e, c0=c0: e.dma_start(out=qs[:], in_=qT[:, c0:c0 + SEG]), writes=["qs"])
            s.dma(lambda e, c0=c0: e.dma_start(out=ks[:], in_=kT[:, c0:c0 + SEG]), writes=["ks"])
            s.dma(lambda e, sgi=sgi: e.dma_start(out=vs[:], in_=vtok[:, sgi * CPS:(sgi + 1) * CPS, :]), writes=["vs"])
            s.dma(lambda e, c0=c0: e.dma_start(out=gls[:], in_=glT[:, c0:c0 + SEG]), writes=["gls"])
            s.dma(lambda e, c0=c0: e.dma_start(out=rs_[:], in_=rT[:, c0:c0 + SEG]), writes=["rs"])
            for pc in range(SEG // 512):
                pzb = pz[pc % 2]
                s.pe(lambda e, pzb=pzb, pc=pc: e.matmul(pzb[0:32, :], lhsT=wgt[:], rhs=gls[:, pc * 512:(pc + 1) * 512], start=True, stop=True),
                     reads=["wgt", "gls"], writes=[("pz", pc % 2)])
                s.act(lambda e, pzb=pzb, pc=pc: e.activation(out=t1[:, pc * 512:(pc + 1) * 512], in_=pzb[0:32, :], func=AF.Exp, scale=-1.0, bias=nbg[:, 0:1]),
                      reads=[("pz", pc % 2), "nbg"], writes=["t1"])
            s.act(lambda e: e.activation(out=t1[:], in_=t1[:], func=AF.Ln, bias=1.0), reads=["t1"], writes=["t1"])
            s.dve(lambda e: e.tensor_tensor_scan(out=cum[:], data0=rmt[:], data1=t1[:], initial=0.0, op0=ALU.mult, op1=ALU.add),
                  reads=["rmt", "t1"], writes=["cum"])
            s.act(lambda e: e.activation(out=eb[:], in_=cum[:], func=AF.Exp, scale=-1.0 / 16), reads=["cum"], writes=["eb"])
            s.act(lambda e: e.activation(out=enb[:], in_=cum[:], func=AF.Exp, scale=1.0 / 16), reads=["cum"], writes=["enb"])
            s.dve(lambda e: e.scalar_tensor_tensor(out=qt_[:], in0=qs[:], scalar=32.0 ** -0.5, in1=eb[:], op0=ALU.mult, op1=ALU.mult),
                  reads=["qs", "eb"], writes=["qt"])
            s.dve(lambda e: e.tensor_tensor(out=kt_[:], in0=ks[:], in1=enb[:], op=ALU.mult), reads=["ks", "enb"], writes=["kt"])
            s.act(lambda e: e.activation(out=sil[:], in_=rs_[:], func=AF.Silu), reads=["rs"], writes=["sil"])
            for c in range(CPS):
                p = cc % 2
                cc += 1
                cs = slice(c * 64, (c + 1) * 64)
                ac = eb[:, c * 64 + 63:c * 64 + 64]
                sp, sn = p, 1 - p
                s.pe(lambda e, p=p, cs=cs: e.transpose(pk[p][:, 0:32], kt_[:, cs], idt[:]), reads=["kt", "idt"], writes=[("pk", p)])
                s.act(lambda e, p=p: e.activation(out=ktok[:, p, :], in_=pk[p][:, 0:32], func=AF.Copy), reads=[("pk", p)], writes=[("ktok", p)])
                s.pe(lambda e, p=p, cs=cs: e.matmul(pt_[p][:, 0:64], lhsT=kt_[:, cs], rhs=qt_[:, cs], start=True, stop=True),
                     reads=["kt", "qt"], writes=[("pt", p)])
                s.dve(lambda e, p=p: e.tensor_tensor(out=am[:, p, :], in0=pt_[p][:, 0:64], in1=trit[:], op=ALU.mult),
                      reads=[("pt", p), "trit"], writes=[("am", p)])
                s.pe(lambda e, p=p, c=c: e.matmul(po[:, 0:64], lhsT=vs[:, c, :], rhs=am[:, p, :], start=True, stop=False),
                     reads=["vs", ("am", p)], writes=["po"])
                s.pe(lambda e, sp=sp, cs=cs: e.matmul(po[:, 0:64], lhsT=S[:, sp, :], rhs=qt_[:, cs], start=False, stop=True),
                     reads=[("S", sp), "qt"], writes=["po"])
                s.act(lambda e, cs=cs: e.activation(out=oTs[:, cs], in_=po[:, 0:64], func=AF.Copy), reads=["po"], writes=["oTs"])
                s.pe(lambda e, p=p, c=c: e.matmul(pu[0:32, 0:64], lhsT=ktok[:, p, :], rhs=vs[:, c, :], start=True, stop=True),
                     reads=[("ktok", p), "vs"], writes=["pu"])
                s.act(lambda e, p=p, ac=ac: e.activation(out=U[:, p, :], in_=pu[0:32, 0:64], func=AF.Copy, scale=ac), reads=["pu", "eb"], writes=[("U", p)])
                s.dve(lambda e, p=p, sp=sp, sn=sn, ac=ac: e.scalar_tensor_tensor(out=S[:, sn, :], in0=S[:, sp, :], scalar=ac, in1=U[:, p, :],
                                                                               op0=ALU.mult, op1=ALU.add),
                      reads=[("S", sp), ("U", p), "eb"], writes=[("S", sn)])
            s.act(lambda e: e.activation(out=sqs[:], in_=oTs[:], func=AF.Square), reads=["oTs"], writes=["sqs"])
            for pc in range(SEG // 512):
                pzb = pz[pc % 2]
                ps_ = slice(pc * 512, (pc + 1) * 512)
                s.pe(lambda e, pzb=pzb, ps_=ps_: e.matmul(pzb[:, :], lhsT=ones[:], rhs=sqs[:, ps_], start=True, stop=True),
                     reads=["ones", "sqs"], writes=[("pz", pc % 2)])
                s.act(lambda e, pzb=pzb: e.activation(out=rsd[:], in_=pzb[:, :], func=AF.Sqrt, scale=1.0 / 64, bias=epst[:, 0:1]),
                      reads=[("pz", pc % 2), "epst"], writes=["rsd"])
                s.dve(lambda e: e.reciprocal(out=rsd[:], in_=rsd[:]), reads=["rsd"], writes=["rsd"])
                s.dve(lambda e, ps_=ps_: e.tensor_tensor(out=oTs[:, ps_], in0=oTs[:, ps_], in1=rsd[:], op=ALU.mult), reads=["oTs", "rsd"], writes=["oTs"])
            s.dve(lambda e: e.scalar_tensor_tensor(out=outb[:], in0=oTs[:], scalar=gnt[:, 0:1], in1=sil[:], op0=ALU.mult, op1=ALU.mult),
                  reads=["oTs", "gnt", "sil"], writes=["outb"])
            s.dma(lambda e, c0=c0: e.dma_start(out=obT[:, c0:c0 + SEG], in_=outb[:]), reads=["outb"])
        s.emit(stack)
        print("G: ops", len(s.ops), "waits", s.n_waits)
    return nc


def host_inputs_G(pf32_full, w_gate_up, b_gate, gla_norm_g):
    maps = []
    rm = np.ones((32, SEG), np.float32)
    rm[:, ::64] = 0.0
    tri = np.triu(np.ones((64, 64), np.float32))
    for c in range(8):
        b, h = divmod(c, 4)
        p = pf32_full[b]
        maps.append({
            "qT": np.ascontiguousarray(p[:, 4 + 32 * h:4 + 32 * (h + 1)].T),
            "kT": np.ascontiguousarray(p[:, 132 + 32 * h:132 + 32 * (h + 1)].T),
            "vtok": np.ascontiguousarray(p[:, 260 + 64 * h:260 + 64 * (h + 1)].reshape(128, 64, 64).transpose(1, 0, 2)),
            "glT": np.ascontiguousarray(p[:, 772:788].T),
            "wg": np.ascontiguousarray(w_gate_up[:, 32 * h:32 * (h + 1)]),
            "bg": np.ascontiguousarray(b_gate[32 * h:32 * (h + 1)].reshape(32, 1)),
            "rT": np.ascontiguousarray(p[:, 516 + 64 * h:516 + 64 * (h + 1)].T),
            "gng": np.ascontiguousarray(gla_norm_g[h].reshape(64, 1)),
            "resetm": rm, "tri": tri, "ident": np.eye(32, dtype=np.float32),
        })
    return maps


DFF = 2816
NFC = 22
UW = 256
SLOTW = 2 + 512
NCOL = 4 * SLOTW


def build_F(final=False):
    nc = bass.Bass("TRN2", target_bir_lowering=False)
    D = lambda name, shape, dt, kind="ExternalInput": nc.dram_tensor(name, shape, dt, kind=kind).ap()
    mixT = D("mixT", [1024, NCOL], BF16)
    xT = D("xT", [1024, NCOL], F32)
    w_out = D("w_out", [1024, 1024], F32)
    w_up = D("w_up", [1024, 2 * DFF], F32)
    w_down = D("w_down", [DFF, 1024], F32)
    g2 = D("g2", [128, 8], F32)
    cw = D("cw", [128, NFC, 4], F32)
    hflag = D("hflag", [128, 4], F32)
    gf = D("gf", [128, 8], F32)
    xoT = D("xoT", [1024, 2048], F32, "ExternalOutput")

    with ExitStack() as stack:
        T = lambda name, shape, dt: stack.enter_context(nc.sbuf_tensor(name, shape, dt))
        P = lambda name, shape, dt: stack.enter_context(nc.psum_tensor(name, shape, dt))
        wo = T("wo", [128, 8, 1024], BF16)
        wu = T("wu", [128, 8, 2 * DFF], BF16)
        wd = T("wd", [128, NFC, 1024], BF16)
        wst = T("wst", [128, 2, 1024], F32)
        g2t = T("g2t", [128, 8], F32)
        gft = T("gft", [128, 8], F32)
        cwt = T("cwt", [128, NFC, 4], F32)
        hft = T("hft", [128, 4], F32)
        ones = T("ones", [128, 128], F32)
        epst = T("epst", [128, 1], F32)
        mx = T("mx", [128, 8, UW], BF16)
        xm = T("xm", [128, 8, UW], F32)
        sq = T("sq", [128, 2, UW], F32)
        rs = T("rs", [128, UW], F32)
        h2 = T("h2", [128, 8, UW], BF16)
        actT = T("actT", [128, NFC, UW], BF16)
        aext = T("aext", [128, 2, 2 + UW], F32)
        cv = T("cv", [128, 2, UW], F32)
        sg = T("sg", [128, 2, UW], F32)
        atail = T("atail", [128, NFC, 2], F32)
        pa = [P("pa%d" % i, [128, 512], F32) for i in range(8)]

        s = Sched(nc)
        for (dst, src, nm) in ((g2t, g2, "g2t"), (gft, gf, "gft"), (cwt, cw, "cwt"), (hft, hflag, "hft")):
            s.dma(lambda e, dst=dst, src=src: e.dma_start(out=dst[:], in_=src), writes=[nm])
        s.dve(lambda e: e.memset(ones[:], 1.0), writes=["ones"])
        s.dve(lambda e: e.memset(epst[:], 1e-6), writes=["epst"])
        wc = 0
        def load_w(dst_fn, src_fn, nrow_chunks, ncols, regname, scale_g):
            nonlocal wc
            for c in range(nrow_chunks):
                for c0 in range(0, ncols, 1024):
                    c1 = min(ncols, c0 + 1024)
                    b = wc % 2
                    wc += 1
                    s.dma(lambda e, b=b, c=c, c0=c0, c1=c1: e.dma_start(out=wst[:, b, 0:c1 - c0], in_=src_fn(c, c0, c1)), writes=[("wst", b)])
                    use_dve = (wc % 2 == 0)
                    if scale_g:
                        if use_dve:
                            s.dve(lambda e, b=b, c=c, c0=c0, c1=c1: e.tensor_scalar(out=dst_fn(c, c0, c1), in0=wst[:, b, 0:c1 - c0], scalar1=g2t[:, c:c + 1],
                                                                                  scalar2=None, op0=ALU.mult),
                                  reads=[("wst", b), "g2t"], writes=[(regname, c)])
                        else:
                            s.act(lambda e, b=b, c=c, c0=c0, c1=c1: e.activation(out=dst_fn(c, c0, c1), in_=wst[:, b, 0:c1 - c0], func=AF.Copy,
                                                                               scale=g2t[:, c:c + 1]),
                                  reads=[("wst", b), "g2t"], writes=[(regname, c)])
                    else:
                        if use_dve:
                            s.dve(lambda e, b=b, c=c, c0=c0, c1=c1: e.tensor_copy(out=dst_fn(c, c0, c1), in_=wst[:, b, 0:c1 - c0]),
                                  reads=[("wst", b)], writes=[(regname, c)])
                        else:
                            s.act(lambda e, b=b, c=c, c0=c0, c1=c1: e.activation(out=dst_fn(c, c0, c1), in_=wst[:, b, 0:c1 - c0], func=AF.Copy),
                                  reads=[("wst", b)], writes=[(regname, c)])
        load_w(lambda c, c0, c1: wo[:, c, c0:c1], lambda c, c0, c1: w_out[c * 128:(c + 1) * 128, c0:c1], 8, 1024, "wo", False)
        load_w(lambda c, c0, c1: wu[:, c, c0:c1], lambda c, c0, c1: w_up[c * 128:(c + 1) * 128, c0:c1], 8, 2 * DFF, "wu", True)
        load_w(lambda c, c0, c1: wd[:, c, c0:c1], lambda c, c0, c1: w_down[c * 128:(c + 1) * 128, c0:c1], NFC, 1024, "wd", False)
        WO = [("wo", c) for c in range(8)]
        WU = [("wu", c) for c in range(8)]
        WD = [("wd", c) for c in range(NFC)]

        def unit(k, col0, n, halo, out0):
            s.dma(lambda e: e.dma_start(out=mx[:, :, 0:n], in_=mixT[:, col0:col0 + n].rearrange("(c p) t -> p c t", p=128)), writes=["mx"])
            s.dma(lambda e: e.dma_start(out=xm[:, :, 0:n], in_=xT[:, col0:col0 + n].rearrange("(c p) t -> p c t", p=128)), writes=["xm"])
            for dc in range(8):
                pt = pa[dc % 2]
                for c in range(8):
                    s.pe(lambda e, pt=pt, c=c, dc=dc: e.matmul(pt[:, 0:n], lhsT=wo[:, c, dc * 128:(dc + 1) * 128], rhs=mx[:, c, 0:n],
                                                              start=(c == 0), stop=(c == 7)),
                         reads=WO + ["mx"], writes=[("pa", dc % 2)])
                s.dve(lambda e, pt=pt, dc=dc: e.tensor_tensor(out=xm[:, dc, 0:n], in0=pt[:, 0:n], in1=xm[:, dc, 0:n], op=ALU.add),
                      reads=[("pa", dc % 2), "xm"], writes=["xm"])
                s.act(lambda e, dc=dc: e.activation(out=sq[:, dc % 2, 0:n], in_=xm[:, dc, 0:n], func=AF.Square), reads=["xm"], writes=[("sq", dc % 2)])
                s.pe(lambda e, dc=dc: e.matmul(pa[2][:, 0:n], lhsT=ones[:], rhs=sq[:, dc % 2, 0:n], start=(dc == 0), stop=(dc == 7)),
                     reads=["ones", ("sq", dc % 2)], writes=[("pa", 2)])
            s.act(lambda e: e.activation(out=rs[:, 0:n], in_=pa[2][:, 0:n], func=AF.Sqrt, scale=1.0 / 1024, bias=epst[:, 0:1]),
                  reads=[("pa", 2), "epst"], writes=["rs"])
            s.dve(lambda e: e.reciprocal(out=rs[:, 0:n], in_=rs[:, 0:n]), reads=["rs"], writes=["rs"])
            for dc in range(8):
                s.dve(lambda e, dc=dc: e.tensor_tensor(out=h2[:, dc, 0:n], in0=xm[:, dc, 0:n], in1=rs[:, 0:n], op=ALU.mult),
                    reads=["xm", "rs"], writes=["h2"])
            for fc in range(NFC):
                ab = fc % 2
                pA = pa[3 + ab]
                pB = pa[5 + ab]
                for c in range(8):
                    s.pe(lambda e, pA=pA, c=c, fc=fc: e.matmul(pA[:, 0:n], lhsT=wu[:, c, fc * 128:(fc + 1) * 128], rhs=h2[:, c, 0:n],
                                                              start=(c == 0), stop=(c == 7)),
                         reads=WU + ["h2"], writes=[("pa", 3 + ab)])
                if halo:
                    s.dve(lambda e, pA=pA, fc=fc, k=k: e.tensor_scalar(out=atail[:, fc, :], in0=pA[:, 0:2], scalar1=hft[:, k:k + 1], scalar2=None,
                                                                     op0=ALU.mult),
                          reads=[("pa", 3 + ab), "hft"], writes=[("atail", fc)])
                    continue
                for c in range(8):
                    s.pe(lambda e, pB=pB, c=c, fc=fc: e.matmul(pB[:, 0:n], lhsT=wu[:, c, DFF + fc * 128:DFF + (fc + 1) * 128], rhs=h2[:, c, 0:n],
                                                              start=(c == 0), stop=(c == 7)),
                         reads=WU + ["h2"], writes=[("pa", 5 + ab)])
                s.act(lambda e, ab=ab, fc=fc: e.activation(out=aext[:, ab, 0:2], in_=atail[:, fc, :], func=AF.Copy),
                      reads=[("atail", fc)], writes=[("aext", ab)])
                s.act(lambda e, ab=ab, pA=pA: e.activation(out=aext[:, ab, 2:2 + n], in_=pA[:, 0:n], func=AF.Copy),
                      reads=[("pa", 3 + ab)], writes=[("aext", ab)])
                s.act(lambda e, ab=ab, fc=fc: e.activation(out=atail[:, fc, :], in_=aext[:, ab, n:n + 2], func=AF.Copy),
                      reads=[("aext", ab)], writes=[("atail", fc)])
                s.dve(lambda e, ab=ab, fc=fc: e.tensor_scalar(out=cv[:, ab, 0:n], in0=aext[:, ab, 2:2 + n], scalar1=cwt[:, fc, 2:3], scalar2=cwt[:, fc, 3:4],
                                                            op0=ALU.mult, op1=ALU.add),
                      reads=[("aext", ab), "cwt"], writes=[("cv", ab)])
                s.dve(lambda e, ab=ab, fc=fc: e.scalar_tensor_tensor(out=cv[:, ab, 0:n], in0=aext[:, ab, 1:1 + n], scalar=cwt[:, fc, 1:2], in1=cv[:, ab, 0:n],
                                                                   op0=ALU.mult, op1=ALU.add),
                      reads=[("aext", ab), "cwt", ("cv", ab)], writes=[("cv", ab)])
                s.dve(lambda e, ab=ab, fc=fc: e.scalar_tensor_tensor(out=cv[:, ab, 0:n], in0=aext[:, ab, 0:n], scalar=cwt[:, fc, 0:1], in1=cv[:, ab, 0:n],
                                                                   op0=ALU.mult, op1=ALU.add),
                      reads=[("aext", ab), "cwt", ("cv", ab)], writes=[("cv", ab)])
                s.act(lambda e, ab=ab: e.activation(out=sg[:, ab, 0:n], in_=cv[:, ab, 0:n], func=AF.Silu), reads=[("cv", ab)], writes=[("sg", ab)])
                s.dve(lambda e, ab=ab, fc=fc, pB=pB: e.tensor_tensor(out=actT[:, fc, 0:n], in0=pB[:, 0:n], in1=sg[:, ab, 0:n], op=ALU.mult),
                      reads=[("pa", 5 + ab), ("sg", ab)], writes=[("actT", fc)])
            if halo:
                return
            AT = [("actT", fc) for fc in range(NFC)]
            for dc in range(8):
                pt = pa[dc % 2]
                for fc in range(NFC):
                    s.pe(lambda e, pt=pt, fc=fc, dc=dc: e.matmul(pt[:, 0:n], lhsT=wd[:, fc, dc * 128:(dc + 1) * 128], rhs=actT[:, fc, 0:n],
                                                                start=(fc == 0), stop=(fc == NFC - 1)),
                         reads=WD + AT, writes=[("pa", dc % 2)])
                s.dve(lambda e, pt=pt, dc=dc: e.tensor_tensor(out=xm[:, dc, 0:n], in0=pt[:, 0:n], in1=xm[:, dc, 0:n], op=ALU.add),
                      reads=[("pa", dc % 2), "xm"], writes=["xm"])
            if final:
                for dc in range(8):
                    s.act(lambda e, dc=dc: e.activation(out=sq[:, dc % 2, 0:n], in_=xm[:, dc, 0:n], func=AF.Square), reads=["xm"], writes=[("sq", dc % 2)])
                    s.pe(lambda e, dc=dc: e.matmul(pa[2][:, 0:n], lhsT=ones[:], rhs=sq[:, dc % 2, 0:n], start=(dc == 0), stop=(dc == 7)),
                         reads=["ones", ("sq", dc % 2)], writes=[("pa", 2)])
                s.act(lambda e: e.activation(out=rs[:, 0:n], in_=pa[2][:, 0:n], func=AF.Sqrt, scale=1.0 / 1024, bias=epst[:, 0:1]),
                      reads=[("pa", 2), "epst"], writes=["rs"])
                s.dve(lambda e: e.reciprocal(out=rs[:, 0:n], in_=rs[:, 0:n]), reads=["rs"], writes=["rs"])
                for dc in range(8):
                    s.dve(lambda e, dc=dc: e.scalar_tensor_tensor(out=xm[:, dc, 0:n], in0=xm[:, dc, 0:n], scalar=gft[:, dc:dc + 1], in1=rs[:, 0:n],
                                                                op0=ALU.mult, op1=ALU.mult),
                          reads=["xm", "rs", "gft"], writes=["xm"])
            s.dma(lambda e: e.dma_start(out=xoT[:, out0:out0 + n].rearrange("(c p) t -> p c t", p=128), in_=xm[:, :, 0:n]), reads=["xm"])

        for k in range(4):
            unit(k, k * SLOTW, 2, True, None)
            for u in range(512 // UW):
                unit(k, k * SLOTW + 2 + u * UW, UW, False, k * 512 + u * UW)
        s.emit(stack)
        print("F: ops", len(s.ops), "waits", s.n_waits)
    return nc


def host_inputs_F(mix_list, x_full, w_out, norm2_g, w_up, conv_w, conv_b, w_down, final_g):
    bf = ml_dtypes.bfloat16
    maps = []
    cw = np.zeros((128, NFC, 4), np.float32)
    cw[:, :, 0:3] = conv_w.T.reshape(NFC, 128, 3).transpose(1, 0, 2)
    cw[:, :, 3] = conv_b.reshape(NFC, 128).T
    for c in range(8):
        b, j = divmod(c, 4)
        mcols, xcols = [], []
        hf = np.ones((128, 4), np.float32)
        for k in range(4):
            g = 4 * k + j
            t0 = 512 * g
            if g == 0:
                mcols.append(np.zeros((2, 1024), bf))
                xcols.append(np.zeros((2, 1024), np.float32))
                hf[:, k] = 0.0
            else:
                mcols.append(mix_list[b][t0 - 2:t0])
                xcols.append(x_full[b, t0 - 2:t0])
            mcols.append(mix_list[b][t0:t0 + 512])
            xcols.append(x_full[b, t0:t0 + 512])
        maps.append({
            "mixT": np.ascontiguousarray(np.concatenate(mcols, 0).T), "xT": np.ascontiguousarray(np.concatenate(xcols, 0).T),
            "w_out": np.ascontiguousarray(w_out), "w_up": np.ascontiguousarray(w_up), "w_down": np.ascontiguousarray(w_down),
            "g2": np.ascontiguousarray(norm2_g.reshape(8, 128).T), "cw": cw, "hflag": hf,
            "gf": np.ascontiguousarray(final_g.reshape(8, 128).T),
        })
    return maps


_CACHE = {}
CHECK = None


def _get(name, fn):
    if name not in _CACHE:
        _CACHE[name] = fn()
    return _CACHE[name]


def _run(nc, maps):
    res = run_bass_kernel_spmd(nc, maps, core_ids=list(range(8)))
    return res.results


def forward(x, positions, norm1_g, w_in, w_gate_up, b_gate, gla_norm_g, w_pool, pool_scale, w_out, norm2_g, w_up, conv_w, conv_b,
            w_down, final_norm_g):
    bf = ml_dtypes.bfloat16
    x = np.asarray(x, np.float32)
    positions = np.asarray(positions, np.int32)
    depth = norm1_g.shape[0]
    ncA = _get("A", build_A)
    ncB = _get("B", build_B)
    ncG = _get("G", build_G)
    for l in range(depth):
        last = (l == depth - 1)
        rA = _run(ncA, host_inputs_A(x, positions, np.asarray(norm1_g[l]), np.asarray(w_in[l]), np.asarray(w_pool[l]), np.asarray(pool_scale[l])))
        pbf = [np.asarray(r["pbf"]) for r in rA]
        pf32 = [np.asarray(r["pf32"]) for r in rA]
        ocT = [np.asarray(r["ocT"]) for r in rA]
        if CHECK:
            CHECK("A", l, dict(pbf=pbf, pf32=pf32, ocT=ocT))
        rB = _run(ncB, host_inputs_B(pbf, pf32))
        oaT = [np.asarray(r["oaT"]) for r in rB]
        if CHECK:
            CHECK("B", l, dict(oaT=oaT))
        pf_full = []
        for b in range(2):
            full = np.zeros((8192, pf32[0].shape[1]), np.float32)
            for j in range(4):
                for k in range(4):
                    g = 4 * k + j
                    full[512 * g:512 * (g + 1)] = pf32[b * 4 + j][512 * k:512 * (k + 1)]
            pf_full.append(full)
        rG = _run(ncG, host_inputs_G(pf_full, np.asarray(w_gate_up[l]), np.asarray(b_gate[l]), np.asarray(gla_norm_g[l])))
        obT = [np.asarray(r["obT"]) for r in rG]
        if CHECK:
            CHECK("G", l, dict(obT=obT))
        mix = []
        for b in range(2):
            m = np.zeros((8192, 1024), bf)
            for j in range(4):
                c = b * 4 + j
                oa = oaT[c].transpose(2, 1, 0).reshape(2048, 512)
                oc = ocT[c].transpose(2, 1, 0).reshape(2048, 256)
                for k in range(4):
                    g = 4 * k + j
                    m[512 * g:512 * (g + 1), 0:512] = oa[512 * k:512 * (k + 1)]
                    m[512 * g:512 * (g + 1), 768:1024] = oc[512 * k:512 * (k + 1)]
            for h in range(4):
                m[:, 512 + 64 * h:512 + 64 * (h + 1)] = obT[b * 4 + h].T
            mix.append(m)
        if CHECK:
            CHECK("mix", l, dict(mix=mix))
        ncF = _get("F%d" % int(last), lambda: build_F(final=last))
        rF = _run(ncF, host_inputs_F(mix, x, np.asarray(w_out[l]), np.asarray(norm2_g[l]), np.asarray(w_up[l]), np.asarray(conv_w[l]),
                                     np.asarray(conv_b[l]), np.asarray(w_down[l]), np.asarray(final_norm_g)))
        xn = np.zeros_like(x)
        for c in range(8):
            b, j = divmod(c, 4)
            xo = np.asarray(rF[c]["xoT"]).T
            for k in range(4):
                g = 4 * k + j
                xn[b, 512 * g:512 * (g + 1)] = xo[512 * k:512 * (k + 1)]
        x = xn
        if CHECK:
            CHECK("F", l, dict(x=x))
    return x


def kernel(**inputs):
    out = forward(**{k: np.asarray(v) for k, v in inputs.items()})
    return np.ascontiguousarray(out.astype(np.float32))
```

```python
import numpy as np
import concourse.bass as bass
import concourse.mybir as mybir
from concourse.bass_utils import run_bass_kernel_spmd
from contextlib import ExitStack
import math
import ml_dtypes

F32 = mybir.dt.float32
BF16 = mybir.dt.bfloat16
I32 = mybir.dt.int32
ALU = mybir.AluOpType
AF = mybir.ActivationFunctionType
AX = mybir.AxisListType

ENGS = ("pe", "act", "dve", "pool", "sp")


class _Op:
    __slots__ = ("eng", "fn", "reads", "writes", "dma", "deps", "sig", "sigval", "dsem", "idx")


class Sched:
    def __init__(self, nc, n_dma_sems=6):
        self.nc = nc
        self.ops = []
        self.last_w = {}
        self.readers = {}
        self.n_dma_sems = n_dma_sems

    def add(self, eng, fn, reads=(), writes=(), dma=False):
        op = _Op()
        op.eng = eng
        op.fn = fn
        op.reads = tuple(reads)
        op.writes = tuple(writes)
        op.dma = dma
        op.idx = len(self.ops)
        deps = set()
        for r in op.reads:
            w = self.last_w.get(r)
            if w is not None:
                deps.add(w)
        for r in op.writes:
            w = self.last_w.get(r)
            if w is not None:
                deps.add(w)
            for rd in self.readers.get(r, ()):
                deps.add(rd)
        deps.discard(op.idx)
        op.deps = deps
        for r in op.reads:
            self.readers.setdefault(r, []).append(op.idx)
        for r in op.writes:
            self.last_w[r] = op.idx
            self.readers[r] = []
        op.sig = False
        op.sigval = None
        op.dsem = None
        self.ops.append(op)
        return op

    def pe(self, fn, reads=(), writes=()):
        return self.add("pe", fn, reads, writes)

    def act(self, fn, reads=(), writes=()):
        return self.add("act", fn, reads, writes)

    def dve(self, fn, reads=(), writes=()):
        return self.add("dve", fn, reads, writes)

    def pool(self, fn, reads=(), writes=()):
        return self.add("pool", fn, reads, writes)

    def dma(self, fn, reads=(), writes=(), q="sp"):
        return self.add(q, fn, reads, writes, dma=True)

    def emit(self, stack):
        nc = self.nc
        ops = self.ops
        for op in ops:
            for d in op.deps:
                dop = ops[d]
                if dop.eng == op.eng and not dop.dma:
                    if not (set(dop.writes) & set(op.reads)):
                        continue
                dop.sig = True
        for op in ops:
            if op.dma:
                op.sig = True
        esem = {e: stack.enter_context(nc.semaphore("s_" + e)) for e in ENGS}
        dsems = {}
        for q in ENGS:
            if any(o.dma and o.eng == q for o in ops):
                dsems[q] = [stack.enter_context(nc.semaphore("d_%s%d" % (q, i))) for i in range(self.n_dma_sems)]
        ecount = {e: 0 for e in ENGS}
        dcount = {q: [0] * self.n_dma_sems for q in dsems}
        drr = {q: 0 for q in dsems}
        prev_dma_wait = {}
        for op in ops:
            if op.dma:
                k = drr[op.eng]
                drr[op.eng] = (k + 1) % self.n_dma_sems
                prev_dma_wait[op.idx] = dcount[op.eng][k]
                dcount[op.eng][k] += 16
                op.dsem = (op.eng, k)
                op.sigval = dcount[op.eng][k]
            elif op.sig:
                ecount[op.eng] += 1
                op.sigval = ecount[op.eng]
        by_eng = {e: [o for o in ops if o.eng == e] for e in ENGS}
        block = stack.enter_context(nc.Block())
        self.n_waits = 0

        def run(ename, eobj):
            waited = {}
            for op in by_eng[ename]:
                need = {}
                for d in op.deps:
                    dop = ops[d]
                    if dop.dma:
                        key = ("d",) + dop.dsem
                        sem = dsems[dop.dsem[0]][dop.dsem[1]]
                    else:
                        if dop.eng == ename and not (set(dop.writes) & set(op.reads)):
                            continue
                        key = ("e", dop.eng)
                        sem = esem[dop.eng]
                    v = dop.sigval
                    if waited.get(key, 0) >= v:
                        continue
                    if key not in need or need[key][1] < v:
                        need[key] = (sem, v)
                if op.dma:
                    pv = prev_dma_wait[op.idx]
                    key = ("d",) + op.dsem
                    if pv > 0 and waited.get(key, 0) < pv:
                        if key not in need or need[key][1] < pv:
                            need[key] = (dsems[op.dsem[0]][op.dsem[1]], pv)
                for key, (sem, v) in need.items():
                    eobj.wait_ge(sem, v)
                    waited[key] = v
                    self.n_waits += 1
                ins = op.fn(eobj)
                if op.dma:
                    ins.then_inc(dsems[op.dsem[0]][op.dsem[1]], 16)
                elif op.sig:
                    ins.then_inc(esem[ename], 1)
            for q, lst in dsems.items():
                if q == ename:
                    for k, s in enumerate(lst):
                        if dcount[q][k] > 0:
                            eobj.wait_ge(s, dcount[q][k])

        if by_eng["pe"]:
            @block.tensor
            def _(e):
                run("pe", e)
        if by_eng["act"]:
            @block.scalar
            def _(e):
                run("act", e)
        if by_eng["dve"]:
            @block.vector
            def _(e):
                run("dve", e)
        if by_eng["pool"]:
            @block.gpsimd
            def _(e):
                run("pool", e)
        if by_eng["sp"]:
            @block.sync
            def _(e):
                run("sp", e)


NTILE = 20
INW = 2900
CH = [(0, 512), (512, 1024), (1024, 1536), (1536, 2048), (2048, 2560), (2560, 2900)]
UC0 = 2644
BFW = 1856
F32W = INW - BFW
TWO_PI = 2.0 * math.pi


def pool_mats(first):
    Mc = np.zeros((128, 4, 128), np.float32)
    Mh = np.zeros((128, 4, 128), np.float32)
    for gi, w in enumerate((2, 4, 8, 16)):
        for t in range(128):
            cnt = min(t + 1, w) if first else w
            for s in range(t - w + 1, t + 1):
                if s >= 0:
                    Mc[s, gi, t] += 1.0 / cnt
                elif not first:
                    Mh[128 + s, gi, t] += 1.0 / cnt
            Mc[t, gi, t] -= 1.0
    return Mc, Mh


def build_A():
    nc = bass.Bass("TRN2", target_bir_lowering=False)
    D = lambda name, shape, dt, kind="ExternalInput": nc.dram_tensor(name, shape, dt, kind=kind).ap()
    xtok = D("xtok", [NTILE * 128, 1024], F32)
    xT = D("xT", [1024, NTILE * 128], F32)
    w_in = D("w_in", [1024, INW], F32)
    g1 = D("g1", [128, 8], F32)
    pos = D("pos", [128, NTILE], I32)
    inv = D("inv", [128, 8], F32)
    wpool = D("wpool", [64, 4, 64], F32)
    pscale = D("pscale", [64, 4], F32)
    mcur = D("mcur", [128, 5, 4, 128], F32)
    mhal = D("mhal", [128, 5, 4, 128], F32)
    pbf = D("pbf", [2048, BFW], BF16, "ExternalOutput")
    pf32 = D("pf32", [2048, F32W], F32, "ExternalOutput")
    ocT = D("ocT", [64, 4, 2048], BF16, "ExternalOutput")

    with ExitStack() as stack:
        T = lambda name, shape, dt: stack.enter_context(nc.sbuf_tensor(name, shape, dt))
        P = lambda name, shape, dt: stack.enter_context(nc.psum_tensor(name, shape, dt))
        wbf = T("wbf", [128, 8, INW], BF16)
        wst = T("wst", [128, 2, INW], F32)
        g1t = T("g1t", [128, 8], F32)
        posi = T("posi", [128, NTILE], I32)
        posf = T("posf", [128, NTILE], F32)
        invt = T("invt", [128, 8], F32)
        ang = T("ang", [128, NTILE, 8], F32)
        kq = T("kq", [128, NTILE, 8], F32)
        angc = T("angc", [128, NTILE, 8], F32)
        kqi = T("kqi", [128, NTILE, 8], I32)
        cost = T("cost", [128, NTILE, 8], F32)
        sint = T("sint", [128, NTILE, 8], F32)
        wpt = T("wpt", [64, 4, 64], F32)
        pst = T("pst", [64, 4], F32)
        mct = T("mct", [128, 5, 4, 128], F32)
        mht = T("mht", [128, 5, 4, 128], F32)
        xtk = T("xtk", [128, 2, 1024], F32)
        sqj = T("sqj", [128, 1024], BF16)
        ss = T("ss", [128, 2], F32)
        rstd = T("rstd", [128, 2], F32)
        epst = T("epst", [128, 1], F32)
        xTt = T("xTt", [128, 2, 8, 128], F32)
        hT = T("hT", [128, 2, 8, 128], BF16)
        proj = T("proj", [128, 2, INW], F32)
        pb16 = T("pb16", [128, 2, BFW], BF16)
        rt = T("rt", [128, 4, 16, 8], F32)
        pooled = T("pooled", [64, 2, 4, 128], F32)
        oct_ = T("oct", [64, 2, 4, 128], BF16)
        ps = [P("ps%d" % i, [128, 512], F32) for i in range(4)]
        pp = [P("pp%d" % i, [64, 4, 128], F32) for i in range(2)]
        py = [P("py%d" % i, [64, 4, 128], F32) for i in range(2)]

        s = Sched(nc)
        s.dma(lambda e: e.dma_start(out=g1t[:], in_=g1), writes=["g1t"])
        s.dma(lambda e: e.dma_start(out=posi[:], in_=pos), writes=["posi"])
        s.dma(lambda e: e.dma_start(out=invt[:], in_=inv), writes=["invt"])
        s.dma(lambda e: e.dma_start(out=wpt[:], in_=wpool), writes=["wpt"])
        s.dma(lambda e: e.dma_start(out=pst[:], in_=pscale), writes=["pst"])
        s.dma(lambda e: e.dma_start(out=mct[:], in_=mcur), writes=["mct"])
        s.dma(lambda e: e.dma_start(out=mht[:], in_=mhal), writes=["mht"])
        s.dve(lambda e: e.memset(epst[:], 1e-6), writes=["epst"])
        s.dve(lambda e: e.tensor_copy(out=posf[:], in_=posi[:]), reads=["posi"], writes=["posf"])
        for t in range(NTILE):
            s.dve(lambda e, t=t: e.tensor_scalar(out=ang[:, t, :], in0=invt[:], scalar1=posf[:, t:t + 1], scalar2=None, op0=ALU.mult),
                  reads=["invt", "posf"], writes=["ang"])
        s.dve(lambda e: e.tensor_scalar(out=kqi[:], in0=ang[:], scalar1=1.0 / TWO_PI, scalar2=None, op0=ALU.mult), reads=["ang"], writes=["kqi"])
        s.dve(lambda e: e.tensor_copy(out=kq[:], in_=kqi[:]), reads=["kqi"], writes=["kq"])
        s.dve(lambda e: e.scalar_tensor_tensor(out=ang[:], in0=kq[:], scalar=-TWO_PI, in1=ang[:], op0=ALU.mult, op1=ALU.add),
              reads=["kq", "ang"], writes=["ang"])
        def wrap(y, name):
            s.dve(lambda e: e.tensor_scalar(out=kq[:], in0=y[:], scalar1=math.pi, scalar2=-TWO_PI, op0=ALU.is_gt, op1=ALU.mult),
                  reads=[name], writes=["kq"])
            s.dve(lambda e: e.tensor_tensor(out=y[:], in0=y[:], in1=kq[:], op=ALU.add), reads=[name, "kq"], writes=[name])
            s.dve(lambda e: e.tensor_scalar(out=kq[:], in0=y[:], scalar1=-math.pi, scalar2=TWO_PI, op0=ALU.is_lt, op1=ALU.mult),
                  reads=[name], writes=["kq"])
            s.dve(lambda e: e.tensor_tensor(out=y[:], in0=y[:], in1=kq[:], op=ALU.add), reads=[name, "kq"], writes=[name])
        s.dve(lambda e: e.tensor_scalar(out=angc[:], in0=ang[:], scalar1=math.pi / 2, scalar2=None, op0=ALU.add), reads=["ang"], writes=["angc"])
        wrap(ang, "ang")
        wrap(angc, "angc")
        s.act(lambda e: e.activation(out=sint[:], in_=ang[:], func=AF.Sin), reads=["ang"], writes=["sint"])
        s.act(lambda e: e.activation(out=cost[:], in_=angc[:], func=AF.Sin), reads=["angc"], writes=["cost"])

        for c in range(8):
            b = c % 2
            s.dma(lambda e, c=c, b=b: e.dma_start(out=wst[:, b, :], in_=w_in[c * 128:(c + 1) * 128, :]), writes=[("wst", b)])
            eng = s.dve if c % 2 == 0 else s.pool
            eng(lambda e, c=c, b=b: e.tensor_scalar(out=wbf[:, c, :], in0=wst[:, b, :], scalar1=g1t[:, c:c + 1], scalar2=None, op0=ALU.mult),
                reads=[("wst", b), "g1t"], writes=[("wbf", c)])

        own = 0
        for t in range(NTILE):
            k, i = divmod(t, 5)
            halo = (i == 0)
            b = t % 2
            pb = (t - 1) % 2
            s.dma(lambda e, t=t, b=b: e.dma_start(out=xtk[:, b, :], in_=xtok[t * 128:(t + 1) * 128, :]), writes=[("xtk", b)])
            s.dma(lambda e, t=t, b=b: e.dma_start(out=xTt[:, b, :, :], in_=xT[:, t * 128:(t + 1) * 128].rearrange("(c p) t -> p c t", p=128)),
                  writes=[("xTt", b)])
            s.act(lambda e, b=b: e.activation(out=sqj[:], in_=xtk[:, b, :], func=AF.Square, accum_out=ss[:, b:b + 1]),
                  reads=[("xtk", b)], writes=["sqj", ("ss", b)])
            s.act(lambda e, b=b: e.activation(out=ss[:, b:b + 1], in_=ss[:, b:b + 1], func=AF.Sqrt, scale=1.0 / 1024, bias=epst[:, 0:1]),
                  reads=[("ss", b), "epst"], writes=[("ss", b)])
            s.dve(lambda e, b=b: e.reciprocal(out=rstd[:, b:b + 1], in_=ss[:, b:b + 1]), reads=[("ss", b)], writes=[("rstd", b)])
            s.pool(lambda e, b=b: e.tensor_copy(out=hT[:, b, :, :], in_=xTt[:, b, :, :]), reads=[("xTt", b)], writes=[("hT", b)])
            chunks = [5] if halo else list(range(6))
            for n in chunks:
                n0, n1 = CH[n]
                pt = ps[n % 4]
                for c in range(8):
                    s.pe(lambda e, pt=pt, b=b, c=c, n0=n0, n1=n1: e.matmul(pt[:, 0:n1 - n0], lhsT=hT[:, b, c, :], rhs=wbf[:, c, n0:n1],
                                                                         start=(c == 0), stop=(c == 7)),
                         reads=[("hT", b), ("wbf", c)], writes=[("ps", n % 4)])
                if n % 2 == 0:
                    s.act(lambda e, pt=pt, b=b, n0=n0, n1=n1: e.activation(out=proj[:, b, n0:n1], in_=pt[:, 0:n1 - n0], func=AF.Copy,
                                                                         scale=rstd[:, b:b + 1]),
                          reads=[("ps", n % 4), ("rstd", b)], writes=[("proj", b, n)])
                else:
                    s.dve(lambda e, pt=pt, b=b, n0=n0, n1=n1: e.tensor_scalar(out=proj[:, b, n0:n1], in0=pt[:, 0:n1 - n0],
                                                                            scalar1=rstd[:, b:b + 1], scalar2=None, op0=ALU.mult),
                          reads=[("ps", n % 4), ("rstd", b)], writes=[("proj", b, n)])
            if not halo:
                for (c0, nh, regs) in ((0, 16, [("proj", b, 0), ("proj", b, 1)]), (1536, 5, [("proj", b, 3)])):
                    v = proj[:, b, c0:c0 + nh * 64].rearrange("p (h d) -> p h d", d=64)
                    x1 = v[:, :, 0:8]
                    x2 = v[:, :, 8:16]
                    cb = cost[:, t, :].unsqueeze(1).to_broadcast([128, nh, 8])
                    sb = sint[:, t, :].unsqueeze(1).to_broadcast([128, nh, 8])
                    t0 = rt[:, 0, 0:nh, :]
                    t1 = rt[:, 1, 0:nh, :]
                    t2 = rt[:, 2, 0:nh, :]
                    t3 = rt[:, 3, 0:nh, :]
                    R = regs + ["cost", "sint"]
                    s.dve(lambda e, t0=t0, x1=x1, cb=cb: e.tensor_tensor(out=t0, in0=x1, in1=cb, op=ALU.mult), reads=R, writes=["rt0"])
                    s.dve(lambda e, t1=t1, x2=x2, sb=sb: e.tensor_tensor(out=t1, in0=x2, in1=sb, op=ALU.mult), reads=R, writes=["rt1"])
                    s.dve(lambda e, t2=t2, x2=x2, cb=cb: e.tensor_tensor(out=t2, in0=x2, in1=cb, op=ALU.mult), reads=R, writes=["rt2"])
                    s.dve(lambda e, t3=t3, x1=x1, sb=sb: e.tensor_tensor(out=t3, in0=x1, in1=sb, op=ALU.mult), reads=R, writes=["rt3"])
                    s.dve(lambda e, t0=t0, t1=t1, x1=x1: e.tensor_tensor(out=x1, in0=t0, in1=t1, op=ALU.subtract),
                          reads=["rt0", "rt1", "rt2", "rt3"], writes=regs)
                    s.dve(lambda e, t2=t2, t3=t3, x2=x2: e.tensor_tensor(out=x2, in0=t2, in1=t3, op=ALU.add),
                          reads=["rt0", "rt1", "rt2", "rt3"], writes=regs)
                r0 = own * 128
                s.act(lambda e, b=b: e.activation(out=pb16[:, b, :], in_=proj[:, b, 0:BFW], func=AF.Copy),
                      reads=[("proj", b, n) for n in range(4)], writes=[("pb16", b)])
                s.dma(lambda e, b=b, r0=r0: e.dma_start(out=pbf[r0:r0 + 128, :], in_=pb16[:, b, :]), reads=[("pb16", b)])
                s.dma(lambda e, b=b, r0=r0: e.dma_start(out=pf32[r0:r0 + 128, :], in_=proj[:, b, BFW:INW]),
                      reads=[("proj", b, n) for n in (3, 4, 5)])
                mi = 0 if i > 1 else 1 + k
                ob = own % 2
                for gi in range(4):
                    s.pe(lambda e, ob=ob, b=b, gi=gi, mi=mi: e.matmul(pp[ob][:, gi, :], lhsT=proj[:, b, UC0 + gi * 64:UC0 + (gi + 1) * 64],
                                                                  rhs=mct[:, mi, gi, :], start=True, stop=False),
                         reads=[("proj", b, 5), "mct"], writes=[("pp", ob)])
                    s.pe(lambda e, ob=ob, pb=pb, gi=gi, mi=mi: e.matmul(pp[ob][:, gi, :], lhsT=proj[:, pb, UC0 + gi * 64:UC0 + (gi + 1) * 64],
                                                                    rhs=mht[:, mi, gi, :], start=False, stop=True),
                         reads=[("proj", pb, 5), "mht"], writes=[("pp", ob)])
                s.act(lambda e, ob=ob: e.activation(out=pooled[:, ob, :, :], in_=pp[ob][:], func=AF.Copy),
                      reads=[("pp", ob)], writes=[("pooled", ob)])
                for gi in range(4):
                    s.pe(lambda e, ob=ob, gi=gi: e.matmul(py[ob][:, gi, :], lhsT=wpt[:, gi, :], rhs=pooled[:, ob, gi, :], start=True, stop=True),
                         reads=[("pooled", ob), "wpt"], writes=[("py", ob)])
                for gi in range(4):
                    s.dve(lambda e, ob=ob, gi=gi: e.tensor_scalar(out=oct_[:, ob, gi, :], in0=py[ob][:, gi, :], scalar1=pst[:, gi:gi + 1],
                                                                scalar2=None, op0=ALU.mult),
                          reads=[("py", ob), "pst"], writes=[("oct", ob)])
                s.dma(lambda e, ob=ob, r0=r0: e.dma_start(out=ocT[:, :, r0:r0 + 128], in_=oct_[:, ob, :, :]), reads=[("oct", ob)])
                own += 1
        s.emit(stack)
        print("A: ops", len(s.ops), "waits", s.n_waits)
    return nc


def host_inputs_A(x, positions, norm1_g, w_in, w_pool, pool_scale):
    inv = (500000.0 ** (-np.arange(0, 16, 2, dtype=np.float32) / 16)).astype(np.float32)
    McG, MhG = pool_mats(False)
    McF, MhF = pool_mats(True)
    maps = []
    for c in range(8):
        b, j = divmod(c, 4)
        rows = []
        posl = []
        mcur = np.zeros((128, 5, 4, 128), np.float32)
        mhal = np.zeros((128, 5, 4, 128), np.float32)
        mcur[:, 0], mhal[:, 0] = McG, MhG
        for k in range(4):
            g = 4 * k + j
            t0 = 512 * g
            if g == 0:
                rows.append(np.zeros((128, 1024), np.float32))
                posl.append(np.zeros((128,), np.int32))
                mcur[:, 1 + k], mhal[:, 1 + k] = McF, MhF
            else:
                rows.append(x[b, t0 - 128:t0])
                posl.append(positions[b, t0 - 128:t0])
                mcur[:, 1 + k], mhal[:, 1 + k] = McG, MhG
            rows.append(x[b, t0:t0 + 512])
            posl.append(positions[b, t0:t0 + 512])
        xt = np.ascontiguousarray(np.concatenate(rows, 0))
        pl = np.concatenate(posl, 0).astype(np.int32)
        maps.append({
            "xtok": xt, "xT": np.ascontiguousarray(xt.T), "w_in": np.ascontiguousarray(w_in),
            "g1": np.ascontiguousarray(norm1_g.reshape(8, 128).T),
            "pos": np.ascontiguousarray(pl.reshape(NTILE, 128).T),
            "inv": np.ascontiguousarray(np.broadcast_to(inv[None, :], (128, 8))),
            "wpool": np.ascontiguousarray(w_pool.transpose(1, 0, 2)),
            "pscale": np.ascontiguousarray(pool_scale.reshape(4, 64).T),
            "mcur": mcur, "mhal": mhal,
        })
    return maps


NIT = 16
NEG = -1.0e30


def build_B(nslot=4, nit=NIT):
    nc = bass.Bass("TRN2", target_bir_lowering=False)
    D = lambda name, shape, dt, kind="ExternalInput": nc.dram_tensor(name, shape, dt, kind=kind).ap()
    kT = D("kT", [128, 4, 8192], BF16)
    vv = D("v", [8192, 512], BF16)
    kiT = D("kiT", [64, 8192], BF16)
    qT = D("qT", [128, 4, 4, 512], BF16)
    qiT = D("qiT", [64, 4, 4, 512], BF16)
    wi = D("wi", [128, 16, 4], F32)
    qrel = D("qrel", [128, 16], F32)
    kpos = D("kpos", [128, 2048], F32)
    ident = D("ident", [128, 128], BF16)
    oaT = D("oaT", [64, 8, 2048], BF16, "ExternalOutput")

    with ExitStack() as stack:
        T = lambda name, shape, dt: stack.enter_context(nc.sbuf_tensor(name, shape, dt))
        P = lambda name, shape, dt: stack.enter_context(nc.psum_tensor(name, shape, dt))
        kit = T("kit", [64, 8192], BF16)
        sc = T("sc", [128, 8192], F32)
        mk = T("mk", [128, 4, 8192], BF16)
        kpt = T("kpt", [128, 2048], F32)
        rr = T("rr", [128, 4, 512], F32)
        qTt = T("qTt", [128, 1, 4, 512], BF16)
        qit = T("qit", [64, 1, 4, 512], BF16)
        wit = T("wit", [128, 16, 4], F32)
        qrt = T("qrt", [128, 16], F32)
        idt = T("idt", [128, 128], BF16)
        kTs = T("kTs", [128, 2, 4, 512], BF16)
        vraw = T("vraw", [128, 2, 4, 512], BF16)
        vt = T("vt", [128, 2, 4, 520], BF16)
        E = T("E", [128, 3, 512], BF16)
        Pm = T("Pm", [128, 3, 512], BF16)
        mT = T("mT", [128, 2, 512], BF16)
        sm = T("sm", [128, 8], F32)
        ones1 = T("ones1", [128, 64], F32)
        rec = T("rec", [128, 512], F32)
        bcs = T("bcs", [64, 512], F32)
        oT = T("oT", [64, 2, 512], BF16)
        pb = [P("pb%d" % i, [128, 512], F32) for i in range(8)]
        pTb = pb[2][:].bitcast(BF16)

        s = Sched(nc)
        s.dma(lambda e: e.dma_start(out=kit[:], in_=kiT), writes=["kit"])
        s.dma(lambda e: e.dma_start(out=wit[:], in_=wi), writes=["wit"])
        s.dma(lambda e: e.dma_start(out=qrt[:], in_=qrel), writes=["qrt"])
        s.dma(lambda e: e.dma_start(out=kpt[:], in_=kpos), writes=["kpt"])
        s.dma(lambda e: e.dma_start(out=idt[:], in_=ident), writes=["idt"])
        s.pool(lambda e: e.memset(ones1[:], 1.0), writes=["ones1"])
        s.dve(lambda e: e.memset(vt[:], 1.0), writes=[("vt", 0), ("vt", 1)])
        cbias = rr[:].rearrange("p h n -> p (h n)")
        RRALL = [("rr", h) for h in range(4)]

        kbc = 0
        ec = 0
        mtc = 0
        otc = 0
        for k in range(nslot):
            L = 2048 * (k + 1)
            nkc = L // 512
            nkb = L // 128
            qb = 0
            s.dma(lambda e, k=k, qb=qb: e.dma_start(out=qTt[:, qb, :, :], in_=qT[:, k, :, :]), writes=[("qTt", qb)])
            s.dma(lambda e, k=k, qb=qb: e.dma_start(out=qit[:, qb, :, :], in_=qiT[:, k, :, :]), writes=[("qit", qb)])
            for qt in range(4):
                g = 4 * k + qt
                for n in range(nkc):
                    for h in range(4):
                        s.pe(lambda e, h=h, qb=qb, qt=qt, n=n: e.matmul(pb[h][:], lhsT=qit[:, qb, h, qt * 128:(qt + 1) * 128],
                                                                     rhs=kit[:, n * 512:(n + 1) * 512], start=True, stop=True),
                             reads=[("qit", qb), "kit"], writes=[("pb", h)])
                        s.act(lambda e, h=h: e.activation(out=rr[:, h, :], in_=pb[h][:], func=AF.Relu), reads=[("pb", h)], writes=[("rr", h)])
                        if h == 0:
                            s.dve(lambda e, n=n, g=g: e.tensor_scalar(out=sc[:, n * 512:(n + 1) * 512], in0=rr[:, 0, :], scalar1=wit[:, g, 0:1],
                                                                    scalar2=None, op0=ALU.mult),
                                  reads=[("rr", 0), "wit"], writes=[("sc", n)])
                        else:
                            s.dve(lambda e, n=n, g=g, h=h: e.scalar_tensor_tensor(out=sc[:, n * 512:(n + 1) * 512], in0=rr[:, h, :],
                                                                                 scalar=wit[:, g, h:h + 1], in1=sc[:, n * 512:(n + 1) * 512],
                                                                                 op0=ALU.mult, op1=ALU.add),
                                  reads=[("rr", h), "wit", ("sc", n)], writes=[("sc", n)])
                allsc = [("sc", n) for n in range(nkc)]
                s.dve(lambda e, L=L: e.tensor_reduce(out=sm[:, 5:6], in_=sc[:, 0:L], axis=AX.X, op=ALU.max, apply_absolute_value=True),
                      reads=allsc, writes=["rmax"])
                s.dve(lambda e: e.tensor_scalar(out=sm[:, 0:1], in0=sm[:, 5:6], scalar1=-1.0, scalar2=None, op0=ALU.mult), reads=["rmax"], writes=["lo"])
                s.dve(lambda e: e.tensor_scalar(out=sm[:, 1:2], in0=sm[:, 5:6], scalar1=2.0, scalar2=None, op0=ALU.mult), reads=["rmax"], writes=["range"])
                s.dve(lambda e, g=g: e.tensor_scalar(out=cbias[:], in0=kpt[:], scalar1=qrt[:, g:g + 1], scalar2=NEG, op0=ALU.is_gt, op1=ALU.mult),
                      reads=["kpt", "qrt"], writes=RRALL)
                s.dve(lambda e, L=L: e.tensor_tensor(out=sc[:, L - 2048:L], in0=sc[:, L - 2048:L], in1=cbias[:], op=ALU.add),
                      reads=allsc + RRALL, writes=allsc)
                Lh = max(512, int(round(0.4 * L / 512.0)) * 512)
                nact = float(L - Lh)
                MKA, MKB = ("mk", qt, "a"), ("mk", qt, "b")
                for it in range(1, nit + 1):
                    f = 2.0 ** (-it)
                    s.dve(lambda e, f=f: e.tensor_scalar(out=sm[:, 2:3], in0=sm[:, 1:2], scalar1=f, scalar2=sm[:, 0:1], op0=ALU.mult, op1=ALU.add),
                          reads=["range", "lo"], writes=["mid"])
                    s.dve(lambda e, Lh=Lh, qt=qt: e.tensor_scalar(out=mk[:, qt, 0:Lh], in0=sc[:, 0:Lh], scalar1=sm[:, 2:3], scalar2=None,
                                                                op0=ALU.is_ge, op1=ALU.add, accum_out=sm[:, 3:4]),
                          reads=allsc + ["mid"], writes=[MKA, "cnt"])
                    s.act(lambda e, Lh=Lh, L=L, qt=qt: e.activation(out=mk[:, qt, Lh:L], in_=sc[:, Lh:L], func=AF.Sign, scale=-1.0, bias=sm[:, 2:3],
                                                                   accum_out=sm[:, 6:7]),
                          reads=allsc + ["mid"], writes=[MKB, "sgn"])
                    s.dve(lambda e: e.scalar_tensor_tensor(out=sm[:, 7:8], in0=sm[:, 3:4], scalar=2.0, in1=sm[:, 6:7], op0=ALU.mult, op1=ALU.subtract),
                          reads=["cnt", "sgn"], writes=["tt"])
                    s.dve(lambda e, f=f, nact=nact: e.tensor_scalar(out=sm[:, 4:5], in0=sm[:, 7:8], scalar1=511.0 - nact, scalar2=f, op0=ALU.is_ge, op1=ALU.mult),
                          reads=["tt"], writes=["pred"])
                    s.dve(lambda e: e.scalar_tensor_tensor(out=sm[:, 0:1], in0=sm[:, 4:5], scalar=sm[:, 1:2], in1=sm[:, 0:1], op0=ALU.mult, op1=ALU.add),
                          reads=["pred", "range", "lo"], writes=["lo"])
                s.dve(lambda e, L=L, qt=qt: e.tensor_scalar(out=mk[:, qt, 0:L], in0=sc[:, 0:L], scalar1=sm[:, 0:1], scalar2=None, op0=ALU.is_ge),
                      reads=allsc + ["lo"], writes=[MKA, MKB])
            steps = [(hp, kb, hl) for hp in range(2) for kb in range(nkb) for hl in range(4)]
            nst = len(steps)
            info = {}

            def load_sb(hp, sbk):
                nonlocal kbc
                kbuf = kbc % 2
                kbc += 1
                info[("kbuf", hp, sbk)] = kbuf
                s.dma(lambda e, sbk=sbk, kbuf=kbuf: e.dma_start(out=kTs[:, kbuf, :, :], in_=kT[:, :, sbk * 512:(sbk + 1) * 512]),
                      writes=[("kTs", kbuf)])
                s.dma(lambda e, sbk=sbk, kbuf=kbuf: e.dma_start(out=vraw[:, kbuf, :, :], in_=vv[sbk * 512:(sbk + 1) * 512, :].rearrange("(kb p) c -> p kb c", p=128)),
                      writes=[("vraw", kbuf)])
                for kl_ in range(4):
                    s.dve(lambda e, kbuf=kbuf, kl_=kl_: e.tensor_copy(out=vt[:, kbuf, kl_, :].rearrange("p (h c) -> p h c", c=65)[:, :, 0:64],
                                                                   in_=vraw[:, kbuf, kl_, :].rearrange("p (h d) -> p h d", d=64)),
                          reads=[("vraw", kbuf)], writes=[("vt", kbuf)])

            def pre(hp, kb):
                nonlocal mtc
                sbk, kl = divmod(kb, 4)
                mb = mtc % 2
                mtc += 1
                info[("mb", hp, kb)] = mb
                for qt in range(4):
                    s.pe(lambda e, qt=qt, kb=kb: e.transpose(pTb[:, qt * 128:(qt + 1) * 128], mk[:, qt, kb * 128:(kb + 1) * 128], idt[:]),
                         reads=[("mk", qt, "a"), ("mk", qt, "b"), "idt"], writes=[("pb", 2)])
                s.act(lambda e, mb=mb: e.activation(out=mT[:, mb, :], in_=pTb[:, 0:512], func=AF.Copy), reads=[("pb", 2)], writes=[("mT", mb)])

            def ST(i):
                hp, kb, hl = steps[i]
                sbk, kl = divmod(kb, 4)
                kbuf = info[("kbuf", hp, sbk)]
                h = hp * 4 + hl
                pr, hh = divmod(h, 2)
                sb = i % 2
                s.pe(lambda e, sb=sb, kbuf=kbuf, pr=pr, hh=hh, kl=kl: e.matmul(pb[sb][:], lhsT=kTs[hh * 64:(hh + 1) * 64, kbuf, pr, kl * 128:(kl + 1) * 128],
                                                                            rhs=qTt[hh * 64:(hh + 1) * 64, 0, pr, :], start=True, stop=True),
                     reads=[("kTs", kbuf), ("qTt", 0)], writes=[("pb", sb)])

            def rest_a(i):
                sb = i % 2
                eb = i % 3
                s.act(lambda e, sb=sb, eb=eb: e.activation(out=E[:, eb, :], in_=pb[sb][:], func=AF.Exp, scale=0.125),
                      reads=[("pb", sb)], writes=[("E", eb)])

            def rest(i):
                nonlocal otc
                hp, kb, hl = steps[i]
                sbk, kl = divmod(kb, 4)
                kbuf = info[("kbuf", hp, sbk)]
                mb = info[("mb", hp, kb)]
                h = hp * 4 + hl
                sb = i % 2
                eb = i % 3
                eng = s.dve if (i % 2 == 0) else s.pool
                eng(lambda e, eb=eb, mb=mb: e.tensor_tensor(out=Pm[:, eb, :], in0=E[:, eb, :], in1=mT[:, mb, :], op=ALU.mult),
                    reads=[("E", eb), ("mT", mb)], writes=[("Pm", eb)])
                s.pe(lambda e, hl=hl, kbuf=kbuf, h=h, eb=eb, kb=kb, kl=kl: e.matmul(pb[4 + hl][0:65, :], lhsT=vt[:, kbuf, kl, h * 65:(h + 1) * 65], rhs=Pm[:, eb, :],
                                                                                 start=(kb == 0), stop=(kb == nkb - 1)),
                     reads=[("vt", kbuf), ("Pm", eb)], writes=[("pb", 4 + hl)])
                if kb == nkb - 1:
                    ob = otc % 2
                    otc += 1
                    s.dve(lambda e, hl=hl: e.reciprocal(out=rec[64:65, :], in_=pb[4 + hl][64:65, :]), reads=[("pb", 4 + hl)], writes=["rec"])
                    s.pe(lambda e: e.matmul(pb[3][0:64, :], lhsT=ones1[64:65, :], rhs=rec[64:65, :], start=True, stop=True),
                         reads=["ones1", "rec"], writes=[("pb", 3)])
                    s.act(lambda e: e.activation(out=bcs[:], in_=pb[3][0:64, :], func=AF.Copy), reads=[("pb", 3)], writes=["bcs"])
                    s.dve(lambda e, hl=hl, ob=ob: e.tensor_tensor(out=oT[:, ob, :], in0=pb[4 + hl][0:64, :], in1=bcs[:], op=ALU.mult),
                          reads=[("pb", 4 + hl), "bcs"], writes=[("oT", ob)])
                    s.dma(lambda e, h=h, k=k, ob=ob: e.dma_start(out=oaT[:, h, k * 512:(k + 1) * 512], in_=oT[:, ob, :]), reads=[("oT", ob)])

            DPIPE = 2
            order = [(hp_, sb_) for hp_ in range(2) for sb_ in range(nkb // 4)]
            load_sb(*order[0])
            for j in range(min(DPIPE, nst)):
                if steps[j][2] == 0:
                    pre(steps[j][0], steps[j][1])
                ST(j)
            for i in range(nst):
                if steps[i][2] == 0 and steps[i][1] % 4 == 0:
                    oi = order.index((steps[i][0], steps[i][1] // 4))
                    if oi + 1 < len(order):
                        load_sb(*order[oi + 1])
                rest_a(i)
                j = i + DPIPE
                if j < nst:
                    if steps[j][2] == 0:
                        pre(steps[j][0], steps[j][1])
                    ST(j)
                rest(i)
        s.emit(stack)
        print("B: ops", len(s.ops), "waits", s.n_waits)
    return nc


def host_inputs_B(pbf_list, pf32_list):
    bf = ml_dtypes.bfloat16
    maps = []
    full = []
    for b in range(2):
        ka = np.zeros((8192, 512), bf)
        va = np.zeros((8192, 512), bf)
        ki = np.zeros((8192, 64), bf)
        for j in range(4):
            c = b * 4 + j
            for k in range(4):
                g = 4 * k + j
                blk = pbf_list[c][512 * k:512 * (k + 1)]
                ka[512 * g:512 * (g + 1)] = blk[:, 512:1024]
                va[512 * g:512 * (g + 1)] = blk[:, 1024:1536]
                ki[512 * g:512 * (g + 1)] = blk[:, 1792:1856]
        kTl = np.ascontiguousarray(ka.reshape(8192, 4, 2, 64).transpose(2, 3, 1, 0).reshape(128, 4, 8192))
        full.append((kTl, np.ascontiguousarray(va), np.ascontiguousarray(ki.T)))
    kpos = np.ascontiguousarray(np.broadcast_to(np.arange(2048, dtype=np.float32)[None, :], (128, 2048)))
    ident = np.eye(128).astype(bf)
    for c in range(8):
        b, j = divmod(c, 4)
        p = pbf_list[c]
        qa = p[:, 0:512].reshape(4, 512, 4, 2, 64)
        qTl = np.ascontiguousarray(qa.transpose(3, 4, 0, 2, 1).reshape(128, 4, 4, 512))
        qi = p[:, 1536:1792].reshape(4, 512, 4, 64)
        qiTl = np.ascontiguousarray(qi.transpose(3, 0, 2, 1))
        wi = np.ascontiguousarray(pf32_list[c][:, 0:4].reshape(16, 128, 4).transpose(1, 0, 2))
        qrel = np.zeros((128, 16), np.float32)
        for k in range(4):
            g = 4 * k + j
            for qt in range(4):
                qrel[:, 4 * k + qt] = 512 * g + 128 * qt + np.arange(128) - 2048 * k
        maps.append({"kT": full[b][0], "v": full[b][1], "kiT": full[b][2], "qT": qTl, "qiT": qiTl, "wi": wi, "qrel": qrel,
                     "kpos": kpos, "ident": ident})
    return maps


SEG = 2048
NSEG = 4
CPS = SEG // 64


def build_G():
    nc = bass.Bass("TRN2", target_bir_lowering=False)
    D = lambda name, shape, dt, kind="ExternalInput": nc.dram_tensor(name, shape, dt, kind=kind).ap()
    qT = D("qT", [32, 8192], F32)
    kT = D("kT", [32, 8192], F32)
    vtok = D("vtok", [64, 128, 64], F32)
    glT = D("glT", [16, 8192], F32)
    wg = D("wg", [16, 32], F32)
    bg = D("bg", [32, 1], F32)
    rT = D("rT", [64, 8192], F32)
    gng = D("gng", [64, 1], F32)
    resetm = D("resetm", [32, SEG], F32)
    tri = D("tri", [64, 64], F32)
    ident = D("ident", [32, 32], F32)
    obT = D("obT", [64, 8192], BF16, "ExternalOutput")

    with ExitStack() as stack:
        T = lambda name, shape, dt: stack.enter_context(nc.sbuf_tensor(name, shape, dt))
        P = lambda name, shape, dt: stack.enter_context(nc.psum_tensor(name, shape, dt))
        qs = T("qs", [32, SEG], F32)
        ks = T("ks", [32, SEG], F32)
        vs = T("vs", [64, CPS, 64], F32)
        gls = T("gls", [16, SEG], F32)
        rs_ = T("rs", [64, SEG], F32)
        wgt = T("wgt", [16, 32], F32)
        bgt = T("bgt", [32, 1], F32)
        nbg = T("nbg", [32, 1], F32)
        gnt = T("gnt", [64, 1], F32)
        rmt = T("rmt", [32, SEG], F32)
        trit = T("trit", [64, 64], F32)
        idt = T("idt", [32, 32], F32)
        ones = T("ones", [64, 64], F32)
        epst = T("epst", [64, 1], F32)
        t1 = T("t1", [32, SEG], F32)
        cum = T("cum", [32, SEG], F32)
        eb = T("eb", [32, SEG], F32)
        enb = T("enb", [32, SEG], F32)
        qt_ = T("qt", [32, SEG], F32)
        kt_ = T("kt", [32, SEG], F32)
        ktok = T("ktok", [64, 2, 32], F32)
        am = T("am", [64, 2, 64], F32)
        U = T("U", [32, 2, 64], F32)
        S = T("S", [32, 2, 64], F32)
        oTs = T("oTs", [64, SEG], F32)
        sqs = T("sqs", [64, SEG], F32)
        rsd = T("rsd", [64, 512], F32)
        sil = T("sil", [64, SEG], F32)
        outb = T("outb", [64, SEG], BF16)
        pz = [P("pz%d" % i, [64, 512], F32) for i in range(2)]
        pk = [P("pk%d" % i, [64, 512], F32) for i in range(2)]
        pt_ = [P("pt%d" % i, [64, 512], F32) for i in range(2)]
        po = P("po", [64, 512], F32)
        pu = P("pu", [64, 512], F32)

        s = Sched(nc)
        for (dst, src, nm) in ((wgt, wg, "wgt"), (bgt, bg, "bgt"), (gnt, gng, "gnt"), (rmt, resetm, "rmt"), (trit, tri, "trit"), (idt, ident, "idt")):
            s.dma(lambda e, dst=dst, src=src: e.dma_start(out=dst[:], in_=src), writes=[nm])
        s.dve(lambda e: e.memset(ones[:], 1.0), writes=["ones"])
        s.dve(lambda e: e.memset(epst[:], 1e-6), writes=["epst"])
        s.dve(lambda e: e.memset(S[:], 0.0), writes=[("S", 0), ("S", 1)])
        s.dve(lambda e: e.tensor_scalar(out=nbg[:], in0=bgt[:], scalar1=-1.0, scalar2=None, op0=ALU.mult), reads=["bgt"], writes=["nbg"])
        cc = 0
        for sgi in range(NSEG):
            c0 = sgi * SEG
            s.dma(lambda e, c0=c0: e.dma_start(out=qs[:], in_=qT[:, c0:c0 + SEG]), writes=["qs"])
            s.dma(lambda e, c0=c0: e.dma_start(out=ks[:], in_=kT[:, c0:c0 + SEG]), writes=["ks"])
            s.dma(lambda e, sgi=sgi: e.dma_start(out=vs[:], in_=vtok[:, sgi * CPS:(sgi + 1) * CPS, :]), writes=["vs"])
            s.dma(lambda e, c0=c0: e.dma_start(out=gls[:], in_=glT[:, c0:c0 + SEG]), writes=["gls"])
            s.dma(lambda e, c0=c0: e.dma_start(out=rs_[:], in_=rT[:, c0:c0 + SEG]), writes=["rs"])
            for pc in range(SEG // 512):
                pzb = pz[pc % 2]
                s.pe(lambda e, pzb=pzb, pc=pc: e.matmul(pzb[0:32, :], lhsT=wgt[:], rhs=gls[:, pc * 512:(pc + 1) * 512], start=True, stop=True),
                     reads=["wgt", "gls"], writes=[("pz", pc % 2)])
                s.act(lambda e, pzb=pzb, pc=pc: e.activation(out=t1[:, pc * 512:(pc + 1) * 512], in_=pzb[0:32, :], func=AF.Exp, scale=-1.0, bias=nbg[:, 0:1]),
                      reads=[("pz", pc % 2), "nbg"], writes=["t1"])
            s.act(lambda e: e.activation(out=t1[:], in_=t1[:], func=AF.Ln, bias=1.0), reads=["t1"], writes=["t1"])
            s.dve(lambda e: e.tensor_tensor_scan(out=cum[:], data0=rmt[:], data1=t1[:], initial=0.0, op0=ALU.mult, op1=ALU.add),
                  reads=["rmt", "t1"], writes=["cum"])
            s.act(lambda e: e.activation(out=eb[:], in_=cum[:], func=AF.Exp, scale=-1.0 / 16), reads=["cum"], writes=["eb"])
            s.act(lambda e: e.activation(out=enb[:], in_=cum[:], func=AF.Exp, scale=1.0 / 16), reads=["cum"], writes=["enb"])
            s.dve(lambda e: e.scalar_tensor_tensor(out=qt_[:], in0=qs[:], scalar=32.0 ** -0.5, in1=eb[:], op0=ALU.mult, op1=ALU.mult),
                  reads=["qs", "eb"], writes=["qt"])
            s.dve(lambda e: e.tensor_tensor(out=kt_[:], in0=ks[:], in1=enb[:], op=ALU.mult), reads=["ks", "enb"], writes=["kt"])
            s.act(lambda e: e.activation(out=sil[:], in_=rs_[:], func=AF.Silu), reads=["rs"], writes=["sil"])
            def first_half(c, p):
                cs = slice(c * 64, (c + 1) * 64)
                s.pe(lambda e, p=p, cs=cs: e.transpose(pk[p][:, 0:32], kt_[:, cs], idt[:]), reads=["kt", "idt"], writes=[("pk", p)])
                s.act(lambda e, p=p: e.activation(out=ktok[:, p, :], in_=pk[p][:, 0:32], func=AF.Copy), reads=[("pk", p)], writes=[("ktok", p)])
                s.pe(lambda e, p=p, cs=cs: e.matmul(pt_[p][:, 0:64], lhsT=kt_[:, cs], rhs=qt_[:, cs], start=True, stop=True),
                     reads=["kt", "qt"], writes=[("pt", p)])
                s.dve(lambda e, p=p: e.tensor_tensor(out=am[:, p, :], in0=pt_[p][:, 0:64], in1=trit[:], op=ALU.mult),
                      reads=[("pt", p), "trit"], writes=[("am", p)])

            def second_half(c, p):
                cs = slice(c * 64, (c + 1) * 64)
                ac = eb[:, c * 64 + 63:c * 64 + 64]
                sp, sn = p, 1 - p
                s.pe(lambda e, p=p, c=c: e.matmul(po[:, 0:64], lhsT=vs[:, c, :], rhs=am[:, p, :], start=True, stop=False),
                     reads=["vs", ("am", p)], writes=["po"])
                s.pe(lambda e, sp=sp, cs=cs: e.matmul(po[:, 0:64], lhsT=S[:, sp, :], rhs=qt_[:, cs], start=False, stop=True),
                     reads=[("S", sp), "qt"], writes=["po"])
                s.act(lambda e, cs=cs: e.activation(out=oTs[:, cs], in_=po[:, 0:64], func=AF.Copy), reads=["po"], writes=["oTs"])
                s.pe(lambda e, p=p, c=c: e.matmul(pu[0:32, 0:64], lhsT=ktok[:, p, :], rhs=vs[:, c, :], start=True, stop=True),
                     reads=[("ktok", p), "vs"], writes=["pu"])
                s.act(lambda e, p=p, ac=ac: e.activation(out=U[:, p, :], in_=pu[0:32, 0:64], func=AF.Copy, scale=ac), reads=["pu", "eb"], writes=[("U", p)])
                s.dve(lambda e, p=p, sp=sp, sn=sn, ac=ac: e.scalar_tensor_tensor(out=S[:, sn, :], in0=S[:, sp, :], scalar=ac, in1=U[:, p, :],
                                                                               op0=ALU.mult, op1=ALU.add),
                      reads=[("S", sp), ("U", p), "eb"], writes=[("S", sn)])

            first_half(0, cc % 2)
            for c in range(CPS):
                p = cc % 2
                cc += 1
                if c + 1 < CPS:
                    first_half(c + 1, cc % 2)
                second_half(c, p)
            s.act(lambda e: e.activation(out=sqs[:], in_=oTs[:], func=AF.Square), reads=["oTs"], writes=["sqs"])
            for pc in range(SEG // 512):
                pzb = pz[pc % 2]
                ps_ = slice(pc * 512, (pc + 1) * 512)
                s.pe(lambda e, pzb=pzb, ps_=ps_: e.matmul(pzb[:, :], lhsT=ones[:], rhs=sqs[:, ps_], start=True, stop=True),
                     reads=["ones", "sqs"], writes=[("pz", pc % 2)])
                s.act(lambda e, pzb=pzb: e.activation(out=rsd[:], in_=pzb[:, :], func=AF.Sqrt, scale=1.0 / 64, bias=epst[:, 0:1]),
                      reads=[("pz", pc % 2), "epst"], writes=["rsd"])
                s.dve(lambda e: e.reciprocal(out=rsd[:], in_=rsd[:]), reads=["rsd"], writes=["rsd"])
                s.dve(lambda e, ps_=ps_: e.tensor_tensor(out=oTs[:, ps_], in0=oTs[:, ps_], in1=rsd[:], op=ALU.mult), reads=["oTs", "rsd"], writes=["oTs"])
            s.dve(lambda e: e.scalar_tensor_tensor(out=outb[:], in0=oTs[:], scalar=gnt[:, 0:1], in1=sil[:], op0=ALU.mult, op1=ALU.mult),
                  reads=["oTs", "gnt", "sil"], writes=["outb"])
            s.dma(lambda e, c0=c0: e.dma_start(out=obT[:, c0:c0 + SEG], in_=outb[:]), reads=["outb"])
        s.emit(stack)
        print("G: ops", len(s.ops), "waits", s.n_waits)
    return nc


def host_inputs_G(pf32_full, w_gate_up, b_gate, gla_norm_g):
    maps = []
    rm = np.ones((32, SEG), np.float32)
    rm[:, ::64] = 0.0
    tri = np.triu(np.ones((64, 64), np.float32))
    for c in range(8):
        b, h = divmod(c, 4)
        p = pf32_full[b]
        maps.append({
            "qT": np.ascontiguousarray(p[:, 4 + 32 * h:4 + 32 * (h + 1)].T),
            "kT": np.ascontiguousarray(p[:, 132 + 32 * h:132 + 32 * (h + 1)].T),
            "vtok": np.ascontiguousarray(p[:, 260 + 64 * h:260 + 64 * (h + 1)].reshape(128, 64, 64).transpose(1, 0, 2)),
            "glT": np.ascontiguousarray(p[:, 772:788].T),
            "wg": np.ascontiguousarray(w_gate_up[:, 32 * h:32 * (h + 1)]),
            "bg": np.ascontiguousarray(b_gate[32 * h:32 * (h + 1)].reshape(32, 1)),
            "rT": np.ascontiguousarray(p[:, 516 + 64 * h:516 + 64 * (h + 1)].T),
            "gng": np.ascontiguousarray(gla_norm_g[h].reshape(64, 1)),
            "resetm": rm, "tri": tri, "ident": np.eye(32, dtype=np.float32),
        })
    return maps


DFF = 2816
NFC = 22
UW = 256
SLOTW = 2 + 512
NCOL = 4 * SLOTW


def build_F(final=False):
    nc = bass.Bass("TRN2", target_bir_lowering=False)
    D = lambda name, shape, dt, kind="ExternalInput": nc.dram_tensor(name, shape, dt, kind=kind).ap()
    mixT = D("mixT", [1024, NCOL], BF16)
    xT = D("xT", [1024, NCOL], F32)
    w_out = D("w_out", [1024, 1024], F32)
    w_up = D("w_up", [1024, 2 * DFF], F32)
    w_down = D("w_down", [DFF, 1024], F32)
    g2 = D("g2", [128, 8], F32)
    cw = D("cw", [128, NFC, 4], F32)
    hflag = D("hflag", [128, 4], F32)
    gf = D("gf", [128, 8], F32)
    xoT = D("xoT", [1024, 2048], F32, "ExternalOutput")

    with ExitStack() as stack:
        T = lambda name, shape, dt: stack.enter_context(nc.sbuf_tensor(name, shape, dt))
        P = lambda name, shape, dt: stack.enter_context(nc.psum_tensor(name, shape, dt))
        wo = T("wo", [128, 8, 1024], BF16)
        wu = T("wu", [128, 8, 2 * DFF], BF16)
        wd = T("wd", [128, NFC, 1024], BF16)
        wst = T("wst", [128, 2, 1024], F32)
        g2t = T("g2t", [128, 8], F32)
        gft = T("gft", [128, 8], F32)
        cwt = T("cwt", [128, NFC, 4], F32)
        hft = T("hft", [128, 4], F32)
        ones = T("ones", [128, 128], F32)
        epst = T("epst", [128, 1], F32)
        mx = T("mx", [128, 8, UW], BF16)
        xm = T("xm", [128, 8, UW], F32)
        sq = T("sq", [128, 2, UW], F32)
        rs = T("rs", [128, UW], F32)
        h2 = T("h2", [128, 8, UW], BF16)
        actT = T("actT", [128, NFC, UW], BF16)
        aext = T("aext", [128, 2, 2 + UW], F32)
        cv = T("cv", [128, 2, UW], F32)
        sg = T("sg", [128, 2, UW], F32)
        atail = T("atail", [128, NFC, 2], F32)
        pa = [P("pa%d" % i, [128, 512], F32) for i in range(8)]

        s = Sched(nc)
        for (dst, src, nm) in ((g2t, g2, "g2t"), (gft, gf, "gft"), (cwt, cw, "cwt"), (hft, hflag, "hft")):
            s.dma(lambda e, dst=dst, src=src: e.dma_start(out=dst[:], in_=src), writes=[nm])
        s.dve(lambda e: e.memset(ones[:], 1.0), writes=["ones"])
        s.dve(lambda e: e.memset(epst[:], 1e-6), writes=["epst"])
        wc = 0
        def load_w(dst_fn, src_fn, nrow_chunks, ncols, regname, scale_g):
            nonlocal wc
            for c in range(nrow_chunks):
                for c0 in range(0, ncols, 1024):
                    c1 = min(ncols, c0 + 1024)
                    b = wc % 2
                    wc += 1
                    s.dma(lambda e, b=b, c=c, c0=c0, c1=c1: e.dma_start(out=wst[:, b, 0:c1 - c0], in_=src_fn(c, c0, c1)), writes=[("wst", b)])
                    use_dve = (wc % 2 == 0)
                    if scale_g:
                        if use_dve:
                            s.dve(lambda e, b=b, c=c, c0=c0, c1=c1: e.tensor_scalar(out=dst_fn(c, c0, c1), in0=wst[:, b, 0:c1 - c0], scalar1=g2t[:, c:c + 1],
                                                                                  scalar2=None, op0=ALU.mult),
                                  reads=[("wst", b), "g2t"], writes=[(regname, c)])
                        else:
                            s.act(lambda e, b=b, c=c, c0=c0, c1=c1: e.activation(out=dst_fn(c, c0, c1), in_=wst[:, b, 0:c1 - c0], func=AF.Copy,
                                                                               scale=g2t[:, c:c + 1]),
                                  reads=[("wst", b), "g2t"], writes=[(regname, c)])
                    else:
                        if use_dve:
                            s.dve(lambda e, b=b, c=c, c0=c0, c1=c1: e.tensor_copy(out=dst_fn(c, c0, c1), in_=wst[:, b, 0:c1 - c0]),
                                  reads=[("wst", b)], writes=[(regname, c)])
                        else:
                            s.act(lambda e, b=b, c=c, c0=c0, c1=c1: e.activation(out=dst_fn(c, c0, c1), in_=wst[:, b, 0:c1 - c0], func=AF.Copy),
                                  reads=[("wst", b)], writes=[(regname, c)])
        load_w(lambda c, c0, c1: wo[:, c, c0:c1], lambda c, c0, c1: w_out[c * 128:(c + 1) * 128, c0:c1], 8, 1024, "wo", False)
        load_w(lambda c, c0, c1: wu[:, c, c0:c1], lambda c, c0, c1: w_up[c * 128:(c + 1) * 128, c0:c1], 8, 2 * DFF, "wu", True)
        load_w(lambda c, c0, c1: wd[:, c, c0:c1], lambda c, c0, c1: w_down[c * 128:(c + 1) * 128, c0:c1], NFC, 1024, "wd", False)
        WO = [("wo", c) for c in range(8)]
        WU = [("wu", c) for c in range(8)]
        WD = [("wd", c) for c in range(NFC)]

        def unit(k, col0, n, halo, out0):
            s.dma(lambda e: e.dma_start(out=mx[:, :, 0:n], in_=mixT[:, col0:col0 + n].rearrange("(c p) t -> p c t", p=128)), writes=["mx"])
            s.dma(lambda e: e.dma_start(out=xm[:, :, 0:n], in_=xT[:, col0:col0 + n].rearrange("(c p) t -> p c t", p=128)), writes=["xm"])
            for dc in range(8):
                pt = pa[dc % 2]
                for c in range(8):
                    s.pe(lambda e, pt=pt, c=c, dc=dc: e.matmul(pt[:, 0:n], lhsT=wo[:, c, dc * 128:(dc + 1) * 128], rhs=mx[:, c, 0:n],
                                                              start=(c == 0), stop=(c == 7)),
                         reads=WO + ["mx"], writes=[("pa", dc % 2)])
                s.dve(lambda e, pt=pt, dc=dc: e.tensor_tensor(out=xm[:, dc, 0:n], in0=pt[:, 0:n], in1=xm[:, dc, 0:n], op=ALU.add),
                      reads=[("pa", dc % 2), "xm"], writes=["xm"])
                s.act(lambda e, dc=dc: e.activation(out=sq[:, dc % 2, 0:n], in_=xm[:, dc, 0:n], func=AF.Square), reads=["xm"], writes=[("sq", dc % 2)])
                s.pe(lambda e, dc=dc: e.matmul(pa[2][:, 0:n], lhsT=ones[:], rhs=sq[:, dc % 2, 0:n], start=(dc == 0), stop=(dc == 7)),
                     reads=["ones", ("sq", dc % 2)], writes=[("pa", 2)])
            s.act(lambda e: e.activation(out=rs[:, 0:n], in_=pa[2][:, 0:n], func=AF.Sqrt, scale=1.0 / 1024, bias=epst[:, 0:1]),
                  reads=[("pa", 2), "epst"], writes=["rs"])
            s.dve(lambda e: e.reciprocal(out=rs[:, 0:n], in_=rs[:, 0:n]), reads=["rs"], writes=["rs"])
            for dc in range(8):
                s.dve(lambda e, dc=dc: e.tensor_tensor(out=h2[:, dc, 0:n], in0=xm[:, dc, 0:n], in1=rs[:, 0:n], op=ALU.mult),
                    reads=["xm", "rs"], writes=["h2"])
            for fc in range(NFC):
                ab = fc % 2
                pA = pa[3 + ab]
                pB = pa[5 + ab]
                for c in range(8):
                    s.pe(lambda e, pA=pA, c=c, fc=fc: e.matmul(pA[:, 0:n], lhsT=wu[:, c, fc * 128:(fc + 1) * 128], rhs=h2[:, c, 0:n],
                                                              start=(c == 0), stop=(c == 7)),
                         reads=WU + ["h2"], writes=[("pa", 3 + ab)])
                if halo:
                    s.dve(lambda e, pA=pA, fc=fc, k=k: e.tensor_scalar(out=atail[:, fc, :], in0=pA[:, 0:2], scalar1=hft[:, k:k + 1], scalar2=None,
                                                                     op0=ALU.mult),
                          reads=[("pa", 3 + ab), "hft"], writes=[("atail", fc)])
                    continue
                for c in range(8):
                    s.pe(lambda e, pB=pB, c=c, fc=fc: e.matmul(pB[:, 0:n], lhsT=wu[:, c, DFF + fc * 128:DFF + (fc + 1) * 128], rhs=h2[:, c, 0:n],
                                                              start=(c == 0), stop=(c == 7)),
                         reads=WU + ["h2"], writes=[("pa", 5 + ab)])
                s.act(lambda e, ab=ab, fc=fc: e.activation(out=aext[:, ab, 0:2], in_=atail[:, fc, :], func=AF.Copy),
                      reads=[("atail", fc)], writes=[("aext", ab)])
                s.act(lambda e, ab=ab, pA=pA: e.activation(out=aext[:, ab, 2:2 + n], in_=pA[:, 0:n], func=AF.Copy),
                      reads=[("pa", 3 + ab)], writes=[("aext", ab)])
                s.act(lambda e, ab=ab, fc=fc: e.activation(out=atail[:, fc, :], in_=aext[:, ab, n:n + 2], func=AF.Copy),
                      reads=[("aext", ab)], writes=[("atail", fc)])
                s.dve(lambda e, ab=ab, fc=fc: e.tensor_scalar(out=cv[:, ab, 0:n], in0=aext[:, ab, 2:2 + n], scalar1=cwt[:, fc, 2:3], scalar2=cwt[:, fc, 3:4],
                                                            op0=ALU.mult, op1=ALU.add),
                      reads=[("aext", ab), "cwt"], writes=[("cv", ab)])
                s.dve(lambda e, ab=ab, fc=fc: e.scalar_tensor_tensor(out=cv[:, ab, 0:n], in0=aext[:, ab, 1:1 + n], scalar=cwt[:, fc, 1:2], in1=cv[:, ab, 0:n],
                                                                   op0=ALU.mult, op1=ALU.add),
                      reads=[("aext", ab), "cwt", ("cv", ab)], writes=[("cv", ab)])
                s.dve(lambda e, ab=ab, fc=fc: e.scalar_tensor_tensor(out=cv[:, ab, 0:n], in0=aext[:, ab, 0:n], scalar=cwt[:, fc, 0:1], in1=cv[:, ab, 0:n],
                                                                   op0=ALU.mult, op1=ALU.add),
                      reads=[("aext", ab), "cwt", ("cv", ab)], writes=[("cv", ab)])
                s.act(lambda e, ab=ab: e.activation(out=sg[:, ab, 0:n], in_=cv[:, ab, 0:n], func=AF.Silu), reads=[("cv", ab)], writes=[("sg", ab)])
                s.dve(lambda e, ab=ab, fc=fc, pB=pB: e.tensor_tensor(out=actT[:, fc, 0:n], in0=pB[:, 0:n], in1=sg[:, ab, 0:n], op=ALU.mult),
                      reads=[("pa", 5 + ab), ("sg", ab)], writes=[("actT", fc)])
            if halo:
                return
            AT = [("actT", fc) for fc in range(NFC)]
            for dc in range(8):
                pt = pa[dc % 2]
                for fc in range(NFC):
                    s.pe(lambda e, pt=pt, fc=fc, dc=dc: e.matmul(pt[:, 0:n], lhsT=wd[:, fc, dc * 128:(dc + 1) * 128], rhs=actT[:, fc, 0:n],
                                                                start=(fc == 0), stop=(fc == NFC - 1)),
                         reads=WD + AT, writes=[("pa", dc % 2)])
                s.dve(lambda e, pt=pt, dc=dc: e.tensor_tensor(out=xm[:, dc, 0:n], in0=pt[:, 0:n], in1=xm[:, dc, 0:n], op=ALU.add),
                      reads=[("pa", dc % 2), "xm"], writes=["xm"])
            if final:
                for dc in range(8):
                    s.act(lambda e, dc=dc: e.activation(out=sq[:, dc % 2, 0:n], in_=xm[:, dc, 0:n], func=AF.Square), reads=["xm"], writes=[("sq", dc % 2)])
                    s.pe(lambda e, dc=dc: e.matmul(pa[2][:, 0:n], lhsT=ones[:], rhs=sq[:, dc % 2, 0:n], start=(dc == 0), stop=(dc == 7)),
                         reads=["ones", ("sq", dc % 2)], writes=[("pa", 2)])
                s.act(lambda e: e.activation(out=rs[:, 0:n], in_=pa[2][:, 0:n], func=AF.Sqrt, scale=1.0 / 1024, bias=epst[:, 0:1]),
                      reads=[("pa", 2), "epst"], writes=["rs"])
                s.dve(lambda e: e.reciprocal(out=rs[:, 0:n], in_=rs[:, 0:n]), reads=["rs"], writes=["rs"])
                for dc in range(8):
                    s.dve(lambda e, dc=dc: e.scalar_tensor_tensor(out=xm[:, dc, 0:n], in0=xm[:, dc, 0:n], scalar=gft[:, dc:dc + 1], in1=rs[:, 0:n],
                                                                op0=ALU.mult, op1=ALU.mult),
                          reads=["xm", "rs", "gft"], writes=["xm"])
            s.dma(lambda e: e.dma_start(out=xoT[:, out0:out0 + n].rearrange("(c p) t -> p c t", p=128), in_=xm[:, :, 0:n]), reads=["xm"])

        for k in range(4):
            unit(k, k * SLOTW, 2, True, None)
            for u in range(512 // UW):
                unit(k, k * SLOTW + 2 + u * UW, UW, False, k * 512 + u * UW)
        s.emit(stack)
        print("F: ops", len(s.ops), "waits", s.n_waits)
    return nc


def host_inputs_F(mix_list, x_full, w_out, norm2_g, w_up, conv_w, conv_b, w_down, final_g):
    bf = ml_dtypes.bfloat16
    maps = []
    cw = np.zeros((128, NFC, 4), np.float32)
    cw[:, :, 0:3] = conv_w.T.reshape(NFC, 128, 3).transpose(1, 0, 2)
    cw[:, :, 3] = conv_b.reshape(NFC, 128).T
    for c in range(8):
        b, j = divmod(c, 4)
        mcols, xcols = [], []
        hf = np.ones((128, 4), np.float32)
        for k in range(4):
            g = 4 * k + j
            t0 = 512 * g
            if g == 0:
                mcols.append(np.zeros((2, 1024), bf))
                xcols.append(np.zeros((2, 1024), np.float32))
                hf[:, k] = 0.0
            else:
                mcols.append(mix_list[b][t0 - 2:t0])
                xcols.append(x_full[b, t0 - 2:t0])
            mcols.append(mix_list[b][t0:t0 + 512])
            xcols.append(x_full[b, t0:t0 + 512])
        maps.append({
            "mixT": np.ascontiguousarray(np.concatenate(mcols, 0).T), "xT": np.ascontiguousarray(np.concatenate(xcols, 0).T),
            "w_out": np.ascontiguousarray(w_out), "w_up": np.ascontiguousarray(w_up), "w_down": np.ascontiguousarray(w_down),
            "g2": np.ascontiguousarray(norm2_g.reshape(8, 128).T), "cw": cw, "hflag": hf,
            "gf": np.ascontiguousarray(final_g.reshape(8, 128).T),
        })
    return maps


_CACHE = {}
CHECK = None


def _get(name, fn):
    if name not in _CACHE:
        _CACHE[name] = fn()
    return _CACHE[name]


def _run(nc, maps):
    res = run_bass_kernel_spmd(nc, maps, core_ids=list(range(8)))
    return res.results


def forward(x, positions, norm1_g, w_in, w_gate_up, b_gate, gla_norm_g, w_pool, pool_scale, w_out, norm2_g, w_up, conv_w, conv_b,
            w_down, final_norm_g):
    bf = ml_dtypes.bfloat16
    x = np.asarray(x, np.float32)
    positions = np.asarray(positions, np.int32)
    depth = norm1_g.shape[0]
    ncA = _get("A", build_A)
    ncB = _get("B", build_B)
    ncG = _get("G", build_G)
    for l in range(depth):
        last = (l == depth - 1)
        rA = _run(ncA, host_inputs_A(x, positions, np.asarray(norm1_g[l]), np.asarray(w_in[l]), np.asarray(w_pool[l]), np.asarray(pool_scale[l])))
        pbf = [np.asarray(r["pbf"]) for r in rA]
        pf32 = [np.asarray(r["pf32"]) for r in rA]
        ocT = [np.asarray(r["ocT"]) for r in rA]
        if CHECK:
            CHECK("A", l, dict(pbf=pbf, pf32=pf32, ocT=ocT))
        rB = _run(ncB, host_inputs_B(pbf, pf32))
        oaT = [np.asarray(r["oaT"]) for r in rB]
        if CHECK:
            CHECK("B", l, dict(oaT=oaT))
        pf_full = []
        for b in range(2):
            full = np.zeros((8192, pf32[0].shape[1]), np.float32)
            for j in range(4):
                for k in range(4):
                    g = 4 * k + j
                    full[512 * g:512 * (g + 1)] = pf32[b * 4 + j][512 * k:512 * (k + 1)]
            pf_full.append(full)
        rG = _run(ncG, host_inputs_G(pf_full, np.asarray(w_gate_up[l]), np.asarray(b_gate[l]), np.asarray(gla_norm_g[l])))
        obT = [np.asarray(r["obT"]) for r in rG]
        if CHECK:
            CHECK("G", l, dict(obT=obT))
        mix = []
        for b in range(2):
            m = np.zeros((8192, 1024), bf)
            for j in range(4):
                c = b * 4 + j
                oa = oaT[c].transpose(2, 1, 0).reshape(2048, 512)
                oc = ocT[c].transpose(2, 1, 0).reshape(2048, 256)
                for k in range(4):
                    g = 4 * k + j
                    m[512 * g:512 * (g + 1), 0:512] = oa[512 * k:512 * (k + 1)]
                    m[512 * g:512 * (g + 1), 768:1024] = oc[512 * k:512 * (k + 1)]
            for h in range(4):
                m[:, 512 + 64 * h:512 + 64 * (h + 1)] = obT[b * 4 + h].T
            mix.append(m)
        if CHECK:
            CHECK("mix", l, dict(mix=mix))
        ncF = _get("F%d" % int(last), lambda: build_F(final=last))
        rF = _run(ncF, host_inputs_F(mix, x, np.asarray(w_out[l]), np.asarray(norm2_g[l]), np.asarray(w_up[l]), np.asarray(conv_w[l]),
                                     np.asarray(conv_b[l]), np.asarray(w_down[l]), np.asarray(final_norm_g)))
        xn = np.zeros_like(x)
        for c in range(8):
            b, j = divmod(c, 4)
            xo = np.asarray(rF[c]["xoT"]).T
            for k in range(4):
                g = 4 * k + j
                xn[b, 512 * g:512 * (g + 1)] = xo[512 * k:512 * (k + 1)]
        x = xn
        if CHECK:
            CHECK("F", l, dict(x=x))
    return x


def kernel(**inputs):
    out = forward(**{k: np.asarray(v) for k, v in inputs.items()})
    return np.ascontiguousarray(out.astype(np.float32))
```

```python
import numpy as np
import concourse.bass as bass
import concourse.mybir as mybir
from concourse.bass_utils import run_bass_kernel_spmd
from contextlib import ExitStack
import math
import ml_dtypes

F32 = mybir.dt.float32
BF16 = mybir.dt.bfloat16
I32 = mybir.dt.int32
ALU = mybir.AluOpType
AF = mybir.ActivationFunctionType
AX = mybir.AxisListType

ENGS = ("pe", "act", "dve", "pool", "sp")


class _Op:
    __slots__ = ("eng", "fn", "reads", "writes", "dma", "deps", "sig", "sigval", "dsem", "idx")


class Sched:
    def __init__(self, nc, n_dma_sems=6):
        self.nc = nc
        self.ops = []
        self.last_w = {}
        self.readers = {}
        self.n_dma_sems = n_dma_sems

    def add(self, eng, fn, reads=(), writes=(), dma=False):
        op = _Op()
        op.eng = eng
        op.fn = fn
        op.reads = tuple(reads)
        op.writes = tuple(writes)
        op.dma = dma
        op.idx = len(self.ops)
        deps = set()
        for r in op.reads:
            w = self.last_w.get(r)
            if w is not None:
                deps.add(w)
        for r in op.writes:
            w = self.last_w.get(r)
            if w is not None:
                deps.add(w)
            for rd in self.readers.get(r, ()):
                deps.add(rd)
        deps.discard(op.idx)
        op.deps = deps
        for r in op.reads:
            self.readers.setdefault(r, []).append(op.idx)
        for r in op.writes:
            self.last_w[r] = op.idx
            self.readers[r] = []
        op.sig = False
        op.sigval = None
        op.dsem = None
        self.ops.append(op)
        return op

    def pe(self, fn, reads=(), writes=()):
        return self.add("pe", fn, reads, writes)

    def act(self, fn, reads=(), writes=()):
        return self.add("act", fn, reads, writes)

    def dve(self, fn, reads=(), writes=()):
        return self.add("dve", fn, reads, writes)

    def pool(self, fn, reads=(), writes=()):
        return self.add("pool", fn, reads, writes)

    def dma(self, fn, reads=(), writes=(), q="sp"):
        return self.add(q, fn, reads, writes, dma=True)

    def emit(self, stack):
        nc = self.nc
        ops = self.ops
        for op in ops:
            for d in op.deps:
                dop = ops[d]
                if dop.eng == op.eng and not dop.dma:
                    if not (set(dop.writes) & set(op.reads)):
                        continue
                dop.sig = True
        for op in ops:
            if op.dma:
                op.sig = True
        esem = {e: stack.enter_context(nc.semaphore("s_" + e)) for e in ENGS}
        dsems = {}
        for q in ENGS:
            if any(o.dma and o.eng == q for o in ops):
                dsems[q] = [stack.enter_context(nc.semaphore("d_%s%d" % (q, i))) for i in range(self.n_dma_sems)]
        ecount = {e: 0 for e in ENGS}
        dcount = {q: [0] * self.n_dma_sems for q in dsems}
        drr = {q: 0 for q in dsems}
        prev_dma_wait = {}
        for op in ops:
            if op.dma:
                k = drr[op.eng]
                drr[op.eng] = (k + 1) % self.n_dma_sems
                prev_dma_wait[op.idx] = dcount[op.eng][k]
                dcount[op.eng][k] += 16
                op.dsem = (op.eng, k)
                op.sigval = dcount[op.eng][k]
            elif op.sig:
                ecount[op.eng] += 1
                op.sigval = ecount[op.eng]
        by_eng = {e: [o for o in ops if o.eng == e] for e in ENGS}
        block = stack.enter_context(nc.Block())
        self.n_waits = 0

        def run(ename, eobj):
            waited = {}
            for op in by_eng[ename]:
                need = {}
                for d in op.deps:
                    dop = ops[d]
                    if dop.dma:
                        key = ("d",) + dop.dsem
                        sem = dsems[dop.dsem[0]][dop.dsem[1]]
                    else:
                        if dop.eng == ename and not (set(dop.writes) & set(op.reads)):
                            continue
                        key = ("e", dop.eng)
                        sem = esem[dop.eng]
                    v = dop.sigval
                    if waited.get(key, 0) >= v:
                        continue
                    if key not in need or need[key][1] < v:
                        need[key] = (sem, v)
                if op.dma:
                    pv = prev_dma_wait[op.idx]
                    key = ("d",) + op.dsem
                    if pv > 0 and waited.get(key, 0) < pv:
                        if key not in need or need[key][1] < pv:
                            need[key] = (dsems[op.dsem[0]][op.dsem[1]], pv)
                for key, (sem, v) in need.items():
                    eobj.wait_ge(sem, v)
                    waited[key] = v
                    self.n_waits += 1
                ins = op.fn(eobj)
                if op.dma:
                    ins.then_inc(dsems[op.dsem[0]][op.dsem[1]], 16)
                elif op.sig:
                    ins.then_inc(esem[ename], 1)
            for q, lst in dsems.items():
                if q == ename:
                    for k, s in enumerate(lst):
                        if dcount[q][k] > 0:
                            eobj.wait_ge(s, dcount[q][k])

        if by_eng["pe"]:
            @block.tensor
            def _(e):
                run("pe", e)
        if by_eng["act"]:
            @block.scalar
            def _(e):
                run("act", e)
        if by_eng["dve"]:
            @block.vector
            def _(e):
                run("dve", e)
        if by_eng["pool"]:
            @block.gpsimd
            def _(e):
                run("pool", e)
        if by_eng["sp"]:
            @block.sync
            def _(e):
                run("sp", e)


NTILE = 20
INW = 2900
CH = [(0, 512), (512, 1024), (1024, 1536), (1536, 2048), (2048, 2560), (2560, 2900)]
UC0 = 2644
BFW = 1856
F32W = INW - BFW
TWO_PI = 2.0 * math.pi


def pool_mats(first):
    Mc = np.zeros((128, 4, 128), np.float32)
    Mh = np.zeros((128, 4, 128), np.float32)
    for gi, w in enumerate((2, 4, 8, 16)):
        for t in range(128):
            cnt = min(t + 1, w) if first else w
            for s in range(t - w + 1, t + 1):
                if s >= 0:
                    Mc[s, gi, t] += 1.0 / cnt
                elif not first:
                    Mh[128 + s, gi, t] += 1.0 / cnt
            Mc[t, gi, t] -= 1.0
    return Mc, Mh


def build_A():
    nc = bass.Bass("TRN2", target_bir_lowering=False)
    D = lambda name, shape, dt, kind="ExternalInput": nc.dram_tensor(name, shape, dt, kind=kind).ap()
    xtok = D("xtok", [NTILE * 128, 1024], F32)
    xT = D("xT", [1024, NTILE * 128], F32)
    w_in = D("w_in", [1024, INW], F32)
    g1 = D("g1", [128, 8], F32)
    pos = D("pos", [128, NTILE], I32)
    inv = D("inv", [128, 8], F32)
    wpool = D("wpool", [64, 4, 64], F32)
    pscale = D("pscale", [64, 4], F32)
    mcur = D("mcur", [128, 5, 4, 128], F32)
    mhal = D("mhal", [128, 5, 4, 128], F32)
    pbf = D("pbf", [2048, BFW], BF16, "ExternalOutput")
    pf32 = D("pf32", [2048, F32W], F32, "ExternalOutput")
    ocT = D("ocT", [64, 4, 2048], BF16, "ExternalOutput")

    with ExitStack() as stack:
        T = lambda name, shape, dt: stack.enter_context(nc.sbuf_tensor(name, shape, dt))
        P = lambda name, shape, dt: stack.enter_context(nc.psum_tensor(name, shape, dt))
        wbf = T("wbf", [128, 8, INW], BF16)
        wst = T("wst", [128, 2, INW], F32)
        g1t = T("g1t", [128, 8], F32)
        posi = T("posi", [128, NTILE], I32)
        posf = T("posf", [128, NTILE], F32)
        invt = T("invt", [128, 8], F32)
        ang = T("ang", [128, NTILE, 8], F32)
        kq = T("kq", [128, NTILE, 8], F32)
        angc = T("angc", [128, NTILE, 8], F32)
        kqi = T("kqi", [128, NTILE, 8], I32)
        cost = T("cost", [128, NTILE, 8], F32)
        sint = T("sint", [128, NTILE, 8], F32)
        wpt = T("wpt", [64, 4, 64], F32)
        pst = T("pst", [64, 4], F32)
        mct = T("mct", [128, 5, 4, 128], F32)
        mht = T("mht", [128, 5, 4, 128], F32)
        xtk = T("xtk", [128, 2, 1024], F32)
        sqj = T("sqj", [128, 1024], BF16)
        ss = T("ss", [128, 2], F32)
        rstd = T("rstd", [128, 2], F32)
        epst = T("epst", [128, 1], F32)
        xTt = T("xTt", [128, 2, 8, 128], F32)
        hT = T("hT", [128, 2, 8, 128], BF16)
        proj = T("proj", [128, 2, INW], F32)
        pb16 = T("pb16", [128, 2, BFW], BF16)
        rt = T("rt", [128, 4, 16, 8], F32)
        pooled = T("pooled", [64, 2, 4, 128], F32)
        oct_ = T("oct", [64, 2, 4, 128], BF16)
        ps = [P("ps%d" % i, [128, 512], F32) for i in range(4)]
        pp = [P("pp%d" % i, [64, 4, 128], F32) for i in range(2)]
        py = [P("py%d" % i, [64, 4, 128], F32) for i in range(2)]

        s = Sched(nc)
        s.dma(lambda e: e.dma_start(out=g1t[:], in_=g1), writes=["g1t"])
        s.dma(lambda e: e.dma_start(out=posi[:], in_=pos), writes=["posi"])
        s.dma(lambda e: e.dma_start(out=invt[:], in_=inv), writes=["invt"])
        s.dma(lambda e: e.dma_start(out=wpt[:], in_=wpool), writes=["wpt"])
        s.dma(lambda e: e.dma_start(out=pst[:], in_=pscale), writes=["pst"])
        s.dma(lambda e: e.dma_start(out=mct[:], in_=mcur), writes=["mct"])
        s.dma(lambda e: e.dma_start(out=mht[:], in_=mhal), writes=["mht"])
        s.dve(lambda e: e.memset(epst[:], 1e-6), writes=["epst"])
        s.dve(lambda e: e.tensor_copy(out=posf[:], in_=posi[:]), reads=["posi"], writes=["posf"])
        for t in range(NTILE):
            s.dve(lambda e, t=t: e.tensor_scalar(out=ang[:, t, :], in0=invt[:], scalar1=posf[:, t:t + 1], scalar2=None, op0=ALU.mult),
                  reads=["invt", "posf"], writes=["ang"])
        s.dve(lambda e: e.tensor_scalar(out=kqi[:], in0=ang[:], scalar1=1.0 / TWO_PI, scalar2=None, op0=ALU.mult), reads=["ang"], writes=["kqi"])
        s.dve(lambda e: e.tensor_copy(out=kq[:], in_=kqi[:]), reads=["kqi"], writes=["kq"])
        s.dve(lambda e: e.scalar_tensor_tensor(out=ang[:], in0=kq[:], scalar=-TWO_PI, in1=ang[:], op0=ALU.mult, op1=ALU.add),
              reads=["kq", "ang"], writes=["ang"])
        def wrap(y, name):
            s.dve(lambda e: e.tensor_scalar(out=kq[:], in0=y[:], scalar1=math.pi, scalar2=-TWO_PI, op0=ALU.is_gt, op1=ALU.mult),
                  reads=[name], writes=["kq"])
            s.dve(lambda e: e.tensor_tensor(out=y[:], in0=y[:], in1=kq[:], op=ALU.add), reads=[name, "kq"], writes=[name])
            s.dve(lambda e: e.tensor_scalar(out=kq[:], in0=y[:], scalar1=-math.pi, scalar2=TWO_PI, op0=ALU.is_lt, op1=ALU.mult),
                  reads=[name], writes=["kq"])
            s.dve(lambda e: e.tensor_tensor(out=y[:], in0=y[:], in1=kq[:], op=ALU.add), reads=[name, "kq"], writes=[name])
        s.dve(lambda e: e.tensor_scalar(out=angc[:], in0=ang[:], scalar1=math.pi / 2, scalar2=None, op0=ALU.add), reads=["ang"], writes=["angc"])
        wrap(ang, "ang")
        wrap(angc, "angc")
        s.act(lambda e: e.activation(out=sint[:], in_=ang[:], func=AF.Sin), reads=["ang"], writes=["sint"])
        s.act(lambda e: e.activation(out=cost[:], in_=angc[:], func=AF.Sin), reads=["angc"], writes=["cost"])

        for c in range(8):
            b = c % 2
            s.dma(lambda e, c=c, b=b: e.dma_start(out=wst[:, b, :], in_=w_in[c * 128:(c + 1) * 128, :]), writes=[("wst", b)])
            eng = s.dve if c % 2 == 0 else s.pool
            eng(lambda e, c=c, b=b: e.tensor_scalar(out=wbf[:, c, :], in0=wst[:, b, :], scalar1=g1t[:, c:c + 1], scalar2=None, op0=ALU.mult),
                reads=[("wst", b), "g1t"], writes=[("wbf", c)])

        own = 0
        for t in range(NTILE):
            k, i = divmod(t, 5)
            halo = (i == 0)
            b = t % 2
            pb = (t - 1) % 2
            s.dma(lambda e, t=t, b=b: e.dma_start(out=xtk[:, b, :], in_=xtok[t * 128:(t + 1) * 128, :]), writes=[("xtk", b)])
            s.dma(lambda e, t=t, b=b: e.dma_start(out=xTt[:, b, :, :], in_=xT[:, t * 128:(t + 1) * 128].rearrange("(c p) t -> p c t", p=128)),
                  writes=[("xTt", b)])
            s.act(lambda e, b=b: e.activation(out=sqj[:], in_=xtk[:, b, :], func=AF.Square, accum_out=ss[:, b:b + 1]),
                  reads=[("xtk", b)], writes=["sqj", ("ss", b)])
            s.act(lambda e, b=b: e.activation(out=ss[:, b:b + 1], in_=ss[:, b:b + 1], func=AF.Sqrt, scale=1.0 / 1024, bias=epst[:, 0:1]),
                  reads=[("ss", b), "epst"], writes=[("ss", b)])
            s.dve(lambda e, b=b: e.reciprocal(out=rstd[:, b:b + 1], in_=ss[:, b:b + 1]), reads=[("ss", b)], writes=[("rstd", b)])
            s.pool(lambda e, b=b: e.tensor_copy(out=hT[:, b, :, :], in_=xTt[:, b, :, :]), reads=[("xTt", b)], writes=[("hT", b)])
            chunks = [5] if halo else list(range(6))
            for n in chunks:
                n0, n1 = CH[n]
                pt = ps[n % 4]
                for c in range(8):
                    s.pe(lambda e, pt=pt, b=b, c=c, n0=n0, n1=n1: e.matmul(pt[:, 0:n1 - n0], lhsT=hT[:, b, c, :], rhs=wbf[:, c, n0:n1],
                                                                         start=(c == 0), stop=(c == 7)),
                         reads=[("hT", b), ("wbf", c)], writes=[("ps", n % 4)])
                if n % 2 == 0:
                    s.act(lambda e, pt=pt, b=b, n0=n0, n1=n1: e.activation(out=proj[:, b, n0:n1], in_=pt[:, 0:n1 - n0], func=AF.Copy,
                                                                         scale=rstd[:, b:b + 1]),
                          reads=[("ps", n % 4), ("rstd", b)], writes=[("proj", b, n)])
                else:
                    s.dve(lambda e, pt=pt, b=b, n0=n0, n1=n1: e.tensor_scalar(out=proj[:, b, n0:n1], in0=pt[:, 0:n1 - n0],
                                                                            scalar1=rstd[:, b:b + 1], scalar2=None, op0=ALU.mult),
                          reads=[("ps", n % 4), ("rstd", b)], writes=[("proj", b, n)])
            if not halo:
                for (c0, nh, regs) in ((0, 16, [("proj", b, 0), ("proj", b, 1)]), (1536, 5, [("proj", b, 3)])):
                    v = proj[:, b, c0:c0 + nh * 64].rearrange("p (h d) -> p h d", d=64)
                    x1 = v[:, :, 0:8]
                    x2 = v[:, :, 8:16]
                    cb = cost[:, t, :].unsqueeze(1).to_broadcast([128, nh, 8])
                    sb = sint[:, t, :].unsqueeze(1).to_broadcast([128, nh, 8])
                    t0 = rt[:, 0, 0:nh, :]
                    t1 = rt[:, 1, 0:nh, :]
                    t2 = rt[:, 2, 0:nh, :]
                    t3 = rt[:, 3, 0:nh, :]
                    R = regs + ["cost", "sint"]
                    s.dve(lambda e, t0=t0, x1=x1, cb=cb: e.tensor_tensor(out=t0, in0=x1, in1=cb, op=ALU.mult), reads=R, writes=["rt0"])
                    s.dve(lambda e, t1=t1, x2=x2, sb=sb: e.tensor_tensor(out=t1, in0=x2, in1=sb, op=ALU.mult), reads=R, writes=["rt1"])
                    s.dve(lambda e, t2=t2, x2=x2, cb=cb: e.tensor_tensor(out=t2, in0=x2, in1=cb, op=ALU.mult), reads=R, writes=["rt2"])
                    s.dve(lambda e, t3=t3, x1=x1, sb=sb: e.tensor_tensor(out=t3, in0=x1, in1=sb, op=ALU.mult), reads=R, writes=["rt3"])
                    s.dve(lambda e, t0=t0, t1=t1, x1=x1: e.tensor_tensor(out=x1, in0=t0, in1=t1, op=ALU.subtract),
                          reads=["rt0", "rt1", "rt2", "rt3"], writes=regs)
                    s.dve(lambda e, t2=t2, t3=t3, x2=x2: e.tensor_tensor(out=x2, in0=t2, in1=t3, op=ALU.add),
                          reads=["rt0", "rt1", "rt2", "rt3"], writes=regs)
                r0 = own * 128
                s.act(lambda e, b=b: e.activation(out=pb16[:, b, :], in_=proj[:, b, 0:BFW], func=AF.Copy),
                      reads=[("proj", b, n) for n in range(4)], writes=[("pb16", b)])
                s.dma(lambda e, b=b, r0=r0: e.dma_start(out=pbf[r0:r0 + 128, :], in_=pb16[:, b, :]), reads=[("pb16", b)])
                s.dma(lambda e, b=b, r0=r0: e.dma_start(out=pf32[r0:r0 + 128, :], in_=proj[:, b, BFW:INW]),
                      reads=[("proj", b, n) for n in (3, 4, 5)])
                mi = 0 if i > 1 else 1 + k
                ob = own % 2
                for gi in range(4):
                    s.pe(lambda e, ob=ob, b=b, gi=gi, mi=mi: e.matmul(pp[ob][:, gi, :], lhsT=proj[:, b, UC0 + gi * 64:UC0 + (gi + 1) * 64],
                                                                  rhs=mct[:, mi, gi, :], start=True, stop=False),
                         reads=[("proj", b, 5), "mct"], writes=[("pp", ob)])
                    s.pe(lambda e, ob=ob, pb=pb, gi=gi, mi=mi: e.matmul(pp[ob][:, gi, :], lhsT=proj[:, pb, UC0 + gi * 64:UC0 + (gi + 1) * 64],
                                                                    rhs=mht[:, mi, gi, :], start=False, stop=True),
                         reads=[("proj", pb, 5), "mht"], writes=[("pp", ob)])
                s.act(lambda e, ob=ob: e.activation(out=pooled[:, ob, :, :], in_=pp[ob][:], func=AF.Copy),
                      reads=[("pp", ob)], writes=[("pooled", ob)])
                for gi in range(4):
                    s.pe(lambda e, ob=ob, gi=gi: e.matmul(py[ob][:, gi, :], lhsT=wpt[:, gi, :], rhs=pooled[:, ob, gi, :], start=True, stop=True),
                         reads=[("pooled", ob), "wpt"], writes=[("py", ob)])
                for gi in range(4):
                    s.dve(lambda e, ob=ob, gi=gi: e.tensor_scalar(out=oct_[:, ob, gi, :], in0=py[ob][:, gi, :], scalar1=pst[:, gi:gi + 1],
                                                                scalar2=None, op0=ALU.mult),
                          reads=[("py", ob), "pst"], writes=[("oct", ob)])
                s.dma(lambda e, ob=ob, r0=r0: e.dma_start(out=ocT[:, :, r0:r0 + 128], in_=oct_[:, ob, :, :]), reads=[("oct", ob)])
                own += 1
        s.emit(stack)
        print("A: ops", len(s.ops), "waits", s.n_waits)
    return nc


def host_inputs_A(x, positions, norm1_g, w_in, w_pool, pool_scale):
    inv = (500000.0 ** (-np.arange(0, 16, 2, dtype=np.float32) / 16)).astype(np.float32)
    McG, MhG = pool_mats(False)
    McF, MhF = pool_mats(True)
    maps = []
    for c in range(8):
        b, j = divmod(c, 4)
        rows = []
        posl = []
        mcur = np.zeros((128, 5, 4, 128), np.float32)
        mhal = np.zeros((128, 5, 4, 128), np.float32)
        mcur[:, 0], mhal[:, 0] = McG, MhG
        for k in range(4):
            g = 4 * k + j
            t0 = 512 * g
            if g == 0:
                rows.append(np.zeros((128, 1024), np.float32))
                posl.append(np.zeros((128,), np.int32))
                mcur[:, 1 + k], mhal[:, 1 + k] = McF, MhF
            else:
                rows.append(x[b, t0 - 128:t0])
                posl.append(positions[b, t0 - 128:t0])
                mcur[:, 1 + k], mhal[:, 1 + k] = McG, MhG
            rows.append(x[b, t0:t0 + 512])
            posl.append(positions[b, t0:t0 + 512])
        xt = np.ascontiguousarray(np.concatenate(rows, 0))
        pl = np.concatenate(posl, 0).astype(np.int32)
        maps.append({
            "xtok": xt, "xT": np.ascontiguousarray(xt.T), "w_in": np.ascontiguousarray(w_in),
            "g1": np.ascontiguousarray(norm1_g.reshape(8, 128).T),
            "pos": np.ascontiguousarray(pl.reshape(NTILE, 128).T),
            "inv": np.ascontiguousarray(np.broadcast_to(inv[None, :], (128, 8))),
            "wpool": np.ascontiguousarray(w_pool.transpose(1, 0, 2)),
            "pscale": np.ascontiguousarray(pool_scale.reshape(4, 64).T),
            "mcur": mcur, "mhal": mhal,
        })
    return maps


NIT = 16
NEG = -1.0e30


def build_B(nslot=4, nit=NIT):
    nc = bass.Bass("TRN2", target_bir_lowering=False)
    D = lambda name, shape, dt, kind="ExternalInput": nc.dram_tensor(name, shape, dt, kind=kind).ap()
    kT = D("kT", [128, 4, 8192], BF16)
    vv = D("v", [8192, 512], BF16)
    kiT = D("kiT", [64, 8192], BF16)
    qT = D("qT", [128, 4, 4, 512], BF16)
    qiT = D("qiT", [64, 4, 4, 512], BF16)
    wi = D("wi", [128, 16, 4], F32)
    qrel = D("qrel", [128, 16], F32)
    kpos = D("kpos", [128, 2048], F32)
    ident = D("ident", [128, 128], BF16)
    oaT = D("oaT", [64, 8, 2048], BF16, "ExternalOutput")

    with ExitStack() as stack:
        T = lambda name, shape, dt: stack.enter_context(nc.sbuf_tensor(name, shape, dt))
        P = lambda name, shape, dt: stack.enter_context(nc.psum_tensor(name, shape, dt))
        kit = T("kit", [64, 8192], BF16)
        sc = T("sc", [128, 8192], F32)
        mk = T("mk", [128, 4, 8192], BF16)
        kpt = T("kpt", [128, 2048], F32)
        rr = T("rr", [128, 4, 512], F32)
        qTt = T("qTt", [128, 1, 4, 512], BF16)
        qit = T("qit", [64, 1, 4, 512], BF16)
        wit = T("wit", [128, 16, 4], F32)
        qrt = T("qrt", [128, 16], F32)
        idt = T("idt", [128, 128], BF16)
        kTs = T("kTs", [128, 2, 4, 512], BF16)
        vraw = T("vraw", [128, 2, 4, 512], BF16)
        vt = T("vt", [128, 2, 4, 520], BF16)
        E = T("E", [128, 3, 512], BF16)
        Pm = T("Pm", [128, 3, 512], BF16)
        mT = T("mT", [128, 2, 512], BF16)
        sm = T("sm", [128, 8], F32)
        ones1 = T("ones1", [128, 64], F32)
        rec = T("rec", [128, 512], F32)
        bcs = T("bcs", [64, 512], F32)
        oT = T("oT", [64, 2, 512], BF16)
        pb = [P("pb%d" % i, [128, 512], F32) for i in range(8)]
        pTb = pb[2][:].bitcast(BF16)

        s = Sched(nc)
        s.dma(lambda e: e.dma_start(out=kit[:], in_=kiT), writes=["kit"])
        s.dma(lambda e: e.dma_start(out=wit[:], in_=wi), writes=["wit"])
        s.dma(lambda e: e.dma_start(out=qrt[:], in_=qrel), writes=["qrt"])
        s.dma(lambda e: e.dma_start(out=kpt[:], in_=kpos), writes=["kpt"])
        s.dma(lambda e: e.dma_start(out=idt[:], in_=ident), writes=["idt"])
        s.pool(lambda e: e.memset(ones1[:], 1.0), writes=["ones1"])
        s.dve(lambda e: e.memset(vt[:], 1.0), writes=[("vt", 0), ("vt", 1)])
        cbias = rr[:].rearrange("p h n -> p (h n)")
        RRALL = [("rr", h) for h in range(4)]

        kbc = 0
        ec = 0
        mtc = 0
        otc = 0
        for k in range(nslot):
            L = 2048 * (k + 1)
            nkc = L // 512
            nkb = L // 128
            qb = 0
            s.dma(lambda e, k=k, qb=qb: e.dma_start(out=qTt[:, qb, :, :], in_=qT[:, k, :, :]), writes=[("qTt", qb)])
            s.dma(lambda e, k=k, qb=qb: e.dma_start(out=qit[:, qb, :, :], in_=qiT[:, k, :, :]), writes=[("qit", qb)])
            for qt in range(4):
                g = 4 * k + qt
                for n in range(nkc):
                    for h in range(4):
                        s.pe(lambda e, h=h, qb=qb, qt=qt, n=n: e.matmul(pb[h][:], lhsT=qit[:, qb, h, qt * 128:(qt + 1) * 128],
                                                                     rhs=kit[:, n * 512:(n + 1) * 512], start=True, stop=True),
                             reads=[("qit", qb), "kit"], writes=[("pb", h)])
                        s.act(lambda e, h=h: e.activation(out=rr[:, h, :], in_=pb[h][:], func=AF.Relu), reads=[("pb", h)], writes=[("rr", h)])
                        if h == 0:
                            s.dve(lambda e, n=n, g=g: e.tensor_scalar(out=sc[:, n * 512:(n + 1) * 512], in0=rr[:, 0, :], scalar1=wit[:, g, 0:1],
                                                                    scalar2=None, op0=ALU.mult),
                                  reads=[("rr", 0), "wit"], writes=[("sc", n)])
                        else:
                            s.dve(lambda e, n=n, g=g, h=h: e.scalar_tensor_tensor(out=sc[:, n * 512:(n + 1) * 512], in0=rr[:, h, :],
                                                                                 scalar=wit[:, g, h:h + 1], in1=sc[:, n * 512:(n + 1) * 512],
                                                                                 op0=ALU.mult, op1=ALU.add),
                                  reads=[("rr", h), "wit", ("sc", n)], writes=[("sc", n)])
                allsc = [("sc", n) for n in range(nkc)]
                s.dve(lambda e, L=L: e.tensor_reduce(out=sm[:, 5:6], in_=sc[:, 0:L], axis=AX.X, op=ALU.max, apply_absolute_value=True),
                      reads=allsc, writes=["rmax"])
                s.dve(lambda e: e.tensor_scalar(out=sm[:, 0:1], in0=sm[:, 5:6], scalar1=-1.0, scalar2=None, op0=ALU.mult), reads=["rmax"], writes=["lo"])
                s.dve(lambda e: e.tensor_scalar(out=sm[:, 1:2], in0=sm[:, 5:6], scalar1=2.0, scalar2=None, op0=ALU.mult), reads=["rmax"], writes=["range"])
                s.dve(lambda e, g=g: e.tensor_scalar(out=cbias[:], in0=kpt[:], scalar1=qrt[:, g:g + 1], scalar2=NEG, op0=ALU.is_gt, op1=ALU.mult),
                      reads=["kpt", "qrt"], writes=RRALL)
                s.dve(lambda e, L=L: e.tensor_tensor(out=sc[:, L - 2048:L], in0=sc[:, L - 2048:L], in1=cbias[:], op=ALU.add),
                      reads=allsc + RRALL, writes=allsc)
                Lh = max(512, int(round(0.4 * L / 512.0)) * 512)
                nact = float(L - Lh)
                MKA, MKB = ("mk", qt, "a"), ("mk", qt, "b")
                for it in range(1, nit + 1):
                    f = 2.0 ** (-it)
                    s.dve(lambda e, f=f: e.tensor_scalar(out=sm[:, 2:3], in0=sm[:, 1:2], scalar1=f, scalar2=sm[:, 0:1], op0=ALU.mult, op1=ALU.add),
                          reads=["range", "lo"], writes=["mid"])
                    s.dve(lambda e, Lh=Lh, qt=qt: e.tensor_scalar(out=mk[:, qt, 0:Lh], in0=sc[:, 0:Lh], scalar1=sm[:, 2:3], scalar2=None,
                                                                op0=ALU.is_ge, op1=ALU.add, accum_out=sm[:, 3:4]),
                          reads=allsc + ["mid"], writes=[MKA, "cnt"])
                    s.act(lambda e, Lh=Lh, L=L, qt=qt: e.activation(out=mk[:, qt, Lh:L], in_=sc[:, Lh:L], func=AF.Sign, scale=-1.0, bias=sm[:, 2:3],
                                                                   accum_out=sm[:, 6:7]),
                          reads=allsc + ["mid"], writes=[MKB, "sgn"])
                    s.dve(lambda e: e.scalar_tensor_tensor(out=sm[:, 7:8], in0=sm[:, 3:4], scalar=2.0, in1=sm[:, 6:7], op0=ALU.mult, op1=ALU.subtract),
                          reads=["cnt", "sgn"], writes=["tt"])
                    s.dve(lambda e, f=f, nact=nact: e.tensor_scalar(out=sm[:, 4:5], in0=sm[:, 7:8], scalar1=511.0 - nact, scalar2=f, op0=ALU.is_ge, op1=ALU.mult),
                          reads=["tt"], writes=["pred"])
                    s.dve(lambda e: e.scalar_tensor_tensor(out=sm[:, 0:1], in0=sm[:, 4:5], scalar=sm[:, 1:2], in1=sm[:, 0:1], op0=ALU.mult, op1=ALU.add),
                          reads=["pred", "range", "lo"], writes=["lo"])
                s.dve(lambda e, L=L, qt=qt: e.tensor_scalar(out=mk[:, qt, 0:L], in0=sc[:, 0:L], scalar1=sm[:, 0:1], scalar2=None, op0=ALU.is_ge),
                      reads=allsc + ["lo"], writes=[MKA, MKB])
            steps = [(hp, kb, hl) for hp in range(2) for kb in range(nkb) for hl in range(4)]
            nst = len(steps)
            info = {}

            def load_sb(hp, sbk):
                nonlocal kbc
                kbuf = kbc % 2
                kbc += 1
                info[("kbuf", hp, sbk)] = kbuf
                s.dma(lambda e, sbk=sbk, kbuf=kbuf: e.dma_start(out=kTs[:, kbuf, :, :], in_=kT[:, :, sbk * 512:(sbk + 1) * 512]),
                      writes=[("kTs", kbuf)])
                s.dma(lambda e, sbk=sbk, kbuf=kbuf: e.dma_start(out=vraw[:, kbuf, :, :], in_=vv[sbk * 512:(sbk + 1) * 512, :].rearrange("(kb p) c -> p kb c", p=128)),
                      writes=[("vraw", kbuf)])
                for kl_ in range(4):
                    s.dve(lambda e, kbuf=kbuf, kl_=kl_: e.tensor_copy(out=vt[:, kbuf, kl_, :].rearrange("p (h c) -> p h c", c=65)[:, :, 0:64],
                                                                   in_=vraw[:, kbuf, kl_, :].rearrange("p (h d) -> p h d", d=64)),
                          reads=[("vraw", kbuf)], writes=[("vt", kbuf)])

            def pre(hp, kb):
                nonlocal mtc
                sbk, kl = divmod(kb, 4)
                mb = mtc % 2
                mtc += 1
                info[("mb", hp, kb)] = mb
                for qt in range(4):
                    s.pe(lambda e, qt=qt, kb=kb: e.transpose(pTb[:, qt * 128:(qt + 1) * 128], mk[:, qt, kb * 128:(kb + 1) * 128], idt[:]),
                         reads=[("mk", qt, "a"), ("mk", qt, "b"), "idt"], writes=[("pb", 2)])
                s.act(lambda e, mb=mb: e.activation(out=mT[:, mb, :], in_=pTb[:, 0:512], func=AF.Copy), reads=[("pb", 2)], writes=[("mT", mb)])

            def ST(i):
                hp, kb, hl = steps[i]
                sbk, kl = divmod(kb, 4)
                kbuf = info[("kbuf", hp, sbk)]
                h = hp * 4 + hl
                pr, hh = divmod(h, 2)
                sb = i % 2
                s.pe(lambda e, sb=sb, kbuf=kbuf, pr=pr, hh=hh, kl=kl: e.matmul(pb[sb][:], lhsT=kTs[hh * 64:(hh + 1) * 64, kbuf, pr, kl * 128:(kl + 1) * 128],
                                                                            rhs=qTt[hh * 64:(hh + 1) * 64, 0, pr, :], start=True, stop=True),
                     reads=[("kTs", kbuf), ("qTt", 0)], writes=[("pb", sb)])

            def rest_a(i):
                sb = i % 2
                eb = i % 3
                s.act(lambda e, sb=sb, eb=eb: e.activation(out=E[:, eb, :], in_=pb[sb][:], func=AF.Exp, scale=0.125),
                      reads=[("pb", sb)], writes=[("E", eb)])

            def rest(i):
                nonlocal otc
                hp, kb, hl = steps[i]
                sbk, kl = divmod(kb, 4)
                kbuf = info[("kbuf", hp, sbk)]
                mb = info[("mb", hp, kb)]
                h = hp * 4 + hl
                sb = i % 2
                eb = i % 3
                eng = s.dve if (i % 2 == 0) else s.pool
                eng(lambda e, eb=eb, mb=mb: e.tensor_tensor(out=Pm[:, eb, :], in0=E[:, eb, :], in1=mT[:, mb, :], op=ALU.mult),
                    reads=[("E", eb), ("mT", mb)], writes=[("Pm", eb)])
                s.pe(lambda e, hl=hl, kbuf=kbuf, h=h, eb=eb, kb=kb, kl=kl: e.matmul(pb[4 + hl][0:65, :], lhsT=vt[:, kbuf, kl, h * 65:(h + 1) * 65], rhs=Pm[:, eb, :],
                                                                                 start=(kb == 0), stop=(kb == nkb - 1)),
                     reads=[("vt", kbuf), ("Pm", eb)], writes=[("pb", 4 + hl)])
                if kb == nkb - 1:
                    ob = otc % 2
                    otc += 1
                    s.dve(lambda e, hl=hl: e.reciprocal(out=rec[64:65, :], in_=pb[4 + hl][64:65, :]), reads=[("pb", 4 + hl)], writes=["rec"])
                    s.pe(lambda e: e.matmul(pb[3][0:64, :], lhsT=ones1[64:65, :], rhs=rec[64:65, :], start=True, stop=True),
                         reads=["ones1", "rec"], writes=[("pb", 3)])
                    s.act(lambda e: e.activation(out=bcs[:], in_=pb[3][0:64, :], func=AF.Copy), reads=[("pb", 3)], writes=["bcs"])
                    s.dve(lambda e, hl=hl, ob=ob: e.tensor_tensor(out=oT[:, ob, :], in0=pb[4 + hl][0:64, :], in1=bcs[:], op=ALU.mult),
                          reads=[("pb", 4 + hl), "bcs"], writes=[("oT", ob)])
                    s.dma(lambda e, h=h, k=k, ob=ob: e.dma_start(out=oaT[:, h, k * 512:(k + 1) * 512], in_=oT[:, ob, :]), reads=[("oT", ob)])

            DPIPE = 2
            order = [(hp_, sb_) for hp_ in range(2) for sb_ in range(nkb // 4)]
            load_sb(*order[0])
            for j in range(min(DPIPE, nst)):
                if steps[j][2] == 0:
                    pre(steps[j][0], steps[j][1])
                ST(j)
            for i in range(0, nst, 2):
                if steps[i][2] == 0 and steps[i][1] % 4 == 0:
                    oi = order.index((steps[i][0], steps[i][1] // 4))
                    if oi + 1 < len(order):
                        load_sb(*order[oi + 1])
                rest_a(i)
                rest_a(i + 1)
                for j in (i + DPIPE, i + DPIPE + 1):
                    if j < nst:
                        if steps[j][2] == 0:
                            pre(steps[j][0], steps[j][1])
                        ST(j)
                rest(i)
                rest(i + 1)
        s.emit(stack)
        print("B: ops", len(s.ops), "waits", s.n_waits)
    return nc


def host_inputs_B(pbf_list, pf32_list):
    bf = ml_dtypes.bfloat16
    maps = []
    full = []
    for b in range(2):
        ka = np.zeros((8192, 512), bf)
        va = np.zeros((8192, 512), bf)
        ki = np.zeros((8192, 64), bf)
        for j in range(4):
            c = b * 4 + j
            for k in range(4):
                g = 4 * k + j
                blk = pbf_list[c][512 * k:512 * (k + 1)]
                ka[512 * g:512 * (g + 1)] = blk[:, 512:1024]
                va[512 * g:512 * (g + 1)] = blk[:, 1024:1536]
                ki[512 * g:512 * (g + 1)] = blk[:, 1792:1856]
        kTl = np.ascontiguousarray(ka.reshape(8192, 4, 2, 64).transpose(2, 3, 1, 0).reshape(128, 4, 8192))
        full.append((kTl, np.ascontiguousarray(va), np.ascontiguousarray(ki.T)))
    kpos = np.ascontiguousarray(np.broadcast_to(np.arange(2048, dtype=np.float32)[None, :], (128, 2048)))
    ident = np.eye(128).astype(bf)
    for c in range(8):
        b, j = divmod(c, 4)
        p = pbf_list[c]
        qa = p[:, 0:512].reshape(4, 512, 4, 2, 64)
        qTl = np.ascontiguousarray(qa.transpose(3, 4, 0, 2, 1).reshape(128, 4, 4, 512))
        qi = p[:, 1536:1792].reshape(4, 512, 4, 64)
        qiTl = np.ascontiguousarray(qi.transpose(3, 0, 2, 1))
        wi = np.ascontiguousarray(pf32_list[c][:, 0:4].reshape(16, 128, 4).transpose(1, 0, 2))
        qrel = np.zeros((128, 16), np.float32)
        for k in range(4):
            g = 4 * k + j
            for qt in range(4):
                qrel[:, 4 * k + qt] = 512 * g + 128 * qt + np.arange(128) - 2048 * k
        maps.append({"kT": full[b][0], "v": full[b][1], "kiT": full[b][2], "qT": qTl, "qiT": qiTl, "wi": wi, "qrel": qrel,
                     "kpos": kpos, "ident": ident})
    return maps


SEG = 2048
NSEG = 4
CPS = SEG // 64


def build_G():
    nc = bass.Bass("TRN2", target_bir_lowering=False)
    D = lambda name, shape, dt, kind="ExternalInput": nc.dram_tensor(name, shape, dt, kind=kind).ap()
    qT = D("qT", [32, 8192], F32)
    kT = D("kT", [32, 8192], F32)
    vtok = D("vtok", [64, 128, 64], F32)
    glT = D("glT", [16, 8192], F32)
    wg = D("wg", [16, 32], F32)
    bg = D("bg", [32, 1], F32)
    rT = D("rT", [64, 8192], F32)
    gng = D("gng", [64, 1], F32)
    resetm = D("resetm", [32, SEG], F32)
    tri = D("tri", [64, 64], F32)
    ident = D("ident", [32, 32], F32)
    obT = D("obT", [64, 8192], BF16, "ExternalOutput")

    with ExitStack() as stack:
        T = lambda name, shape, dt: stack.enter_context(nc.sbuf_tensor(name, shape, dt))
        P = lambda name, shape, dt: stack.enter_context(nc.psum_tensor(name, shape, dt))
        qs = T("qs", [32, SEG], F32)
        ks = T("ks", [32, SEG], F32)
        vs = T("vs", [64, CPS, 64], F32)
        gls = T("gls", [16, SEG], F32)
        rs_ = T("rs", [64, SEG], F32)
        wgt = T("wgt", [16, 32], F32)
        bgt = T("bgt", [32, 1], F32)
        nbg = T("nbg", [32, 1], F32)
        gnt = T("gnt", [64, 1], F32)
        rmt = T("rmt", [32, SEG], F32)
        trit = T("trit", [64, 64], F32)
        idt = T("idt", [32, 32], F32)
        ones = T("ones", [64, 64], F32)
        epst = T("epst", [64, 1], F32)
        t1 = T("t1", [32, SEG], F32)
        cum = T("cum", [32, SEG], F32)
        eb = T("eb", [32, SEG], F32)
        enb = T("enb", [32, SEG], F32)
        qt_ = T("qt", [32, SEG], F32)
        kt_ = T("kt", [32, SEG], F32)
        ktok = T("ktok", [64, 2, 32], F32)
        am = T("am", [64, 2, 64], F32)
        U = T("U", [32, 2, 64], F32)
        S = T("S", [32, 2, 64], F32)
        oTs = T("oTs", [64, SEG], F32)
        sqs = T("sqs", [64, SEG], F32)
        rsd = T("rsd", [64, 512], F32)
        sil = T("sil", [64, SEG], F32)
        outb = T("outb", [64, SEG], BF16)
        pz = [P("pz%d" % i, [64, 512], F32) for i in range(2)]
        pk = [P("pk%d" % i, [64, 512], F32) for i in range(2)]
        pt_ = [P("pt%d" % i, [64, 512], F32) for i in range(2)]
        po = P("po", [64, 512], F32)
        pu = P("pu", [64, 512], F32)

        s = Sched(nc)
        for (dst, src, nm) in ((wgt, wg, "wgt"), (bgt, bg, "bgt"), (gnt, gng, "gnt"), (rmt, resetm, "rmt"), (trit, tri, "trit"), (idt, ident, "idt")):
            s.dma(lambda e, dst=dst, src=src: e.dma_start(out=dst[:], in_=src), writes=[nm])
        s.dve(lambda e: e.memset(ones[:], 1.0), writes=["ones"])
        s.dve(lambda e: e.memset(epst[:], 1e-6), writes=["epst"])
        s.dve(lambda e: e.memset(S[:], 0.0), writes=[("S", 0), ("S", 1)])
        s.dve(lambda e: e.tensor_scalar(out=nbg[:], in0=bgt[:], scalar1=-1.0, scalar2=None, op0=ALU.mult), reads=["bgt"], writes=["nbg"])
        cc = 0
        for sgi in range(NSEG):
            c0 = sgi * SEG
            s.dma(lambda e, c0=c0: e.dma_start(out=qs[:], in_=qT[:, c0:c0 + SEG]), writes=["qs"])
            s.dma(lambda e, c0=c0: e.dma_start(out=ks[:], in_=kT[:, c0:c0 + SEG]), writes=["ks"])
            s.dma(lambda e, sgi=sgi: e.dma_start(out=vs[:], in_=vtok[:, sgi * CPS:(sgi + 1) * CPS, :]), writes=["vs"])
            s.dma(lambda e, c0=c0: e.dma_start(out=gls[:], in_=glT[:, c0:c0 + SEG]), writes=["gls"])
            s.dma(lambda e, c0=c0: e.dma_start(out=rs_[:], in_=rT[:, c0:c0 + SEG]), writes=["rs"])
            for pc in range(SEG // 512):
                pzb = pz[pc % 2]
                s.pe(lambda e, pzb=pzb, pc=pc: e.matmul(pzb[0:32, :], lhsT=wgt[:], rhs=gls[:, pc * 512:(pc + 1) * 512], start=True, stop=True),
                     reads=["wgt", "gls"], writes=[("pz", pc % 2)])
                s.act(lambda e, pzb=pzb, pc=pc: e.activation(out=t1[:, pc * 512:(pc + 1) * 512], in_=pzb[0:32, :], func=AF.Exp, scale=-1.0, bias=nbg[:, 0:1]),
                      reads=[("pz", pc % 2), "nbg"], writes=["t1"])
            s.act(lambda e: e.activation(out=t1[:], in_=t1[:], func=AF.Ln, bias=1.0), reads=["t1"], writes=["t1"])
            s.dve(lambda e: e.tensor_tensor_scan(out=cum[:], data0=rmt[:], data1=t1[:], initial=0.0, op0=ALU.mult, op1=ALU.add),
                  reads=["rmt", "t1"], writes=["cum"])
            s.act(lambda e: e.activation(out=eb[:], in_=cum[:], func=AF.Exp, scale=-1.0 / 16), reads=["cum"], writes=["eb"])
            s.act(lambda e: e.activation(out=enb[:], in_=cum[:], func=AF.Exp, scale=1.0 / 16), reads=["cum"], writes=["enb"])
            s.dve(lambda e: e.scalar_tensor_tensor(out=qt_[:], in0=qs[:], scalar=32.0 ** -0.5, in1=eb[:], op0=ALU.mult, op1=ALU.mult),
                  reads=["qs", "eb"], writes=["qt"])
            s.dve(lambda e: e.tensor_tensor(out=kt_[:], in0=ks[:], in1=enb[:], op=ALU.mult), reads=["ks", "enb"], writes=["kt"])
            s.act(lambda e: e.activation(out=sil[:], in_=rs_[:], func=AF.Silu), reads=["rs"], writes=["sil"])
            def first_half(c, p):
                cs = slice(c * 64, (c + 1) * 64)
                s.pe(lambda e, p=p, cs=cs: e.transpose(pk[p][:, 0:32], kt_[:, cs], idt[:]), reads=["kt", "idt"], writes=[("pk", p)])
                s.act(lambda e, p=p: e.activation(out=ktok[:, p, :], in_=pk[p][:, 0:32], func=AF.Copy), reads=[("pk", p)], writes=[("ktok", p)])
                s.pe(lambda e, p=p, cs=cs: e.matmul(pt_[p][:, 0:64], lhsT=kt_[:, cs], rhs=qt_[:, cs], start=True, stop=True),
                     reads=["kt", "qt"], writes=[("pt", p)])
                s.dve(lambda e, p=p: e.tensor_tensor(out=am[:, p, :], in0=pt_[p][:, 0:64], in1=trit[:], op=ALU.mult),
                      reads=[("pt", p), "trit"], writes=[("am", p)])

            def second_half(c, p):
                cs = slice(c * 64, (c + 1) * 64)
                ac = eb[:, c * 64 + 63:c * 64 + 64]
                sp, sn = p, 1 - p
                s.pe(lambda e, p=p, c=c: e.matmul(po[:, 0:64], lhsT=vs[:, c, :], rhs=am[:, p, :], start=True, stop=False),
                     reads=["vs", ("am", p)], writes=["po"])
                s.pe(lambda e, sp=sp, cs=cs: e.matmul(po[:, 0:64], lhsT=S[:, sp, :], rhs=qt_[:, cs], start=False, stop=True),
                     reads=[("S", sp), "qt"], writes=["po"])
                s.act(lambda e, cs=cs: e.activation(out=oTs[:, cs], in_=po[:, 0:64], func=AF.Copy), reads=["po"], writes=["oTs"])
                s.pe(lambda e, p=p, c=c: e.matmul(pu[0:32, 0:64], lhsT=ktok[:, p, :], rhs=vs[:, c, :], start=True, stop=True),
                     reads=[("ktok", p), "vs"], writes=["pu"])
                s.act(lambda e, p=p, ac=ac: e.activation(out=U[:, p, :], in_=pu[0:32, 0:64], func=AF.Copy, scale=ac), reads=["pu", "eb"], writes=[("U", p)])
                s.dve(lambda e, p=p, sp=sp, sn=sn, ac=ac: e.scalar_tensor_tensor(out=S[:, sn, :], in0=S[:, sp, :], scalar=ac, in1=U[:, p, :],
                                                                               op0=ALU.mult, op1=ALU.add),
                      reads=[("S", sp), ("U", p), "eb"], writes=[("S", sn)])

            first_half(0, cc % 2)
            for c in range(CPS):
                p = cc % 2
                cc += 1
                if c + 1 < CPS:
                    first_half(c + 1, cc % 2)
                second_half(c, p)
            s.act(lambda e: e.activation(out=sqs[:], in_=oTs[:], func=AF.Square), reads=["oTs"], writes=["sqs"])
            for pc in range(SEG // 512):
                pzb = pz[pc % 2]
                ps_ = slice(pc * 512, (pc + 1) * 512)
                s.pe(lambda e, pzb=pzb, ps_=ps_: e.matmul(pzb[:, :], lhsT=ones[:], rhs=sqs[:, ps_], start=True, stop=True),
                     reads=["ones", "sqs"], writes=[("pz", pc % 2)])
                s.act(lambda e, pzb=pzb: e.activation(out=rsd[:], in_=pzb[:, :], func=AF.Sqrt, scale=1.0 / 64, bias=epst[:, 0:1]),
                      reads=[("pz", pc % 2), "epst"], writes=["rsd"])
                s.dve(lambda e: e.reciprocal(out=rsd[:], in_=rsd[:]), reads=["rsd"], writes=["rsd"])
                s.dve(lambda e, ps_=ps_: e.tensor_tensor(out=oTs[:, ps_], in0=oTs[:, ps_], in1=rsd[:], op=ALU.mult), reads=["oTs", "rsd"], writes=["oTs"])
            s.dve(lambda e: e.scalar_tensor_tensor(out=outb[:], in0=oTs[:], scalar=gnt[:, 0:1], in1=sil[:], op0=ALU.mult, op1=ALU.mult),
                  reads=["oTs", "gnt", "sil"], writes=["outb"])
            s.dma(lambda e, c0=c0: e.dma_start(out=obT[:, c0:c0 + SEG], in_=outb[:]), reads=["outb"])
        s.emit(stack)
        print("G: ops", len(s.ops), "waits", s.n_waits)
    return nc


def host_inputs_G(pf32_full, w_gate_up, b_gate, gla_norm_g):
    maps = []
    rm = np.ones((32, SEG), np.float32)
    rm[:, ::64] = 0.0
    tri = np.triu(np.ones((64, 64), np.float32))
    for c in range(8):
        b, h = divmod(c, 4)
        p = pf32_full[b]
        maps.append({
            "qT": np.ascontiguousarray(p[:, 4 + 32 * h:4 + 32 * (h + 1)].T),
            "kT": np.ascontiguousarray(p[:, 132 + 32 * h:132 + 32 * (h + 1)].T),
            "vtok": np.ascontiguousarray(p[:, 260 + 64 * h:260 + 64 * (h + 1)].reshape(128, 64, 64).transpose(1, 0, 2)),
            "glT": np.ascontiguousarray(p[:, 772:788].T),
            "wg": np.ascontiguousarray(w_gate_up[:, 32 * h:32 * (h + 1)]),
            "bg": np.ascontiguousarray(b_gate[32 * h:32 * (h + 1)].reshape(32, 1)),
            "rT": np.ascontiguousarray(p[:, 516 + 64 * h:516 + 64 * (h + 1)].T),
            "gng": np.ascontiguousarray(gla_norm_g[h].reshape(64, 1)),
            "resetm": rm, "tri": tri, "ident": np.eye(32, dtype=np.float32),
        })
    return maps


DFF = 2816
NFC = 22
UW = 256
SLOTW = 2 + 512
NCOL = 4 * SLOTW


def build_F(final=False):
    nc = bass.Bass("TRN2", target_bir_lowering=False)
    D = lambda name, shape, dt, kind="ExternalInput": nc.dram_tensor(name, shape, dt, kind=kind).ap()
    mixT = D("mixT", [1024, NCOL], BF16)
    xT = D("xT", [1024, NCOL], F32)
    w_out = D("w_out", [1024, 1024], F32)
    w_up = D("w_up", [1024, 2 * DFF], F32)
    w_down = D("w_down", [DFF, 1024], F32)
    g2 = D("g2", [128, 8], F32)
    cw = D("cw", [128, NFC, 4], F32)
    hflag = D("hflag", [128, 4], F32)
    gf = D("gf", [128, 8], F32)
    xoT = D("xoT", [1024, 2048], F32, "ExternalOutput")

    with ExitStack() as stack:
        T = lambda name, shape, dt: stack.enter_context(nc.sbuf_tensor(name, shape, dt))
        P = lambda name, shape, dt: stack.enter_context(nc.psum_tensor(name, shape, dt))
        wo = T("wo", [128, 8, 1024], BF16)
        wu = T("wu", [128, 8, 2 * DFF], BF16)
        wd = T("wd", [128, NFC, 1024], BF16)
        wst = T("wst", [128, 2, 1024], F32)
        g2t = T("g2t", [128, 8], F32)
        gft = T("gft", [128, 8], F32)
        cwt = T("cwt", [128, NFC, 4], F32)
        hft = T("hft", [128, 4], F32)
        ones = T("ones", [128, 128], F32)
        epst = T("epst", [128, 1], F32)
        mx = T("mx", [128, 8, UW], BF16)
        xm = T("xm", [128, 8, UW], F32)
        sq = T("sq", [128, 2, UW], F32)
        rs = T("rs", [128, UW], F32)
        h2 = T("h2", [128, 8, UW], BF16)
        actT = T("actT", [128, NFC, UW], BF16)
        aext = T("aext", [128, 2, 2 + UW], F32)
        cv = T("cv", [128, 2, UW], F32)
        sg = T("sg", [128, 2, UW], F32)
        atail = T("atail", [128, NFC, 2], F32)
        pa = [P("pa%d" % i, [128, 512], F32) for i in range(8)]

        s = Sched(nc)
        for (dst, src, nm) in ((g2t, g2, "g2t"), (gft, gf, "gft"), (cwt, cw, "cwt"), (hft, hflag, "hft")):
            s.dma(lambda e, dst=dst, src=src: e.dma_start(out=dst[:], in_=src), writes=[nm])
        s.dve(lambda e: e.memset(ones[:], 1.0), writes=["ones"])
        s.dve(lambda e: e.memset(epst[:], 1e-6), writes=["epst"])
        wc = 0
        def load_w(dst_fn, src_fn, nrow_chunks, ncols, regname, scale_g):
            nonlocal wc
            for c in range(nrow_chunks):
                for c0 in range(0, ncols, 1024):
                    c1 = min(ncols, c0 + 1024)
                    b = wc % 2
                    wc += 1
                    s.dma(lambda e, b=b, c=c, c0=c0, c1=c1: e.dma_start(out=wst[:, b, 0:c1 - c0], in_=src_fn(c, c0, c1)), writes=[("wst", b)])
                    use_dve = (wc % 2 == 0)
                    if scale_g:
                        if use_dve:
                            s.dve(lambda e, b=b, c=c, c0=c0, c1=c1: e.tensor_scalar(out=dst_fn(c, c0, c1), in0=wst[:, b, 0:c1 - c0], scalar1=g2t[:, c:c + 1],
                                                                                  scalar2=None, op0=ALU.mult),
                                  reads=[("wst", b), "g2t"], writes=[(regname, c)])
                        else:
                            s.act(lambda e, b=b, c=c, c0=c0, c1=c1: e.activation(out=dst_fn(c, c0, c1), in_=wst[:, b, 0:c1 - c0], func=AF.Copy,
                                                                               scale=g2t[:, c:c + 1]),
                                  reads=[("wst", b), "g2t"], writes=[(regname, c)])
                    else:
                        if use_dve:
                            s.dve(lambda e, b=b, c=c, c0=c0, c1=c1: e.tensor_copy(out=dst_fn(c, c0, c1), in_=wst[:, b, 0:c1 - c0]),
                                  reads=[("wst", b)], writes=[(regname, c)])
                        else:
                            s.act(lambda e, b=b, c=c, c0=c0, c1=c1: e.activation(out=dst_fn(c, c0, c1), in_=wst[:, b, 0:c1 - c0], func=AF.Copy),
                                  reads=[("wst", b)], writes=[(regname, c)])
        load_w(lambda c, c0, c1: wo[:, c, c0:c1], lambda c, c0, c1: w_out[c * 128:(c + 1) * 128, c0:c1], 8, 1024, "wo", False)
        load_w(lambda c, c0, c1: wu[:, c, c0:c1], lambda c, c0, c1: w_up[c * 128:(c + 1) * 128, c0:c1], 8, 2 * DFF, "wu", True)
        load_w(lambda c, c0, c1: wd[:, c, c0:c1], lambda c, c0, c1: w_down[c * 128:(c + 1) * 128, c0:c1], NFC, 1024, "wd", False)
        WO = [("wo", c) for c in range(8)]
        WU = [("wu", c) for c in range(8)]
        WD = [("wd", c) for c in range(NFC)]

        def unit(k, col0, n, halo, out0):
            s.dma(lambda e: e.dma_start(out=mx[:, :, 0:n], in_=mixT[:, col0:col0 + n].rearrange("(c p) t -> p c t", p=128)), writes=["mx"])
            s.dma(lambda e: e.dma_start(out=xm[:, :, 0:n], in_=xT[:, col0:col0 + n].rearrange("(c p) t -> p c t", p=128)), writes=["xm"])
            for dc in range(8):
                pt = pa[dc % 2]
                for c in range(8):
                    s.pe(lambda e, pt=pt, c=c, dc=dc: e.matmul(pt[:, 0:n], lhsT=wo[:, c, dc * 128:(dc + 1) * 128], rhs=mx[:, c, 0:n],
                                                              start=(c == 0), stop=(c == 7)),
                         reads=WO + ["mx"], writes=[("pa", dc % 2)])
                s.dve(lambda e, pt=pt, dc=dc: e.tensor_tensor(out=xm[:, dc, 0:n], in0=pt[:, 0:n], in1=xm[:, dc, 0:n], op=ALU.add),
                      reads=[("pa", dc % 2), "xm"], writes=["xm"])
                s.act(lambda e, dc=dc: e.activation(out=sq[:, dc % 2, 0:n], in_=xm[:, dc, 0:n], func=AF.Square), reads=["xm"], writes=[("sq", dc % 2)])
                s.pe(lambda e, dc=dc: e.matmul(pa[2][:, 0:n], lhsT=ones[:], rhs=sq[:, dc % 2, 0:n], start=(dc == 0), stop=(dc == 7)),
                     reads=["ones", ("sq", dc % 2)], writes=[("pa", 2)])
            s.act(lambda e: e.activation(out=rs[:, 0:n], in_=pa[2][:, 0:n], func=AF.Sqrt, scale=1.0 / 1024, bias=epst[:, 0:1]),
                  reads=[("pa", 2), "epst"], writes=["rs"])
            s.dve(lambda e: e.reciprocal(out=rs[:, 0:n], in_=rs[:, 0:n]), reads=["rs"], writes=["rs"])
            for dc in range(8):
                s.dve(lambda e, dc=dc: e.tensor_tensor(out=h2[:, dc, 0:n], in0=xm[:, dc, 0:n], in1=rs[:, 0:n], op=ALU.mult),
                    reads=["xm", "rs"], writes=["h2"])
            for fc in range(NFC):
                ab = fc % 2
                pA = pa[3 + ab]
                pB = pa[5 + ab]
                for c in range(8):
                    s.pe(lambda e, pA=pA, c=c, fc=fc: e.matmul(pA[:, 0:n], lhsT=wu[:, c, fc * 128:(fc + 1) * 128], rhs=h2[:, c, 0:n],
                                                              start=(c == 0), stop=(c == 7)),
                         reads=WU + ["h2"], writes=[("pa", 3 + ab)])
                if halo:
                    s.dve(lambda e, pA=pA, fc=fc, k=k: e.tensor_scalar(out=atail[:, fc, :], in0=pA[:, 0:2], scalar1=hft[:, k:k + 1], scalar2=None,
                                                                     op0=ALU.mult),
                          reads=[("pa", 3 + ab), "hft"], writes=[("atail", fc)])
                    continue
                for c in range(8):
                    s.pe(lambda e, pB=pB, c=c, fc=fc: e.matmul(pB[:, 0:n], lhsT=wu[:, c, DFF + fc * 128:DFF + (fc + 1) * 128], rhs=h2[:, c, 0:n],
                                                              start=(c == 0), stop=(c == 7)),
                         reads=WU + ["h2"], writes=[("pa", 5 + ab)])
                s.act(lambda e, ab=ab, fc=fc: e.activation(out=aext[:, ab, 0:2], in_=atail[:, fc, :], func=AF.Copy),
                      reads=[("atail", fc)], writes=[("aext", ab)])
                s.act(lambda e, ab=ab, pA=pA: e.activation(out=aext[:, ab, 2:2 + n], in_=pA[:, 0:n], func=AF.Copy),
                      reads=[("pa", 3 + ab)], writes=[("aext", ab)])
                s.act(lambda e, ab=ab, fc=fc: e.activation(out=atail[:, fc, :], in_=aext[:, ab, n:n + 2], func=AF.Copy),
                      reads=[("aext", ab)], writes=[("atail", fc)])
                s.dve(lambda e, ab=ab, fc=fc: e.tensor_scalar(out=cv[:, ab, 0:n], in0=aext[:, ab, 2:2 + n], scalar1=cwt[:, fc, 2:3], scalar2=cwt[:, fc, 3:4],
                                                            op0=ALU.mult, op1=ALU.add),
                      reads=[("aext", ab), "cwt"], writes=[("cv", ab)])
                s.dve(lambda e, ab=ab, fc=fc: e.scalar_tensor_tensor(out=cv[:, ab, 0:n], in0=aext[:, ab, 1:1 + n], scalar=cwt[:, fc, 1:2], in1=cv[:, ab, 0:n],
                                                                   op0=ALU.mult, op1=ALU.add),
                      reads=[("aext", ab), "cwt", ("cv", ab)], writes=[("cv", ab)])
                s.dve(lambda e, ab=ab, fc=fc: e.scalar_tensor_tensor(out=cv[:, ab, 0:n], in0=aext[:, ab, 0:n], scalar=cwt[:, fc, 0:1], in1=cv[:, ab, 0:n],
                                                                   op0=ALU.mult, op1=ALU.add),
                      reads=[("aext", ab), "cwt", ("cv", ab)], writes=[("cv", ab)])
                s.act(lambda e, ab=ab: e.activation(out=sg[:, ab, 0:n], in_=cv[:, ab, 0:n], func=AF.Silu), reads=[("cv", ab)], writes=[("sg", ab)])
                s.dve(lambda e, ab=ab, fc=fc, pB=pB: e.tensor_tensor(out=actT[:, fc, 0:n], in0=pB[:, 0:n], in1=sg[:, ab, 0:n], op=ALU.mult),
                      reads=[("pa", 5 + ab), ("sg", ab)], writes=[("actT", fc)])
            if halo:
                return
            AT = [("actT", fc) for fc in range(NFC)]
            for dc in range(8):
                pt = pa[dc % 2]
                for fc in range(NFC):
                    s.pe(lambda e, pt=pt, fc=fc, dc=dc: e.matmul(pt[:, 0:n], lhsT=wd[:, fc, dc * 128:(dc + 1) * 128], rhs=actT[:, fc, 0:n],
                                                                start=(fc == 0), stop=(fc == NFC - 1)),
                         reads=WD + AT, writes=[("pa", dc % 2)])
                s.dve(lambda e, pt=pt, dc=dc: e.tensor_tensor(out=xm[:, dc, 0:n], in0=pt[:, 0:n], in1=xm[:, dc, 0:n], op=ALU.add),
                      reads=[("pa", dc % 2), "xm"], writes=["xm"])
            if final:
                for dc in range(8):
                    s.act(lambda e, dc=dc: e.activation(out=sq[:, dc % 2, 0:n], in_=xm[:, dc, 0:n], func=AF.Square), reads=["xm"], writes=[("sq", dc % 2)])
                    s.pe(lambda e, dc=dc: e.matmul(pa[2][:, 0:n], lhsT=ones[:], rhs=sq[:, dc % 2, 0:n], start=(dc == 0), stop=(dc == 7)),
                         reads=["ones", ("sq", dc % 2)], writes=[("pa", 2)])
                s.act(lambda e: e.activation(out=rs[:, 0:n], in_=pa[2][:, 0:n], func=AF.Sqrt, scale=1.0 / 1024, bias=epst[:, 0:1]),
                      reads=[("pa", 2), "epst"], writes=["rs"])
                s.dve(lambda e: e.reciprocal(out=rs[:, 0:n], in_=rs[:, 0:n]), reads=["rs"], writes=["rs"])
                for dc in range(8):
                    s.dve(lambda e, dc=dc: e.scalar_tensor_tensor(out=xm[:, dc, 0:n], in0=xm[:, dc, 0:n], scalar=gft[:, dc:dc + 1], in1=rs[:, 0:n],
                                                                op0=ALU.mult, op1=ALU.mult),
                          reads=["xm", "rs", "gft"], writes=["xm"])
            s.dma(lambda e: e.dma_start(out=xoT[:, out0:out0 + n].rearrange("(c p) t -> p c t", p=128), in_=xm[:, :, 0:n]), reads=["xm"])

        for k in range(4):
            unit(k, k * SLOTW, 2, True, None)
            for u in range(512 // UW):
                unit(k, k * SLOTW + 2 + u * UW, UW, False, k * 512 + u * UW)
        s.emit(stack)
        print("F: ops", len(s.ops), "waits", s.n_waits)
    return nc


def host_inputs_F(mix_list, x_full, w_out, norm2_g, w_up, conv_w, conv_b, w_down, final_g):
    bf = ml_dtypes.bfloat16
    maps = []
    cw = np.zeros((128, NFC, 4), np.float32)
    cw[:, :, 0:3] = conv_w.T.reshape(NFC, 128, 3).transpose(1, 0, 2)
    cw[:, :, 3] = conv_b.reshape(NFC, 128).T
    for c in range(8):
        b, j = divmod(c, 4)
        mcols, xcols = [], []
        hf = np.ones((128, 4), np.float32)
        for k in range(4):
            g = 4 * k + j
            t0 = 512 * g
            if g == 0:
                mcols.append(np.zeros((2, 1024), bf))
                xcols.append(np.zeros((2, 1024), np.float32))
                hf[:, k] = 0.0
            else:
                mcols.append(mix_list[b][t0 - 2:t0])
                xcols.append(x_full[b, t0 - 2:t0])
            mcols.append(mix_list[b][t0:t0 + 512])
            xcols.append(x_full[b, t0:t0 + 512])
        maps.append({
            "mixT": np.ascontiguousarray(np.concatenate(mcols, 0).T), "xT": np.ascontiguousarray(np.concatenate(xcols, 0).T),
            "w_out": np.ascontiguousarray(w_out), "w_up": np.ascontiguousarray(w_up), "w_down": np.ascontiguousarray(w_down),
            "g2": np.ascontiguousarray(norm2_g.reshape(8, 128).T), "cw": cw, "hflag": hf,
            "gf": np.ascontiguousarray(final_g.reshape(8, 128).T),
        })
    return maps


_CACHE = {}
CHECK = None


def _get(name, fn):
    if name not in _CACHE:
        _CACHE[name] = fn()
    return _CACHE[name]


def _run(nc, maps):
    res = run_bass_kernel_spmd(nc, maps, core_ids=list(range(8)))
    return res.results


def forward(x, positions, norm1_g, w_in, w_gate_up, b_gate, gla_norm_g, w_pool, pool_scale, w_out, norm2_g, w_up, conv_w, conv_b,
            w_down, final_norm_g):
    bf = ml_dtypes.bfloat16
    x = np.asarray(x, np.float32)
    positions = np.asarray(positions, np.int32)
    depth = norm1_g.shape[0]
    ncA = _get("A", build_A)
    ncB = _get("B", build_B)
    ncG = _get("G", build_G)
    for l in range(depth):
        last = (l == depth - 1)
        rA = _run(ncA, host_inputs_A(x, positions, np.asarray(norm1_g[l]), np.asarray(w_in[l]), np.asarray(w_pool[l]), np.asarray(pool_scale[l])))
        pbf = [np.asarray(r["pbf"]) for r in rA]
        pf32 = [np.asarray(r["pf32"]) for r in rA]
        ocT = [np.asarray(r["ocT"]) for r in rA]
        if CHECK:
            CHECK("A", l, dict(pbf=pbf, pf32=pf32, ocT=ocT))
        rB = _run(ncB, host_inputs_B(pbf, pf32))
        oaT = [np.asarray(r["oaT"]) for r in rB]
        if CHECK:
            CHECK("B", l, dict(oaT=oaT))
        pf_full = []
        for b in range(2):
            full = np.zeros((8192, pf32[0].shape[1]), np.float32)
            for j in range(4):
                for k in range(4):
                    g = 4 * k + j
                    full[512 * g:512 * (g + 1)] = pf32[b * 4 + j][512 * k:512 * (k + 1)]
            pf_full.append(full)
        rG = _run(ncG, host_inputs_G(pf_full, np.asarray(w_gate_up[l]), np.asarray(b_gate[l]), np.asarray(gla_norm_g[l])))
        obT = [np.asarray(r["obT"]) for r in rG]
        if CHECK:
            CHECK("G", l, dict(obT=obT))
        mix = []
        for b in range(2):
            m = np.zeros((8192, 1024), bf)
            for j in range(4):
                c = b * 4 + j
                oa = oaT[c].transpose(2, 1, 0).reshape(2048, 512)
                oc = ocT[c].transpose(2, 1, 0).reshape(2048, 256)
                for k in range(4):
                    g = 4 * k + j
                    m[512 * g:512 * (g + 1), 0:512] = oa[512 * k:512 * (k + 1)]
                    m[512 * g:512 * (g + 1), 768:1024] = oc[512 * k:512 * (k + 1)]
            for h in range(4):
                m[:, 512 + 64 * h:512 + 64 * (h + 1)] = obT[b * 4 + h].T
            mix.append(m)
        if CHECK:
            CHECK("mix", l, dict(mix=mix))
        ncF = _get("F%d" % int(last), lambda: build_F(final=last))
        rF = _run(ncF, host_inputs_F(mix, x, np.asarray(w_out[l]), np.asarray(norm2_g[l]), np.asarray(w_up[l]), np.asarray(conv_w[l]),
                                     np.asarray(conv_b[l]), np.asarray(w_down[l]), np.asarray(final_norm_g)))
        xn = np.zeros_like(x)
        for c in range(8):
            b, j = divmod(c, 4)
            xo = np.asarray(rF[c]["xoT"]).T
            for k in range(4):
                g = 4 * k + j
                xn[b, 512 * g:512 * (g + 1)] = xo[512 * k:512 * (k + 1)]
        x = xn
        if CHECK:
            CHECK("F", l, dict(x=x))
    return x


def kernel(**inputs):
    out = forward(**{k: np.asarray(v) for k, v in inputs.items()})
    return np.ascontiguousarray(out.astype(np.float32))
```

```python
import numpy as np
import concourse.bass as bass
import concourse.mybir as mybir
from concourse.bass_utils import run_bass_kernel_spmd
from contextlib import ExitStack
import math
import ml_dtypes

F32 = mybir.dt.float32
BF16 = mybir.dt.bfloat16
I32 = mybir.dt.int32
ALU = mybir.AluOpType
AF = mybir.ActivationFunctionType
AX = mybir.AxisListType

ENGS = ("pe", "act", "dve", "pool", "sp")


class _Op:
    __slots__ = ("eng", "fn", "reads", "writes", "dma", "deps", "sig", "sigval", "dsem", "idx")


class Sched:
    def __init__(self, nc, n_dma_sems=6):
        self.nc = nc
        self.ops = []
        self.last_w = {}
        self.readers = {}
        self.n_dma_sems = n_dma_sems

    def add(self, eng, fn, reads=(), writes=(), dma=False):
        op = _Op()
        op.eng = eng
        op.fn = fn
        op.reads = tuple(reads)
        op.writes = tuple(writes)
        op.dma = dma
        op.idx = len(self.ops)
        deps = set()
        for r in op.reads:
            w = self.last_w.get(r)
            if w is not None:
                deps.add(w)
        for r in op.writes:
            w = self.last_w.get(r)
            if w is not None:
                deps.add(w)
            for rd in self.readers.get(r, ()):
                deps.add(rd)
        deps.discard(op.idx)
        op.deps = deps
        for r in op.reads:
            self.readers.setdefault(r, []).append(op.idx)
        for r in op.writes:
            self.last_w[r] = op.idx
            self.readers[r] = []
        op.sig = False
        op.sigval = None
        op.dsem = None
        self.ops.append(op)
        return op

    def pe(self, fn, reads=(), writes=()):
        return self.add("pe", fn, reads, writes)

    def act(self, fn, reads=(), writes=()):
        return self.add("act", fn, reads, writes)

    def dve(self, fn, reads=(), writes=()):
        return self.add("dve", fn, reads, writes)

    def pool(self, fn, reads=(), writes=()):
        return self.add("pool", fn, reads, writes)

    def dma(self, fn, reads=(), writes=(), q="sp"):
        return self.add(q, fn, reads, writes, dma=True)

    def emit(self, stack):
        nc = self.nc
        ops = self.ops
        for op in ops:
            for d in op.deps:
                dop = ops[d]
                if dop.eng == op.eng and not dop.dma:
                    if not (set(dop.writes) & set(op.reads)):
                        continue
                dop.sig = True
        for op in ops:
            if op.dma:
                op.sig = True
        esem = {e: stack.enter_context(nc.semaphore("s_" + e)) for e in ENGS}
        dsems = {}
        for q in ENGS:
            if any(o.dma and o.eng == q for o in ops):
                dsems[q] = [stack.enter_context(nc.semaphore("d_%s%d" % (q, i))) for i in range(self.n_dma_sems)]
        ecount = {e: 0 for e in ENGS}
        dcount = {q: [0] * self.n_dma_sems for q in dsems}
        drr = {q: 0 for q in dsems}
        prev_dma_wait = {}
        for op in ops:
            if op.dma:
                k = drr[op.eng]
                drr[op.eng] = (k + 1) % self.n_dma_sems
                prev_dma_wait[op.idx] = dcount[op.eng][k]
                dcount[op.eng][k] += 16
                op.dsem = (op.eng, k)
                op.sigval = dcount[op.eng][k]
            elif op.sig:
                ecount[op.eng] += 1
                op.sigval = ecount[op.eng]
        by_eng = {e: [o for o in ops if o.eng == e] for e in ENGS}
        block = stack.enter_context(nc.Block())
        self.n_waits = 0

        def run(ename, eobj):
            waited = {}
            for op in by_eng[ename]:
                need = {}
                for d in op.deps:
                    dop = ops[d]
                    if dop.dma:
                        key = ("d",) + dop.dsem
                        sem = dsems[dop.dsem[0]][dop.dsem[1]]
                    else:
                        if dop.eng == ename and not (set(dop.writes) & set(op.reads)):
                            continue
                        key = ("e", dop.eng)
                        sem = esem[dop.eng]
                    v = dop.sigval
                    if waited.get(key, 0) >= v:
                        continue
                    if key not in need or need[key][1] < v:
                        need[key] = (sem, v)
                if op.dma:
                    pv = prev_dma_wait[op.idx]
                    key = ("d",) + op.dsem
                    if pv > 0 and waited.get(key, 0) < pv:
                        if key not in need or need[key][1] < pv:
                            need[key] = (dsems[op.dsem[0]][op.dsem[1]], pv)
                for key, (sem, v) in need.items():
                    eobj.wait_ge(sem, v)
                    waited[key] = v
                    self.n_waits += 1
                ins = op.fn(eobj)
                if op.dma:
                    ins.then_inc(dsems[op.dsem[0]][op.dsem[1]], 16)
                elif op.sig:
                    ins.then_inc(esem[ename], 1)
            for q, lst in dsems.items():
                if q == ename:
                    for k, s in enumerate(lst):
                        if dcount[q][k] > 0:
                            eobj.wait_ge(s, dcount[q][k])

        if by_eng["pe"]:
            @block.tensor
            def _(e):
                run("pe", e)
        if by_eng["act"]:
            @block.scalar
            def _(e):
                run("act", e)
        if by_eng["dve"]:
            @block.vector
            def _(e):
                run("dve", e)
        if by_eng["pool"]:
            @block.gpsimd
            def _(e):
                run("pool", e)
        if by_eng["sp"]:
            @block.sync
            def _(e):
                run("sp", e)


NTILE = 20
INW = 2900
CH = [(0, 512), (512, 1024), (1024, 1536), (1536, 2048), (2048, 2560), (2560, 2900)]
UC0 = 2644
BFW = 1856
F32W = INW - BFW
TWO_PI = 2.0 * math.pi


def pool_mats(first):
    Mc = np.zeros((128, 4, 128), np.float32)
    Mh = np.zeros((128, 4, 128), np.float32)
    for gi, w in enumerate((2, 4, 8, 16)):
        for t in range(128):
            cnt = min(t + 1, w) if first else w
            for s in range(t - w + 1, t + 1):
                if s >= 0:
                    Mc[s, gi, t] += 1.0 / cnt
                elif not first:
                    Mh[128 + s, gi, t] += 1.0 / cnt
            Mc[t, gi, t] -= 1.0
    return Mc, Mh


def build_A():
    nc = bass.Bass("TRN2", target_bir_lowering=False)
    D = lambda name, shape, dt, kind="ExternalInput": nc.dram_tensor(name, shape, dt, kind=kind).ap()
    xtok = D("xtok", [NTILE * 128, 1024], F32)
    xT = D("xT", [1024, NTILE * 128], F32)
    w_in = D("w_in", [1024, INW], F32)
    g1 = D("g1", [128, 8], F32)
    pos = D("pos", [128, NTILE], I32)
    inv = D("inv", [128, 8], F32)
    wpool = D("wpool", [64, 4, 64], F32)
    pscale = D("pscale", [64, 4], F32)
    mcur = D("mcur", [128, 5, 4, 128], F32)
    mhal = D("mhal", [128, 5, 4, 128], F32)
    pbf = D("pbf", [2048, BFW], BF16, "ExternalOutput")
    pf32 = D("pf32", [2048, F32W], F32, "ExternalOutput")
    ocT = D("ocT", [64, 4, 2048], BF16, "ExternalOutput")

    with ExitStack() as stack:
        T = lambda name, shape, dt: stack.enter_context(nc.sbuf_tensor(name, shape, dt))
        P = lambda name, shape, dt: stack.enter_context(nc.psum_tensor(name, shape, dt))
        wbf = T("wbf", [128, 8, INW], BF16)
        wst = T("wst", [128, 2, INW], F32)
        g1t = T("g1t", [128, 8], F32)
        posi = T("posi", [128, NTILE], I32)
        posf = T("posf", [128, NTILE], F32)
        invt = T("invt", [128, 8], F32)
        ang = T("ang", [128, NTILE, 8], F32)
        kq = T("kq", [128, NTILE, 8], F32)
        angc = T("angc", [128, NTILE, 8], F32)
        kqi = T("kqi", [128, NTILE, 8], I32)
        cost = T("cost", [128, NTILE, 8], F32)
        sint = T("sint", [128, NTILE, 8], F32)
        wpt = T("wpt", [64, 4, 64], F32)
        pst = T("pst", [64, 4], F32)
        mct = T("mct", [128, 5, 4, 128], F32)
        mht = T("mht", [128, 5, 4, 128], F32)
        xtk = T("xtk", [128, 2, 1024], F32)
        sqj = T("sqj", [128, 1024], BF16)
        ss = T("ss", [128, 2], F32)
        rstd = T("rstd", [128, 2], F32)
        epst = T("epst", [128, 1], F32)
        xTt = T("xTt", [128, 2, 8, 128], F32)
        hT = T("hT", [128, 2, 8, 128], BF16)
        proj = T("proj", [128, 2, INW], F32)
        pb16 = T("pb16", [128, 2, BFW], BF16)
        rt = T("rt", [128, 4, 16, 8], F32)
        pooled = T("pooled", [64, 2, 4, 128], F32)
        oct_ = T("oct", [64, 2, 4, 128], BF16)
        ps = [P("ps%d" % i, [128, 512], F32) for i in range(4)]
        pp = [P("pp%d" % i, [64, 4, 128], F32) for i in range(2)]
        py = [P("py%d" % i, [64, 4, 128], F32) for i in range(2)]

        s = Sched(nc)
        s.dma(lambda e: e.dma_start(out=g1t[:], in_=g1), writes=["g1t"])
        s.dma(lambda e: e.dma_start(out=posi[:], in_=pos), writes=["posi"])
        s.dma(lambda e: e.dma_start(out=invt[:], in_=inv), writes=["invt"])
        s.dma(lambda e: e.dma_start(out=wpt[:], in_=wpool), writes=["wpt"])
        s.dma(lambda e: e.dma_start(out=pst[:], in_=pscale), writes=["pst"])
        s.dma(lambda e: e.dma_start(out=mct[:], in_=mcur), writes=["mct"])
        s.dma(lambda e: e.dma_start(out=mht[:], in_=mhal), writes=["mht"])
        s.dve(lambda e: e.memset(epst[:], 1e-6), writes=["epst"])
        s.dve(lambda e: e.tensor_copy(out=posf[:], in_=posi[:]), reads=["posi"], writes=["posf"])
        for t in range(NTILE):
            s.dve(lambda e, t=t: e.tensor_scalar(out=ang[:, t, :], in0=invt[:], scalar1=posf[:, t:t + 1], scalar2=None, op0=ALU.mult),
                  reads=["invt", "posf"], writes=["ang"])
        s.dve(lambda e: e.tensor_scalar(out=kqi[:], in0=ang[:], scalar1=1.0 / TWO_PI, scalar2=None, op0=ALU.mult), reads=["ang"], writes=["kqi"])
        s.dve(lambda e: e.tensor_copy(out=kq[:], in_=kqi[:]), reads=["kqi"], writes=["kq"])
        s.dve(lambda e: e.scalar_tensor_tensor(out=ang[:], in0=kq[:], scalar=-TWO_PI, in1=ang[:], op0=ALU.mult, op1=ALU.add),
              reads=["kq", "ang"], writes=["ang"])
        def wrap(y, name):
            s.dve(lambda e: e.tensor_scalar(out=kq[:], in0=y[:], scalar1=math.pi, scalar2=-TWO_PI, op0=ALU.is_gt, op1=ALU.mult),
                  reads=[name], writes=["kq"])
            s.dve(lambda e: e.tensor_tensor(out=y[:], in0=y[:], in1=kq[:], op=ALU.add), reads=[name, "kq"], writes=[name])
            s.dve(lambda e: e.tensor_scalar(out=kq[:], in0=y[:], scalar1=-math.pi, scalar2=TWO_PI, op0=ALU.is_lt, op1=ALU.mult),
                  reads=[name], writes=["kq"])
            s.dve(lambda e: e.tensor_tensor(out=y[:], in0=y[:], in1=kq[:], op=ALU.add), reads=[name, "kq"], writes=[name])
        s.dve(lambda e: e.tensor_scalar(out=angc[:], in0=ang[:], scalar1=math.pi / 2, scalar2=None, op0=ALU.add), reads=["ang"], writes=["angc"])
        wrap(ang, "ang")
        wrap(angc, "angc")
        s.act(lambda e: e.activation(out=sint[:], in_=ang[:], func=AF.Sin), reads=["ang"], writes=["sint"])
        s.act(lambda e: e.activation(out=cost[:], in_=angc[:], func=AF.Sin), reads=["angc"], writes=["cost"])

        for c in range(8):
            b = c % 2
            s.dma(lambda e, c=c, b=b: e.dma_start(out=wst[:, b, :], in_=w_in[c * 128:(c + 1) * 128, :]), writes=[("wst", b)])
            if c % 2 == 0:
                s.dve(lambda e, c=c, b=b: e.tensor_scalar(out=wbf[:, c, :], in0=wst[:, b, :], scalar1=g1t[:, c:c + 1], scalar2=None, op0=ALU.mult),
                      reads=[("wst", b), "g1t"], writes=[("wbf", c)])
            else:
                s.act(lambda e, c=c, b=b: e.activation(out=wbf[:, c, :], in_=wst[:, b, :], func=AF.Copy, scale=g1t[:, c:c + 1]),
                      reads=[("wst", b), "g1t"], writes=[("wbf", c)])

        own = 0
        for t in range(NTILE):
            k, i = divmod(t, 5)
            halo = (i == 0)
            b = t % 2
            pb = (t - 1) % 2
            s.dma(lambda e, t=t, b=b: e.dma_start(out=xtk[:, b, :], in_=xtok[t * 128:(t + 1) * 128, :]), writes=[("xtk", b)])
            s.dma(lambda e, t=t, b=b: e.dma_start(out=xTt[:, b, :, :], in_=xT[:, t * 128:(t + 1) * 128].rearrange("(c p) t -> p c t", p=128)),
                  writes=[("xTt", b)])
            s.act(lambda e, b=b: e.activation(out=sqj[:], in_=xtk[:, b, :], func=AF.Square, accum_out=ss[:, b:b + 1]),
                  reads=[("xtk", b)], writes=["sqj", ("ss", b)])
            s.act(lambda e, b=b: e.activation(out=ss[:, b:b + 1], in_=ss[:, b:b + 1], func=AF.Sqrt, scale=1.0 / 1024, bias=epst[:, 0:1]),
                  reads=[("ss", b), "epst"], writes=[("ss", b)])
            s.dve(lambda e, b=b: e.reciprocal(out=rstd[:, b:b + 1], in_=ss[:, b:b + 1]), reads=[("ss", b)], writes=[("rstd", b)])
            s.pool(lambda e, b=b: e.tensor_copy(out=hT[:, b, :, :], in_=xTt[:, b, :, :]), reads=[("xTt", b)], writes=[("hT", b)])
            chunks = [5] if halo else list(range(6))
            for n in chunks:
                n0, n1 = CH[n]
                pt = ps[n % 4]
                for c in range(8):
                    s.pe(lambda e, pt=pt, b=b, c=c, n0=n0, n1=n1: e.matmul(pt[:, 0:n1 - n0], lhsT=hT[:, b, c, :], rhs=wbf[:, c, n0:n1],
                                                                         start=(c == 0), stop=(c == 7)),
                         reads=[("hT", b), ("wbf", c)], writes=[("ps", n % 4)])
                if n % 2 == 0:
                    s.act(lambda e, pt=pt, b=b, n0=n0, n1=n1: e.activation(out=proj[:, b, n0:n1], in_=pt[:, 0:n1 - n0], func=AF.Copy,
                                                                         scale=rstd[:, b:b + 1]),
                          reads=[("ps", n % 4), ("rstd", b)], writes=[("proj", b, n)])
                else:
                    s.dve(lambda e, pt=pt, b=b, n0=n0, n1=n1: e.tensor_scalar(out=proj[:, b, n0:n1], in0=pt[:, 0:n1 - n0],
                                                                            scalar1=rstd[:, b:b + 1], scalar2=None, op0=ALU.mult),
                          reads=[("ps", n % 4), ("rstd", b)], writes=[("proj", b, n)])
            if not halo:
                for (c0, nh, regs) in ((0, 16, [("proj", b, 0), ("proj", b, 1)]), (1536, 5, [("proj", b, 3)])):
                    v = proj[:, b, c0:c0 + nh * 64].rearrange("p (h d) -> p h d", d=64)
                    x1 = v[:, :, 0:8]
                    x2 = v[:, :, 8:16]
                    cb = cost[:, t, :].unsqueeze(1).to_broadcast([128, nh, 8])
                    sb = sint[:, t, :].unsqueeze(1).to_broadcast([128, nh, 8])
                    t0 = rt[:, 0, 0:nh, :]
                    t1 = rt[:, 1, 0:nh, :]
                    t2 = rt[:, 2, 0:nh, :]
                    t3 = rt[:, 3, 0:nh, :]
                    R = regs + ["cost", "sint"]
                    s.dve(lambda e, t0=t0, x1=x1, cb=cb: e.tensor_tensor(out=t0, in0=x1, in1=cb, op=ALU.mult), reads=R, writes=["rt0"])
                    s.dve(lambda e, t1=t1, x2=x2, sb=sb: e.tensor_tensor(out=t1, in0=x2, in1=sb, op=ALU.mult), reads=R, writes=["rt1"])
                    s.dve(lambda e, t2=t2, x2=x2, cb=cb: e.tensor_tensor(out=t2, in0=x2, in1=cb, op=ALU.mult), reads=R, writes=["rt2"])
                    s.dve(lambda e, t3=t3, x1=x1, sb=sb: e.tensor_tensor(out=t3, in0=x1, in1=sb, op=ALU.mult), reads=R, writes=["rt3"])
                    s.dve(lambda e, t0=t0, t1=t1, x1=x1: e.tensor_tensor(out=x1, in0=t0, in1=t1, op=ALU.subtract),
                          reads=["rt0", "rt1", "rt2", "rt3"], writes=regs)
                    s.dve(lambda e, t2=t2, t3=t3, x2=x2: e.tensor_tensor(out=x2, in0=t2, in1=t3, op=ALU.add),
                          reads=["rt0", "rt1", "rt2", "rt3"], writes=regs)
                r0 = own * 128
                s.act(lambda e, b=b: e.activation(out=pb16[:, b, :], in_=proj[:, b, 0:BFW], func=AF.Copy),
                      reads=[("proj", b, n) for n in range(4)], writes=[("pb16", b)])
                s.dma(lambda e, b=b, r0=r0: e.dma_start(out=pbf[r0:r0 + 128, :], in_=pb16[:, b, :]), reads=[("pb16", b)])
                s.dma(lambda e, b=b, r0=r0: e.dma_start(out=pf32[r0:r0 + 128, :], in_=proj[:, b, BFW:INW]),
                      reads=[("proj", b, n) for n in (3, 4, 5)])
                mi = 0 if i > 1 else 1 + k
                ob = own % 2
                for gi in range(4):
                    s.pe(lambda e, ob=ob, b=b, gi=gi, mi=mi: e.matmul(pp[ob][:, gi, :], lhsT=proj[:, b, UC0 + gi * 64:UC0 + (gi + 1) * 64],
                                                                  rhs=mct[:, mi, gi, :], start=True, stop=False),
                         reads=[("proj", b, 5), "mct"], writes=[("pp", ob)])
                    s.pe(lambda e, ob=ob, pb=pb, gi=gi, mi=mi: e.matmul(pp[ob][:, gi, :], lhsT=proj[:, pb, UC0 + gi * 64:UC0 + (gi + 1) * 64],
                                                                    rhs=mht[:, mi, gi, :], start=False, stop=True),
                         reads=[("proj", pb, 5), "mht"], writes=[("pp", ob)])
                s.act(lambda e, ob=ob: e.activation(out=pooled[:, ob, :, :], in_=pp[ob][:], func=AF.Copy),
                      reads=[("pp", ob)], writes=[("pooled", ob)])
                for gi in range(4):
                    s.pe(lambda e, ob=ob, gi=gi: e.matmul(py[ob][:, gi, :], lhsT=wpt[:, gi, :], rhs=pooled[:, ob, gi, :], start=True, stop=True),
                         reads=[("pooled", ob), "wpt"], writes=[("py", ob)])
                for gi in range(4):
                    s.dve(lambda e, ob=ob, gi=gi: e.tensor_scalar(out=oct_[:, ob, gi, :], in0=py[ob][:, gi, :], scalar1=pst[:, gi:gi + 1],
                                                                scalar2=None, op0=ALU.mult),
                          reads=[("py", ob), "pst"], writes=[("oct", ob)])
                s.dma(lambda e, ob=ob, r0=r0: e.dma_start(out=ocT[:, :, r0:r0 + 128], in_=oct_[:, ob, :, :]), reads=[("oct", ob)])
                own += 1
        s.emit(stack)
        print("A: ops", len(s.ops), "waits", s.n_waits)
    return nc


def host_inputs_A(x, positions, norm1_g, w_in, w_pool, pool_scale):
    inv = (500000.0 ** (-np.arange(0, 16, 2, dtype=np.float32) / 16)).astype(np.float32)
    McG, MhG = pool_mats(False)
    McF, MhF = pool_mats(True)
    maps = []
    for c in range(8):
        b, j = divmod(c, 4)
        rows = []
        posl = []
        mcur = np.zeros((128, 5, 4, 128), np.float32)
        mhal = np.zeros((128, 5, 4, 128), np.float32)
        mcur[:, 0], mhal[:, 0] = McG, MhG
        for k in range(4):
            g = 4 * k + j
            t0 = 512 * g
            if g == 0:
                rows.append(np.zeros((128, 1024), np.float32))
                posl.append(np.zeros((128,), np.int32))
                mcur[:, 1 + k], mhal[:, 1 + k] = McF, MhF
            else:
                rows.append(x[b, t0 - 128:t0])
                posl.append(positions[b, t0 - 128:t0])
                mcur[:, 1 + k], mhal[:, 1 + k] = McG, MhG
            rows.append(x[b, t0:t0 + 512])
            posl.append(positions[b, t0:t0 + 512])
        xt = np.ascontiguousarray(np.concatenate(rows, 0))
        pl = np.concatenate(posl, 0).astype(np.int32)
        maps.append({
            "xtok": xt, "xT": np.ascontiguousarray(xt.T), "w_in": np.ascontiguousarray(w_in),
            "g1": np.ascontiguousarray(norm1_g.reshape(8, 128).T),
            "pos": np.ascontiguousarray(pl.reshape(NTILE, 128).T),
            "inv": np.ascontiguousarray(np.broadcast_to(inv[None, :], (128, 8))),
            "wpool": np.ascontiguousarray(w_pool.transpose(1, 0, 2)),
            "pscale": np.ascontiguousarray(pool_scale.reshape(4, 64).T),
            "mcur": mcur, "mhal": mhal,
        })
    return maps


NIT = 16
NEG = -1.0e30


def build_B(nslot=4, nit=NIT):
    nc = bass.Bass("TRN2", target_bir_lowering=False)
    D = lambda name, shape, dt, kind="ExternalInput": nc.dram_tensor(name, shape, dt, kind=kind).ap()
    kT = D("kT", [128, 4, 8192], BF16)
    vv = D("v", [8192, 512], BF16)
    kiT = D("kiT", [64, 8192], BF16)
    qT = D("qT", [128, 4, 4, 512], BF16)
    qiT = D("qiT", [64, 4, 4, 512], BF16)
    wi = D("wi", [128, 16, 4], F32)
    qrel = D("qrel", [128, 16], F32)
    kpos = D("kpos", [128, 2048], F32)
    ident = D("ident", [128, 128], BF16)
    oaT = D("oaT", [64, 8, 2048], BF16, "ExternalOutput")

    with ExitStack() as stack:
        T = lambda name, shape, dt: stack.enter_context(nc.sbuf_tensor(name, shape, dt))
        P = lambda name, shape, dt: stack.enter_context(nc.psum_tensor(name, shape, dt))
        kit = T("kit", [64, 8192], BF16)
        sc = T("sc", [128, 8192], F32)
        mk = T("mk", [128, 4, 8192], BF16)
        kpt = T("kpt", [128, 2048], F32)
        rr = T("rr", [128, 4, 512], F32)
        qTt = T("qTt", [128, 1, 4, 512], BF16)
        qit = T("qit", [64, 1, 4, 512], BF16)
        wit = T("wit", [128, 16, 4], F32)
        qrt = T("qrt", [128, 16], F32)
        idt = T("idt", [128, 128], BF16)
        kTs = T("kTs", [128, 2, 4, 512], BF16)
        vraw = T("vraw", [128, 2, 4, 512], BF16)
        vt = T("vt", [128, 2, 4, 520], BF16)
        E = T("E", [128, 3, 512], BF16)
        Pm = T("Pm", [128, 3, 512], BF16)
        mT = T("mT", [128, 2, 512], BF16)
        sm = T("sm", [128, 8], F32)
        ones1 = T("ones1", [128, 64], F32)
        rec = T("rec", [128, 512], F32)
        bcs = T("bcs", [64, 512], F32)
        oT = T("oT", [64, 2, 512], BF16)
        pb = [P("pb%d" % i, [128, 512], F32) for i in range(8)]
        pTb = pb[2][:].bitcast(BF16)

        s = Sched(nc)
        s.dma(lambda e: e.dma_start(out=kit[:], in_=kiT), writes=["kit"])
        s.dma(lambda e: e.dma_start(out=wit[:], in_=wi), writes=["wit"])
        s.dma(lambda e: e.dma_start(out=qrt[:], in_=qrel), writes=["qrt"])
        s.dma(lambda e: e.dma_start(out=kpt[:], in_=kpos), writes=["kpt"])
        s.dma(lambda e: e.dma_start(out=idt[:], in_=ident), writes=["idt"])
        s.pool(lambda e: e.memset(ones1[:], 1.0), writes=["ones1"])
        s.dve(lambda e: e.memset(vt[:], 1.0), writes=[("vt", 0), ("vt", 1)])
        cbias = rr[:].rearrange("p h n -> p (h n)")
        RRALL = [("rr", h) for h in range(4)]

        kbc = 0
        ec = 0
        mtc = 0
        otc = 0
        for k in range(nslot):
            L = 2048 * (k + 1)
            nkc = L // 512
            nkb = L // 128
            qb = 0
            s.dma(lambda e, k=k, qb=qb: e.dma_start(out=qTt[:, qb, :, :], in_=qT[:, k, :, :]), writes=[("qTt", qb)])
            s.dma(lambda e, k=k, qb=qb: e.dma_start(out=qit[:, qb, :, :], in_=qiT[:, k, :, :]), writes=[("qit", qb)])
            for qt in range(4):
                g = 4 * k + qt
                for n in range(nkc):
                    for h in range(4):
                        s.pe(lambda e, h=h, qb=qb, qt=qt, n=n: e.matmul(pb[h][:], lhsT=qit[:, qb, h, qt * 128:(qt + 1) * 128],
                                                                     rhs=kit[:, n * 512:(n + 1) * 512], start=True, stop=True),
                             reads=[("qit", qb), "kit"], writes=[("pb", h)])
                        s.act(lambda e, h=h: e.activation(out=rr[:, h, :], in_=pb[h][:], func=AF.Relu), reads=[("pb", h)], writes=[("rr", h)])
                        if h == 0:
                            s.dve(lambda e, n=n, g=g: e.tensor_scalar(out=sc[:, n * 512:(n + 1) * 512], in0=rr[:, 0, :], scalar1=wit[:, g, 0:1],
                                                                    scalar2=None, op0=ALU.mult),
                                  reads=[("rr", 0), "wit"], writes=[("sc", n)])
                        else:
                            s.dve(lambda e, n=n, g=g, h=h: e.scalar_tensor_tensor(out=sc[:, n * 512:(n + 1) * 512], in0=rr[:, h, :],
                                                                                 scalar=wit[:, g, h:h + 1], in1=sc[:, n * 512:(n + 1) * 512],
                                                                                 op0=ALU.mult, op1=ALU.add),
                                  reads=[("rr", h), "wit", ("sc", n)], writes=[("sc", n)])
                allsc = [("sc", n) for n in range(nkc)]
                s.dve(lambda e, L=L: e.tensor_reduce(out=sm[:, 5:6], in_=sc[:, 0:L], axis=AX.X, op=ALU.max, apply_absolute_value=True),
                      reads=allsc, writes=["rmax"])
                s.dve(lambda e: e.tensor_scalar(out=sm[:, 0:1], in0=sm[:, 5:6], scalar1=-1.0, scalar2=None, op0=ALU.mult), reads=["rmax"], writes=["lo"])
                s.dve(lambda e: e.tensor_scalar(out=sm[:, 1:2], in0=sm[:, 5:6], scalar1=2.0, scalar2=None, op0=ALU.mult), reads=["rmax"], writes=["range"])
                s.dve(lambda e, g=g: e.tensor_scalar(out=cbias[:], in0=kpt[:], scalar1=qrt[:, g:g + 1], scalar2=NEG, op0=ALU.is_gt, op1=ALU.mult),
                      reads=["kpt", "qrt"], writes=RRALL)
                s.dve(lambda e, L=L: e.tensor_tensor(out=sc[:, L - 2048:L], in0=sc[:, L - 2048:L], in1=cbias[:], op=ALU.add),
                      reads=allsc + RRALL, writes=allsc)
                Lh = max(512, int(round(0.4 * L / 512.0)) * 512)
                nact = float(L - Lh)
                MKA, MKB = ("mk", qt, "a"), ("mk", qt, "b")
                for it in range(1, nit + 1):
                    f = 2.0 ** (-it)
                    s.dve(lambda e, f=f: e.tensor_scalar(out=sm[:, 2:3], in0=sm[:, 1:2], scalar1=f, scalar2=sm[:, 0:1], op0=ALU.mult, op1=ALU.add),
                          reads=["range", "lo"], writes=["mid"])
                    s.dve(lambda e, Lh=Lh, qt=qt: e.tensor_scalar(out=mk[:, qt, 0:Lh], in0=sc[:, 0:Lh], scalar1=sm[:, 2:3], scalar2=None,
                                                                op0=ALU.is_ge, op1=ALU.add, accum_out=sm[:, 3:4]),
                          reads=allsc + ["mid"], writes=[MKA, "cnt"])
                    s.act(lambda e, Lh=Lh, L=L, qt=qt: e.activation(out=mk[:, qt, Lh:L], in_=sc[:, Lh:L], func=AF.Sign, scale=-1.0, bias=sm[:, 2:3],
                                                                   accum_out=sm[:, 6:7]),
                          reads=allsc + ["mid"], writes=[MKB, "sgn"])
                    s.dve(lambda e: e.scalar_tensor_tensor(out=sm[:, 7:8], in0=sm[:, 3:4], scalar=2.0, in1=sm[:, 6:7], op0=ALU.mult, op1=ALU.subtract),
                          reads=["cnt", "sgn"], writes=["tt"])
                    s.dve(lambda e, f=f, nact=nact: e.tensor_scalar(out=sm[:, 4:5], in0=sm[:, 7:8], scalar1=511.0 - nact, scalar2=f, op0=ALU.is_ge, op1=ALU.mult),
                          reads=["tt"], writes=["pred"])
                    s.dve(lambda e: e.scalar_tensor_tensor(out=sm[:, 0:1], in0=sm[:, 4:5], scalar=sm[:, 1:2], in1=sm[:, 0:1], op0=ALU.mult, op1=ALU.add),
                          reads=["pred", "range", "lo"], writes=["lo"])
                s.dve(lambda e, L=L, qt=qt: e.tensor_scalar(out=mk[:, qt, 0:L], in0=sc[:, 0:L], scalar1=sm[:, 0:1], scalar2=None, op0=ALU.is_ge),
                      reads=allsc + ["lo"], writes=[MKA, MKB])
            steps = [(hp, kb, hl) for hp in range(2) for kb in range(nkb) for hl in range(4)]
            nst = len(steps)
            info = {}

            def load_sb(hp, sbk):
                nonlocal kbc
                kbuf = kbc % 2
                kbc += 1
                info[("kbuf", hp, sbk)] = kbuf
                s.dma(lambda e, sbk=sbk, kbuf=kbuf: e.dma_start(out=kTs[:, kbuf, :, :], in_=kT[:, :, sbk * 512:(sbk + 1) * 512]),
                      writes=[("kTs", kbuf)])
                s.dma(lambda e, sbk=sbk, kbuf=kbuf: e.dma_start(out=vraw[:, kbuf, :, :], in_=vv[sbk * 512:(sbk + 1) * 512, :].rearrange("(kb p) c -> p kb c", p=128)),
                      writes=[("vraw", kbuf)])
                for kl_ in range(4):
                    s.dve(lambda e, kbuf=kbuf, kl_=kl_: e.tensor_copy(out=vt[:, kbuf, kl_, :].rearrange("p (h c) -> p h c", c=65)[:, :, 0:64],
                                                                   in_=vraw[:, kbuf, kl_, :].rearrange("p (h d) -> p h d", d=64)),
                          reads=[("vraw", kbuf)], writes=[("vt", kbuf)])

            def pre(hp, kb):
                nonlocal mtc
                sbk, kl = divmod(kb, 4)
                mb = mtc % 2
                mtc += 1
                info[("mb", hp, kb)] = mb
                for qt in range(4):
                    s.pe(lambda e, qt=qt, kb=kb: e.transpose(pTb[:, qt * 128:(qt + 1) * 128], mk[:, qt, kb * 128:(kb + 1) * 128], idt[:]),
                         reads=[("mk", qt, "a"), ("mk", qt, "b"), "idt"], writes=[("pb", 2)])
                s.act(lambda e, mb=mb: e.activation(out=mT[:, mb, :], in_=pTb[:, 0:512], func=AF.Copy), reads=[("pb", 2)], writes=[("mT", mb)])

            def ST(i):
                hp, kb, hl = steps[i]
                sbk, kl = divmod(kb, 4)
                kbuf = info[("kbuf", hp, sbk)]
                h = hp * 4 + hl
                pr, hh = divmod(h, 2)
                sb = i % 2
                s.pe(lambda e, sb=sb, kbuf=kbuf, pr=pr, hh=hh, kl=kl: e.matmul(pb[sb][:], lhsT=kTs[hh * 64:(hh + 1) * 64, kbuf, pr, kl * 128:(kl + 1) * 128],
                                                                            rhs=qTt[hh * 64:(hh + 1) * 64, 0, pr, :], start=True, stop=True),
                     reads=[("kTs", kbuf), ("qTt", 0)], writes=[("pb", sb)])

            def rest_a(i):
                sb = i % 2
                eb = i % 3
                s.act(lambda e, sb=sb, eb=eb: e.activation(out=E[:, eb, :], in_=pb[sb][:], func=AF.Exp, scale=0.125),
                      reads=[("pb", sb)], writes=[("E", eb)])

            def rest(i):
                nonlocal otc
                hp, kb, hl = steps[i]
                sbk, kl = divmod(kb, 4)
                kbuf = info[("kbuf", hp, sbk)]
                mb = info[("mb", hp, kb)]
                h = hp * 4 + hl
                sb = i % 2
                eb = i % 3
                eng = s.dve if (i % 2 == 0) else s.pool
                eng(lambda e, eb=eb, mb=mb: e.tensor_tensor(out=Pm[:, eb, :], in0=E[:, eb, :], in1=mT[:, mb, :], op=ALU.mult),
                    reads=[("E", eb), ("mT", mb)], writes=[("Pm", eb)])
                s.pe(lambda e, hl=hl, kbuf=kbuf, h=h, eb=eb, kb=kb, kl=kl: e.matmul(pb[4 + hl][0:65, :], lhsT=vt[:, kbuf, kl, h * 65:(h + 1) * 65], rhs=Pm[:, eb, :],
                                                                                 start=(kb == 0), stop=(kb == nkb - 1)),
                     reads=[("vt", kbuf), ("Pm", eb)], writes=[("pb", 4 + hl)])
                if kb == nkb - 1:
                    ob = otc % 2
                    otc += 1
                    s.dve(lambda e, hl=hl: e.reciprocal(out=rec[64:65, :], in_=pb[4 + hl][64:65, :]), reads=[("pb", 4 + hl)], writes=["rec"])
                    s.pe(lambda e: e.matmul(pb[3][0:64, :], lhsT=ones1[64:65, :], rhs=rec[64:65, :], start=True, stop=True),
                         reads=["ones1", "rec"], writes=[("pb", 3)])
                    s.act(lambda e: e.activation(out=bcs[:], in_=pb[3][0:64, :], func=AF.Copy), reads=[("pb", 3)], writes=["bcs"])
                    s.dve(lambda e, hl=hl, ob=ob: e.tensor_tensor(out=oT[:, ob, :], in0=pb[4 + hl][0:64, :], in1=bcs[:], op=ALU.mult),
                          reads=[("pb", 4 + hl), "bcs"], writes=[("oT", ob)])
                    s.dma(lambda e, h=h, k=k, ob=ob: e.dma_start(out=oaT[:, h, k * 512:(k + 1) * 512], in_=oT[:, ob, :]), reads=[("oT", ob)])

            DPIPE = 2
            order = [(hp_, sb_) for hp_ in range(2) for sb_ in range(nkb // 4)]
            load_sb(*order[0])
            for j in range(min(DPIPE, nst)):
                if steps[j][2] == 0:
                    pre(steps[j][0], steps[j][1])
                ST(j)
            for i in range(0, nst, 2):
                if steps[i][2] == 0 and steps[i][1] % 4 == 0:
                    oi = order.index((steps[i][0], steps[i][1] // 4))
                    if oi + 1 < len(order):
                        load_sb(*order[oi + 1])
                rest_a(i)
                rest_a(i + 1)
                for j in (i + DPIPE, i + DPIPE + 1):
                    if j < nst:
                        if steps[j][2] == 0:
                            pre(steps[j][0], steps[j][1])
                        ST(j)
                rest(i)
                rest(i + 1)
        s.emit(stack)
        print("B: ops", len(s.ops), "waits", s.n_waits)
    return nc


def host_inputs_B(pbf_list, pf32_list):
    bf = ml_dtypes.bfloat16
    maps = []
    full = []
    for b in range(2):
        ka = np.zeros((8192, 512), bf)
        va = np.zeros((8192, 512), bf)
        ki = np.zeros((8192, 64), bf)
        for j in range(4):
            c = b * 4 + j
            for k in range(4):
                g = 4 * k + j
                blk = pbf_list[c][512 * k:512 * (k + 1)]
                ka[512 * g:512 * (g + 1)] = blk[:, 512:1024]
                va[512 * g:512 * (g + 1)] = blk[:, 1024:1536]
                ki[512 * g:512 * (g + 1)] = blk[:, 1792:1856]
        kTl = np.ascontiguousarray(ka.reshape(8192, 4, 2, 64).transpose(2, 3, 1, 0).reshape(128, 4, 8192))
        full.append((kTl, np.ascontiguousarray(va), np.ascontiguousarray(ki.T)))
    kpos = np.ascontiguousarray(np.broadcast_to(np.arange(2048, dtype=np.float32)[None, :], (128, 2048)))
    ident = np.eye(128).astype(bf)
    for c in range(8):
        b, j = divmod(c, 4)
        p = pbf_list[c]
        qa = p[:, 0:512].reshape(4, 512, 4, 2, 64)
        qTl = np.ascontiguousarray(qa.transpose(3, 4, 0, 2, 1).reshape(128, 4, 4, 512))
        qi = p[:, 1536:1792].reshape(4, 512, 4, 64)
        qiTl = np.ascontiguousarray(qi.transpose(3, 0, 2, 1))
        wi = np.ascontiguousarray(pf32_list[c][:, 0:4].reshape(16, 128, 4).transpose(1, 0, 2))
        qrel = np.zeros((128, 16), np.float32)
        for k in range(4):
            g = 4 * k + j
            for qt in range(4):
                qrel[:, 4 * k + qt] = 512 * g + 128 * qt + np.arange(128) - 2048 * k
        maps.append({"kT": full[b][0], "v": full[b][1], "kiT": full[b][2], "qT": qTl, "qiT": qiTl, "wi": wi, "qrel": qrel,
                     "kpos": kpos, "ident": ident})
    return maps


SEG = 2048
NSEG = 4
CPS = SEG // 64


def build_G():
    nc = bass.Bass("TRN2", target_bir_lowering=False)
    D = lambda name, shape, dt, kind="ExternalInput": nc.dram_tensor(name, shape, dt, kind=kind).ap()
    qT = D("qT", [32, 8192], F32)
    kT = D("kT", [32, 8192], F32)
    vtok = D("vtok", [64, 128, 64], F32)
    glT = D("glT", [16, 8192], F32)
    wg = D("wg", [16, 32], F32)
    bg = D("bg", [32, 1], F32)
    rT = D("rT", [64, 8192], F32)
    gng = D("gng", [64, 1], F32)
    resetm = D("resetm", [32, SEG], F32)
    tri = D("tri", [64, 64], F32)
    ident = D("ident", [32, 32], F32)
    obT = D("obT", [64, 8192], BF16, "ExternalOutput")

    with ExitStack() as stack:
        T = lambda name, shape, dt: stack.enter_context(nc.sbuf_tensor(name, shape, dt))
        P = lambda name, shape, dt: stack.enter_context(nc.psum_tensor(name, shape, dt))
        qs = T("qs", [32, SEG], F32)
        ks = T("ks", [32, SEG], F32)
        vs = T("vs", [64, CPS, 64], F32)
        gls = T("gls", [16, SEG], F32)
        rs_ = T("rs", [64, SEG], F32)
        wgt = T("wgt", [16, 32], F32)
        bgt = T("bgt", [32, 1], F32)
        nbg = T("nbg", [32, 1], F32)
        gnt = T("gnt", [64, 1], F32)
        rmt = T("rmt", [32, SEG], F32)
        trit = T("trit", [64, 64], F32)
        idt = T("idt", [32, 32], F32)
        ones = T("ones", [64, 64], F32)
        epst = T("epst", [64, 1], F32)
        t1 = T("t1", [32, SEG], F32)
        cum = T("cum", [32, SEG], F32)
        eb = T("eb", [32, SEG], F32)
        enb = T("enb", [32, SEG], F32)
        qt_ = T("qt", [32, SEG], F32)
        kt_ = T("kt", [32, SEG], F32)
        ktok = T("ktok", [64, 2, 32], F32)
        am = T("am", [64, 2, 64], F32)
        U = T("U", [32, 2, 64], F32)
        S = T("S", [32, 2, 64], F32)
        oTs = T("oTs", [64, SEG], F32)
        sqs = T("sqs", [64, SEG], F32)
        rsd = T("rsd", [64, 512], F32)
        sil = T("sil", [64, SEG], F32)
        outb = T("outb", [64, SEG], BF16)
        pz = [P("pz%d" % i, [64, 512], F32) for i in range(2)]
        pk = [P("pk%d" % i, [64, 512], F32) for i in range(2)]
        pt_ = [P("pt%d" % i, [64, 512], F32) for i in range(2)]
        po = P("po", [64, 512], F32)
        pu = P("pu", [64, 512], F32)

        s = Sched(nc)
        for (dst, src, nm) in ((wgt, wg, "wgt"), (bgt, bg, "bgt"), (gnt, gng, "gnt"), (rmt, resetm, "rmt"), (trit, tri, "trit"), (idt, ident, "idt")):
            s.dma(lambda e, dst=dst, src=src: e.dma_start(out=dst[:], in_=src), writes=[nm])
        s.dve(lambda e: e.memset(ones[:], 1.0), writes=["ones"])
        s.dve(lambda e: e.memset(epst[:], 1e-6), writes=["epst"])
        s.dve(lambda e: e.memset(S[:], 0.0), writes=[("S", 0), ("S", 1)])
        s.dve(lambda e: e.tensor_scalar(out=nbg[:], in0=bgt[:], scalar1=-1.0, scalar2=None, op0=ALU.mult), reads=["bgt"], writes=["nbg"])
        cc = 0
        for sgi in range(NSEG):
            c0 = sgi * SEG
            s.dma(lambda e, c0=c0: e.dma_start(out=qs[:], in_=qT[:, c0:c0 + SEG]), writes=["qs"])
            s.dma(lambda e, c0=c0: e.dma_start(out=ks[:], in_=kT[:, c0:c0 + SEG]), writes=["ks"])
            s.dma(lambda e, sgi=sgi: e.dma_start(out=vs[:], in_=vtok[:, sgi * CPS:(sgi + 1) * CPS, :]), writes=["vs"])
            s.dma(lambda e, c0=c0: e.dma_start(out=gls[:], in_=glT[:, c0:c0 + SEG]), writes=["gls"])
            s.dma(lambda e, c0=c0: e.dma_start(out=rs_[:], in_=rT[:, c0:c0 + SEG]), writes=["rs"])
            for pc in range(SEG // 512):
                pzb = pz[pc % 2]
                s.pe(lambda e, pzb=pzb, pc=pc: e.matmul(pzb[0:32, :], lhsT=wgt[:], rhs=gls[:, pc * 512:(pc + 1) * 512], start=True, stop=True),
                     reads=["wgt", "gls"], writes=[("pz", pc % 2)])
                s.act(lambda e, pzb=pzb, pc=pc: e.activation(out=t1[:, pc * 512:(pc + 1) * 512], in_=pzb[0:32, :], func=AF.Exp, scale=-1.0, bias=nbg[:, 0:1]),
                      reads=[("pz", pc % 2), "nbg"], writes=["t1"])
            s.act(lambda e: e.activation(out=t1[:], in_=t1[:], func=AF.Ln, bias=1.0), reads=["t1"], writes=["t1"])
            s.dve(lambda e: e.tensor_tensor_scan(out=cum[:], data0=rmt[:], data1=t1[:], initial=0.0, op0=ALU.mult, op1=ALU.add),
                  reads=["rmt", "t1"], writes=["cum"])
            s.act(lambda e: e.activation(out=eb[:], in_=cum[:], func=AF.Exp, scale=-1.0 / 16), reads=["cum"], writes=["eb"])
            s.act(lambda e: e.activation(out=enb[:], in_=cum[:], func=AF.Exp, scale=1.0 / 16), reads=["cum"], writes=["enb"])
            s.dve(lambda e: e.scalar_tensor_tensor(out=qt_[:], in0=qs[:], scalar=32.0 ** -0.5, in1=eb[:], op0=ALU.mult, op1=ALU.mult),
                  reads=["qs", "eb"], writes=["qt"])
            s.dve(lambda e: e.tensor_tensor(out=kt_[:], in0=ks[:], in1=enb[:], op=ALU.mult), reads=["ks", "enb"], writes=["kt"])
            s.act(lambda e: e.activation(out=sil[:], in_=rs_[:], func=AF.Silu), reads=["rs"], writes=["sil"])
            def first_half(c, p):
                cs = slice(c * 64, (c + 1) * 64)
                s.pe(lambda e, p=p, cs=cs: e.transpose(pk[p][:, 0:32], kt_[:, cs], idt[:]), reads=["kt", "idt"], writes=[("pk", p)])
                s.act(lambda e, p=p: e.activation(out=ktok[:, p, :], in_=pk[p][:, 0:32], func=AF.Copy), reads=[("pk", p)], writes=[("ktok", p)])
                s.pe(lambda e, p=p, cs=cs: e.matmul(pt_[p][:, 0:64], lhsT=kt_[:, cs], rhs=qt_[:, cs], start=True, stop=True),
                     reads=["kt", "qt"], writes=[("pt", p)])
                s.dve(lambda e, p=p: e.tensor_tensor(out=am[:, p, :], in0=pt_[p][:, 0:64], in1=trit[:], op=ALU.mult),
                      reads=[("pt", p), "trit"], writes=[("am", p)])

            def second_half(c, p):
                cs = slice(c * 64, (c + 1) * 64)
                ac = eb[:, c * 64 + 63:c * 64 + 64]
                sp, sn = p, 1 - p
                s.pe(lambda e, p=p, c=c: e.matmul(po[:, 0:64], lhsT=vs[:, c, :], rhs=am[:, p, :], start=True, stop=False),
                     reads=["vs", ("am", p)], writes=["po"])
                s.pe(lambda e, sp=sp, cs=cs: e.matmul(po[:, 0:64], lhsT=S[:, sp, :], rhs=qt_[:, cs], start=False, stop=True),
                     reads=[("S", sp), "qt"], writes=["po"])
                s.act(lambda e, cs=cs: e.activation(out=oTs[:, cs], in_=po[:, 0:64], func=AF.Copy), reads=["po"], writes=["oTs"])
                s.pe(lambda e, p=p, c=c: e.matmul(pu[0:32, 0:64], lhsT=ktok[:, p, :], rhs=vs[:, c, :], start=True, stop=True),
                     reads=[("ktok", p), "vs"], writes=["pu"])
                s.act(lambda e, p=p, ac=ac: e.activation(out=U[:, p, :], in_=pu[0:32, 0:64], func=AF.Copy, scale=ac), reads=["pu", "eb"], writes=[("U", p)])
                s.dve(lambda e, p=p, sp=sp, sn=sn, ac=ac: e.scalar_tensor_tensor(out=S[:, sn, :], in0=S[:, sp, :], scalar=ac, in1=U[:, p, :],
                                                                               op0=ALU.mult, op1=ALU.add),
                      reads=[("S", sp), ("U", p), "eb"], writes=[("S", sn)])

            first_half(0, cc % 2)
            for c in range(CPS):
                p = cc % 2
                cc += 1
                if c + 1 < CPS:
                    first_half(c + 1, cc % 2)
                second_half(c, p)
            s.act(lambda e: e.activation(out=sqs[:], in_=oTs[:], func=AF.Square), reads=["oTs"], writes=["sqs"])
            for pc in range(SEG // 512):
                pzb = pz[pc % 2]
                ps_ = slice(pc * 512, (pc + 1) * 512)
                s.pe(lambda e, pzb=pzb, ps_=ps_: e.matmul(pzb[:, :], lhsT=ones[:], rhs=sqs[:, ps_], start=True, stop=True),
                     reads=["ones", "sqs"], writes=[("pz", pc % 2)])
                s.act(lambda e, pzb=pzb: e.activation(out=rsd[:], in_=pzb[:, :], func=AF.Sqrt, scale=1.0 / 64, bias=epst[:, 0:1]),
                      reads=[("pz", pc % 2), "epst"], writes=["rsd"])
                s.dve(lambda e: e.reciprocal(out=rsd[:], in_=rsd[:]), reads=["rsd"], writes=["rsd"])
                s.dve(lambda e, ps_=ps_: e.tensor_tensor(out=oTs[:, ps_], in0=oTs[:, ps_], in1=rsd[:], op=ALU.mult), reads=["oTs", "rsd"], writes=["oTs"])
            s.dve(lambda e: e.scalar_tensor_tensor(out=outb[:], in0=oTs[:], scalar=gnt[:, 0:1], in1=sil[:], op0=ALU.mult, op1=ALU.mult),
                  reads=["oTs", "gnt", "sil"], writes=["outb"])
            s.dma(lambda e, c0=c0: e.dma_start(out=obT[:, c0:c0 + SEG], in_=outb[:]), reads=["outb"])
        s.emit(stack)
        print("G: ops", len(s.ops), "waits", s.n_waits)
    return nc


def host_inputs_G(pf32_full, w_gate_up, b_gate, gla_norm_g):
    maps = []
    rm = np.ones((32, SEG), np.float32)
    rm[:, ::64] = 0.0
    tri = np.triu(np.ones((64, 64), np.float32))
    for c in range(8):
        b, h = divmod(c, 4)
        p = pf32_full[b]
        maps.append({
            "qT": np.ascontiguousarray(p[:, 4 + 32 * h:4 + 32 * (h + 1)].T),
            "kT": np.ascontiguousarray(p[:, 132 + 32 * h:132 + 32 * (h + 1)].T),
            "vtok": np.ascontiguousarray(p[:, 260 + 64 * h:260 + 64 * (h + 1)].reshape(128, 64, 64).transpose(1, 0, 2)),
            "glT": np.ascontiguousarray(p[:, 772:788].T),
            "wg": np.ascontiguousarray(w_gate_up[:, 32 * h:32 * (h + 1)]),
            "bg": np.ascontiguousarray(b_gate[32 * h:32 * (h + 1)].reshape(32, 1)),
            "rT": np.ascontiguousarray(p[:, 516 + 64 * h:516 + 64 * (h + 1)].T),
            "gng": np.ascontiguousarray(gla_norm_g[h].reshape(64, 1)),
            "resetm": rm, "tri": tri, "ident": np.eye(32, dtype=np.float32),
        })
    return maps


DFF = 2816
NFC = 22
UW = 256
SLOTW = 2 + 512
NCOL = 4 * SLOTW


def build_F(final=False):
    nc = bass.Bass("TRN2", target_bir_lowering=False)
    D = lambda name, shape, dt, kind="ExternalInput": nc.dram_tensor(name, shape, dt, kind=kind).ap()
    mixT = D("mixT", [1024, NCOL], BF16)
    xT = D("xT", [1024, NCOL], F32)
    w_out = D("w_out", [1024, 1024], F32)
    w_up = D("w_up", [1024, 2 * DFF], F32)
    w_down = D("w_down", [DFF, 1024], F32)
    g2 = D("g2", [128, 8], F32)
    cw = D("cw", [128, NFC, 4], F32)
    hflag = D("hflag", [128, 4], F32)
    gf = D("gf", [128, 8], F32)
    xoT = D("xoT", [1024, 2048], F32, "ExternalOutput")

    with ExitStack() as stack:
        T = lambda name, shape, dt: stack.enter_context(nc.sbuf_tensor(name, shape, dt))
        P = lambda name, shape, dt: stack.enter_context(nc.psum_tensor(name, shape, dt))
        wo = T("wo", [128, 8, 1024], BF16)
        wu = T("wu", [128, 8, 2 * DFF], BF16)
        wd = T("wd", [128, NFC, 1024], BF16)
        wst = T("wst", [128, 2, 1024], F32)
        g2t = T("g2t", [128, 8], F32)
        gft = T("gft", [128, 8], F32)
        cwt = T("cwt", [128, NFC, 4], F32)
        hft = T("hft", [128, 4], F32)
        ones = T("ones", [128, 128], F32)
        epst = T("epst", [128, 1], F32)
        mx = T("mx", [128, 8, UW], BF16)
        xm = T("xm", [128, 8, UW], F32)
        sq = T("sq", [128, 2, UW], F32)
        rs = T("rs", [128, UW], F32)
        h2 = T("h2", [128, 8, UW], BF16)
        actT = T("actT", [128, NFC, UW], BF16)
        aext = T("aext", [128, 2, 2 + UW], F32)
        cv = T("cv", [128, 2, UW], F32)
        sg = T("sg", [128, 2, UW], F32)
        atail = T("atail", [128, NFC, 2], F32)
        pa = [P("pa%d" % i, [128, 512], F32) for i in range(8)]

        s = Sched(nc)
        for (dst, src, nm) in ((g2t, g2, "g2t"), (gft, gf, "gft"), (cwt, cw, "cwt"), (hft, hflag, "hft")):
            s.dma(lambda e, dst=dst, src=src: e.dma_start(out=dst[:], in_=src), writes=[nm])
        s.dve(lambda e: e.memset(ones[:], 1.0), writes=["ones"])
        s.dve(lambda e: e.memset(epst[:], 1e-6), writes=["epst"])
        wc = 0
        def load_w(dst_fn, src_fn, nrow_chunks, ncols, regname, scale_g):
            nonlocal wc
            for c in range(nrow_chunks):
                for c0 in range(0, ncols, 1024):
                    c1 = min(ncols, c0 + 1024)
                    b = wc % 2
                    wc += 1
                    s.dma(lambda e, b=b, c=c, c0=c0, c1=c1: e.dma_start(out=wst[:, b, 0:c1 - c0], in_=src_fn(c, c0, c1)), writes=[("wst", b)])
                    use_dve = (wc % 2 == 0)
                    if scale_g:
                        if use_dve:
                            s.dve(lambda e, b=b, c=c, c0=c0, c1=c1: e.tensor_scalar(out=dst_fn(c, c0, c1), in0=wst[:, b, 0:c1 - c0], scalar1=g2t[:, c:c + 1],
                                                                                  scalar2=None, op0=ALU.mult),
                                  reads=[("wst", b), "g2t"], writes=[(regname, c)])
                        else:
                            s.act(lambda e, b=b, c=c, c0=c0, c1=c1: e.activation(out=dst_fn(c, c0, c1), in_=wst[:, b, 0:c1 - c0], func=AF.Copy,
                                                                               scale=g2t[:, c:c + 1]),
                                  reads=[("wst", b), "g2t"], writes=[(regname, c)])
                    else:
                        if use_dve:
                            s.dve(lambda e, b=b, c=c, c0=c0, c1=c1: e.tensor_copy(out=dst_fn(c, c0, c1), in_=wst[:, b, 0:c1 - c0]),
                                  reads=[("wst", b)], writes=[(regname, c)])
                        else:
                            s.act(lambda e, b=b, c=c, c0=c0, c1=c1: e.activation(out=dst_fn(c, c0, c1), in_=wst[:, b, 0:c1 - c0], func=AF.Copy),
                                  reads=[("wst", b)], writes=[(regname, c)])
        load_w(lambda c, c0, c1: wo[:, c, c0:c1], lambda c, c0, c1: w_out[c * 128:(c + 1) * 128, c0:c1], 8, 1024, "wo", False)
        load_w(lambda c, c0, c1: wu[:, c, c0:c1], lambda c, c0, c1: w_up[c * 128:(c + 1) * 128, c0:c1], 8, 2 * DFF, "wu", True)
        load_w(lambda c, c0, c1: wd[:, c, c0:c1], lambda c, c0, c1: w_down[c * 128:(c + 1) * 128, c0:c1], NFC, 1024, "wd", False)
        WO = [("wo", c) for c in range(8)]
        WU = [("wu", c) for c in range(8)]
        WD = [("wd", c) for c in range(NFC)]

        def unit(k, col0, n, halo, out0):
            s.dma(lambda e: e.dma_start(out=mx[:, :, 0:n], in_=mixT[:, col0:col0 + n].rearrange("(c p) t -> p c t", p=128)), writes=["mx"])
            s.dma(lambda e: e.dma_start(out=xm[:, :, 0:n], in_=xT[:, col0:col0 + n].rearrange("(c p) t -> p c t", p=128)), writes=["xm"])
            for dc in range(8):
                pt = pa[dc % 2]
                for c in range(8):
                    s.pe(lambda e, pt=pt, c=c, dc=dc: e.matmul(pt[:, 0:n], lhsT=wo[:, c, dc * 128:(dc + 1) * 128], rhs=mx[:, c, 0:n],
                                                              start=(c == 0), stop=(c == 7)),
                         reads=WO + ["mx"], writes=[("pa", dc % 2)])
                s.dve(lambda e, pt=pt, dc=dc: e.tensor_tensor(out=xm[:, dc, 0:n], in0=pt[:, 0:n], in1=xm[:, dc, 0:n], op=ALU.add),
                      reads=[("pa", dc % 2), "xm"], writes=["xm"])
                s.act(lambda e, dc=dc: e.activation(out=sq[:, dc % 2, 0:n], in_=xm[:, dc, 0:n], func=AF.Square), reads=["xm"], writes=[("sq", dc % 2)])
                s.pe(lambda e, dc=dc: e.matmul(pa[2][:, 0:n], lhsT=ones[:], rhs=sq[:, dc % 2, 0:n], start=(dc == 0), stop=(dc == 7)),
                     reads=["ones", ("sq", dc % 2)], writes=[("pa", 2)])
            s.act(lambda e: e.activation(out=rs[:, 0:n], in_=pa[2][:, 0:n], func=AF.Sqrt, scale=1.0 / 1024, bias=epst[:, 0:1]),
                  reads=[("pa", 2), "epst"], writes=["rs"])
            s.dve(lambda e: e.reciprocal(out=rs[:, 0:n], in_=rs[:, 0:n]), reads=["rs"], writes=["rs"])
            for dc in range(8):
                s.dve(lambda e, dc=dc: e.tensor_tensor(out=h2[:, dc, 0:n], in0=xm[:, dc, 0:n], in1=rs[:, 0:n], op=ALU.mult),
                    reads=["xm", "rs"], writes=["h2"])
            for fc in range(NFC):
                ab = fc % 2
                pA = pa[3 + ab]
                pB = pa[5 + ab]
                for c in range(8):
                    s.pe(lambda e, pA=pA, c=c, fc=fc: e.matmul(pA[:, 0:n], lhsT=wu[:, c, fc * 128:(fc + 1) * 128], rhs=h2[:, c, 0:n],
                                                              start=(c == 0), stop=(c == 7)),
                         reads=WU + ["h2"], writes=[("pa", 3 + ab)])
                if halo:
                    s.dve(lambda e, pA=pA, fc=fc, k=k: e.tensor_scalar(out=atail[:, fc, :], in0=pA[:, 0:2], scalar1=hft[:, k:k + 1], scalar2=None,
                                                                     op0=ALU.mult),
                          reads=[("pa", 3 + ab), "hft"], writes=[("atail", fc)])
                    continue
                for c in range(8):
                    s.pe(lambda e, pB=pB, c=c, fc=fc: e.matmul(pB[:, 0:n], lhsT=wu[:, c, DFF + fc * 128:DFF + (fc + 1) * 128], rhs=h2[:, c, 0:n],
                                                              start=(c == 0), stop=(c == 7)),
                         reads=WU + ["h2"], writes=[("pa", 5 + ab)])
                s.act(lambda e, ab=ab, fc=fc: e.activation(out=aext[:, ab, 0:2], in_=atail[:, fc, :], func=AF.Copy),
                      reads=[("atail", fc)], writes=[("aext", ab)])
                s.act(lambda e, ab=ab, pA=pA: e.activation(out=aext[:, ab, 2:2 + n], in_=pA[:, 0:n], func=AF.Copy),
                      reads=[("pa", 3 + ab)], writes=[("aext", ab)])
                s.act(lambda e, ab=ab, fc=fc: e.activation(out=atail[:, fc, :], in_=aext[:, ab, n:n + 2], func=AF.Copy),
                      reads=[("aext", ab)], writes=[("atail", fc)])
                s.dve(lambda e, ab=ab, fc=fc: e.tensor_scalar(out=cv[:, ab, 0:n], in0=aext[:, ab, 2:2 + n], scalar1=cwt[:, fc, 2:3], scalar2=cwt[:, fc, 3:4],
                                                            op0=ALU.mult, op1=ALU.add),
                      reads=[("aext", ab), "cwt"], writes=[("cv", ab)])
                s.dve(lambda e, ab=ab, fc=fc: e.scalar_tensor_tensor(out=cv[:, ab, 0:n], in0=aext[:, ab, 1:1 + n], scalar=cwt[:, fc, 1:2], in1=cv[:, ab, 0:n],
                                                                   op0=ALU.mult, op1=ALU.add),
                      reads=[("aext", ab), "cwt", ("cv", ab)], writes=[("cv", ab)])
                s.dve(lambda e, ab=ab, fc=fc: e.scalar_tensor_tensor(out=cv[:, ab, 0:n], in0=aext[:, ab, 0:n], scalar=cwt[:, fc, 0:1], in1=cv[:, ab, 0:n],
                                                                   op0=ALU.mult, op1=ALU.add),
                      reads=[("aext", ab), "cwt", ("cv", ab)], writes=[("cv", ab)])
                s.act(lambda e, ab=ab: e.activation(out=sg[:, ab, 0:n], in_=cv[:, ab, 0:n], func=AF.Silu), reads=[("cv", ab)], writes=[("sg", ab)])
                s.dve(lambda e, ab=ab, fc=fc, pB=pB: e.tensor_tensor(out=actT[:, fc, 0:n], in0=pB[:, 0:n], in1=sg[:, ab, 0:n], op=ALU.mult),
                      reads=[("pa", 5 + ab), ("sg", ab)], writes=[("actT", fc)])
            if halo:
                return
            AT = [("actT", fc) for fc in range(NFC)]
            for dc in range(8):
                pt = pa[dc % 2]
                for fc in range(NFC):
                    s.pe(lambda e, pt=pt, fc=fc, dc=dc: e.matmul(pt[:, 0:n], lhsT=wd[:, fc, dc * 128:(dc + 1) * 128], rhs=actT[:, fc, 0:n],
                                                                start=(fc == 0), stop=(fc == NFC - 1)),
                         reads=WD + AT, writes=[("pa", dc % 2)])
                s.dve(lambda e, pt=pt, dc=dc: e.tensor_tensor(out=xm[:, dc, 0:n], in0=pt[:, 0:n], in1=xm[:, dc, 0:n], op=ALU.add),
                      reads=[("pa", dc % 2), "xm"], writes=["xm"])
            if final:
                for dc in range(8):
                    s.act(lambda e, dc=dc: e.activation(out=sq[:, dc % 2, 0:n], in_=xm[:, dc, 0:n], func=AF.Square), reads=["xm"], writes=[("sq", dc % 2)])
                    s.pe(lambda e, dc=dc: e.matmul(pa[2][:, 0:n], lhsT=ones[:], rhs=sq[:, dc % 2, 0:n], start=(dc == 0), stop=(dc == 7)),
                         reads=["ones", ("sq", dc % 2)], writes=[("pa", 2)])
                s.act(lambda e: e.activation(out=rs[:, 0:n], in_=pa[2][:, 0:n], func=AF.Sqrt, scale=1.0 / 1024, bias=epst[:, 0:1]),
                      reads=[("pa", 2), "epst"], writes=["rs"])
                s.dve(lambda e: e.reciprocal(out=rs[:, 0:n], in_=rs[:, 0:n]), reads=["rs"], writes=["rs"])
                for dc in range(8):
                    s.dve(lambda e, dc=dc: e.scalar_tensor_tensor(out=xm[:, dc, 0:n], in0=xm[:, dc, 0:n], scalar=gft[:, dc:dc + 1], in1=rs[:, 0:n],
                                                                op0=ALU.mult, op1=ALU.mult),
                          reads=["xm", "rs", "gft"], writes=["xm"])
            s.dma(lambda e: e.dma_start(out=xoT[:, out0:out0 + n].rearrange("(c p) t -> p c t", p=128), in_=xm[:, :, 0:n]), reads=["xm"])

        for k in range(4):
            unit(k, k * SLOTW, 2, True, None)
            for u in range(512 // UW):
                unit(k, k * SLOTW + 2 + u * UW, UW, False, k * 512 + u * UW)
        s.emit(stack)
        print("F: ops", len(s.ops), "waits", s.n_waits)
    return nc


def host_inputs_F(mix_list, x_full, w_out, norm2_g, w_up, conv_w, conv_b, w_down, final_g):
    bf = ml_dtypes.bfloat16
    maps = []
    cw = np.zeros((128, NFC, 4), np.float32)
    cw[:, :, 0:3] = conv_w.T.reshape(NFC, 128, 3).transpose(1, 0, 2)
    cw[:, :, 3] = conv_b.reshape(NFC, 128).T
    for c in range(8):
        b, j = divmod(c, 4)
        mcols, xcols = [], []
        hf = np.ones((128, 4), np.float32)
        for k in range(4):
            g = 4 * k + j
            t0 = 512 * g
            if g == 0:
                mcols.append(np.zeros((2, 1024), bf))
                xcols.append(np.zeros((2, 1024), np.float32))
                hf[:, k] = 0.0
            else:
                mcols.append(mix_list[b][t0 - 2:t0])
                xcols.append(x_full[b, t0 - 2:t0])
            mcols.append(mix_list[b][t0:t0 + 512])
            xcols.append(x_full[b, t0:t0 + 512])
        maps.append({
            "mixT": np.ascontiguousarray(np.concatenate(mcols, 0).T), "xT": np.ascontiguousarray(np.concatenate(xcols, 0).T),
            "w_out": np.ascontiguousarray(w_out), "w_up": np.ascontiguousarray(w_up), "w_down": np.ascontiguousarray(w_down),
            "g2": np.ascontiguousarray(norm2_g.reshape(8, 128).T), "cw": cw, "hflag": hf,
            "gf": np.ascontiguousarray(final_g.reshape(8, 128).T),
        })
    return maps


_CACHE = {}
CHECK = None


def _get(name, fn):
    if name not in _CACHE:
        _CACHE[name] = fn()
    return _CACHE[name]


def _run(nc, maps):
    res = run_bass_kernel_spmd(nc, maps, core_ids=list(range(8)))
    return res.results


def forward(x, positions, norm1_g, w_in, w_gate_up, b_gate, gla_norm_g, w_pool, pool_scale, w_out, norm2_g, w_up, conv_w, conv_b,
            w_down, final_norm_g):
    bf = ml_dtypes.bfloat16
    x = np.asarray(x, np.float32)
    positions = np.asarray(positions, np.int32)
    depth = norm1_g.shape[0]
    ncA = _get("A", build_A)
    ncB = _get("B", build_B)
    ncG = _get("G", build_G)
    for l in range(depth):
        last = (l == depth - 1)
        rA = _run(ncA, host_inputs_A(x, positions, np.asarray(norm1_g[l]), np.asarray(w_in[l]), np.asarray(w_pool[l]), np.asarray(pool_scale[l])))
        pbf = [np.asarray(r["pbf"]) for r in rA]
        pf32 = [np.asarray(r["pf32"]) for r in rA]
        ocT = [np.asarray(r["ocT"]) for r in rA]
        if CHECK:
            CHECK("A", l, dict(pbf=pbf, pf32=pf32, ocT=ocT))
        rB = _run(ncB, host_inputs_B(pbf, pf32))
        oaT = [np.asarray(r["oaT"]) for r in rB]
        if CHECK:
            CHECK("B", l, dict(oaT=oaT))
        pf_full = []
        for b in range(2):
            full = np.zeros((8192, pf32[0].shape[1]), np.float32)
            for j in range(4):
                for k in range(4):
                    g = 4 * k + j
                    full[512 * g:512 * (g + 1)] = pf32[b * 4 + j][512 * k:512 * (k + 1)]
            pf_full.append(full)
        rG = _run(ncG, host_inputs_G(pf_full, np.asarray(w_gate_up[l]), np.asarray(b_gate[l]), np.asarray(gla_norm_g[l])))
        obT = [np.asarray(r["obT"]) for r in rG]
        if CHECK:
            CHECK("G", l, dict(obT=obT))
        mix = []
        for b in range(2):
            m = np.zeros((8192, 1024), bf)
            for j in range(4):
                c = b * 4 + j
                oa = oaT[c].transpose(2, 1, 0).reshape(2048, 512)
                oc = ocT[c].transpose(2, 1, 0).reshape(2048, 256)
                for k in range(4):
                    g = 4 * k + j
                    m[512 * g:512 * (g + 1), 0:512] = oa[512 * k:512 * (k + 1)]
                    m[512 * g:512 * (g + 1), 768:1024] = oc[512 * k:512 * (k + 1)]
            for h in range(4):
                m[:, 512 + 64 * h:512 + 64 * (h + 1)] = obT[b * 4 + h].T
            mix.append(m)
        if CHECK:
            CHECK("mix", l, dict(mix=mix))
        ncF = _get("F%d" % int(last), lambda: build_F(final=last))
        rF = _run(ncF, host_inputs_F(mix, x, np.asarray(w_out[l]), np.asarray(norm2_g[l]), np.asarray(w_up[l]), np.asarray(conv_w[l]),
                                     np.asarray(conv_b[l]), np.asarray(w_down[l]), np.asarray(final_norm_g)))
        xn = np.zeros_like(x)
        for c in range(8):
            b, j = divmod(c, 4)
            xo = np.asarray(rF[c]["xoT"]).T
            for k in range(4):
                g = 4 * k + j
                xn[b, 512 * g:512 * (g + 1)] = xo[512 * k:512 * (k + 1)]
        x = xn
        if CHECK:
            CHECK("F", l, dict(x=x))
    return x


def kernel(**inputs):
    out = forward(**{k: np.asarray(v) for k, v in inputs.items()})
    return np.ascontiguousarray(out.astype(np.float32))
```

```python
import numpy as np
import concourse.bass as bass
import concourse.mybir as mybir
from concourse.bass_utils import run_bass_kernel_spmd
from contextlib import ExitStack
import math
import ml_dtypes

F32 = mybir.dt.float32
BF16 = mybir.dt.bfloat16
I32 = mybir.dt.int32
ALU = mybir.AluOpType
AF = mybir.ActivationFunctionType
AX = mybir.AxisListType

ENGS = ("pe", "act", "dve", "pool", "sp")


class _Op:
    __slots__ = ("eng", "fn", "reads", "writes", "dma", "deps", "sig", "sigval", "dsem", "idx")


class Sched:
    def __init__(self, nc, n_dma_sems=6):
        self.nc = nc
        self.ops = []
        self.last_w = {}
        self.readers = {}
        self.n_dma_sems = n_dma_sems

    def add(self, eng, fn, reads=(), writes=(), dma=False):
        op = _Op()
        op.eng = eng
        op.fn = fn
        op.reads = tuple(reads)
        op.writes = tuple(writes)
        op.dma = dma
        op.idx = len(self.ops)
        deps = set()
        for r in op.reads:
            w = self.last_w.get(r)
            if w is not None:
                deps.add(w)
        for r in op.writes:
            w = self.last_w.get(r)
            if w is not None:
                deps.add(w)
            for rd in self.readers.get(r, ()):
                deps.add(rd)
        deps.discard(op.idx)
        op.deps = deps
        for r in op.reads:
            self.readers.setdefault(r, []).append(op.idx)
        for r in op.writes:
            self.last_w[r] = op.idx
            self.readers[r] = []
        op.sig = False
        op.sigval = None
        op.dsem = None
        self.ops.append(op)
        return op

    def pe(self, fn, reads=(), writes=()):
        return self.add("pe", fn, reads, writes)

    def act(self, fn, reads=(), writes=()):
        return self.add("act", fn, reads, writes)

    def dve(self, fn, reads=(), writes=()):
        return self.add("dve", fn, reads, writes)

    def pool(self, fn, reads=(), writes=()):
        return self.add("pool", fn, reads, writes)

    def dma(self, fn, reads=(), writes=(), q="sp"):
        return self.add(q, fn, reads, writes, dma=True)

    def emit(self, stack):
        nc = self.nc
        ops = self.ops
        for op in ops:
            for d in op.deps:
                dop = ops[d]
                if dop.eng == op.eng and not dop.dma:
                    if not (set(dop.writes) & set(op.reads)):
                        continue
                dop.sig = True
        for op in ops:
            if op.dma:
                op.sig = True
        esem = {e: stack.enter_context(nc.semaphore("s_" + e)) for e in ENGS}
        dsems = {}
        for q in ENGS:
            if any(o.dma and o.eng == q for o in ops):
                dsems[q] = [stack.enter_context(nc.semaphore("d_%s%d" % (q, i))) for i in range(self.n_dma_sems)]
        ecount = {e: 0 for e in ENGS}
        dcount = {q: [0] * self.n_dma_sems for q in dsems}
        drr = {q: 0 for q in dsems}
        prev_dma_wait = {}
        for op in ops:
            if op.dma:
                k = drr[op.eng]
                drr[op.eng] = (k + 1) % self.n_dma_sems
                prev_dma_wait[op.idx] = dcount[op.eng][k]
                dcount[op.eng][k] += 16
                op.dsem = (op.eng, k)
                op.sigval = dcount[op.eng][k]
            elif op.sig:
                ecount[op.eng] += 1
                op.sigval = ecount[op.eng]
        by_eng = {e: [o for o in ops if o.eng == e] for e in ENGS}
        block = stack.enter_context(nc.Block())
        self.n_waits = 0

        def run(ename, eobj):
            waited = {}
            for op in by_eng[ename]:
                need = {}
                for d in op.deps:
                    dop = ops[d]
                    if dop.dma:
                        key = ("d",) + dop.dsem
                        sem = dsems[dop.dsem[0]][dop.dsem[1]]
                    else:
                        if dop.eng == ename and not (set(dop.writes) & set(op.reads)):
                            continue
                        key = ("e", dop.eng)
                        sem = esem[dop.eng]
                    v = dop.sigval
                    if waited.get(key, 0) >= v:
                        continue
                    if key not in need or need[key][1] < v:
                        need[key] = (sem, v)
                if op.dma:
                    pv = prev_dma_wait[op.idx]
                    key = ("d",) + op.dsem
                    if pv > 0 and waited.get(key, 0) < pv:
                        if key not in need or need[key][1] < pv:
                            need[key] = (dsems[op.dsem[0]][op.dsem[1]], pv)
                for key, (sem, v) in need.items():
                    eobj.wait_ge(sem, v)
                    waited[key] = v
                    self.n_waits += 1
                ins = op.fn(eobj)
                if op.dma:
                    ins.then_inc(dsems[op.dsem[0]][op.dsem[1]], 16)
                elif op.sig:
                    ins.then_inc(esem[ename], 1)
            for q, lst in dsems.items():
                if q == ename:
                    for k, s in enumerate(lst):
                        if dcount[q][k] > 0:
                            eobj.wait_ge(s, dcount[q][k])

        if by_eng["pe"]:
            @block.tensor
            def _(e):
                run("pe", e)
        if by_eng["act"]:
            @block.scalar
            def _(e):
                run("act", e)
        if by_eng["dve"]:
            @block.vector
            def _(e):
                run("dve", e)
        if by_eng["pool"]:
            @block.gpsimd
            def _(e):
                run("pool", e)
        if by_eng["sp"]:
            @block.sync
            def _(e):
                run("sp", e)


NTILE = 20
INW = 2900
CH = [(0, 512), (512, 1024), (1024, 1536), (1536, 2048), (2048, 2560), (2560, 2900)]
UC0 = 2644
BFW = 1856
F32W = INW - BFW
TWO_PI = 2.0 * math.pi


def pool_mats(first):
    Mc = np.zeros((128, 4, 128), np.float32)
    Mh = np.zeros((128, 4, 128), np.float32)
    for gi, w in enumerate((2, 4, 8, 16)):
        for t in range(128):
            cnt = min(t + 1, w) if first else w
            for s in range(t - w + 1, t + 1):
                if s >= 0:
                    Mc[s, gi, t] += 1.0 / cnt
                elif not first:
                    Mh[128 + s, gi, t] += 1.0 / cnt
            Mc[t, gi, t] -= 1.0
    return Mc, Mh


def build_A():
    nc = bass.Bass("TRN2", target_bir_lowering=False)
    D = lambda name, shape, dt, kind="ExternalInput": nc.dram_tensor(name, shape, dt, kind=kind).ap()
    xtok = D("xtok", [NTILE * 128, 1024], F32)
    xT = D("xT", [1024, NTILE * 128], F32)
    w_in = D("w_in", [1024, INW], F32)
    g1 = D("g1", [128, 8], F32)
    pos = D("pos", [128, NTILE], I32)
    inv = D("inv", [128, 8], F32)
    wpool = D("wpool", [64, 4, 64], F32)
    pscale = D("pscale", [64, 4], F32)
    mcur = D("mcur", [128, 5, 4, 128], F32)
    mhal = D("mhal", [128, 5, 4, 128], F32)
    pbf = D("pbf", [2048, BFW], BF16, "ExternalOutput")
    pf32 = D("pf32", [2048, F32W], F32, "ExternalOutput")
    ocT = D("ocT", [64, 4, 2048], BF16, "ExternalOutput")

    with ExitStack() as stack:
        T = lambda name, shape, dt: stack.enter_context(nc.sbuf_tensor(name, shape, dt))
        P = lambda name, shape, dt: stack.enter_context(nc.psum_tensor(name, shape, dt))
        wbf = T("wbf", [128, 8, INW], BF16)
        wst = T("wst", [128, 2, INW], F32)
        g1t = T("g1t", [128, 8], F32)
        posi = T("posi", [128, NTILE], I32)
        posf = T("posf", [128, NTILE], F32)
        invt = T("invt", [128, 8], F32)
        ang = T("ang", [128, NTILE, 8], F32)
        kq = T("kq", [128, NTILE, 8], F32)
        angc = T("angc", [128, NTILE, 8], F32)
        kqi = T("kqi", [128, NTILE, 8], I32)
        cost = T("cost", [128, NTILE, 8], F32)
        sint = T("sint", [128, NTILE, 8], F32)
        wpt = T("wpt", [64, 4, 64], F32)
        pst = T("pst", [64, 4], F32)
        mct = T("mct", [128, 5, 4, 128], F32)
        mht = T("mht", [128, 5, 4, 128], F32)
        xtk = T("xtk", [128, 2, 1024], F32)
        sqj = T("sqj", [128, 1024], BF16)
        ss = T("ss", [128, 2], F32)
        rstd = T("rstd", [128, 2], F32)
        epst = T("epst", [128, 1], F32)
        xTt = T("xTt", [128, 2, 8, 128], F32)
        hT = T("hT", [128, 2, 8, 128], BF16)
        proj = T("proj", [128, 2, INW], F32)
        pb16 = T("pb16", [128, 2, BFW], BF16)
        rt = T("rt", [128, 4, 16, 8], F32)
        pooled = T("pooled", [64, 2, 4, 128], F32)
        oct_ = T("oct", [64, 2, 4, 128], BF16)
        ps = [P("ps%d" % i, [128, 512], F32) for i in range(4)]
        pp = [P("pp%d" % i, [64, 4, 128], F32) for i in range(2)]
        py = [P("py%d" % i, [64, 4, 128], F32) for i in range(2)]

        s = Sched(nc)
        s.dma(lambda e: e.dma_start(out=g1t[:], in_=g1), writes=["g1t"])
        s.dma(lambda e: e.dma_start(out=posi[:], in_=pos), writes=["posi"])
        s.dma(lambda e: e.dma_start(out=invt[:], in_=inv), writes=["invt"])
        s.dma(lambda e: e.dma_start(out=wpt[:], in_=wpool), writes=["wpt"])
        s.dma(lambda e: e.dma_start(out=pst[:], in_=pscale), writes=["pst"])
        s.dma(lambda e: e.dma_start(out=mct[:], in_=mcur), writes=["mct"])
        s.dma(lambda e: e.dma_start(out=mht[:], in_=mhal), writes=["mht"])
        s.dve(lambda e: e.memset(epst[:], 1e-6), writes=["epst"])
        s.dve(lambda e: e.tensor_copy(out=posf[:], in_=posi[:]), reads=["posi"], writes=["posf"])
        for t in range(NTILE):
            s.dve(lambda e, t=t: e.tensor_scalar(out=ang[:, t, :], in0=invt[:], scalar1=posf[:, t:t + 1], scalar2=None, op0=ALU.mult),
                  reads=["invt", "posf"], writes=["ang"])
        s.dve(lambda e: e.tensor_scalar(out=kqi[:], in0=ang[:], scalar1=1.0 / TWO_PI, scalar2=None, op0=ALU.mult), reads=["ang"], writes=["kqi"])
        s.dve(lambda e: e.tensor_copy(out=kq[:], in_=kqi[:]), reads=["kqi"], writes=["kq"])
        s.dve(lambda e: e.scalar_tensor_tensor(out=ang[:], in0=kq[:], scalar=-TWO_PI, in1=ang[:], op0=ALU.mult, op1=ALU.add),
              reads=["kq", "ang"], writes=["ang"])
        def wrap(y, name):
            s.dve(lambda e: e.tensor_scalar(out=kq[:], in0=y[:], scalar1=math.pi, scalar2=-TWO_PI, op0=ALU.is_gt, op1=ALU.mult),
                  reads=[name], writes=["kq"])
            s.dve(lambda e: e.tensor_tensor(out=y[:], in0=y[:], in1=kq[:], op=ALU.add), reads=[name, "kq"], writes=[name])
            s.dve(lambda e: e.tensor_scalar(out=kq[:], in0=y[:], scalar1=-math.pi, scalar2=TWO_PI, op0=ALU.is_lt, op1=ALU.mult),
                  reads=[name], writes=["kq"])
            s.dve(lambda e: e.tensor_tensor(out=y[:], in0=y[:], in1=kq[:], op=ALU.add), reads=[name, "kq"], writes=[name])
        s.dve(lambda e: e.tensor_scalar(out=angc[:], in0=ang[:], scalar1=math.pi / 2, scalar2=None, op0=ALU.add), reads=["ang"], writes=["angc"])
        wrap(ang, "ang")
        wrap(angc, "angc")
        s.act(lambda e: e.activation(out=sint[:], in_=ang[:], func=AF.Sin), reads=["ang"], writes=["sint"])
        s.act(lambda e: e.activation(out=cost[:], in_=angc[:], func=AF.Sin), reads=["angc"], writes=["cost"])

        for c in range(8):
            b = c % 2
            s.dma(lambda e, c=c, b=b: e.dma_start(out=wst[:, b, :], in_=w_in[c * 128:(c + 1) * 128, :]), writes=[("wst", b)])
            if c % 2 == 0:
                s.dve(lambda e, c=c, b=b: e.tensor_scalar(out=wbf[:, c, :], in0=wst[:, b, :], scalar1=g1t[:, c:c + 1], scalar2=None, op0=ALU.mult),
                      reads=[("wst", b), "g1t"], writes=[("wbf", c)])
            else:
                s.act(lambda e, c=c, b=b: e.activation(out=wbf[:, c, :], in_=wst[:, b, :], func=AF.Copy, scale=g1t[:, c:c + 1]),
                      reads=[("wst", b), "g1t"], writes=[("wbf", c)])

        own = 0
        for t in range(NTILE):
            k, i = divmod(t, 5)
            halo = (i == 0)
            b = t % 2
            pb = (t - 1) % 2
            s.dma(lambda e, t=t, b=b: e.dma_start(out=xtk[:, b, :], in_=xtok[t * 128:(t + 1) * 128, :]), writes=[("xtk", b)])
            s.dma(lambda e, t=t, b=b: e.dma_start(out=xTt[:, b, :, :], in_=xT[:, t * 128:(t + 1) * 128].rearrange("(c p) t -> p c t", p=128)),
                  writes=[("xTt", b)])
            s.act(lambda e, b=b: e.activation(out=sqj[:], in_=xtk[:, b, :], func=AF.Square, accum_out=ss[:, b:b + 1]),
                  reads=[("xtk", b)], writes=["sqj", ("ss", b)])
            s.act(lambda e, b=b: e.activation(out=ss[:, b:b + 1], in_=ss[:, b:b + 1], func=AF.Sqrt, scale=1.0 / 1024, bias=epst[:, 0:1]),
                  reads=[("ss", b), "epst"], writes=[("ss", b)])
            s.dve(lambda e, b=b: e.reciprocal(out=rstd[:, b:b + 1], in_=ss[:, b:b + 1]), reads=[("ss", b)], writes=[("rstd", b)])
            s.pool(lambda e, b=b: e.tensor_copy(out=hT[:, b, :, :], in_=xTt[:, b, :, :]), reads=[("xTt", b)], writes=[("hT", b)])
            chunks = [5] if halo else list(range(6))
            for n in chunks:
                n0, n1 = CH[n]
                pt = ps[n % 4]
                for c in range(8):
                    s.pe(lambda e, pt=pt, b=b, c=c, n0=n0, n1=n1: e.matmul(pt[:, 0:n1 - n0], lhsT=hT[:, b, c, :], rhs=wbf[:, c, n0:n1],
                                                                         start=(c == 0), stop=(c == 7)),
                         reads=[("hT", b), ("wbf", c)], writes=[("ps", n % 4)])
                if n % 2 == 0:
                    s.act(lambda e, pt=pt, b=b, n0=n0, n1=n1: e.activation(out=proj[:, b, n0:n1], in_=pt[:, 0:n1 - n0], func=AF.Copy,
                                                                         scale=rstd[:, b:b + 1]),
                          reads=[("ps", n % 4), ("rstd", b)], writes=[("proj", b, n)])
                else:
                    s.dve(lambda e, pt=pt, b=b, n0=n0, n1=n1: e.tensor_scalar(out=proj[:, b, n0:n1], in0=pt[:, 0:n1 - n0],
                                                                            scalar1=rstd[:, b:b + 1], scalar2=None, op0=ALU.mult),
                          reads=[("ps", n % 4), ("rstd", b)], writes=[("proj", b, n)])
            if not halo:
                for (c0, nh, regs) in ((0, 16, [("proj", b, 0), ("proj", b, 1)]), (1536, 5, [("proj", b, 3)])):
                    v = proj[:, b, c0:c0 + nh * 64].rearrange("p (h d) -> p h d", d=64)
                    x1 = v[:, :, 0:8]
                    x2 = v[:, :, 8:16]
                    cb = cost[:, t, :].unsqueeze(1).to_broadcast([128, nh, 8])
                    sb = sint[:, t, :].unsqueeze(1).to_broadcast([128, nh, 8])
                    t0 = rt[:, 0, 0:nh, :]
                    t1 = rt[:, 1, 0:nh, :]
                    t2 = rt[:, 2, 0:nh, :]
                    t3 = rt[:, 3, 0:nh, :]
                    R = regs + ["cost", "sint"]
                    s.dve(lambda e, t0=t0, x1=x1, cb=cb: e.tensor_tensor(out=t0, in0=x1, in1=cb, op=ALU.mult), reads=R, writes=["rt0"])
                    s.dve(lambda e, t1=t1, x2=x2, sb=sb: e.tensor_tensor(out=t1, in0=x2, in1=sb, op=ALU.mult), reads=R, writes=["rt1"])
                    s.dve(lambda e, t2=t2, x2=x2, cb=cb: e.tensor_tensor(out=t2, in0=x2, in1=cb, op=ALU.mult), reads=R, writes=["rt2"])
                    s.dve(lambda e, t3=t3, x1=x1, sb=sb: e.tensor_tensor(out=t3, in0=x1, in1=sb, op=ALU.mult), reads=R, writes=["rt3"])
                    s.dve(lambda e, t0=t0, t1=t1, x1=x1: e.tensor_tensor(out=x1, in0=t0, in1=t1, op=ALU.subtract),
                          reads=["rt0", "rt1", "rt2", "rt3"], writes=regs)
                    s.dve(lambda e, t2=t2, t3=t3, x2=x2: e.tensor_tensor(out=x2, in0=t2, in1=t3, op=ALU.add),
                          reads=["rt0", "rt1", "rt2", "rt3"], writes=regs)
                r0 = own * 128
                s.act(lambda e, b=b: e.activation(out=pb16[:, b, :], in_=proj[:, b, 0:BFW], func=AF.Copy),
                      reads=[("proj", b, n) for n in range(4)], writes=[("pb16", b)])
                s.dma(lambda e, b=b, r0=r0: e.dma_start(out=pbf[r0:r0 + 128, :], in_=pb16[:, b, :]), reads=[("pb16", b)])
                s.dma(lambda e, b=b, r0=r0: e.dma_start(out=pf32[r0:r0 + 128, :], in_=proj[:, b, BFW:INW]),
                      reads=[("proj", b, n) for n in (3, 4, 5)])
                mi = 0 if i > 1 else 1 + k
                ob = own % 2
                for gi in range(4):
                    s.pe(lambda e, ob=ob, b=b, gi=gi, mi=mi: e.matmul(pp[ob][:, gi, :], lhsT=proj[:, b, UC0 + gi * 64:UC0 + (gi + 1) * 64],
                                                                  rhs=mct[:, mi, gi, :], start=True, stop=False),
                         reads=[("proj", b, 5), "mct"], writes=[("pp", ob)])
                    s.pe(lambda e, ob=ob, pb=pb, gi=gi, mi=mi: e.matmul(pp[ob][:, gi, :], lhsT=proj[:, pb, UC0 + gi * 64:UC0 + (gi + 1) * 64],
                                                                    rhs=mht[:, mi, gi, :], start=False, stop=True),
                         reads=[("proj", pb, 5), "mht"], writes=[("pp", ob)])
                s.act(lambda e, ob=ob: e.activation(out=pooled[:, ob, :, :], in_=pp[ob][:], func=AF.Copy),
                      reads=[("pp", ob)], writes=[("pooled", ob)])
                for gi in range(4):
                    s.pe(lambda e, ob=ob, gi=gi: e.matmul(py[ob][:, gi, :], lhsT=wpt[:, gi, :], rhs=pooled[:, ob, gi, :], start=True, stop=True),
                         reads=[("pooled", ob), "wpt"], writes=[("py", ob)])
                for gi in range(4):
                    s.dve(lambda e, ob=ob, gi=gi: e.tensor_scalar(out=oct_[:, ob, gi, :], in0=py[ob][:, gi, :], scalar1=pst[:, gi:gi + 1],
                                                                scalar2=None, op0=ALU.mult),
                          reads=[("py", ob), "pst"], writes=[("oct", ob)])
                s.dma(lambda e, ob=ob, r0=r0: e.dma_start(out=ocT[:, :, r0:r0 + 128], in_=oct_[:, ob, :, :]), reads=[("oct", ob)])
                own += 1
        s.emit(stack)
        print("A: ops", len(s.ops), "waits", s.n_waits)
    return nc


def host_inputs_A(x, positions, norm1_g, w_in, w_pool, pool_scale):
    inv = (500000.0 ** (-np.arange(0, 16, 2, dtype=np.float32) / 16)).astype(np.float32)
    McG, MhG = pool_mats(False)
    McF, MhF = pool_mats(True)
    maps = []
    for c in range(8):
        b, j = divmod(c, 4)
        rows = []
        posl = []
        mcur = np.zeros((128, 5, 4, 128), np.float32)
        mhal = np.zeros((128, 5, 4, 128), np.float32)
        mcur[:, 0], mhal[:, 0] = McG, MhG
        for k in range(4):
            g = 4 * k + j
            t0 = 512 * g
            if g == 0:
                rows.append(np.zeros((128, 1024), np.float32))
                posl.append(np.zeros((128,), np.int32))
                mcur[:, 1 + k], mhal[:, 1 + k] = McF, MhF
            else:
                rows.append(x[b, t0 - 128:t0])
                posl.append(positions[b, t0 - 128:t0])
                mcur[:, 1 + k], mhal[:, 1 + k] = McG, MhG
            rows.append(x[b, t0:t0 + 512])
            posl.append(positions[b, t0:t0 + 512])
        xt = np.ascontiguousarray(np.concatenate(rows, 0))
        pl = np.concatenate(posl, 0).astype(np.int32)
        maps.append({
            "xtok": xt, "xT": np.ascontiguousarray(xt.T), "w_in": np.ascontiguousarray(w_in),
            "g1": np.ascontiguousarray(norm1_g.reshape(8, 128).T),
            "pos": np.ascontiguousarray(pl.reshape(NTILE, 128).T),
            "inv": np.ascontiguousarray(np.broadcast_to(inv[None, :], (128, 8))),
            "wpool": np.ascontiguousarray(w_pool.transpose(1, 0, 2)),
            "pscale": np.ascontiguousarray(pool_scale.reshape(4, 64).T),
            "mcur": mcur, "mhal": mhal,
        })
    return maps


NIT = 16
NEG = -1.0e30


def build_B(nslot=4, nit=NIT):
    nc = bass.Bass("TRN2", target_bir_lowering=False)
    D = lambda name, shape, dt, kind="ExternalInput": nc.dram_tensor(name, shape, dt, kind=kind).ap()
    kT = D("kT", [128, 4, 8192], BF16)
    vv = D("v", [8192, 512], BF16)
    kiT = D("kiT", [64, 8192], BF16)
    qT = D("qT", [128, 4, 4, 512], BF16)
    qiT = D("qiT", [64, 4, 4, 512], BF16)
    wi = D("wi", [128, 16, 4], F32)
    qrel = D("qrel", [128, 16], F32)
    kpos = D("kpos", [128, 2048], F32)
    ident = D("ident", [128, 128], BF16)
    oaT = D("oaT", [64, 8, 2048], BF16, "ExternalOutput")

    with ExitStack() as stack:
        T = lambda name, shape, dt: stack.enter_context(nc.sbuf_tensor(name, shape, dt))
        P = lambda name, shape, dt: stack.enter_context(nc.psum_tensor(name, shape, dt))
        kit = T("kit", [64, 8192], BF16)
        sc = T("sc", [128, 8192], F32)
        mk = T("mk", [128, 4, 8192], BF16)
        kpt = T("kpt", [128, 2048], F32)
        rr = T("rr", [128, 4, 512], F32)
        qTt = T("qTt", [128, 1, 4, 512], BF16)
        qit = T("qit", [64, 1, 4, 512], BF16)
        wit = T("wit", [128, 16, 4], F32)
        qrt = T("qrt", [128, 16], F32)
        idt = T("idt", [128, 128], BF16)
        kTs = T("kTs", [128, 2, 4, 512], BF16)
        vraw = T("vraw", [128, 2, 4, 512], BF16)
        vt = T("vt", [128, 2, 4, 520], BF16)
        E = T("E", [128, 3, 512], BF16)
        Pm = T("Pm", [128, 3, 512], BF16)
        mT = T("mT", [128, 2, 512], BF16)
        sm = T("sm", [128, 8], F32)
        ones1 = T("ones1", [128, 64], F32)
        rec = T("rec", [128, 512], F32)
        bcs = T("bcs", [64, 512], F32)
        oT = T("oT", [64, 2, 512], BF16)
        pb = [P("pb%d" % i, [128, 512], F32) for i in range(8)]
        pTb = pb[2][:].bitcast(BF16)

        s = Sched(nc)
        s.dma(lambda e: e.dma_start(out=kit[:], in_=kiT), writes=["kit"])
        s.dma(lambda e: e.dma_start(out=wit[:], in_=wi), writes=["wit"])
        s.dma(lambda e: e.dma_start(out=qrt[:], in_=qrel), writes=["qrt"])
        s.dma(lambda e: e.dma_start(out=kpt[:], in_=kpos), writes=["kpt"])
        s.dma(lambda e: e.dma_start(out=idt[:], in_=ident), writes=["idt"])
        s.pool(lambda e: e.memset(ones1[:], 1.0), writes=["ones1"])
        s.dve(lambda e: e.memset(vt[:], 1.0), writes=[("vt", 0), ("vt", 1)])
        cbias = rr[:].rearrange("p h n -> p (h n)")
        RRALL = [("rr", h) for h in range(4)]

        kbc = 0
        ec = 0
        mtc = 0
        otc = 0
        for k in range(nslot):
            L = 2048 * (k + 1)
            nkc = L // 512
            nkb = L // 128
            qb = 0
            s.dma(lambda e, k=k, qb=qb: e.dma_start(out=qTt[:, qb, :, :], in_=qT[:, k, :, :]), writes=[("qTt", qb)])
            s.dma(lambda e, k=k, qb=qb: e.dma_start(out=qit[:, qb, :, :], in_=qiT[:, k, :, :]), writes=[("qit", qb)])
            for qt in range(4):
                g = 4 * k + qt
                for n in range(nkc):
                    for h in range(4):
                        s.pe(lambda e, h=h, qb=qb, qt=qt, n=n: e.matmul(pb[h][:], lhsT=qit[:, qb, h, qt * 128:(qt + 1) * 128],
                                                                     rhs=kit[:, n * 512:(n + 1) * 512], start=True, stop=True),
                             reads=[("qit", qb), "kit"], writes=[("pb", h)])
                        s.act(lambda e, h=h: e.activation(out=rr[:, h, :], in_=pb[h][:], func=AF.Relu), reads=[("pb", h)], writes=[("rr", h)])
                        if h == 0:
                            s.dve(lambda e, n=n, g=g: e.tensor_scalar(out=sc[:, n * 512:(n + 1) * 512], in0=rr[:, 0, :], scalar1=wit[:, g, 0:1],
                                                                    scalar2=None, op0=ALU.mult),
                                  reads=[("rr", 0), "wit"], writes=[("sc", n)])
                        else:
                            s.dve(lambda e, n=n, g=g, h=h: e.scalar_tensor_tensor(out=sc[:, n * 512:(n + 1) * 512], in0=rr[:, h, :],
                                                                                 scalar=wit[:, g, h:h + 1], in1=sc[:, n * 512:(n + 1) * 512],
                                                                                 op0=ALU.mult, op1=ALU.add),
                                  reads=[("rr", h), "wit", ("sc", n)], writes=[("sc", n)])
                allsc = [("sc", n) for n in range(nkc)]
                s.dve(lambda e, L=L: e.tensor_reduce(out=sm[:, 5:6], in_=sc[:, 0:L], axis=AX.X, op=ALU.max, apply_absolute_value=True),
                      reads=allsc, writes=["rmax"])
                s.dve(lambda e: e.tensor_scalar(out=sm[:, 0:1], in0=sm[:, 5:6], scalar1=-1.0, scalar2=None, op0=ALU.mult), reads=["rmax"], writes=["lo"])
                s.dve(lambda e: e.tensor_scalar(out=sm[:, 1:2], in0=sm[:, 5:6], scalar1=2.0, scalar2=None, op0=ALU.mult), reads=["rmax"], writes=["range"])
                s.dve(lambda e, g=g: e.tensor_scalar(out=cbias[:], in0=kpt[:], scalar1=qrt[:, g:g + 1], scalar2=NEG, op0=ALU.is_gt, op1=ALU.mult),
                      reads=["kpt", "qrt"], writes=RRALL)
                s.dve(lambda e, L=L: e.tensor_tensor(out=sc[:, L - 2048:L], in0=sc[:, L - 2048:L], in1=cbias[:], op=ALU.add),
                      reads=allsc + RRALL, writes=allsc)
                Lh = max(512, int(round(0.4 * L / 512.0)) * 512)
                nact = float(L - Lh)
                MKA, MKB = ("mk", qt, "a"), ("mk", qt, "b")
                for it in range(1, nit + 1):
                    f = 2.0 ** (-it)
                    s.dve(lambda e, f=f: e.tensor_scalar(out=sm[:, 2:3], in0=sm[:, 1:2], scalar1=f, scalar2=sm[:, 0:1], op0=ALU.mult, op1=ALU.add),
                          reads=["range", "lo"], writes=["mid"])
                    s.dve(lambda e, Lh=Lh, qt=qt: e.tensor_scalar(out=mk[:, qt, 0:Lh], in0=sc[:, 0:Lh], scalar1=sm[:, 2:3], scalar2=None,
                                                                op0=ALU.is_ge, op1=ALU.add, accum_out=sm[:, 3:4]),
                          reads=allsc + ["mid"], writes=[MKA, "cnt"])
                    s.act(lambda e, Lh=Lh, L=L, qt=qt: e.activation(out=mk[:, qt, Lh:L], in_=sc[:, Lh:L], func=AF.Sign, scale=-1.0, bias=sm[:, 2:3],
                                                                   accum_out=sm[:, 6:7]),
                          reads=allsc + ["mid"], writes=[MKB, "sgn"])
                    s.dve(lambda e: e.scalar_tensor_tensor(out=sm[:, 7:8], in0=sm[:, 3:4], scalar=2.0, in1=sm[:, 6:7], op0=ALU.mult, op1=ALU.subtract),
                          reads=["cnt", "sgn"], writes=["tt"])
                    s.dve(lambda e, f=f, nact=nact: e.tensor_scalar(out=sm[:, 4:5], in0=sm[:, 7:8], scalar1=511.0 - nact, scalar2=f, op0=ALU.is_ge, op1=ALU.mult),
                          reads=["tt"], writes=["pred"])
                    s.dve(lambda e: e.scalar_tensor_tensor(out=sm[:, 0:1], in0=sm[:, 4:5], scalar=sm[:, 1:2], in1=sm[:, 0:1], op0=ALU.mult, op1=ALU.add),
                          reads=["pred", "range", "lo"], writes=["lo"])
                s.dve(lambda e, L=L, qt=qt: e.tensor_scalar(out=mk[:, qt, 0:L], in0=sc[:, 0:L], scalar1=sm[:, 0:1], scalar2=None, op0=ALU.is_ge),
                      reads=allsc + ["lo"], writes=[MKA, MKB])
            steps = [(hp, kb, hl) for hp in range(2) for kb in range(nkb) for hl in range(4)]
            nst = len(steps)
            info = {}

            def load_sb(hp, sbk):
                nonlocal kbc
                kbuf = kbc % 2
                kbc += 1
                info[("kbuf", hp, sbk)] = kbuf
                s.dma(lambda e, sbk=sbk, kbuf=kbuf: e.dma_start(out=kTs[:, kbuf, :, :], in_=kT[:, :, sbk * 512:(sbk + 1) * 512]),
                      writes=[("kTs", kbuf)])
                s.dma(lambda e, sbk=sbk, kbuf=kbuf: e.dma_start(out=vraw[:, kbuf, :, :], in_=vv[sbk * 512:(sbk + 1) * 512, :].rearrange("(kb p) c -> p kb c", p=128)),
                      writes=[("vraw", kbuf)])
                for kl_ in range(4):
                    s.dve(lambda e, kbuf=kbuf, kl_=kl_: e.tensor_copy(out=vt[:, kbuf, kl_, :].rearrange("p (h c) -> p h c", c=65)[:, :, 0:64],
                                                                   in_=vraw[:, kbuf, kl_, :].rearrange("p (h d) -> p h d", d=64)),
                          reads=[("vraw", kbuf)], writes=[("vt", kbuf)])

            def pre(hp, kb):
                nonlocal mtc
                sbk, kl = divmod(kb, 4)
                mb = mtc % 2
                mtc += 1
                info[("mb", hp, kb)] = mb
                for qt in range(4):
                    s.pe(lambda e, qt=qt, kb=kb: e.transpose(pTb[:, qt * 128:(qt + 1) * 128], mk[:, qt, kb * 128:(kb + 1) * 128], idt[:]),
                         reads=[("mk", qt, "a"), ("mk", qt, "b"), "idt"], writes=[("pb", 2)])
                s.act(lambda e, mb=mb: e.activation(out=mT[:, mb, :], in_=pTb[:, 0:512], func=AF.Copy), reads=[("pb", 2)], writes=[("mT", mb)])

            def ST(i):
                hp, kb, hl = steps[i]
                sbk, kl = divmod(kb, 4)
                kbuf = info[("kbuf", hp, sbk)]
                h = hp * 4 + hl
                pr, hh = divmod(h, 2)
                sb = i % 2
                s.pe(lambda e, sb=sb, kbuf=kbuf, pr=pr, hh=hh, kl=kl: e.matmul(pb[sb][:], lhsT=kTs[hh * 64:(hh + 1) * 64, kbuf, pr, kl * 128:(kl + 1) * 128],
                                                                            rhs=qTt[hh * 64:(hh + 1) * 64, 0, pr, :], start=True, stop=True),
                     reads=[("kTs", kbuf), ("qTt", 0)], writes=[("pb", sb)])

            def rest_a(i):
                sb = i % 2
                eb = i % 3
                s.act(lambda e, sb=sb, eb=eb: e.activation(out=E[:, eb, :], in_=pb[sb][:], func=AF.Exp, scale=0.125),
                      reads=[("pb", sb)], writes=[("E", eb)])

            def rest(i):
                nonlocal otc
                hp, kb, hl = steps[i]
                sbk, kl = divmod(kb, 4)
                kbuf = info[("kbuf", hp, sbk)]
                mb = info[("mb", hp, kb)]
                h = hp * 4 + hl
                sb = i % 2
                eb = i % 3
                eng = s.dve if (i % 2 == 0) else s.pool
                eng(lambda e, eb=eb, mb=mb: e.tensor_tensor(out=Pm[:, eb, :], in0=E[:, eb, :], in1=mT[:, mb, :], op=ALU.mult),
                    reads=[("E", eb), ("mT", mb)], writes=[("Pm", eb)])
                s.pe(lambda e, hl=hl, kbuf=kbuf, h=h, eb=eb, kb=kb, kl=kl: e.matmul(pb[4 + hl][0:65, :], lhsT=vt[:, kbuf, kl, h * 65:(h + 1) * 65], rhs=Pm[:, eb, :],
                                                                                 start=(kb == 0), stop=(kb == nkb - 1)),
                     reads=[("vt", kbuf), ("Pm", eb)], writes=[("pb", 4 + hl)])
                if kb == nkb - 1:
                    ob = otc % 2
                    otc += 1
                    s.dve(lambda e, hl=hl: e.reciprocal(out=rec[64:65, :], in_=pb[4 + hl][64:65, :]), reads=[("pb", 4 + hl)], writes=["rec"])
                    s.pe(lambda e: e.matmul(pb[3][0:64, :], lhsT=ones1[64:65, :], rhs=rec[64:65, :], start=True, stop=True),
                         reads=["ones1", "rec"], writes=[("pb", 3)])
                    s.act(lambda e: e.activation(out=bcs[:], in_=pb[3][0:64, :], func=AF.Copy), reads=[("pb", 3)], writes=["bcs"])
                    s.dve(lambda e, hl=hl, ob=ob: e.tensor_tensor(out=oT[:, ob, :], in0=pb[4 + hl][0:64, :], in1=bcs[:], op=ALU.mult),
                          reads=[("pb", 4 + hl), "bcs"], writes=[("oT", ob)])
                    s.dma(lambda e, h=h, k=k, ob=ob: e.dma_start(out=oaT[:, h, k * 512:(k + 1) * 512], in_=oT[:, ob, :]), reads=[("oT", ob)])

            DPIPE = 2
            order = [(hp_, sb_) for hp_ in range(2) for sb_ in range(nkb // 4)]
            load_sb(*order[0])
            for j in range(min(DPIPE, nst)):
                if steps[j][2] == 0:
                    pre(steps[j][0], steps[j][1])
                ST(j)
            for i in range(0, nst, 2):
                if steps[i][2] == 0 and steps[i][1] % 4 == 0:
                    oi = order.index((steps[i][0], steps[i][1] // 4))
                    if oi + 1 < len(order):
                        load_sb(*order[oi + 1])
                rest_a(i)
                rest_a(i + 1)
                for j in (i + DPIPE, i + DPIPE + 1):
                    if j < nst:
                        if steps[j][2] == 0:
                            pre(steps[j][0], steps[j][1])
                        ST(j)
                rest(i)
                rest(i + 1)
        s.emit(stack)
        print("B: ops", len(s.ops), "waits", s.n_waits)
    return nc


def host_inputs_B(pbf_list, pf32_list):
    bf = ml_dtypes.bfloat16
    maps = []
    full = []
    for b in range(2):
        ka = np.zeros((8192, 512), bf)
        va = np.zeros((8192, 512), bf)
        ki = np.zeros((8192, 64), bf)
        for j in range(4):
            c = b * 4 + j
            for k in range(4):
                g = 4 * k + j
                blk = pbf_list[c][512 * k:512 * (k + 1)]
                ka[512 * g:512 * (g + 1)] = blk[:, 512:1024]
                va[512 * g:512 * (g + 1)] = blk[:, 1024:1536]
                ki[512 * g:512 * (g + 1)] = blk[:, 1792:1856]
        kTl = np.ascontiguousarray(ka.reshape(8192, 4, 2, 64).transpose(2, 3, 1, 0).reshape(128, 4, 8192))
        full.append((kTl, np.ascontiguousarray(va), np.ascontiguousarray(ki.T)))
    kpos = np.ascontiguousarray(np.broadcast_to(np.arange(2048, dtype=np.float32)[None, :], (128, 2048)))
    ident = np.eye(128).astype(bf)
    for c in range(8):
        b, j = divmod(c, 4)
        p = pbf_list[c]
        qa = p[:, 0:512].reshape(4, 512, 4, 2, 64)
        qTl = np.ascontiguousarray(qa.transpose(3, 4, 0, 2, 1).reshape(128, 4, 4, 512))
        qi = p[:, 1536:1792].reshape(4, 512, 4, 64)
        qiTl = np.ascontiguousarray(qi.transpose(3, 0, 2, 1))
        wi = np.ascontiguousarray(pf32_list[c][:, 0:4].reshape(16, 128, 4).transpose(1, 0, 2))
        qrel = np.zeros((128, 16), np.float32)
        for k in range(4):
            g = 4 * k + j
            for qt in range(4):
                qrel[:, 4 * k + qt] = 512 * g + 128 * qt + np.arange(128) - 2048 * k
        maps.append({"kT": full[b][0], "v": full[b][1], "kiT": full[b][2], "qT": qTl, "qiT": qiTl, "wi": wi, "qrel": qrel,
                     "kpos": kpos, "ident": ident})
    return maps


SEG = 2048
NSEG = 4
CPS = SEG // 64


def build_G():
    nc = bass.Bass("TRN2", target_bir_lowering=False)
    D = lambda name, shape, dt, kind="ExternalInput": nc.dram_tensor(name, shape, dt, kind=kind).ap()
    qT = D("qT", [32, 8192], F32)
    kT = D("kT", [32, 8192], F32)
    vtok = D("vtok", [64, 128, 64], F32)
    glT = D("glT", [16, 8192], F32)
    wg = D("wg", [16, 32], F32)
    bg = D("bg", [32, 1], F32)
    rT = D("rT", [64, 8192], F32)
    gng = D("gng", [64, 1], F32)
    resetm = D("resetm", [32, SEG], F32)
    tri = D("tri", [64, 64], F32)
    ident = D("ident", [32, 32], F32)
    obT = D("obT", [64, 8192], BF16, "ExternalOutput")

    with ExitStack() as stack:
        T = lambda name, shape, dt: stack.enter_context(nc.sbuf_tensor(name, shape, dt))
        P = lambda name, shape, dt: stack.enter_context(nc.psum_tensor(name, shape, dt))
        qs = T("qs", [32, SEG], F32)
        ks = T("ks", [32, SEG], F32)
        vs = T("vs", [64, CPS, 64], F32)
        gls = T("gls", [16, SEG], F32)
        rs_ = T("rs", [64, SEG], F32)
        wgt = T("wgt", [16, 32], F32)
        bgt = T("bgt", [32, 1], F32)
        nbg = T("nbg", [32, 1], F32)
        gnt = T("gnt", [64, 1], F32)
        rmt = T("rmt", [32, SEG], F32)
        trit = T("trit", [64, 64], F32)
        idt = T("idt", [32, 32], F32)
        ones = T("ones", [64, 64], F32)
        epst = T("epst", [64, 1], F32)
        t1 = T("t1", [32, SEG], F32)
        cum = T("cum", [32, SEG], F32)
        eb = T("eb", [32, SEG], F32)
        enb = T("enb", [32, SEG], F32)
        qt_ = T("qt", [32, SEG], F32)
        kt_ = T("kt", [32, SEG], F32)
        ktok = T("ktok", [64, 2, 32], F32)
        am = T("am", [64, 2, 64], F32)
        U = T("U", [32, 2, 64], F32)
        S = T("S", [32, 4, 64], F32)
        oTs = T("oTs", [64, SEG], F32)
        sqs = T("sqs", [64, SEG], F32)
        rsd = T("rsd", [64, 512], F32)
        sil = T("sil", [64, SEG], F32)
        outb = T("outb", [64, SEG], BF16)
        pz = [P("pz%d" % i, [64, 512], F32) for i in range(2)]
        pk = [P("pk%d" % i, [64, 512], F32) for i in range(2)]
        pt_ = [P("pt%d" % i, [64, 512], F32) for i in range(2)]
        po = P("po", [64, 512], F32)
        pu = P("pu", [64, 512], F32)

        s = Sched(nc)
        for (dst, src, nm) in ((wgt, wg, "wgt"), (bgt, bg, "bgt"), (gnt, gng, "gnt"), (rmt, resetm, "rmt"), (trit, tri, "trit"), (idt, ident, "idt")):
            s.dma(lambda e, dst=dst, src=src: e.dma_start(out=dst[:], in_=src), writes=[nm])
        s.dve(lambda e: e.memset(ones[:], 1.0), writes=["ones"])
        s.dve(lambda e: e.memset(epst[:], 1e-6), writes=["epst"])
        s.dve(lambda e: e.memset(S[:], 0.0), writes=[("S", 0), ("S", 1), ("S", 2), ("S", 3)])
        s.dve(lambda e: e.tensor_scalar(out=nbg[:], in0=bgt[:], scalar1=-1.0, scalar2=None, op0=ALU.mult), reads=["bgt"], writes=["nbg"])
        cc = 0
        for sgi in range(NSEG):
            c0 = sgi * SEG
            s.dma(lambda e, c0=c0: e.dma_start(out=qs[:], in_=qT[:, c0:c0 + SEG]), writes=["qs"])
            s.dma(lambda e, c0=c0: e.dma_start(out=ks[:], in_=kT[:, c0:c0 + SEG]), writes=["ks"])
            s.dma(lambda e, sgi=sgi: e.dma_start(out=vs[:], in_=vtok[:, sgi * CPS:(sgi + 1) * CPS, :]), writes=["vs"])
            s.dma(lambda e, c0=c0: e.dma_start(out=gls[:], in_=glT[:, c0:c0 + SEG]), writes=["gls"])
            s.dma(lambda e, c0=c0: e.dma_start(out=rs_[:], in_=rT[:, c0:c0 + SEG]), writes=["rs"])
            for pc in range(SEG // 512):
                pzb = pz[pc % 2]
                s.pe(lambda e, pzb=pzb, pc=pc: e.matmul(pzb[0:32, :], lhsT=wgt[:], rhs=gls[:, pc * 512:(pc + 1) * 512], start=True, stop=True),
                     reads=["wgt", "gls"], writes=[("pz", pc % 2)])
                s.act(lambda e, pzb=pzb, pc=pc: e.activation(out=t1[:, pc * 512:(pc + 1) * 512], in_=pzb[0:32, :], func=AF.Exp, scale=-1.0, bias=nbg[:, 0:1]),
                      reads=[("pz", pc % 2), "nbg"], writes=["t1"])
            s.act(lambda e: e.activation(out=t1[:], in_=t1[:], func=AF.Ln, bias=1.0), reads=["t1"], writes=["t1"])
            s.dve(lambda e: e.tensor_tensor_scan(out=cum[:], data0=rmt[:], data1=t1[:], initial=0.0, op0=ALU.mult, op1=ALU.add),
                  reads=["rmt", "t1"], writes=["cum"])
            s.act(lambda e: e.activation(out=eb[:], in_=cum[:], func=AF.Exp, scale=-1.0 / 16), reads=["cum"], writes=["eb"])
            s.act(lambda e: e.activation(out=enb[:], in_=cum[:], func=AF.Exp, scale=1.0 / 16), reads=["cum"], writes=["enb"])
            s.dve(lambda e: e.scalar_tensor_tensor(out=qt_[:], in0=qs[:], scalar=32.0 ** -0.5, in1=eb[:], op0=ALU.mult, op1=ALU.mult),
                  reads=["qs", "eb"], writes=["qt"])
            s.dve(lambda e: e.tensor_tensor(out=kt_[:], in0=ks[:], in1=enb[:], op=ALU.mult), reads=["ks", "enb"], writes=["kt"])
            s.act(lambda e: e.activation(out=sil[:], in_=rs_[:], func=AF.Silu), reads=["rs"], writes=["sil"])
            def first_half(c, p):
                cs = slice(c * 64, (c + 1) * 64)
                ac = eb[:, c * 64 + 63:c * 64 + 64]
                s.pe(lambda e, p=p, cs=cs: e.transpose(pk[p][:, 0:32], kt_[:, cs], idt[:]), reads=["kt", "idt"], writes=[("pk", p)])
                s.act(lambda e, p=p: e.activation(out=ktok[:, p, :], in_=pk[p][:, 0:32], func=AF.Copy), reads=[("pk", p)], writes=[("ktok", p)])
                s.pe(lambda e, p=p, cs=cs: e.matmul(pt_[p][:, 0:64], lhsT=kt_[:, cs], rhs=qt_[:, cs], start=True, stop=True),
                     reads=["kt", "qt"], writes=[("pt", p)])
                s.dve(lambda e, p=p: e.tensor_tensor(out=am[:, p, :], in0=pt_[p][:, 0:64], in1=trit[:], op=ALU.mult),
                      reads=[("pt", p), "trit"], writes=[("am", p)])
                s.pe(lambda e, p=p, c=c: e.matmul(pu[0:32, 0:64], lhsT=ktok[:, p, :], rhs=vs[:, c, :], start=True, stop=True),
                     reads=[("ktok", p), "vs"], writes=["pu"])
                s.act(lambda e, p=p, ac=ac: e.activation(out=U[:, p, :], in_=pu[0:32, 0:64], func=AF.Copy, scale=ac), reads=["pu", "eb"], writes=[("U", p)])

            def second_half(c, p, sp, sn):
                cs = slice(c * 64, (c + 1) * 64)
                ac = eb[:, c * 64 + 63:c * 64 + 64]
                s.dve(lambda e, p=p, sp=sp, sn=sn, ac=ac: e.scalar_tensor_tensor(out=S[:, sn, :], in0=S[:, sp, :], scalar=ac, in1=U[:, p, :],
                                                                               op0=ALU.mult, op1=ALU.add),
                      reads=[("S", sp), ("U", p), "eb"], writes=[("S", sn)])
                s.pe(lambda e, p=p, c=c: e.matmul(po[:, 0:64], lhsT=vs[:, c, :], rhs=am[:, p, :], start=True, stop=False),
                     reads=["vs", ("am", p)], writes=["po"])
                s.pe(lambda e, sp=sp, cs=cs: e.matmul(po[:, 0:64], lhsT=S[:, sp, :], rhs=qt_[:, cs], start=False, stop=True),
                     reads=[("S", sp), "qt"], writes=["po"])
                s.act(lambda e, cs=cs: e.activation(out=oTs[:, cs], in_=po[:, 0:64], func=AF.Copy), reads=["po"], writes=["oTs"])

            first_half(0, cc % 2)
            for c in range(CPS):
                p = cc % 2
                sp, sn = cc % 4, (cc + 1) % 4
                cc += 1
                if c + 1 < CPS:
                    first_half(c + 1, cc % 2)
                second_half(c, p, sp, sn)
            s.act(lambda e: e.activation(out=sqs[:], in_=oTs[:], func=AF.Square), reads=["oTs"], writes=["sqs"])
            for pc in range(SEG // 512):
                pzb = pz[pc % 2]
                ps_ = slice(pc * 512, (pc + 1) * 512)
                s.pe(lambda e, pzb=pzb, ps_=ps_: e.matmul(pzb[:, :], lhsT=ones[:], rhs=sqs[:, ps_], start=True, stop=True),
                     reads=["ones", "sqs"], writes=[("pz", pc % 2)])
                s.act(lambda e, pzb=pzb: e.activation(out=rsd[:], in_=pzb[:, :], func=AF.Sqrt, scale=1.0 / 64, bias=epst[:, 0:1]),
                      reads=[("pz", pc % 2), "epst"], writes=["rsd"])
                s.dve(lambda e: e.reciprocal(out=rsd[:], in_=rsd[:]), reads=["rsd"], writes=["rsd"])
                s.dve(lambda e, ps_=ps_: e.tensor_tensor(out=oTs[:, ps_], in0=oTs[:, ps_], in1=rsd[:], op=ALU.mult), reads=["oTs", "rsd"], writes=["oTs"])
            s.dve(lambda e: e.scalar_tensor_tensor(out=outb[:], in0=oTs[:], scalar=gnt[:, 0:1], in1=sil[:], op0=ALU.mult, op1=ALU.mult),
                  reads=["oTs", "gnt", "sil"], writes=["outb"])
            s.dma(lambda e, c0=c0: e.dma_start(out=obT[:, c0:c0 + SEG], in_=outb[:]), reads=["outb"])
        s.emit(stack)
        print("G: ops", len(s.ops), "waits", s.n_waits)
    return nc


def host_inputs_G(pf32_full, w_gate_up, b_gate, gla_norm_g):
    maps = []
    rm = np.ones((32, SEG), np.float32)
    rm[:, ::64] = 0.0
    tri = np.triu(np.ones((64, 64), np.float32))
    for c in range(8):
        b, h = divmod(c, 4)
        p = pf32_full[b]
        maps.append({
            "qT": np.ascontiguousarray(p[:, 4 + 32 * h:4 + 32 * (h + 1)].T),
            "kT": np.ascontiguousarray(p[:, 132 + 32 * h:132 + 32 * (h + 1)].T),
            "vtok": np.ascontiguousarray(p[:, 260 + 64 * h:260 + 64 * (h + 1)].reshape(128, 64, 64).transpose(1, 0, 2)),
            "glT": np.ascontiguousarray(p[:, 772:788].T),
            "wg": np.ascontiguousarray(w_gate_up[:, 32 * h:32 * (h + 1)]),
            "bg": np.ascontiguousarray(b_gate[32 * h:32 * (h + 1)].reshape(32, 1)),
            "rT": np.ascontiguousarray(p[:, 516 + 64 * h:516 + 64 * (h + 1)].T),
            "gng": np.ascontiguousarray(gla_norm_g[h].reshape(64, 1)),
            "resetm": rm, "tri": tri, "ident": np.eye(32, dtype=np.float32),
        })
    return maps


DFF = 2816
NFC = 22
UW = 256
SLOTW = 2 + 512
NCOL = 4 * SLOTW


def build_F(final=False):
    nc = bass.Bass("TRN2", target_bir_lowering=False)
    D = lambda name, shape, dt, kind="ExternalInput": nc.dram_tensor(name, shape, dt, kind=kind).ap()
    mixT = D("mixT", [1024, NCOL], BF16)
    xT = D("xT", [1024, NCOL], F32)
    w_out = D("w_out", [1024, 1024], F32)
    w_up = D("w_up", [1024, 2 * DFF], F32)
    w_down = D("w_down", [DFF, 1024], F32)
    g2 = D("g2", [128, 8], F32)
    cw = D("cw", [128, NFC, 4], F32)
    hflag = D("hflag", [128, 4], F32)
    gf = D("gf", [128, 8], F32)
    xoT = D("xoT", [1024, 2048], F32, "ExternalOutput")

    with ExitStack() as stack:
        T = lambda name, shape, dt: stack.enter_context(nc.sbuf_tensor(name, shape, dt))
        P = lambda name, shape, dt: stack.enter_context(nc.psum_tensor(name, shape, dt))
        wo = T("wo", [128, 8, 1024], BF16)
        wu = T("wu", [128, 8, 2 * DFF], BF16)
        wd = T("wd", [128, NFC, 1024], BF16)
        wst = T("wst", [128, 2, 1024], F32)
        g2t = T("g2t", [128, 8], F32)
        gft = T("gft", [128, 8], F32)
        cwt = T("cwt", [128, NFC, 4], F32)
        hft = T("hft", [128, 4], F32)
        ones = T("ones", [128, 128], F32)
        epst = T("epst", [128, 1], F32)
        mx = T("mx", [128, 8, UW], BF16)
        xm = T("xm", [128, 8, UW], F32)
        sq = T("sq", [128, 2, UW], F32)
        rs = T("rs", [128, UW], F32)
        h2 = T("h2", [128, 8, UW], BF16)
        actT = T("actT", [128, NFC, UW], BF16)
        aext = T("aext", [128, 2, 2 + UW], F32)
        cv = T("cv", [128, 2, UW], F32)
        sg = T("sg", [128, 2, UW], F32)
        atail = T("atail", [128, NFC, 2], F32)
        pa = [P("pa%d" % i, [128, 512], F32) for i in range(8)]

        s = Sched(nc)
        for (dst, src, nm) in ((g2t, g2, "g2t"), (gft, gf, "gft"), (cwt, cw, "cwt"), (hft, hflag, "hft")):
            s.dma(lambda e, dst=dst, src=src: e.dma_start(out=dst[:], in_=src), writes=[nm])
        s.dve(lambda e: e.memset(ones[:], 1.0), writes=["ones"])
        s.dve(lambda e: e.memset(epst[:], 1e-6), writes=["epst"])
        wc = 0
        def load_w(dst_fn, src_fn, nrow_chunks, ncols, regname, scale_g):
            nonlocal wc
            for c in range(nrow_chunks):
                for c0 in range(0, ncols, 1024):
                    c1 = min(ncols, c0 + 1024)
                    b = wc % 2
                    wc += 1
                    s.dma(lambda e, b=b, c=c, c0=c0, c1=c1: e.dma_start(out=wst[:, b, 0:c1 - c0], in_=src_fn(c, c0, c1)), writes=[("wst", b)])
                    use_dve = (wc % 2 == 0)
                    if scale_g:
                        if use_dve:
                            s.dve(lambda e, b=b, c=c, c0=c0, c1=c1: e.tensor_scalar(out=dst_fn(c, c0, c1), in0=wst[:, b, 0:c1 - c0], scalar1=g2t[:, c:c + 1],
                                                                                  scalar2=None, op0=ALU.mult),
                                  reads=[("wst", b), "g2t"], writes=[(regname, c)])
                        else:
                            s.act(lambda e, b=b, c=c, c0=c0, c1=c1: e.activation(out=dst_fn(c, c0, c1), in_=wst[:, b, 0:c1 - c0], func=AF.Copy,
                                                                               scale=g2t[:, c:c + 1]),
                                  reads=[("wst", b), "g2t"], writes=[(regname, c)])
                    else:
                        if use_dve:
                            s.dve(lambda e, b=b, c=c, c0=c0, c1=c1: e.tensor_copy(out=dst_fn(c, c0, c1), in_=wst[:, b, 0:c1 - c0]),
                                  reads=[("wst", b)], writes=[(regname, c)])
                        else:
                            s.act(lambda e, b=b, c=c, c0=c0, c1=c1: e.activation(out=dst_fn(c, c0, c1), in_=wst[:, b, 0:c1 - c0], func=AF.Copy),
                                  reads=[("wst", b)], writes=[(regname, c)])
        load_w(lambda c, c0, c1: wo[:, c, c0:c1], lambda c, c0, c1: w_out[c * 128:(c + 1) * 128, c0:c1], 8, 1024, "wo", False)
        load_w(lambda c, c0, c1: wu[:, c, c0:c1], lambda c, c0, c1: w_up[c * 128:(c + 1) * 128, c0:c1], 8, 2 * DFF, "wu", True)
        load_w(lambda c, c0, c1: wd[:, c, c0:c1], lambda c, c0, c1: w_down[c * 128:(c + 1) * 128, c0:c1], NFC, 1024, "wd", False)
        WO = [("wo", c) for c in range(8)]
        WU = [("wu", c) for c in range(8)]
        WD = [("wd", c) for c in range(NFC)]

        def unit(k, col0, n, halo, out0):
            s.dma(lambda e: e.dma_start(out=mx[:, :, 0:n], in_=mixT[:, col0:col0 + n].rearrange("(c p) t -> p c t", p=128)), writes=["mx"])
            s.dma(lambda e: e.dma_start(out=xm[:, :, 0:n], in_=xT[:, col0:col0 + n].rearrange("(c p) t -> p c t", p=128)), writes=["xm"])
            for dc in range(8):
                pt = pa[dc % 2]
                for c in range(8):
                    s.pe(lambda e, pt=pt, c=c, dc=dc: e.matmul(pt[:, 0:n], lhsT=wo[:, c, dc * 128:(dc + 1) * 128], rhs=mx[:, c, 0:n],
                                                              start=(c == 0), stop=(c == 7)),
                         reads=WO + ["mx"], writes=[("pa", dc % 2)])
                s.dve(lambda e, pt=pt, dc=dc: e.tensor_tensor(out=xm[:, dc, 0:n], in0=pt[:, 0:n], in1=xm[:, dc, 0:n], op=ALU.add),
                      reads=[("pa", dc % 2), "xm"], writes=["xm"])
                s.act(lambda e, dc=dc: e.activation(out=sq[:, dc % 2, 0:n], in_=xm[:, dc, 0:n], func=AF.Square), reads=["xm"], writes=[("sq", dc % 2)])
                s.pe(lambda e, dc=dc: e.matmul(pa[2][:, 0:n], lhsT=ones[:], rhs=sq[:, dc % 2, 0:n], start=(dc == 0), stop=(dc == 7)),
                     reads=["ones", ("sq", dc % 2)], writes=[("pa", 2)])
            s.act(lambda e: e.activation(out=rs[:, 0:n], in_=pa[2][:, 0:n], func=AF.Sqrt, scale=1.0 / 1024, bias=epst[:, 0:1]),
                  reads=[("pa", 2), "epst"], writes=["rs"])
            s.dve(lambda e: e.reciprocal(out=rs[:, 0:n], in_=rs[:, 0:n]), reads=["rs"], writes=["rs"])
            for dc in range(8):
                s.dve(lambda e, dc=dc: e.tensor_tensor(out=h2[:, dc, 0:n], in0=xm[:, dc, 0:n], in1=rs[:, 0:n], op=ALU.mult),
                    reads=["xm", "rs"], writes=["h2"])
            for fc in range(NFC):
                ab = fc % 2
                pA = pa[3 + ab]
                pB = pa[5 + ab]
                for c in range(8):
                    s.pe(lambda e, pA=pA, c=c, fc=fc: e.matmul(pA[:, 0:n], lhsT=wu[:, c, fc * 128:(fc + 1) * 128], rhs=h2[:, c, 0:n],
                                                              start=(c == 0), stop=(c == 7)),
                         reads=WU + ["h2"], writes=[("pa", 3 + ab)])
                if halo:
                    s.dve(lambda e, pA=pA, fc=fc, k=k: e.tensor_scalar(out=atail[:, fc, :], in0=pA[:, 0:2], scalar1=hft[:, k:k + 1], scalar2=None,
                                                                     op0=ALU.mult),
                          reads=[("pa", 3 + ab), "hft"], writes=[("atail", fc)])
                    continue
                for c in range(8):
                    s.pe(lambda e, pB=pB, c=c, fc=fc: e.matmul(pB[:, 0:n], lhsT=wu[:, c, DFF + fc * 128:DFF + (fc + 1) * 128], rhs=h2[:, c, 0:n],
                                                              start=(c == 0), stop=(c == 7)),
                         reads=WU + ["h2"], writes=[("pa", 5 + ab)])
                s.act(lambda e, ab=ab, fc=fc: e.activation(out=aext[:, ab, 0:2], in_=atail[:, fc, :], func=AF.Copy),
                      reads=[("atail", fc)], writes=[("aext", ab)])
                s.act(lambda e, ab=ab, pA=pA: e.activation(out=aext[:, ab, 2:2 + n], in_=pA[:, 0:n], func=AF.Copy),
                      reads=[("pa", 3 + ab)], writes=[("aext", ab)])
                s.act(lambda e, ab=ab, fc=fc: e.activation(out=atail[:, fc, :], in_=aext[:, ab, n:n + 2], func=AF.Copy),
                      reads=[("aext", ab)], writes=[("atail", fc)])
                s.dve(lambda e, ab=ab, fc=fc: e.tensor_scalar(out=cv[:, ab, 0:n], in0=aext[:, ab, 2:2 + n], scalar1=cwt[:, fc, 2:3], scalar2=cwt[:, fc, 3:4],
                                                            op0=ALU.mult, op1=ALU.add),
                      reads=[("aext", ab), "cwt"], writes=[("cv", ab)])
                s.dve(lambda e, ab=ab, fc=fc: e.scalar_tensor_tensor(out=cv[:, ab, 0:n], in0=aext[:, ab, 1:1 + n], scalar=cwt[:, fc, 1:2], in1=cv[:, ab, 0:n],
                                                                   op0=ALU.mult, op1=ALU.add),
                      reads=[("aext", ab), "cwt", ("cv", ab)], writes=[("cv", ab)])
                s.dve(lambda e, ab=ab, fc=fc: e.scalar_tensor_tensor(out=cv[:, ab, 0:n], in0=aext[:, ab, 0:n], scalar=cwt[:, fc, 0:1], in1=cv[:, ab, 0:n],
                                                                   op0=ALU.mult, op1=ALU.add),
                      reads=[("aext", ab), "cwt", ("cv", ab)], writes=[("cv", ab)])
                s.act(lambda e, ab=ab: e.activation(out=sg[:, ab, 0:n], in_=cv[:, ab, 0:n], func=AF.Silu), reads=[("cv", ab)], writes=[("sg", ab)])
                s.dve(lambda e, ab=ab, fc=fc, pB=pB: e.tensor_tensor(out=actT[:, fc, 0:n], in0=pB[:, 0:n], in1=sg[:, ab, 0:n], op=ALU.mult),
                      reads=[("pa", 5 + ab), ("sg", ab)], writes=[("actT", fc)])
            if halo:
                return
            AT = [("actT", fc) for fc in range(NFC)]
            for dc in range(8):
                pt = pa[dc % 2]
                for fc in range(NFC):
                    s.pe(lambda e, pt=pt, fc=fc, dc=dc: e.matmul(pt[:, 0:n], lhsT=wd[:, fc, dc * 128:(dc + 1) * 128], rhs=actT[:, fc, 0:n],
                                                                start=(fc == 0), stop=(fc == NFC - 1)),
                         reads=WD + AT, writes=[("pa", dc % 2)])
                s.dve(lambda e, pt=pt, dc=dc: e.tensor_tensor(out=xm[:, dc, 0:n], in0=pt[:, 0:n], in1=xm[:, dc, 0:n], op=ALU.add),
                      reads=[("pa", dc % 2), "xm"], writes=["xm"])
            if final:
                for dc in range(8):
                    s.act(lambda e, dc=dc: e.activation(out=sq[:, dc % 2, 0:n], in_=xm[:, dc, 0:n], func=AF.Square), reads=["xm"], writes=[("sq", dc % 2)])
                    s.pe(lambda e, dc=dc: e.matmul(pa[2][:, 0:n], lhsT=ones[:], rhs=sq[:, dc % 2, 0:n], start=(dc == 0), stop=(dc == 7)),
                         reads=["ones", ("sq", dc % 2)], writes=[("pa", 2)])
                s.act(lambda e: e.activation(out=rs[:, 0:n], in_=pa[2][:, 0:n], func=AF.Sqrt, scale=1.0 / 1024, bias=epst[:, 0:1]),
                      reads=[("pa", 2), "epst"], writes=["rs"])
                s.dve(lambda e: e.reciprocal(out=rs[:, 0:n], in_=rs[:, 0:n]), reads=["rs"], writes=["rs"])
                for dc in range(8):
                    s.dve(lambda e, dc=dc: e.scalar_tensor_tensor(out=xm[:, dc, 0:n], in0=xm[:, dc, 0:n], scalar=gft[:, dc:dc + 1], in1=rs[:, 0:n],
                                                                op0=ALU.mult, op1=ALU.mult),
                          reads=["xm", "rs", "gft"], writes=["xm"])
            s.dma(lambda e: e.dma_start(out=xoT[:, out0:out0 + n].rearrange("(c p) t -> p c t", p=128), in_=xm[:, :, 0:n]), reads=["xm"])

        for k in range(4):
            unit(k, k * SLOTW, 2, True, None)
            for u in range(512 // UW):
                unit(k, k * SLOTW + 2 + u * UW, UW, False, k * 512 + u * UW)
        s.emit(stack)
        print("F: ops", len(s.ops), "waits", s.n_waits)
    return nc


def host_inputs_F(mix_list, x_full, w_out, norm2_g, w_up, conv_w, conv_b, w_down, final_g):
    bf = ml_dtypes.bfloat16
    maps = []
    cw = np.zeros((128, NFC, 4), np.float32)
    cw[:, :, 0:3] = conv_w.T.reshape(NFC, 128, 3).transpose(1, 0, 2)
    cw[:, :, 3] = conv_b.reshape(NFC, 128).T
    for c in range(8):
        b, j = divmod(c, 4)
        mcols, xcols = [], []
        hf = np.ones((128, 4), np.float32)
        for k in range(4):
            g = 4 * k + j
            t0 = 512 * g
            if g == 0:
                mcols.append(np.zeros((2, 1024), bf))
                xcols.append(np.zeros((2, 1024), np.float32))
                hf[:, k] = 0.0
            else:
                mcols.append(mix_list[b][t0 - 2:t0])
                xcols.append(x_full[b, t0 - 2:t0])
            mcols.append(mix_list[b][t0:t0 + 512])
            xcols.append(x_full[b, t0:t0 + 512])
        maps.append({
            "mixT": np.ascontiguousarray(np.concatenate(mcols, 0).T), "xT": np.ascontiguousarray(np.concatenate(xcols, 0).T),
            "w_out": np.ascontiguousarray(w_out), "w_up": np.ascontiguousarray(w_up), "w_down": np.ascontiguousarray(w_down),
            "g2": np.ascontiguousarray(norm2_g.reshape(8, 128).T), "cw": cw, "hflag": hf,
            "gf": np.ascontiguousarray(final_g.reshape(8, 128).T),
        })
    return maps


_CACHE = {}
CHECK = None


def _get(name, fn):
    if name not in _CACHE:
        _CACHE[name] = fn()
    return _CACHE[name]


def _run(nc, maps):
    res = run_bass_kernel_spmd(nc, maps, core_ids=list(range(8)))
    return res.results


def forward(x, positions, norm1_g, w_in, w_gate_up, b_gate, gla_norm_g, w_pool, pool_scale, w_out, norm2_g, w_up, conv_w, conv_b,
            w_down, final_norm_g):
    bf = ml_dtypes.bfloat16
    x = np.asarray(x, np.float32)
    positions = np.asarray(positions, np.int32)
    depth = norm1_g.shape[0]
    ncA = _get("A", build_A)
    ncB = _get("B", build_B)
    ncG = _get("G", build_G)
    for l in range(depth):
        last = (l == depth - 1)
        rA = _run(ncA, host_inputs_A(x, positions, np.asarray(norm1_g[l]), np.asarray(w_in[l]), np.asarray(w_pool[l]), np.asarray(pool_scale[l])))
        pbf = [np.asarray(r["pbf"]) for r in rA]
        pf32 = [np.asarray(r["pf32"]) for r in rA]
        ocT = [np.asarray(r["ocT"]) for r in rA]
        if CHECK:
            CHECK("A", l, dict(pbf=pbf, pf32=pf32, ocT=ocT))
        rB = _run(ncB, host_inputs_B(pbf, pf32))
        oaT = [np.asarray(r["oaT"]) for r in rB]
        if CHECK:
            CHECK("B", l, dict(oaT=oaT))
        pf_full = []
        for b in range(2):
            full = np.zeros((8192, pf32[0].shape[1]), np.float32)
            for j in range(4):
                for k in range(4):
                    g = 4 * k + j
                    full[512 * g:512 * (g + 1)] = pf32[b * 4 + j][512 * k:512 * (k + 1)]
            pf_full.append(full)
        rG = _run(ncG, host_inputs_G(pf_full, np.asarray(w_gate_up[l]), np.asarray(b_gate[l]), np.asarray(gla_norm_g[l])))
        obT = [np.asarray(r["obT"]) for r in rG]
        if CHECK:
            CHECK("G", l, dict(obT=obT))
        mix = []
        for b in range(2):
            m = np.zeros((8192, 1024), bf)
            for j in range(4):
                c = b * 4 + j
                oa = oaT[c].transpose(2, 1, 0).reshape(2048, 512)
                oc = ocT[c].transpose(2, 1, 0).reshape(2048, 256)
                for k in range(4):
                    g = 4 * k + j
                    m[512 * g:512 * (g + 1), 0:512] = oa[512 * k:512 * (k + 1)]
                    m[512 * g:512 * (g + 1), 768:1024] = oc[512 * k:512 * (k + 1)]
            for h in range(4):
                m[:, 512 + 64 * h:512 + 64 * (h + 1)] = obT[b * 4 + h].T
            mix.append(m)
        if CHECK:
            CHECK("mix", l, dict(mix=mix))
        ncF = _get("F%d" % int(last), lambda: build_F(final=last))
        rF = _run(ncF, host_inputs_F(mix, x, np.asarray(w_out[l]), np.asarray(norm2_g[l]), np.asarray(w_up[l]), np.asarray(conv_w[l]),
                                     np.asarray(conv_b[l]), np.asarray(w_down[l]), np.asarray(final_norm_g)))
        xn = np.zeros_like(x)
        for c in range(8):
            b, j = divmod(c, 4)
            xo = np.asarray(rF[c]["xoT"]).T
            for k in range(4):
                g = 4 * k + j
                xn[b, 512 * g:512 * (g + 1)] = xo[512 * k:512 * (k + 1)]
        x = xn
        if CHECK:
            CHECK("F", l, dict(x=x))
    return x


def kernel(**inputs):
    out = forward(**{k: np.asarray(v) for k, v in inputs.items()})
    return np.ascontiguousarray(out.astype(np.float32))
```
